# Optimizing a Trainium2 kernel written in Bass

```python
import math
import jax, jax.numpy as jnp
from jax import lax
import numpy as np


D_MODEL = 2048
BATCH = 4
SEQ = 2048
DEPTH = 1

GRID_W = 64
CTX_LEN = 256

D_MIX = D_MODEL
D_SSM = D_MIX // 2
D_CONV = D_MIX - D_SSM
SSM_GROUP = 16
SSM_GROUPS = D_SSM // SSM_GROUP
SSM_STATE = 64
DT_MIN = 1e-3
DT_MAX = 1e-1
CONV_WIDTH = 3
IN_SPLITS = (D_SSM, D_SSM + D_CONV, D_SSM + 2 * D_CONV)
D_IN = D_SSM + 3 * D_CONV

N_EXPERTS = 64
N_EXPERT_GROUPS = 8
TOPK_GROUPS = 4
TOP_K = 8
D_EXPERT = 512
D_SHARED = 512
ROUTED_SCALE = 2.5
EXPERT_BLOCK = 128

N_MOD = 6
EPS = 1e-6

kernel_name = 'hybrid_s5_shortconv_moe_dit_layer'


def rmsnorm(x, g):
    xf = x.astype(jnp.float32)
    y = xf * lax.rsqrt(jnp.mean(xf * xf, axis=-1, keepdims=True) + EPS)
    return y.astype(x.dtype) * g


def swiglu(x, wg, wu, wd):
    return (jax.nn.silu(x @ wg) * (x @ wu)) @ wd


def zoh(lam_re, lam_im, b_re, b_im, log_dt):
    lam = lax.complex(lam_re.astype(jnp.float32), lam_im.astype(jnp.float32))
    dt = jnp.exp(log_dt.astype(jnp.float32))[:, None]
    lam_bar = jnp.exp(lam * dt)
    b = lax.complex(b_re.astype(jnp.float32), b_im.astype(jnp.float32))
    b_bar = ((lam_bar - 1.0) / lam)[..., None] * b
    return lam_bar, b_bar


def _ssm_combine(left, right):
    a1, b1 = left
    a2, b2 = right
    return a1 * a2, a2 * b1 + b2


def linear_scan(bu, lam_bar, h0, reverse):
    if reverse:
        bu = jnp.flip(bu, axis=1)
    if h0 is not None:
        bu = bu.at[:, 0].add(lam_bar * h0)
    a = jnp.broadcast_to(lam_bar, (1, bu.shape[1]) + lam_bar.shape)
    _, h = lax.associative_scan(_ssm_combine, (a, bu), axis=1)
    return jnp.flip(h, axis=1) if reverse else h


def ssm_readout(h, c_mat):
    y = jnp.einsum('blgp,gnp->blgn', h, c_mat).real
    return y.reshape(h.shape[0], h.shape[1], D_SSM)


def s5_glu(y, w_glu):
    g = jax.nn.gelu(y)
    ga, gb = jnp.split(g @ w_glu, 2, axis=-1)
    return ga * jax.nn.sigmoid(gb)


def s5_bidirectional(ux, uc, lam_re, lam_im, b_re, b_im, c_re, c_im, log_dt, d_skip, w_glu, ctx_out):
    bsz, seq, _ = ux.shape
    ugx = ux.astype(jnp.float32).reshape(bsz, seq, SSM_GROUPS, SSM_GROUP)
    ugc = uc.astype(jnp.float32).reshape(bsz, uc.shape[1], SSM_GROUPS, SSM_GROUP)
    yx = d_skip * ux
    yc = d_skip * uc if ctx_out else None
    for d in range(2):
        rev = d == 1
        lam_bar, b_bar = zoh(lam_re[d], lam_im[d], b_re[d], b_im[d], log_dt[d])
        c_mat = lax.complex(c_re[d].astype(jnp.float32), c_im[d].astype(jnp.float32))
        hc = linear_scan(jnp.einsum('blgn,gpn->blgp', ugc, b_bar), lam_bar, None, rev)
        h0 = hc[:, 0] if rev else hc[:, -1]
        hx = linear_scan(jnp.einsum('blgn,gpn->blgp', ugx, b_bar), lam_bar, h0, rev)
        yx = yx + ssm_readout(hx, c_mat).astype(ux.dtype)
        if ctx_out:
            yc = yc + ssm_readout(hc, c_mat).astype(uc.dtype)
    return s5_glu(yx, w_glu), (s5_glu(yc, w_glu) if ctx_out else None)


def centred_conv(z, w, b):
    pad = CONV_WIDTH // 2
    n = z.shape[-2]
    zp = jnp.pad(z, [(0, 0)] * (z.ndim - 2) + [(pad, pad), (0, 0)])
    out = b
    for j in range(CONV_WIDTH):
        out = out + zp[..., j:j + n, :] * w[j]
    return out


def short_conv_mixer(bg, cg, v, w, b, rows):
    z = cg * v
    if rows is None:
        y = centred_conv(z, w, b)
    else:
        bsz, seq, ch = z.shape
        y = centred_conv(z.reshape(bsz, rows, GRID_W, ch), w, b).reshape(bsz, seq, ch)
    return bg * y


def merge_heads(ssm_y, conv_y, mix_norm_g, w_out):
    heads = jnp.concatenate([rmsnorm(ssm_y, mix_norm_g[:D_SSM]),
                             rmsnorm(conv_y, mix_norm_g[D_SSM:])], axis=-1)
    return heads @ w_out


def route(h, router_w, router_bias):
    t = h.shape[0]
    scores = jax.nn.sigmoid((h @ router_w).astype(jnp.float32))
    biased = scores + router_bias.astype(jnp.float32)
    grouped = biased.reshape(t, N_EXPERT_GROUPS, N_EXPERTS // N_EXPERT_GROUPS)
    group_score = jnp.sum(lax.top_k(grouped, 2)[0], axis=-1)
    _, top_groups = lax.top_k(group_score, TOPK_GROUPS)
    group_mask = jnp.sum(jax.nn.one_hot(top_groups, N_EXPERT_GROUPS, dtype=jnp.float32), axis=-2) > 0
    expert_mask = jnp.repeat(group_mask, N_EXPERTS // N_EXPERT_GROUPS, axis=-1)
    _, idx = lax.top_k(jnp.where(expert_mask, biased, -jnp.inf), TOP_K)
    w = jnp.take_along_axis(scores, idx, axis=-1)
    w = w / jnp.sum(w, axis=-1, keepdims=True) * ROUTED_SCALE
    return idx, w


def routed_experts(h, idx, wts, w_gate, w_up, w_down):
    t, d = h.shape
    n_assign = t * TOP_K
    flat_e = idx.reshape(n_assign)
    flat_tok = jnp.repeat(jnp.arange(t, dtype=jnp.int32), TOP_K)
    flat_w = wts.reshape(n_assign)
    order = jnp.argsort(flat_e)
    se, stok, sw = flat_e[order], flat_tok[order], flat_w[order]
    counts = jnp.bincount(flat_e, length=N_EXPERTS)
    starts = jnp.cumsum(counts) - counts
    padded = (counts + EXPERT_BLOCK - 1) // EXPERT_BLOCK * EXPERT_BLOCK
    pends = jnp.cumsum(padded)
    pstarts = pends - padded
    dest = pstarts[se] + (jnp.arange(n_assign) - starts[se])
    n_blocks = -(-n_assign // EXPERT_BLOCK) + N_EXPERTS
    n_rows = n_blocks * EXPERT_BLOCK
    rows_tok = jnp.zeros((n_rows,), jnp.int32).at[dest].set(stok)
    rows_w = jnp.zeros((n_rows,), h.dtype).at[dest].set(sw.astype(h.dtype))
    block_start = jnp.arange(n_blocks, dtype=pends.dtype) * EXPERT_BLOCK
    block_e = jnp.minimum(jnp.searchsorted(pends, block_start, side='right'), N_EXPERTS - 1)

    def block_ffn(args):
        tok, e = args
        return swiglu(h[tok], w_gate[e], w_up[e], w_down[e])

    ys = lax.map(block_ffn, (rows_tok.reshape(n_blocks, EXPERT_BLOCK), block_e))
    ys = ys.reshape(n_rows, d) * rows_w[:, None]
    return jnp.zeros_like(h).at[rows_tok].add(ys)


def moe_ffn(h, router_w, router_bias, w_gate, w_up, w_down, ws_gate, ws_up, ws_down):
    shape = h.shape
    ht = h.reshape(-1, shape[-1])
    idx, wts = route(ht, router_w, router_bias)
    routed = routed_experts(ht, idx, wts, w_gate, w_up, w_down)
    shared = swiglu(ht, ws_gate, ws_up, ws_down)
    return (routed + shared).reshape(shape)


def setup_inputs(seed: int = 0) -> dict:
    key = jax.random.key(seed)
    ks = jax.random.split(key, 32)
    f32 = jnp.float32

    def nrm(k, shape, scale):
        return jax.random.normal(k, shape, f32) * scale

    n_idx = jnp.arange(SSM_STATE, dtype=f32)
    return {
        'x': nrm(ks[0], (BATCH, SEQ, D_MODEL), 1.0),
        'c': nrm(ks[1], (BATCH, D_MODEL), 1.0),
        'ctx': nrm(ks[2], (BATCH, CTX_LEN, D_MODEL), 1.0),
        'c_ctx': nrm(ks[3], (D_MODEL,), 1.0),
        'norm1_g': 1.0 + nrm(ks[4], (DEPTH, D_MODEL), 0.02),
        'norm2_g': 1.0 + nrm(ks[5], (DEPTH, D_MODEL), 0.02),
        'w_ada': nrm(ks[6], (DEPTH, D_MODEL, N_MOD * D_MODEL), 0.5 * D_MODEL ** -0.5),
        'b_ada': nrm(ks[7], (DEPTH, N_MOD * D_MODEL), 0.02),
        'w_in': nrm(ks[8], (DEPTH, D_MODEL, D_IN), D_MODEL ** -0.5),
        'ssm_lam_re': -0.5 + nrm(ks[9], (DEPTH, 2, SSM_GROUPS, SSM_STATE), 0.01),
        'ssm_lam_im': math.pi * n_idx + nrm(ks[10], (DEPTH, 2, SSM_GROUPS, SSM_STATE), 0.01),
        'ssm_b_re': nrm(ks[11], (DEPTH, 2, SSM_GROUPS, SSM_STATE, SSM_GROUP), (2 * SSM_GROUP) ** -0.5),
        'ssm_b_im': nrm(ks[12], (DEPTH, 2, SSM_GROUPS, SSM_STATE, SSM_GROUP), (2 * SSM_GROUP) ** -0.5),
        'ssm_c_re': nrm(ks[13], (DEPTH, 2, SSM_GROUPS, SSM_GROUP, SSM_STATE), (2 * SSM_STATE) ** -0.5),
        'ssm_c_im': nrm(ks[14], (DEPTH, 2, SSM_GROUPS, SSM_GROUP, SSM_STATE), (2 * SSM_STATE) ** -0.5),
        'ssm_log_dt': jax.random.uniform(ks[15], (DEPTH, 2, SSM_GROUPS), f32,
                                         minval=math.log(DT_MIN), maxval=math.log(DT_MAX)),
        'ssm_d': nrm(ks[16], (DEPTH, D_SSM), 0.5),
        'ssm_w_glu': nrm(ks[17], (DEPTH, D_SSM, 2 * D_SSM), D_SSM ** -0.5),
        'conv_w': nrm(ks[18], (DEPTH, CONV_WIDTH, D_CONV), CONV_WIDTH ** -0.5),
        'conv_b': nrm(ks[19], (DEPTH, D_CONV), 0.01),
        'mix_norm_g': 1.0 + nrm(ks[20], (DEPTH, D_MIX), 0.02),
        'w_out': nrm(ks[21], (DEPTH, D_MIX, D_MODEL), D_MIX ** -0.5),
        'router_w': nrm(ks[22], (DEPTH, D_MODEL, N_EXPERTS), D_MODEL ** -0.5),
        'router_bias': nrm(ks[23], (DEPTH, N_EXPERTS), 0.01),
        'exp_w_gate': nrm(ks[24], (DEPTH, N_EXPERTS, D_MODEL, D_EXPERT), D_MODEL ** -0.5),
        'exp_w_up': nrm(ks[25], (DEPTH, N_EXPERTS, D_MODEL, D_EXPERT), D_MODEL ** -0.5),
        'exp_w_down': nrm(ks[26], (DEPTH, N_EXPERTS, D_EXPERT, D_MODEL), D_EXPERT ** -0.5),
        'shared_w_gate': nrm(ks[27], (DEPTH, D_MODEL, D_SHARED), D_MODEL ** -0.5),
        'shared_w_up': nrm(ks[28], (DEPTH, D_MODEL, D_SHARED), D_MODEL ** -0.5),
        'shared_w_down': nrm(ks[29], (DEPTH, D_SHARED, D_MODEL), D_SHARED ** -0.5),
        'final_g': 1.0 + nrm(ks[30], (D_MODEL,), 0.02),
    }


def reference(x, c, ctx, c_ctx, norm1_g, norm2_g, w_ada, b_ada, w_in,
              ssm_lam_re, ssm_lam_im, ssm_b_re, ssm_b_im, ssm_c_re, ssm_c_im, ssm_log_dt,
              ssm_d, ssm_w_glu, conv_w, conv_b, mix_norm_g, w_out,
              router_w, router_bias, exp_w_gate, exp_w_up, exp_w_down,
              shared_w_gate, shared_w_up, shared_w_down, final_g):
    rows = x.shape[1] // GRID_W
    for layer in range(DEPTH):
        ctx_out = layer < DEPTH - 1
        mod_x = jnp.split(jax.nn.silu(c) @ w_ada[layer] + b_ada[layer], N_MOD, axis=-1)
        sh1, sc1, g1, sh2, sc2, g2 = [m[:, None, :] for m in mod_x]
        csh1, csc1, cg1, csh2, csc2, cg2 = jnp.split(
            jax.nn.silu(c_ctx) @ w_ada[layer] + b_ada[layer], N_MOD, axis=-1)

        hx = rmsnorm(x, norm1_g[layer]) * (1 + sc1) + sh1
        hc = rmsnorm(ctx, norm1_g[layer]) * (1 + csc1) + csh1
        ux, bx, cx, vx = jnp.split(hx @ w_in[layer], IN_SPLITS, axis=-1)
        if ctx_out:
            uc, bc, cc, vc = jnp.split(hc @ w_in[layer], IN_SPLITS, axis=-1)
        else:
            uc = hc @ w_in[layer][:, :D_SSM]
        ssm_x, ssm_c = s5_bidirectional(ux, uc, ssm_lam_re[layer], ssm_lam_im[layer],
                                        ssm_b_re[layer], ssm_b_im[layer], ssm_c_re[layer],
                                        ssm_c_im[layer], ssm_log_dt[layer], ssm_d[layer],
                                        ssm_w_glu[layer], ctx_out)
        conv_x = short_conv_mixer(bx, cx, vx, conv_w[layer], conv_b[layer], rows)
        x = x + g1 * merge_heads(ssm_x, conv_x, mix_norm_g[layer], w_out[layer])

        hx2 = rmsnorm(x, norm2_g[layer]) * (1 + sc2) + sh2
        x = x + g2 * moe_ffn(hx2, router_w[layer], router_bias[layer], exp_w_gate[layer],
                             exp_w_up[layer], exp_w_down[layer], shared_w_gate[layer],
                             shared_w_up[layer], shared_w_down[layer])

        if ctx_out:
            conv_c = short_conv_mixer(bc, cc, vc, conv_w[layer], conv_b[layer], None)
            ctx = ctx + cg1 * merge_heads(ssm_c, conv_c, mix_norm_g[layer], w_out[layer])
            hc2 = rmsnorm(ctx, norm2_g[layer]) * (1 + csc2) + csh2
            ctx = ctx + cg2 * moe_ffn(hc2, router_w[layer], router_bias[layer], exp_w_gate[layer],
                                      exp_w_up[layer], exp_w_down[layer], shared_w_gate[layer],
                                      shared_w_up[layer], shared_w_down[layer])
    return rmsnorm(x, final_g)
```

```python
from contextlib import ExitStack
import numpy as np
import concourse.bass as bass
import concourse.mybir as mybir
from concourse.bass_utils import run_bass_kernel_spmd

F32 = mybir.dt.float32
BF16 = mybir.dt.bfloat16
I32 = mybir.dt.int32
ALU = mybir.AluOpType
AF = mybir.ActivationFunctionType
AX = mybir.AxisListType

D = 2048
NOWN = 1024
NSEQ = 2304
EPS = 1e-6


class Prog:
    ENG = ("pe", "act", "dve", "pool", "sp")

    def __init__(self, nc, stack):
        self.nc = nc
        self.stack = stack
        self.ops = []
        self.keys = {}
        self.groups = {}
        self.psum_names = set()

    @staticmethod
    def _norm(k):
        return k if isinstance(k, tuple) else (k,)

    def _related(self, key):
        d = self.keys.setdefault(key[0], {})
        for k2 in list(d.keys()):
            n = min(len(k2), len(key))
            if k2[:n] == key[:n]:
                yield k2, d[k2]

    def add(self, eng, fn, reads=(), writes=(), group=None):
        op = dict(id=len(self.ops), eng=eng, fn=fn, deps=set(), group=group, used=False)
        reads = [self._norm(k) for k in reads] + [("__phase",)]
        writes = [self._norm(k) for k in writes]
        pk = [(k[0],) for k in reads + writes if k[0] in self.psum_names]
        reads = [k for k in reads if k[0] not in self.psum_names]
        writes = [k for k in writes if k[0] not in self.psum_names] + sorted(set(pk))
        for key in reads:
            for k2, st in self._related(key):
                if st[0] is not None:
                    op["deps"].add(st[0])
        for key in writes:
            for k2, st in self._related(key):
                if st[0] is not None:
                    op["deps"].add(st[0])
                op["deps"].update(st[1])
        for key in reads:
            d = self.keys.setdefault(key[0], {})
            st = d.setdefault(key, [None, []])
            st[1].append(op["id"])
        for key in writes:
            d = self.keys.setdefault(key[0], {})
            for k2 in list(d.keys()):
                if len(k2) > len(key) and k2[:len(key)] == key:
                    del d[k2]
            d[key] = [op["id"], []]
        op["deps"].discard(op["id"])
        self.ops.append(op)
        return op

    def barrier(self):
        scr = self._bar_scr
        self.add("dve", lambda e: e.memset(scr[:, 0:1], 0.0), writes=[("__phase",), "barscr"])

    def emit(self):
        nc = self.nc
        ops = self.ops
        for op in ops:
            for d in op["deps"]:
                ops[d]["used"] = True
        sems = {}
        for e in self.ENG:
            sems[e] = self.stack.enter_context(nc.semaphore("s_" + e))
        gsem = {}
        cnt = {e: 0 for e in self.ENG}
        gcnt = {}
        for op in ops:
            if op["group"] is not None:
                g = op["group"]
                if g not in gsem:
                    gsem[g] = self.stack.enter_context(nc.semaphore("g_" + str(g)))
                    gcnt[g] = 0
                gcnt[g] += 16
                op["sig"] = (gsem[g], gcnt[g], 16)
            elif op["used"]:
                cnt[op["eng"]] += 1
                op["sig"] = (sems[op["eng"]], cnt[op["eng"]], 1)
            else:
                op["sig"] = None
        per = {e: [o for o in ops if o["eng"] == e] for e in self.ENG}

        def replay(ename, eng):
            waited = {}
            for op in per[ename]:
                need = {}
                for d in op["deps"]:
                    dop = ops[d]
                    if dop["eng"] == "pe" and ename == "pe" and dop["group"] is None:
                        continue
                    s = dop["sig"]
                    assert s is not None
                    key = id(s[0])
                    if key not in need or need[key][1] < s[1]:
                        need[key] = (s[0], s[1])
                for key, (sem, val) in need.items():
                    if waited.get(key, 0) >= val:
                        continue
                    waited[key] = val
                    eng.wait_ge(sem, val)
                if op["fn"] is None:
                    continue
                ins = op["fn"](eng)
                if op["sig"] is not None:
                    ins.then_inc(op["sig"][0], op["sig"][2])

        block = self.stack.enter_context(nc.Block())

        @block.tensor
        def _(eng):
            replay("pe", eng)

        @block.scalar
        def _(eng):
            replay("act", eng)

        @block.vector
        def _(eng):
            replay("dve", eng)

        @block.gpsimd
        def _(eng):
            replay("pool", eng)

        @block.sync
        def _(eng):
            replay("sp", eng)


def build_nc(debug=False, stop=99):
    nc = bass.Bass("TRN2", target_bir_lowering=False)
    dbg = {}

    def din(name, shape, dt=F32):
        return nc.dram_tensor(name, list(shape), dt, kind="ExternalInput").ap()

    xs = din("xs", [2048, D])
    ctxs = din("ctxs", [256, D])
    vecsA = din("vecsA", [96, 128])
    vecsB = din("vecsB", [40, 128])
    b_ada = din("b_ada", [96, 128])
    w_ada = din("w_ada", [D, 6 * D])
    w_in = din("w_in", [D, 4096])
    lamre_p = din("lamre_p", [64, 128])
    lamim_p = din("lamim_p", [64, 128])
    logdt_p = din("logdt_p", [64, 2])
    ssm_b_re = din("ssm_b_re", [2, 64, 64, 16])
    ssm_b_im = din("ssm_b_im", [2, 64, 64, 16])
    ssm_c_re = din("ssm_c_re", [2, 64, 16, 64])
    ssm_c_im = din("ssm_c_im", [2, 64, 16, 64])
    w_glu = din("w_glu", [1024, 2048])
    w_out = din("w_out", [D, D])
    router_w = din("router_w", [D, 64])
    rbias_b = din("rbias_b", [128, 64])
    ew_gate = din("ew_gate", [64, D, 512])
    ew_up = din("ew_up", [64, D, 512])
    ew_down = din("ew_down", [64, 512, D])
    sw_gate = din("sw_gate", [D, 512])
    sw_up = din("sw_up", [D, 512])
    sw_down = din("sw_down", [512, D])
    out = nc.dram_tensor("out", [NOWN, D], F32, kind="ExternalOutput").ap()
    skind = "ExternalOutput" if debug else "Internal"
    scrW1 = nc.dram_tensor("scrW1", [2, 32, 2, 128, 128], BF16, kind=skind).ap()
    scrT = nc.dram_tensor("scrT", [2, 64, 128, 128], BF16, kind=skind).ap()
    scrW2 = nc.dram_tensor("scrW2", [2, 32, 2, 128, 128], BF16, kind=skind).ap()
    scrX1 = nc.dram_tensor("scrX1", [NOWN, D], F32, kind=skind).ap()
    scrU = nc.dram_tensor("scrU", [8, 128, NSEQ - NOWN], BF16, kind=skind).ap()

    def dout(name, shape, dt=F32):
        t = nc.dram_tensor(name, list(shape), dt, kind="ExternalOutput").ap()
        dbg[name] = t
        return t

    with ExitStack() as top:
        P = Prog(nc, top)

        def sb(name, shape, dt=F32, stack=top):
            return stack.enter_context(nc.sbuf_tensor(name, list(shape), dt))

        def ps(name, shape, dt=F32, stack=top):
            P.psum_names.add(name)
            esz = 4 if dt == F32 else 2
            full = stack.enter_context(nc.psum_tensor(name, [128, 2048 // esz], dt))
            n = int(np.prod(shape[1:]))
            v = full[:, 0:n]
            if len(shape) == 3:
                v = v.rearrange("p (a b) -> p a b", b=shape[2])
            return v

        P._bar_scr = sb("barscr", [128, 4])

        ident_f = sb("ident_f", [128, 128])
        ident_b = sb("ident_b", [128, 128], BF16)
        iot = sb("iot", [128, 128], I32)
        iotf = sb("iotf", [128, 128])
        P.add("pool", lambda e: e.iota(iot[:], [[1, 128]], base=0, channel_multiplier=-1), writes=["iot"])
        P.add("dve", lambda e: e.tensor_copy(iotf[:], iot[:]), reads=["iot"], writes=["iotf"])
        P.add("dve", lambda e: e.tensor_single_scalar(ident_f[:], iotf[:], 0.0, ALU.is_equal),
              reads=["iotf"], writes=["ident_f"])
        P.add("dve", lambda e: e.tensor_copy(ident_b[:], ident_f[:]), reads=["ident_f"], writes=["ident_b"])

        if stop < 1:
            d_i = dout('d_ident', [128, 128])
            P.add('sp', lambda e: e.dma_start(out=d_i, in_=ident_f[:]), reads=['ident_f'], writes=['d_ident'], group='dbgi')
            P.add('sp', None, reads=list(dbg.keys()))
            P.emit()
            return nc, dbg
        ones_b = sb("ones_b", [128, 128], BF16)
        ones_f = sb("ones_f", [128, 128], F32)
        P.add("dve", lambda e: e.memset(ones_b[:], 1.0), writes=["ones_b"])
        P.add("dve", lambda e: e.memset(ones_f[:], 1.0), writes=["ones_f"])
        ss2 = sb("ss2", [128, 8, 4], F32)
        vA = sb("vA", [96, 128])
        vB = sb("vB", [40, 128])
        vC = sb("vC", [96, 128])
        colA = sb("colA", [128, 96])
        colB = sb("colB", [128, 40])
        badaT = sb("badaT", [128, 96])
        P.add("sp", lambda e: e.dma_start(out=vA[:], in_=vecsA), writes=["vA"], group="vA")
        P.add("sp", lambda e: e.dma_start(out=vB[:], in_=vecsB), writes=["vB"], group="vB")
        P.add("sp", lambda e: e.dma_start(out=vC[:], in_=b_ada), writes=["vC"], group="vC")
        with ExitStack() as ph:
            pt = ps("pt_small", [128, 3, 128], F32, ph)
            P.add("pe", lambda e: e.transpose(out=pt[:, 0, 0:96], in_=vA[:], identity=ident_f[0:96, 0:96]),
                  reads=["vA", "ident_f"], writes=["pt_small"])
            P.add("pe", lambda e: e.transpose(out=pt[:, 1, 0:40], in_=vB[:], identity=ident_f[0:40, 0:40]),
                  reads=["vB", "ident_f"], writes=["pt_small"])
            P.add("pe", lambda e: e.transpose(out=pt[:, 2, 0:96], in_=vC[:], identity=ident_f[0:96, 0:96]),
                  reads=["vC", "ident_f"], writes=["pt_small"])
            P.add("dve", lambda e: e.tensor_copy(colA[:], pt[:, 0, 0:96]), reads=["pt_small"], writes=["colA"])
            P.add("dve", lambda e: e.tensor_copy(colB[:], pt[:, 1, 0:40]), reads=["pt_small"], writes=["colB"])
            P.add("dve", lambda e: e.tensor_copy(badaT[:], pt[:, 2, 0:96]), reads=["pt_small"], writes=["badaT"])
            P.barrier()

        if stop < 2:
            d_c = dout('d_colA', [128, 96])
            P.add('sp', lambda e: e.dma_start(out=d_c, in_=colA[:]), reads=['colA'], writes=['d_colA'], group='dbgc')
            P.add('sp', None, reads=list(dbg.keys()))
            P.emit()
            return nc, dbg
        sc = sb("sc", [128, 16, 2])
        for j in range(2):
            P.add("act", lambda e, j=j: e.activation(out=sc[:, :, j], in_=colA[:, 16 * j:16 * j + 16], func=AF.Silu),
                  reads=["colA"], writes=[("sc", j)])
        modT = sb("modT", [128, 96, 2])
        with ExitStack() as ph:
            scb = sb("scb", [128, 16, 4], BF16, ph)
            sch = sb("sch", [128, 16, 2], F32, ph)
            P.add("dve", lambda e: e.tensor_copy(scb[:, :, 0:2], sc[:]), reads=["sc"], writes=["scb"])
            P.add("dve", lambda e: e.tensor_copy(sch[:], scb[:, :, 0:2]), reads=["scb"], writes=["sch"])
            P.add("dve", lambda e: e.tensor_tensor(out=sch[:], in0=sc[:], in1=sch[:], op=ALU.subtract), reads=["sc", "sch"], writes=["sch"])
            P.add("dve", lambda e: e.tensor_copy(scb[:, :, 2:4], sch[:]), reads=["sch", "scb"], writes=["scb"])
            pm = ps("pmod", [128, 96, 4], F32, ph)
            wbuf = [sb("wada%d" % i, [128, 2048], BF16, ph) for i in range(4)]
            n = 0
            for kt in range(16):
                for cc in range(6):
                    b = n % 4
                    n += 1
                    for hc in range(2):
                        P.add("pool", lambda e, b=b, kt=kt, cc=cc, hc=hc: e.dma_start(
                            out=wbuf[b][:, hc * 1024:(hc + 1) * 1024],
                            in_=w_ada[kt * 128:(kt + 1) * 128, cc * 2048 + hc * 1024:cc * 2048 + (hc + 1) * 1024]),
                            writes=[("wada", b, hc)], group="wada%d" % b)

                    def mm(e, b=b, kt=kt, cc=cc):
                        ins = None
                        for t in range(16):
                            ins = e.matmul(pm[:, cc * 16 + t, :], lhsT=wbuf[b][:, t * 128:(t + 1) * 128],
                                           rhs=scb[:, kt, :], start=(kt == 0 and cc == 0 and t == 0), stop=(kt == 15),
                                           skip_group_check=True)
                        return ins
                    P.add("pe", mm, reads=[("wada", b), "scb"], writes=["pmod"])
            P.add("dve", lambda e: e.tensor_tensor(out=modT[:], in0=pm[:, :, 0:2], in1=badaT[:].unsqueeze(2).to_broadcast([128, 96, 2]),
                                                  op=ALU.add), reads=["pmod", "badaT"], writes=["modT"])
            P.add("dve", lambda e: e.tensor_tensor(out=modT[:], in0=modT[:], in1=pm[:, :, 2:4], op=ALU.add),
                  reads=["pmod", "modT"], writes=["modT"])
            P.barrier()
        if debug:
            d_mod = dout("d_mod", [128, 192])
            P.add("sp", lambda e: e.dma_start(out=d_mod, in_=modT[:].rearrange("p a b -> p (a b)")), reads=["modT"],
                  writes=["d_mod"], group="dbg0")

        scale1 = sb("scale1", [128, 16, 2])
        P.add("dve", lambda e: e.scalar_tensor_tensor(out=scale1[:], in0=modT[:, 16:32, :], scalar=1.0,
                                                      in1=colA[:, 32:48].unsqueeze(2).to_broadcast([128, 16, 2]),
                                                      op0=ALU.add, op1=ALU.mult),
              reads=["modT", "colA"], writes=["scale1"])

        mixer = top.enter_context(ExitStack())
        uTown = sb("uTown", [128, 8, NOWN], BF16, mixer)
        convT = sb("convT", [128, 8, NOWN], BF16, mixer)
        ss = sb("ss", [128, 24], F32, mixer)
        with ExitStack() as ph:
            w_u = sb("w_u", [128, 16, 1024], BF16, ph)
            ustage = [sb("ustage%d" % i, [128, 512], BF16, ph) for i in range(2)]
            w_in_v = w_in.rearrange("(kt p) c -> p kt c", p=128)
            for kt in range(16):
                P.add("pool", lambda e, kt=kt: e.dma_start(out=w_u[:, kt, :], in_=w_in_v[:, kt, 0:1024]),
                      writes=[("w_u", kt)], group="w_u")
            wch = [[sb("wch%d_%d" % (s_, i), [128, 16, 128], BF16, ph) for i in range(3)] for s_ in range(2)]
            xt = [sb("xt%d" % i, [128, D], F32, ph) for i in range(2)]
            xn = sb("xn", [128, 4, D], BF16, ph)
            hxT = [sb("hxT%d" % i, [128, 16, 512], BF16, ph) for i in range(2)]
            ptr = [ps("ptr%d" % i, [128, 512], BF16, ph) for i in range(2)]
            pmm = [ps("pmm%d" % i, [128, 512], F32, ph) for i in range(4)]
            zc = sb("zc", [128, 512], F32, ph)
            zz = sb("zz", [128, 512], F32, ph)
            yy = sb("yy", [128, 512], F32, ph)
            groups = [("x", 1024, 4, 0, 1024, 0), ("x", 1536, 4, 0, 1536, 1), ("c", 0, 2, 1, 2048, 0),
                      ("x", 0, 4, 0, 0, 1), ("x", 512, 4, 0, 512, 0)]
            nx = 0
            for gi, (src, r0, nt, mj, soff, xb) in enumerate(groups):
                ntok = nt * 128
                for t in range(nt):
                    b = nx % 2
                    tix = nx % 24
                    nx += 1
                    srcap = (xs if src == "x" else ctxs)[r0 + t * 128:r0 + (t + 1) * 128, :]
                    P.add("sp", lambda e, b=b, srcap=srcap: e.dma_start(out=xt[b][:], in_=srcap),
                          writes=[("xt", b)], group="xt%d" % b)
                    P.add("act", lambda e, b=b, tix=tix, t=t: e.activation(out=xn[:, t, :], in_=xt[b][:], func=AF.Square,
                                                                      accum_out=ss[:, tix:tix + 1]),
                          reads=[("xt", b)], writes=[("xn", t), ("ss", tix)])
                    P.add("dve", lambda e, tix=tix: e.tensor_scalar(out=ss[:, tix:tix + 1], in0=ss[:, tix:tix + 1],
                                                                    scalar1=1.0 / D, scalar2=EPS, op0=ALU.mult, op1=ALU.add),
                          reads=[("ss", tix)], writes=[("ss", tix)])
                    P.add("act", lambda e, tix=tix: e.activation(out=ss[:, tix:tix + 1], in_=ss[:, tix:tix + 1], func=AF.Sqrt),
                          reads=[("ss", tix)], writes=[("ss", tix)])
                    P.add("dve", lambda e, tix=tix: e.reciprocal(out=ss[:, tix:tix + 1], in_=ss[:, tix:tix + 1]),
                          reads=[("ss", tix)], writes=[("ss", tix)])
                    P.add("act", lambda e, b=b, tix=tix, t=t: e.activation(
                        out=xn[:, t, :], in_=xt[b][:], func=AF.Copy, scale=ss[:, tix:tix + 1]),
                        reads=[("xt", b), ("ss", tix)], writes=[("xn", t)])
                for ft in range(16):
                    pb = ft % 2

                    def tr(e, ft=ft, nt=nt, pb=pb):
                        ins = None
                        for t in range(nt):
                            ins = e.transpose(out=ptr[pb][:, t * 128:(t + 1) * 128],
                                              in_=xn[:, t, ft * 128:(ft + 1) * 128], identity=ident_b[:])
                        return ins
                    P.add("pe", tr, reads=["xn", "ident_b"], writes=["ptr%d" % pb])
                    if ft % 2 == 0:
                        P.add("dve", lambda e, xb=xb, ft=ft, pb=pb, ntok=ntok, mj=mj: e.tensor_scalar(
                            out=hxT[xb][:, ft, 0:ntok], in0=ptr[pb][:, 0:ntok], scalar1=scale1[:, ft, mj:mj + 1],
                            scalar2=modT[:, ft, mj:mj + 1], op0=ALU.mult, op1=ALU.add),
                            reads=["ptr%d" % pb, "scale1", "modT"], writes=[("hxT", xb, ft)])
                    else:
                        P.add("act", lambda e, xb=xb, ft=ft, pb=pb, ntok=ntok, mj=mj: e.activation(
                            out=hxT[xb][:, ft, 0:ntok], in_=ptr[pb][:, 0:ntok], func=AF.Identity,
                            scale=scale1[:, ft, mj:mj + 1], bias=modT[:, ft, mj:mj + 1]),
                            reads=["ptr%d" % pb, "scale1", "modT"], writes=[("hxT", xb, ft)])
                for ct in range(8):
                    pq = ct % 4

                    def mmu(e, xb=xb, ct=ct, ntok=ntok, pq=pq):
                        ins = None
                        for kt in range(16):
                            ins = e.matmul(pmm[pq][:, 0:ntok], lhsT=w_u[:, kt, ct * 128:(ct + 1) * 128],
                                           rhs=hxT[xb][:, kt, 0:ntok], start=(kt == 0), stop=(kt == 15))
                        return ins
                    P.add("pe", mmu, reads=["w_u", ("hxT", xb)], writes=["pmm%d" % pq])
                    nj = ntok // 8
                    if soff < NOWN:
                        dst = uTown[:, ct, :].rearrange("p (s j) -> p s j", s=8)[:, :, soff // 8:soff // 8 + nj]
                        wk = [("uT", ct, soff)]
                    else:
                        sg = ct % 2
                        dst = ustage[sg][:, 0:ntok].rearrange("p (s j) -> p s j", s=8)
                        wk = [("ustage", sg)]
                    srcv = pmm[pq][:, 0:ntok].rearrange("p (j s) -> p s j", s=8)
                    if ct % 2 == 0:
                        P.add("dve", lambda e, dst=dst, srcv=srcv: e.tensor_copy(dst, srcv),
                              reads=["pmm%d" % pq], writes=wk)
                    else:
                        P.add("act", lambda e, dst=dst, srcv=srcv: e.activation(out=dst, in_=srcv, func=AF.Copy),
                              reads=["pmm%d" % pq], writes=wk)
                    if soff >= NOWN:
                        j0r = (soff - NOWN) // 8
                        P.add("sp", lambda e, ct=ct, sg=sg, ntok=ntok, j0r=j0r, nj=nj: e.dma_start(
                            out=scrU[ct].rearrange("p (s j) -> p s j", s=8)[:, :, j0r:j0r + nj],
                            in_=ustage[sg][:, 0:ntok].rearrange("p (s j) -> p s j", s=8)),
                            reads=[("ustage", sg)], writes=["scrU"], group="ustage%d" % sg)
            for ct in range(8):
                s_ = ct % 2
                for i in range(3):
                    P.add("pool", lambda e, s_=s_, i=i, ct=ct: e.dma_start(
                        out=wch[s_][i][:], in_=w_in_v[:, :, 1024 * (i + 1) + ct * 128:1024 * (i + 1) + (ct + 1) * 128]),
                        writes=[("wch", s_, i)], group="wch%d_%d" % (s_, i))
                for og, (xb, soff) in enumerate([(1, 0), (0, 512)]):
                    if True:
                        for i in range(3):
                            def mmb(e, xb=xb, i=i, s_=s_):
                                ins = None
                                for kt in range(16):
                                    ins = e.matmul(pmm[i][:, :], lhsT=wch[s_][i][:, kt, :],
                                                   rhs=hxT[xb][:, kt, :], start=(kt == 0), stop=(kt == 15))
                                return ins
                            P.add("pe", mmb, reads=[("wch", s_, i), ("hxT", xb)], writes=["pmm%d" % i])
                        P.add("act", lambda e: e.activation(out=zc[:], in_=pmm[1][:], func=AF.Copy),
                              reads=["pmm1"], writes=["zc"])
                        P.add("dve", lambda e: e.tensor_tensor(out=zz[:], in0=zc[:], in1=pmm[2][:], op=ALU.mult),
                              reads=["zc", "pmm2"], writes=["zz"])
                        P.add("dve", lambda e, ct=ct: e.tensor_scalar(
                            out=yy[:], in0=zz[:], scalar1=colB[:, 16 + ct:17 + ct], scalar2=colB[:, 32 + ct:33 + ct],
                            op0=ALU.mult, op1=ALU.add), reads=["zz", "colB"], writes=["yy"])
                        yv = yy[:].rearrange("p (r w) -> p r w", w=64)
                        zv = zz[:].rearrange("p (r w) -> p r w", w=64)
                        P.add("dve", lambda e, ct=ct, yv=yv, zv=zv: e.scalar_tensor_tensor(
                            out=yv[:, :, 1:64], in0=zv[:, :, 0:63], scalar=colB[:, 8 + ct:9 + ct], in1=yv[:, :, 1:64],
                            op0=ALU.mult, op1=ALU.add), reads=["zz", "yy", "colB"], writes=["yy"])
                        P.add("dve", lambda e, ct=ct, yv=yv, zv=zv: e.scalar_tensor_tensor(
                            out=yv[:, :, 0:63], in0=zv[:, :, 1:64], scalar=colB[:, 24 + ct:25 + ct], in1=yv[:, :, 0:63],
                            op0=ALU.mult, op1=ALU.add), reads=["zz", "yy", "colB"], writes=["yy"])
                        P.add("dve", lambda e, ct=ct, soff=soff: e.tensor_tensor(
                            out=convT[:, ct, soff:soff + 512], in0=yy[:], in1=pmm[0][:], op=ALU.mult),
                            reads=["yy", "pmm0"], writes=[("convT", ct, soff)])
            P.barrier()
        if debug:
            d_uT = dout("d_uT", [128, 8 * NOWN], BF16)
            d_convT = dout("d_convT", [128, 8 * NOWN], BF16)
            P.add("sp", lambda e: e.dma_start(out=d_uT, in_=uTown[:].rearrange("p a b -> p (a b)")), reads=["uT"],
                  writes=["d_uT"], group="dbg1")
            P.add("sp", lambda e: e.dma_start(out=d_convT, in_=convT[:].rearrange("p a b -> p (a b)")), reads=["convT"],
                  writes=["d_convT"], group="dbg2")

        import math
        PI = math.pi
        Acplx = sb("Acplx", [128, 2, 64], F32, mixer)
        with ExitStack() as ph:
            raw = sb("s0raw", [64, 3, 128], F32, ph)
            ldt = sb("s0ldt", [64, 2], F32, ph)
            P.add("sp", lambda e: e.dma_start(out=raw[:, 0, :], in_=lamre_p), writes=[("s0raw", 0)], group="s0raw0")
            P.add("sp", lambda e: e.dma_start(out=raw[:, 1, :], in_=lamim_p), writes=[("s0raw", 1)], group="s0raw1")
            P.add("sp", lambda e: e.dma_start(out=ldt[:], in_=logdt_p), writes=["s0ldt"], group="s0ldt")
            P.add("act", lambda e: e.activation(out=ldt[:], in_=ldt[:], func=AF.Exp), reads=["s0ldt"], writes=["s0ldt"])
            P.add("dve", lambda e: e.tensor_copy(raw[:, 2, :].rearrange("q (a b) -> q a b", a=2),
                                                 ldt[:].unsqueeze(2).to_broadcast([64, 2, 64])),
                  reads=["s0ldt"], writes=[("s0raw", 2)])
            LRI = sb("LRI", [128, 3, 64], F32, ph)
            ptq = ps("s0pt", [128, 4, 128], F32, ph)
            for i in range(3):
                P.add("pe", lambda e, i=i: e.transpose(out=ptq[:, i, 0:64], in_=raw[:, i, :], identity=ident_f[0:64, 0:64]),
                      reads=[("s0raw", i), "ident_f"], writes=["s0pt"])
            P.add("dve", lambda e: e.tensor_copy(LRI[:], ptq[:, 0:3, 0:64]), reads=["s0pt"], writes=["LRI"])
            ath = sb("ath", [128, 2, 64], F32, ph)
            P.add("dve", lambda e: e.tensor_tensor(out=ath[:], in0=LRI[:, 0:2, :],
                                                   in1=LRI[:, 2:3, :].to_broadcast([128, 2, 64]), op=ALU.mult),
                  reads=["LRI"], writes=["ath"])
            io8 = sb("io8", [128, 8], I32, ph)
            io8f = sb("io8f", [128, 8], F32, ph)
            KM = sb("KM", [128, 3, 2, 8], F32, ph)
            P.add("pool", lambda e: e.iota(io8[:], [[1, 8]], base=0, channel_multiplier=0), writes=["io8"])
            P.add("dve", lambda e: e.tensor_copy(io8f[:], io8[:]), reads=["io8"], writes=["io8f"])
            kmab = {(0, 0): (-1.0, 7.0), (0, 1): (1.0, 0.0), (1, 0): (-1.0, -1.0), (1, 1): (1.0, -8.0),
                    (2, 0): (1.0, 1.0), (2, 1): (-1.0, 8.0)}
            for (u_, d_), (ka, kb) in kmab.items():
                P.add("dve", lambda e, u_=u_, d_=d_, ka=ka, kb=kb: e.tensor_scalar(
                    out=KM[:, u_, d_, :], in0=io8f[:], scalar1=ka, scalar2=kb, op0=ALU.mult, op1=ALU.add),
                    reads=["io8f"], writes=[("KM", u_, d_)])

            et_ang = sb("et_ang", [128, 2, 32, 8], F32, ph)
            et_ex = sb("et_ex", [128, 2, 32, 8], F32, ph)
            et_tmp = sb("et_tmp", [128, 2, 32, 8], F32, ph)
            et_ti = sb("et_ti", [128, 2, 32, 8], I32, ph)
            et_tf = sb("et_tf", [128, 2, 32, 8], F32, ph)

            def etab(name, mult_ap, L, dst_re, dst_im, rkeys, wkeys):
                shp = [128, 2, 32, L]
                name = "et"
                ang = et_ang[:, :, :, 0:L]
                ex = et_ex[:, :, :, 0:L]
                tmp = et_tmp[:, :, :, 0:L]
                ti = et_ti[:, :, :, 0:L]
                tf = et_tf[:, :, :, 0:L]
                a_b = ath[:, 0, :].rearrange("p (d g) -> p d g", d=2).unsqueeze(3).to_broadcast(shp)
                t_b = ath[:, 1, :].rearrange("p (d g) -> p d g", d=2).unsqueeze(3).to_broadcast(shp)
                P.add("dve", lambda e: e.tensor_tensor(out=ex[:], in0=a_b, in1=mult_ap, op=ALU.mult),
                      reads=["ath"] + rkeys, writes=[name + "_ex"])
                P.add("act", lambda e: e.activation(out=ex[:], in_=ex[:], func=AF.Exp), reads=[name + "_ex"], writes=[name + "_ex"])
                P.add("dve", lambda e: e.tensor_tensor(out=ang[:], in0=t_b, in1=mult_ap, op=ALU.mult),
                      reads=["ath"] + rkeys, writes=[name + "_ang"])
                for (dst, shift) in ((dst_im, 32.0), (dst_re, 32.25)):
                    P.add("dve", lambda e, shift=shift: e.tensor_scalar(out=tmp[:], in0=ang[:], scalar1=1.0 / (2.0 * PI), scalar2=shift,
                                                                        op0=ALU.mult, op1=ALU.add),
                          reads=[name + "_ang"], writes=[name + "_tmp"])
                    P.add("dve", lambda e: e.tensor_copy(ti[:], tmp[:]), reads=[name + "_tmp"], writes=[name + "_ti"])
                    P.add("dve", lambda e: e.tensor_copy(tf[:], ti[:]), reads=[name + "_ti"], writes=[name + "_tf"])
                    P.add("dve", lambda e: e.tensor_tensor(out=tmp[:], in0=tmp[:], in1=tf[:], op=ALU.subtract),
                          reads=[name + "_tmp", name + "_tf"], writes=[name + "_tmp"])
                    P.add("dve", lambda e: e.tensor_single_scalar(tf[:], tmp[:], 0.5, ALU.is_gt),
                          reads=[name + "_tmp"], writes=[name + "_tf"])
                    P.add("dve", lambda e: e.tensor_tensor(out=tmp[:], in0=tmp[:], in1=tf[:], op=ALU.subtract),
                          reads=[name + "_tmp", name + "_tf"], writes=[name + "_tmp"])
                    P.add("act", lambda e: e.activation(out=tmp[:], in_=tmp[:], func=AF.Sin, scale=2.0 * PI), reads=[name + "_tmp"],
                          writes=[name + "_tmp"])
                    P.add("dve", lambda e, dst=dst: e.tensor_tensor(out=dst, in0=tmp[:], in1=ex[:], op=ALU.mult),
                          reads=[name + "_tmp", name + "_ex"], writes=wkeys)

            one1 = sb("one1", [128, 1], F32, ph)
            P.add("dve", lambda e: e.memset(one1[:], 1.0), writes=["one1"])
            E1 = sb("E1", [128, 2, 2, 32, 1], F32, ph)
            etab("e1", one1[:].unsqueeze(2).unsqueeze(3).to_broadcast([128, 2, 32, 1]), 1, E1[:, 0], E1[:, 1], ["one1"], ["E1"])
            eight = sb("eight", [128, 1], F32, ph)
            P.add("dve", lambda e: e.memset(eight[:], 8.0), writes=["eight"])
            etab("e8", eight[:].unsqueeze(2).unsqueeze(3).to_broadcast([128, 2, 32, 1]), 1,
                 Acplx[:, 0, :].rearrange("p (d g o) -> p d g o", d=2, o=1),
                 Acplx[:, 1, :].rearrange("p (d g o) -> p d g o", d=2, o=1), ["eight"], ["Acplx"])
            ET = [sb("ET%d" % u_, [128, 2, 2, 32, 8], F32, ph) for u_ in range(3)]
            for u_ in range(3):
                etab("et%d" % u_, KM[:, u_, :, :].unsqueeze(2).to_broadcast([128, 2, 32, 8]), 8,
                     ET[u_][:, 0], ET[u_][:, 1], ["KM"], ["ET%d" % u_])

            if stop == 10:
                P.add("sp", None, reads=["out", "scrW1", "scrT", "scrW2", "scrU", "scrX1"] + list(dbg.keys()))
                P.emit()
                return nc, dbg
            LR = LRI[:, 0, :]
            LI = LRI[:, 1, :]
            e1r = E1[:, 0].rearrange("p d g o -> p (d g o)")
            e1i = E1[:, 1].rearrange("p d g o -> p (d g o)")
            cf = sb("cf", [128, 6, 64], F32, ph)
            seq_ops = [
                lambda e: e.tensor_scalar(out=cf[:, 0, :], in0=e1r, scalar1=-1.0, scalar2=None, op0=ALU.add),
                lambda e: e.tensor_tensor(out=cf[:, 1, :], in0=LR, in1=LR, op=ALU.mult),
                lambda e: e.tensor_tensor(out=cf[:, 2, :], in0=LI, in1=LI, op=ALU.mult),
                lambda e: e.tensor_tensor(out=cf[:, 1, :], in0=cf[:, 1, :], in1=cf[:, 2, :], op=ALU.add),
                lambda e: e.reciprocal(out=cf[:, 1, :], in_=cf[:, 1, :]),
                lambda e: e.tensor_tensor(out=cf[:, 2, :], in0=cf[:, 0, :], in1=LR, op=ALU.mult),
                lambda e: e.tensor_tensor(out=cf[:, 3, :], in0=e1i, in1=LI, op=ALU.mult),
                lambda e: e.tensor_tensor(out=cf[:, 2, :], in0=cf[:, 2, :], in1=cf[:, 3, :], op=ALU.add),
                lambda e: e.tensor_tensor(out=cf[:, 4, :], in0=cf[:, 2, :], in1=cf[:, 1, :], op=ALU.mult),
                lambda e: e.tensor_tensor(out=cf[:, 2, :], in0=e1i, in1=LR, op=ALU.mult),
                lambda e: e.tensor_tensor(out=cf[:, 3, :], in0=cf[:, 0, :], in1=LI, op=ALU.mult),
                lambda e: e.tensor_tensor(out=cf[:, 2, :], in0=cf[:, 2, :], in1=cf[:, 3, :], op=ALU.subtract),
                lambda e: e.tensor_tensor(out=cf[:, 5, :], in0=cf[:, 2, :], in1=cf[:, 1, :], op=ALU.mult),
            ]
            for f_ in seq_ops:
                P.add("dve", f_, reads=["E1", "LRI", "cf"], writes=["cf"])
            Braw = sb("Braw", [128, 2, 64, 16], F32, ph)
            for i, src_ in enumerate((ssm_b_re, ssm_b_im)):
                for d_ in range(2):
                    v = src_[d_].rearrange("(g2 gp) p m -> gp p g2 m", gp=2)
                    for gp in range(2):
                        P.add("sp", lambda e, i=i, d_=d_, gp=gp, v=v: e.dma_start(
                            out=Braw[gp * 64:(gp + 1) * 64, i, d_ * 32:(d_ + 1) * 32, :], in_=v[gp]),
                            writes=[("Braw", i, d_, gp)], group="Braw")
            bbar = sb("bbar", [128, 2, 64, 16], F32, ph)
            tA = sb("s0tA", [128, 64, 16], F32, ph)
            cre_b = cf[:, 4, :].unsqueeze(2).to_broadcast([128, 64, 16])
            cim_b = cf[:, 5, :].unsqueeze(2).to_broadcast([128, 64, 16])
            P.add("dve", lambda e: e.tensor_tensor(out=bbar[:, 0], in0=Braw[:, 0], in1=cre_b, op=ALU.mult), reads=["Braw", "cf"], writes=[("bbar", 0)])
            P.add("dve", lambda e: e.tensor_tensor(out=tA[:], in0=Braw[:, 1], in1=cim_b, op=ALU.mult), reads=["Braw", "cf"], writes=["s0tA"])
            P.add("dve", lambda e: e.tensor_tensor(out=bbar[:, 0], in0=bbar[:, 0], in1=tA[:], op=ALU.subtract), reads=["s0tA", ("bbar", 0)], writes=[("bbar", 0)])
            P.add("dve", lambda e: e.tensor_tensor(out=bbar[:, 1], in0=Braw[:, 1], in1=cre_b, op=ALU.mult), reads=["Braw", "cf"], writes=[("bbar", 1)])
            P.add("dve", lambda e: e.tensor_tensor(out=tA[:], in0=Braw[:, 0], in1=cim_b, op=ALU.mult), reads=["Braw", "cf", ("bbar", 0)], writes=["s0tA"])
            P.add("dve", lambda e: e.tensor_tensor(out=bbar[:, 1], in0=bbar[:, 1], in1=tA[:], op=ALU.add), reads=["s0tA", ("bbar", 1)], writes=[("bbar", 1)])

            if stop == 11:
                P.add("sp", None, reads=["out", "scrW1", "scrT", "scrW2", "scrU", "scrX1"] + list(dbg.keys()))
                P.emit()
                return nc, dbg
            CT = sb("CT", [128, 2, 64, 16], F32, ph)
            craw = [sb("craw%d" % i, [128, 128], F32, ph) for i in range(2)]
            nb = 0
            for i, src_ in enumerate((ssm_c_re, ssm_c_im)):
                for d_ in range(2):
                    for q in range(4):
                        b_ = nb % 2
                        nb += 1
                        for g2l in range(8):
                            g2 = q * 8 + g2l
                            P.add("sp", lambda e, b_=b_, g2l=g2l, g2=g2, d_=d_, src_=src_: e.dma_start(
                                out=craw[b_][g2l * 16:(g2l + 1) * 16, :].rearrange("n (gp p) -> n gp p", gp=2),
                                in_=src_[d_, 2 * g2:2 * g2 + 2, :, :].rearrange("gp n p -> n gp p")),
                                writes=[("craw", b_, g2l)], group="craw%d" % b_)
                        P.add("pe", lambda e, b_=b_: e.transpose(out=ptq[:, 3, :], in_=craw[b_][:], identity=ident_f[:]),
                              reads=[("craw", b_), "ident_f"], writes=["s0pt"])
                        P.add("dve", lambda e, i=i, d_=d_, q=q: e.tensor_copy(
                            CT[:, i, d_ * 32 + q * 8:d_ * 32 + (q + 1) * 8, :], ptq[:, 3, :].rearrange("p (a n) -> p a n", n=16)),
                            reads=["s0pt"], writes=[("CT", i, d_, q)])

            if stop == 12:
                P.add("sp", None, reads=["out", "scrW1", "scrT", "scrW2", "scrU", "scrX1"] + list(dbg.keys()))
                P.emit()
                return nc, dbg
            mk = sb("mk", [128, 2, 128], F32, ph)
            mi = sb("mki", [128, 2, 128], I32, ph)
            mf = sb("mkf", [128, 2, 128], F32, ph)
            P.add("pool", lambda e: e.iota(mi[:, 0, :], [[1, 128]], base=0, channel_multiplier=0), writes=[("mki", 0)])
            P.add("pool", lambda e: e.iota(mi[:, 1, :], [[0, 128]], base=0, channel_multiplier=1), writes=[("mki", 1)])
            P.add("dve", lambda e: e.tensor_single_scalar(mi[:], mi[:], 4, ALU.arith_shift_right), reads=["mki"], writes=["mki"])
            P.add("dve", lambda e: e.tensor_copy(mf[:], mi[:]), reads=["mki"], writes=["mkf"])
            P.add("dve", lambda e: e.tensor_tensor(out=mk[:, 0, :], in0=mf[:, 0, :], in1=mf[:, 1, :], op=ALU.is_ge), reads=["mkf"], writes=[("mk", 0)])
            P.add("dve", lambda e: e.tensor_tensor(out=mk[:, 1, :], in0=mf[:, 1, :], in1=mf[:, 0, :], op=ALU.is_ge), reads=["mkf"], writes=[("mk", 1)])


            if stop == 13:
                P.add("sp", None, reads=["out", "scrW1", "scrT", "scrW2", "scrU", "scrX1"] + list(dbg.keys()))
                P.emit()
                return nc, dbg
            oR = sb("oR", [128, 32, 8, 16], F32, ph)
            oI = sb("oI", [128, 32, 8, 16], F32, ph)
            o2R = sb("o2R", [128, 32, 8, 16], F32, ph)
            o2I = sb("o2I", [128, 32, 8, 16], F32, ph)
            t1 = sb("s0t1", [128, 32, 8, 16], F32, ph)
            stg = sb("s0stg", [128, 4, 128], BF16, ph)
            stg2 = [sb("s0stg2", [128, 32, 128], BF16, ph)] * 2
            pT = ps("s0pT", [128, 4, 128], F32, ph)
            pT2 = ps("s0pT2", [128, 4, 128], F32, ph)

            def couter(u_, d_, Br, Bi, dR, dI, neg_im, rk, tag):
                shp = [128, 32, 8, 16]
                Er = ET[u_][:, 0, d_].unsqueeze(3).to_broadcast(shp)
                Ei = ET[u_][:, 1, d_].unsqueeze(3).to_broadcast(shp)
                Brb = Br.unsqueeze(2).to_broadcast(shp)
                Bib = Bi.unsqueeze(2).to_broadcast(shp)
                rk = rk + ["ET%d" % u_]
                P.add("dve", lambda e: e.tensor_tensor(out=dR[:], in0=Er, in1=Brb, op=ALU.mult), reads=rk, writes=[tag + "R"])
                P.add("pool", lambda e: e.tensor_tensor(out=t1[:], in0=Ei, in1=Bib, op=ALU.mult), reads=rk, writes=["s0t1"])
                P.add("dve", lambda e: e.tensor_tensor(out=dR[:], in0=dR[:], in1=t1[:], op=ALU.subtract), reads=[tag + "R", "s0t1"], writes=[tag + "R"])
                P.add("dve", lambda e: e.tensor_tensor(out=dI[:], in0=Er, in1=Bib, op=ALU.mult), reads=rk, writes=[tag + "I"])
                P.add("pool", lambda e: e.tensor_tensor(out=t1[:], in0=Ei, in1=Brb, op=ALU.mult), reads=rk + [tag + "R"], writes=["s0t1"])
                if neg_im:
                    P.add("dve", lambda e: e.scalar_tensor_tensor(out=dI[:], in0=dI[:], scalar=-1.0, in1=t1[:], op0=ALU.mult, op1=ALU.subtract),
                          reads=[tag + "I", "s0t1"], writes=[tag + "I"])
                else:
                    P.add("dve", lambda e: e.tensor_tensor(out=dI[:], in0=dI[:], in1=t1[:], op=ALU.add), reads=[tag + "I", "s0t1"], writes=[tag + "I"])

            for d_ in range(2):
                gs = slice(d_ * 32, (d_ + 1) * 32)
                couter(0, d_, bbar[:, 0, gs, :], bbar[:, 1, gs, :], oR, oI, False, ["bbar"], "o")
                for part, src_t, skey in ((0, oR, "oR"), (1, oI, "oI")):
                    for q in range(8):
                        def trw(e, src_t=src_t, q=q):
                            ins = None
                            for j in range(4):
                                ins = e.transpose(out=pT[:, j, :], in_=src_t[:, q * 4 + j].rearrange("p s m -> p (s m)"), identity=ident_f[:])
                            return ins
                        P.add("pe", trw, reads=[skey, "ident_f"], writes=["s0pT"])
                        P.add("act", lambda e: e.activation(out=stg[:], in_=pT[:], func=AF.Copy), reads=["s0pT"], writes=["s0stg"])
                        P.add("sp", lambda e, d_=d_, q=q, part=part: e.dma_start(
                            out=scrW1[d_, q * 4:(q + 1) * 4, part].rearrange("g r c -> r g c"), in_=stg[:]),
                            reads=["s0stg"], writes=["scrW1"], group="s0st")

                if stop == 14:
                    P.add("sp", None, reads=["out", "scrW1", "scrT", "scrW2", "scrU", "scrX1"] + list(dbg.keys()))
                    P.emit()
                    return nc, dbg
                couter(1, d_, bbar[:, 0, gs, :], bbar[:, 1, gs, :], oR, oI, False, ["bbar"], "o")
                couter(2, d_, CT[:, 0, gs, :], CT[:, 1, gs, :], o2R, o2I, True, ["CT"], "o2")
                for part, src_t, skey in ((0, o2R, "o2R"), (1, o2I, "o2I")):
                    P.add("act", lambda e, part=part, src_t=src_t: e.activation(
                        out=stg2[part][:], in_=src_t[:].rearrange("p g t n -> p g (t n)"), func=AF.Copy),
                        reads=[skey], writes=["s0stg2"])
                    P.add("sp", lambda e, d_=d_, part=part: e.dma_start(
                        out=scrW2[d_, :, part].rearrange("g r c -> r g c"), in_=stg2[part][:]),
                        reads=["s0stg2"], writes=["scrW2"], group="s0st2")

                if stop == 15:
                    P.add("sp", None, reads=["out", "scrW1", "scrT", "scrW2", "scrU", "scrX1"] + list(dbg.keys()))
                    P.emit()
                    return nc, dbg
                for q in range(8):
                    for gp in range(2):
                        pTx = pT if gp == 0 else pT2
                        pkey = "s0pT" if gp == 0 else "s0pT2"
                        rs = slice(gp * 64, (gp + 1) * 64)

                        def mmT(e, q=q, gp=gp, pTx=pTx, rs=rs):
                            ins = None
                            for j in range(4):
                                g2 = q * 4 + j
                                e.matmul(pTx[:, j, :], lhsT=oR[rs, g2].rearrange("p s m -> p (s m)"),
                                         rhs=o2R[rs, g2].rearrange("p t n -> p (t n)"), start=True, stop=False)
                                ins = e.matmul(pTx[:, j, :], lhsT=oI[rs, g2].rearrange("p s m -> p (s m)"),
                                               rhs=o2I[rs, g2].rearrange("p t n -> p (t n)"), start=False, stop=True)
                            return ins
                        P.add("pe", mmT, reads=["oR", "oI", "o2R", "o2I"], writes=[pkey])
                        P.add("dve", lambda e, d_=d_, pTx=pTx: e.tensor_tensor(
                            out=stg[:], in0=pTx[:], in1=mk[:, d_:d_ + 1, :].to_broadcast([128, 4, 128]), op=ALU.mult),
                            reads=[pkey, "mk"], writes=["s0stg"])
                        P.add("sp", lambda e, d_=d_, q=q, gp=gp: e.dma_start(
                            out=scrT[d_, q * 8:(q + 1) * 8].rearrange("(j gp) r c -> gp r j c", gp=2)[gp], in_=stg[:]),
                            reads=["s0stg"], writes=["scrT"], group="s0st")
            P.barrier()
        if debug:
            d_A = dout("d_A", [128, 128])
            P.add("sp", lambda e: e.dma_start(out=d_A, in_=Acplx[:].rearrange("p a b -> p (a b)")), reads=["Acplx"],
                  writes=["d_A"], group="dbgA")

        Z = sb("Z", [128, 8, 8, 128], BF16, mixer)
        P.add("pool", lambda e: e.memset(Z[:], 0.0), writes=["Z"])
        for a_ in range(8):
            for b_ in range(8):
                P.add("dve" if (a_ + b_) % 2 else "pool", lambda e, a_=a_, b_=b_: e.tensor_single_scalar(
                    Z[:, a_, b_, 16 * b_:16 * b_ + 16], iotf[:, 16 * b_:16 * b_ + 16], float(16 * (b_ - a_)), ALU.is_equal),
                    reads=["iotf"], writes=[("Z", a_, b_)])
        mix = mixer.enter_context(ExitStack())
        gT = sb("gT", [128, 8, NOWN], BF16, mix)
        ssm = mix.enter_context(ExitStack())
        U = sb("U", [128, 64, 128], BF16, ssm)
        Pt = sb("Pt", [128, 2, 2, 32, 288], BF16, ssm)
        s12 = ssm.enter_context(ExitStack())
        Ur = sb("Ur", [128, 64, 160], BF16, s12)
        with ExitStack() as ph:
            pU = [ps("pU%d" % i, [128, 288], F32, ph) for i in range(3)]
            ucat = [sb("ucat%d" % i, [128, NSEQ], BF16, ph) for i in range(2)]
            for g in range(64):
                ct, gl = g // 8, g % 8
                pb = g % 3
                if gl == 0:
                    P.add("sp", lambda e, ct=ct: e.dma_start(
                        out=ucat[ct % 2][:].rearrange("p (s j) -> p s j", s=8)[:, :, 128:288],
                        in_=scrU[ct].rearrange("p (s j) -> p s j", s=8)), reads=["scrU"],
                        writes=[("ucat", ct % 2, 1)], group="ucat%d" % (ct % 2))
                    P.add("pool", lambda e, ct=ct: e.tensor_copy(
                        ucat[ct % 2][:].rearrange("p (s j) -> p s j", s=8)[:, :, 0:128],
                        uTown[:, ct, :].rearrange("p (s j) -> p s j", s=8)), reads=["uT"],
                        writes=[("ucat", ct % 2, 0)])

                def shf(e, ct=ct, gl=gl, pb=pb):
                    ins = None
                    for s_ in range(8):
                        src = ucat[ct % 2][:, s_ * 288:(s_ + 1) * 288]
                        ins = e.matmul(pU[pb][:, 0:288], lhsT=Z[:, gl, s_, :], rhs=src, start=(s_ == 0), stop=(s_ == 7))
                    return ins
                P.add("pe", shf, reads=["Z", ("ucat", ct % 2)], writes=["pU%d" % pb])
                P.add("dve", lambda e, g=g, pb=pb: e.tensor_copy(U[:, g, :], pU[pb][:, 0:128]), reads=["pU%d" % pb], writes=[("U", g)])
                P.add("act", lambda e, g=g, pb=pb: e.activation(out=Ur[:, g, :], in_=pU[pb][:, 128:288], func=AF.Copy),
                      reads=["pU%d" % pb], writes=[("U", g)])
            P.barrier()
        with ExitStack() as ph:
            w1c = [sb("w1c%d" % i, [128, 8, 2, 128], BF16, ph) for i in range(2)]
            pP = [ps("pP%d" % i, [128, 288], F32, ph) for i in range(3)]
            nn = 0
            for d_ in range(2):
                for q in range(4):
                    wb_ = (d_ * 4 + q) % 2
                    P.add("sp", lambda e, d_=d_, q=q, wb_=wb_: e.dma_start(
                        out=w1c[wb_][:], in_=scrW1[d_, q * 8:(q + 1) * 8].rearrange("g part r c -> r g part c")),
                        reads=["scrW1"], writes=[("w1c", wb_)], group="w1c%d" % wb_)
                    for g2l in range(8):
                        g2 = q * 8 + g2l
                        for part in range(2):
                            pb = nn % 3
                            nn += 1

                            def mmP(e, d_=d_, g2=g2, g2l=g2l, part=part, pb=pb, wb_=wb_):
                                ins = None
                                for gp in range(2):
                                    g = 2 * g2 + gp
                                    lw = w1c[wb_][:, g2l, part, gp * 64:(gp + 1) * 64]
                                    rows = slice(gp * 64, (gp + 1) * 64)
                                    if d_ == 0:
                                        e.matmul(pP[pb][rows, 0:32], lhsT=lw, rhs=Ur[:, g, 128:160], start=True, stop=True)
                                        ins = e.matmul(pP[pb][rows, 32:160], lhsT=lw, rhs=U[:, g, 0:128], start=True, stop=True)
                                    else:
                                        e.matmul(pP[pb][rows, 0:128], lhsT=lw, rhs=U[:, g, 0:128], start=True, stop=True)
                                        ins = e.matmul(pP[pb][rows, 128:288], lhsT=lw, rhs=Ur[:, g, 0:160], start=True, stop=True)
                                return ins
                            P.add("pe", mmP, reads=[("w1c", wb_), "U"], writes=["pP%d" % pb])
                            ncol = 160 if d_ == 0 else 288
                            if nn % 2 == 0:
                                P.add("dve", lambda e, d_=d_, g2=g2, part=part, pb=pb, ncol=ncol: e.tensor_copy(
                                    Pt[:, part, d_, g2, 0:ncol], pP[pb][:, 0:ncol]), reads=["pP%d" % pb], writes=[("Pt", part, d_, g2)])
                            else:
                                P.add("act", lambda e, d_=d_, g2=g2, part=part, pb=pb, ncol=ncol: e.activation(
                                    out=Pt[:, part, d_, g2, 0:ncol], in_=pP[pb][:, 0:ncol], func=AF.Copy),
                                    reads=["pP%d" % pb], writes=[("Pt", part, d_, g2)])
            P.barrier()
        s12.close()
        with ExitStack() as ph:
            St = [sb("St%d" % i, [128, 4, 64], F32, ph) for i in range(2)]
            C4 = sb("C4", [128, 4, 64], F32, ph)
            rt1 = sb("rt1", [128, 4, 64], F32, ph)
            rt2 = sb("rt2", [128, 2, 64], F32, ph)
            P.add("dve", lambda e: e.memset(St[0][:], 0.0), writes=["St0"])
            P.add("dve", lambda e: e.tensor_copy(C4[:, 0:2, :], Acplx[:, 0:1, :].to_broadcast([128, 2, 64])), reads=["Acplx"], writes=["C4"])
            P.add("dve", lambda e: e.tensor_scalar(out=C4[:, 2, :], in0=Acplx[:, 1, :], scalar1=-1.0, scalar2=None, op0=ALU.mult),
                  reads=["Acplx", "C4"], writes=["C4"])
            P.add("dve", lambda e: e.tensor_copy(C4[:, 3, :], Acplx[:, 1, :]), reads=["Acplx", "C4"], writes=["C4"])
            Pt_full = Pt[:]
            pstep = Pt_full.ap[0][0]
            PART = 2 * 32 * 288
            for i in range(288):
                cur, nxt = St[i % 2], St[(i + 1) % 2]
                ck, nk = "St%d" % (i % 2), "St%d" % ((i + 1) % 2)
                qF, qB = i, 287 - i
                if i < 160:
                    cs = slice(0, 64)
                    dd = [[32 * 288 + qB - qF, 2], [288, 32]]
                    off = Pt_full.offset + qF
                    vv = lambda t_, a, b: t_[:, a:b, :].rearrange("p a (d g) -> p a d g", d=2)
                else:
                    cs = slice(32, 64)
                    dd = [[288, 32]]
                    off = Pt_full.offset + 32 * 288 + qB
                    vv = lambda t_, a, b: t_[:, a:b, 32:64]
                pap = bass.AP(Pt_full.tensor, off, [[pstep, 128], [PART, 2]] + dd)
                pap_sw = bass.AP(Pt_full.tensor, off + PART, [[pstep, 128], [-PART, 2]] + dd)
                P.add("dve", lambda e, cur=cur, cs=cs: e.tensor_tensor(out=rt1[:, :, cs], in0=C4[:, :, cs], in1=cur[:, :, cs], op=ALU.mult),
                      reads=[ck, "C4"], writes=["rt1"])
                P.add("dve", lambda e, cs=cs: e.tensor_tensor(out=rt2[:, :, cs], in0=rt1[:, 0:2, cs], in1=rt1[:, 2:4, cs], op=ALU.add),
                      reads=["rt1"], writes=["rt2"])
                P.add("dve", lambda e, nxt=nxt, vv=vv, pap=pap: e.tensor_tensor(out=vv(nxt, 0, 2), in0=vv(rt2, 0, 2), in1=pap, op=ALU.add),
                      reads=["rt2", ("PtS", i)], writes=[(nk, 0)])
                sw_in = bass.AP(rt2[:].tensor, rt2[:].offset + 64 + (32 if i >= 160 else 0), [[rt2[:].ap[0][0], 128], [-64, 2]] + ([[32, 2], [1, 32]] if i < 160 else [[1, 32]]))
                P.add("dve", lambda e, nxt=nxt, vv=vv, pap_sw=pap_sw, sw_in=sw_in: e.tensor_tensor(out=vv(nxt, 2, 4), in0=sw_in, in1=pap_sw, op=ALU.add),
                      reads=["rt2", ("PtS", i)], writes=[(nk, 1)])
                P.add("act", lambda e, nxt=nxt, vv=vv, pap=pap: e.activation(out=pap, in_=vv(nxt, 0, 2), func=AF.Copy),
                      reads=[(nk, 0)], writes=[("PtS", i)])
            P.barrier()
        if debug:
            d_H = dout("d_H", [128, 2 * 2 * 32 * 288], BF16)
            P.add("sp", lambda e: e.dma_start(out=d_H, in_=Pt[:].rearrange("p a b c d -> p (a b c d)")), reads=["Pt", "PtS"],
                  writes=["d_H"], group="dbgH")

        with ExitStack() as ph:
            Ysb = sb("Ysb", [128, 64, 128], BF16, ph)
            tch = [sb("tch%d" % i, [128, 2, 8, 128], BF16, ph) for i in range(1)] * 2
            w2ch = [sb("w2ch%d" % i, [128, 2, 4, 2, 128], BF16, ph) for i in range(1)] * 2
            pY = [ps("pY%d" % i, [128, 4, 128], F32, ph) for i in range(2)]
            for q in range(8):
                b_ = 0
                for d_ in range(2):
                    P.add("sp", lambda e, q=q, b_=b_, d_=d_: e.dma_start(
                        out=tch[b_][:, d_], in_=scrT[d_, q * 8:(q + 1) * 8].rearrange("g r c -> r g c")),
                        reads=["scrT"], writes=[("tch", b_, d_)], group="tch%d" % b_)
                    P.add("sp", lambda e, q=q, b_=b_, d_=d_: e.dma_start(
                        out=w2ch[b_][:, d_], in_=scrW2[d_, q * 4:(q + 1) * 4].rearrange("g part r c -> r g part c")),
                        reads=["scrW2"], writes=[("w2ch", b_, d_)], group="w2ch%d" % b_)
                for hh in range(2):
                    pb = (q * 2 + hh) % 2

                    def mmY(e, q=q, hh=hh, pb=pb, b_=b_):
                        ins = None
                        for j in range(4):
                            gl = hh * 4 + j
                            g = q * 8 + gl
                            g2, gp = g // 2, g % 2
                            g2l = g2 - q * 4
                            rows = slice(gp * 64, (gp + 1) * 64)
                            o = pY[pb][:, j, :]
                            e.matmul(o, lhsT=tch[b_][:, 0, gl, :], rhs=U[:, g, 0:128], start=True, stop=False)
                            e.matmul(o, lhsT=w2ch[b_][rows, 0, g2l, 0, :], rhs=Pt[rows, 0, 0, g2, 31:159], start=False, stop=False)
                            e.matmul(o, lhsT=w2ch[b_][rows, 0, g2l, 1, :], rhs=Pt[rows, 1, 0, g2, 31:159], start=False, stop=False)
                            e.matmul(o, lhsT=tch[b_][:, 1, gl, :], rhs=U[:, g, 0:128], start=False, stop=False)
                            e.matmul(o, lhsT=w2ch[b_][rows, 1, g2l, 0, :], rhs=Pt[rows, 0, 1, g2, 1:129], start=False, stop=False)
                            ins = e.matmul(o, lhsT=w2ch[b_][rows, 1, g2l, 1, :], rhs=Pt[rows, 1, 1, g2, 1:129], start=False, stop=True)
                        return ins
                    P.add("pe", mmY, reads=[("tch", b_), ("w2ch", b_), "U", "Pt", "PtS"], writes=["pY%d" % pb])
                    g0 = q * 8 + hh * 4
                    if hh == 0:
                        P.add("dve", lambda e, g0=g0, pb=pb: e.tensor_copy(Ysb[:, g0:g0 + 4, :], pY[pb][:]), reads=["pY%d" % pb],
                              writes=[("Ysb", g0)])
                    else:
                        P.add("act", lambda e, g0=g0, pb=pb: e.activation(out=Ysb[:, g0:g0 + 4, :], in_=pY[pb][:], func=AF.Copy),
                              reads=["pY%d" % pb], writes=[("Ysb", g0)])
            pZ = [ps("pZ%d" % i, [128, 512], F32, ph) for i in range(2)]
            yf = [sb("yf%d" % i, [128, 512], F32, ph) for i in range(2)]
            ya = [sb("ya%d" % i, [128, 512], F32, ph) for i in range(2)]
            for ct in range(8):
                chains = []
                for half in range(2):
                    pb = half

                    def uns(e, ct=ct, half=half, pb=pb):
                        ins = None
                        ov = pZ[pb][:, :].rearrange("p (j t) -> p t j", t=8)
                        for t_ in range(8):
                            for gl in range(8):
                                ins = e.matmul(ov[:, t_, :], lhsT=Z[:, t_, gl, :], rhs=Ysb[:, ct * 8 + gl, half * 64:(half + 1) * 64],
                                               start=(gl == 0), stop=(gl == 7))
                        return ins
                    P.add("pe", uns, reads=["Z", "Ysb"], writes=["pZ%d" % pb])
                    tk = slice(half * 512, (half + 1) * 512)
                    yk, ak = "yf%d" % pb, "ya%d" % pb
                    chains.append([
                        ("dve", lambda e, ct=ct, pb=pb, half=half: e.scalar_tensor_tensor(
                            out=yf[pb][:].rearrange("p (j s) -> p j s", s=8),
                            in0=uTown[:, ct, :].rearrange("p (s j) -> p j s", s=8)[:, half * 64:(half + 1) * 64, :],
                            scalar=colB[:, ct:ct + 1], in1=pZ[pb][:, :].rearrange("p (j s) -> p j s", s=8), op0=ALU.mult, op1=ALU.add),
                         ["uT", "colB", "pZ%d" % pb], [yk]),
                        ("act", lambda e, pb=pb: e.activation(out=ya[pb][:], in_=yf[pb][:], func=AF.Square), [yk], [ak]),
                        ("dve", lambda e, pb=pb: e.tensor_scalar(out=ya[pb][:], in0=ya[pb][:], scalar1=0.044715, scalar2=1.0,
                                                                 op0=ALU.mult, op1=ALU.add), [ak], [ak]),
                        ("dve", lambda e, pb=pb: e.tensor_tensor(out=ya[pb][:], in0=ya[pb][:], in1=yf[pb][:], op=ALU.mult), [ak, yk], [ak]),
                        ("act", lambda e, pb=pb: e.activation(out=ya[pb][:], in_=ya[pb][:], func=AF.Sigmoid, scale=1.5957691216057308),
                         [ak], [ak]),
                        ("dve", lambda e, pb=pb, ct=ct, tk=tk: e.tensor_tensor(out=gT[:, ct, tk], in0=ya[pb][:], in1=yf[pb][:], op=ALU.mult),
                         [ak, yk], [("gT", ct, half)]),
                    ])
                for k in range(6):
                    for ch in chains:
                        en, f_, rk, wk = ch[k]
                        P.add(en, f_, reads=rk, writes=wk)
            P.barrier()
        ssm.close()
        if debug:
            d_gT = dout("d_gT", [128, 8 * NOWN], BF16)
            P.add("sp", lambda e: e.dma_start(out=d_gT, in_=gT[:].rearrange("p a b -> p (a b)")), reads=["gT"],
                  writes=["d_gT"], group="dbgG")

        ssmT = sb("ssmT", [128, 8, NOWN], BF16, mix)
        rstd = sb("rstdSC", [128, 2, NOWN], F32, mix)
        with ExitStack() as ph:
            wglu = sb("wglu", [128, 8, 2048], BF16, ph)
            wgv = w_glu.rearrange("(kt p) c -> p kt c", p=128)
            for kt in range(8):
                for hc in range(2):
                    P.add("pool", lambda e, kt=kt, hc=hc: e.dma_start(out=wglu[:, kt, hc * 1024:(hc + 1) * 1024],
                                                                      in_=wgv[:, kt, hc * 1024:(hc + 1) * 1024]),
                          writes=[("wglu", kt, hc)], group="wglu")
            pga = [ps("pga%d" % i, [128, 512], F32, ph) for i in range(2)]
            pgb = [ps("pgb%d" % i, [128, 512], F32, ph) for i in range(2)]
            pss = ps("pss", [128, 512], F32, ph)
            sig = [sb("sig%d" % i, [128, 512], F32, ph) for i in range(2)]
            sq = [sb("sq%d" % i, [128, 512], BF16, ph) for i in range(2)]
            pend = []
            for which in range(2):
                for half in range(2):
                    tk = slice(half * 512, (half + 1) * 512)
                    for ot in range(8):
                        b_ = ot % 2
                        if which == 0:
                            def mg(e, ot=ot, b_=b_, tk=tk, off=0, pp=pga):
                                ins = None
                                for kt in range(8):
                                    ins = e.matmul(pp[b_][:, :], lhsT=wglu[:, kt, off + ot * 128:off + (ot + 1) * 128], rhs=gT[:, kt, tk],
                                                   start=(kt == 0), stop=(kt == 7))
                                return ins
                            P.add("pe", mg, reads=["wglu", "gT"], writes=["pga%d" % b_])
                            P.add("pe", lambda e, ot=ot, b_=b_, tk=tk: mg(e, ot, b_, tk, 1024, pgb), reads=["wglu", "gT"], writes=["pgb%d" % b_])
                            P.add("act", lambda e, b_=b_: e.activation(out=sig[b_][:], in_=pgb[b_][:], func=AF.Sigmoid),
                                  reads=["pgb%d" % b_], writes=["sig%d" % b_])
                            P.add("dve", lambda e, b_=b_, ot=ot, tk=tk: e.tensor_tensor(out=ssmT[:, ot, tk], in0=pga[b_][:], in1=sig[b_][:], op=ALU.mult),
                                  reads=["pga%d" % b_, "sig%d" % b_], writes=[("ssmT", ot, half)])
                            srcT, skey = ssmT, ("ssmT", ot, half)
                        else:
                            srcT, skey = convT, "convT"
                        def back(b_=b_, ot=ot, tk=tk, srcT=srcT, skey=skey):
                            P.add("act", lambda e: e.activation(out=sq[b_][:], in_=srcT[:, ot, tk], func=AF.Square),
                                  reads=[skey], writes=["sq%d" % b_])
                            P.add("pe", lambda e: e.matmul(pss[:, :], lhsT=ones_b[:], rhs=sq[b_][:], start=(ot == 0), stop=(ot == 7)),
                                  reads=["ones_b", "sq%d" % b_], writes=["pss"])
                        if pend:
                            pend.pop()()
                        pend.append(back)
                    if pend:
                        pend.pop()()
                    rk = ("rstd", which, half)
                    P.add("dve", lambda e, which=which, tk=tk: e.tensor_scalar(out=rstd[:, which, tk], in0=pss[:, :], scalar1=1.0 / 1024, scalar2=EPS,
                                                                              op0=ALU.mult, op1=ALU.add), reads=["pss"], writes=[rk])
                    P.add("act", lambda e, which=which, tk=tk: e.activation(out=rstd[:, which, tk], in_=rstd[:, which, tk], func=AF.Sqrt),
                          reads=[rk], writes=[rk])
                    P.add("dve", lambda e, which=which, tk=tk: e.reciprocal(out=rstd[:, which, tk], in_=rstd[:, which, tk]), reads=[rk], writes=[rk])
            for ot in range(8):
                P.add("dve", lambda e, ot=ot: e.scalar_tensor_tensor(out=ssmT[:, ot, :], in0=ssmT[:, ot, :], scalar=colA[:, 80 + ot:81 + ot],
                                                                     in1=rstd[:, 0, :], op0=ALU.mult, op1=ALU.mult),
                      reads=["ssmT", "rstd", "colA"], writes=[("ssmT", ot)])
                P.add("dve", lambda e, ot=ot: e.scalar_tensor_tensor(out=convT[:, ot, :], in0=convT[:, ot, :], scalar=colA[:, 88 + ot:89 + ot],
                                                                      in1=rstd[:, 1, :], op0=ALU.mult, op1=ALU.mult),
                      reads=["convT", "rstd", "colA"], writes=[("convT", ot)])
            P.barrier()

        def row_bcast(dst, col_of_ft, rkeys, wkey, stack_ps):
            dgs = [sb(wkey + "_dg%d" % i, [128, 128], F32, stack_ps) for i in range(2)]
            prb = ps(wkey + "_prb", [128, 512], F32, stack_ps)
            for c4 in range(4):
                for j in range(4):
                    ft = c4 * 4 + j
                    b_ = ft % 2
                    P.add("dve", lambda e, ft=ft, b_=b_: e.tensor_scalar(out=dgs[b_][:], in0=ident_f[:], scalar1=col_of_ft(ft), scalar2=None,
                                                                        op0=ALU.mult), reads=["ident_f"] + rkeys, writes=[wkey + "_dg%d" % b_])
                    P.add("pe", lambda e, j=j, b_=b_: e.matmul(prb[:, j * 128:(j + 1) * 128], lhsT=ones_f[:], rhs=dgs[b_][:], start=True, stop=True),
                          reads=["ones_f", wkey + "_dg%d" % b_], writes=[wkey + "_prb"])
                P.add("act", lambda e, c4=c4: e.activation(out=dst[:, c4 * 512:(c4 + 1) * 512], in_=prb[:, :], func=AF.Copy),
                      reads=[wkey + "_prb"], writes=[wkey])

        with ExitStack() as ph:
            g1b = sb("g1b", [128, D], F32, ph)
            with ExitStack() as ph3:
                row_bcast(g1b, lambda ft: modT[:, 32 + ft, 0:1], ["modT"], "g1b", ph3)
                P.barrier()
            woc = [sb("woc%d" % i, [128, 16, 512], BF16, ph) for i in range(2)]
            wov = w_out.rearrange("(kt p) c -> p kt c", p=128)
            po = [ps("po%d" % i, [128, 512], F32, ph) for i in range(3)]
            xp = [sb("xp%d" % i, [128, 512], F32, ph) for i in range(3)]
            x1p = [sb("x1p%d" % i, [128, 512], F32, ph) for i in range(3)]
            jk = sb("jk", [128, 512], BF16, ph)
            n = 0
            for cc in range(4):
                wb_ = cc % 2
                for kt in range(16):
                    P.add("pool", lambda e, kt=kt, cc=cc, wb_=wb_: e.dma_start(out=woc[wb_][:, kt, :], in_=wov[:, kt, cc * 512:(cc + 1) * 512]),
                          writes=[("woc", wb_, kt)], group="woc%d" % wb_)
                for tt in range(8):
                    b_ = n % 3
                    n += 1
                    rows = slice(tt * 128, (tt + 1) * 128)
                    cols = slice(cc * 512, (cc + 1) * 512)
                    P.add("sp", lambda e, b_=b_, rows=rows, cols=cols: e.dma_start(out=xp[b_][:], in_=xs[rows, cols]), writes=[("xp", b_)],
                          group="xp%d" % b_)

                    def mo(e, tt=tt, b_=b_, wb_=wb_):
                        ins = None
                        for ht in range(16):
                            hsrc = ssmT if ht < 8 else convT
                            ins = e.matmul(po[b_][:, :], lhsT=hsrc[:, ht % 8, tt * 128:(tt + 1) * 128], rhs=woc[wb_][:, ht, :],
                                           start=(ht == 0), stop=(ht == 15))
                        return ins
                    P.add("pe", mo, reads=["ssmT", "convT", ("woc", wb_)], writes=["po%d" % b_])
                    P.add("dve", lambda e, b_=b_, cols=cols: e.tensor_tensor(out=x1p[b_][:], in0=po[b_][:, :], in1=g1b[:, cols], op=ALU.mult),
                          reads=["po%d" % b_, "g1b"], writes=[("x1p", b_)])
                    P.add("dve", lambda e, b_=b_: e.tensor_tensor(out=x1p[b_][:], in0=x1p[b_][:], in1=xp[b_][:], op=ALU.add),
                          reads=[("x1p", b_), ("xp", b_)], writes=[("x1p", b_)])
                    P.add("act", lambda e, b_=b_, tt=tt, cc=cc: e.activation(out=jk[:], in_=x1p[b_][:], func=AF.Square,
                                                                            accum_out=ss2[:, tt, cc:cc + 1]),
                          reads=[("x1p", b_)], writes=["jk", ("ss2", tt, cc)])
                    P.add("sp", lambda e, b_=b_, rows=rows, cols=cols: e.dma_start(out=scrX1[rows, cols], in_=x1p[b_][:]),
                          reads=[("x1p", b_)], writes=["scrX1"], group="x1p%d" % b_)
            P.barrier()
        mix.close()
        mixer.close()

        moe = top.enter_context(ExitStack())
        acc = sb("acc", [128, 8, D], F32, moe)
        hx2T = sb("hx2T", [128, 16, NOWN], BF16, moe)
        Wt = sb("Wt", [128, 8, 64], F32, moe)
        with ExitStack() as ph:
            sc2b = sb("sc2b", [128, D], F32, ph)
            sh2b = sb("sh2b", [128, D], F32, ph)
            scale2 = sb("scale2", [128, 16], F32, ph)
            P.add("dve", lambda e: e.scalar_tensor_tensor(out=scale2[:], in0=modT[:, 64:80, 0], scalar=1.0, in1=colA[:, 48:64],
                                                          op0=ALU.add, op1=ALU.mult), reads=["modT", "colA"], writes=["scale2"])
            with ExitStack() as ph3:
                row_bcast(sc2b, lambda ft: scale2[:, ft:ft + 1], ["scale2"], "sc2b", ph3)
                P.barrier()
            with ExitStack() as ph3:
                row_bcast(sh2b, lambda ft: modT[:, 48 + ft, 0:1], ["modT"], "sh2b", ph3)
                P.barrier()
            rw = sb("rw", [128, 16, 64], F32, ph)
            P.add("sp", lambda e: e.dma_start(out=rw[:], in_=router_w.rearrange("(kt p) c -> p kt c", p=128)), writes=["rw"], group="rw")
            rb = sb("rb", [128, 64], F32, ph)
            P.add("sp", lambda e: e.dma_start(out=rb[:], in_=rbias_b), writes=["rb"], group="rb")
            rs2 = sb("rs2", [128, 8], F32, ph)
            P.add("dve", lambda e: e.tensor_reduce(out=rs2[:], in_=ss2[:], axis=AX.X, op=ALU.add), reads=["ss2"], writes=["rs2"])
            P.add("dve", lambda e: e.tensor_scalar(out=rs2[:], in0=rs2[:], scalar1=1.0 / D, scalar2=EPS, op0=ALU.mult, op1=ALU.add),
                  reads=["rs2"], writes=["rs2"])
            P.add("act", lambda e: e.activation(out=rs2[:], in_=rs2[:], func=AF.Sqrt), reads=["rs2"], writes=["rs2"])
            P.add("dve", lambda e: e.reciprocal(out=rs2[:], in_=rs2[:]), reads=["rs2"], writes=["rs2"])
            x1t = [sb("x1t%d" % i, [128, D], F32, ph) for i in range(2)]
            hf = [sb("hf%d" % i, [128, D], F32, ph) for i in range(2)]
            pth = [ps("pth%d" % i, [128, 4, 128], F32, ph) for i in range(2)]
            hfs = [sb("hfs%d" % i, [128, 4, 128], F32, ph) for i in range(2)]
            plgs = [ps("plg%d" % i, [128, 64], F32, ph) for i in range(2)]
            rt = sb("rt", [128, 12, 64], F32, ph)
            m8 = sb("m8", [128, 16], F32, ph)

            def n2A(tt):
                b_ = tt % 2
                rows = slice(tt * 128, (tt + 1) * 128)
                P.add("sp", lambda e, b_=b_, rows=rows: e.dma_start(out=x1t[b_][:], in_=scrX1[rows, :]), reads=["scrX1"],
                      writes=[("x1t", b_)], group="x1t%d" % b_)
                P.add("act", lambda e, b_=b_, tt=tt: e.activation(out=hf[b_][:], in_=x1t[b_][:], func=AF.Copy, scale=rs2[:, tt:tt + 1]),
                      reads=[("x1t", b_), "rs2"], writes=[("hf", b_)])
                P.add("dve", lambda e, b_=b_: e.tensor_tensor(out=hf[b_][:], in0=hf[b_][:], in1=sc2b[:], op=ALU.mult),
                      reads=[("hf", b_), "sc2b"], writes=[("hf", b_)])
                P.add("pool", lambda e, b_=b_: e.tensor_tensor(out=hf[b_][:], in0=hf[b_][:], in1=sh2b[:], op=ALU.add),
                      reads=[("hf", b_), "sh2b"], writes=[("hf", b_)])

            def n2B(tt):
                b_ = tt % 2
                plg = plgs[tt % 2]
                pk = "plg%d" % (tt % 2)
                for f4 in range(4):
                    pb = f4 % 2

                    def trh(e, b_=b_, f4=f4, pb=pb):
                        ins = None
                        for j in range(4):
                            ft = f4 * 4 + j
                            ins = e.transpose(out=pth[pb][:, j, :], in_=hf[b_][:, ft * 128:(ft + 1) * 128], identity=ident_f[:])
                        return ins
                    P.add("pe", trh, reads=[("hf", b_), "ident_f"], writes=["pth%d" % pb])
                    P.add("act", lambda e, pb=pb, f4=f4, tt=tt: e.activation(out=hx2T[:, f4 * 4:(f4 + 1) * 4, tt * 128:(tt + 1) * 128],
                                                                            in_=pth[pb][:], func=AF.Copy),
                          reads=["pth%d" % pb], writes=[("hx2T", f4, tt)])
                    P.add("dve", lambda e, pb=pb: e.tensor_copy(hfs[pb][:], pth[pb][:]), reads=["pth%d" % pb], writes=[("hfs", pb)])

                    def mr(e, pb=pb, f4=f4, plg=plg):
                        ins = None
                        for j in range(4):
                            ft = f4 * 4 + j
                            ins = e.matmul(plg[:, :], lhsT=hfs[pb][:, j, :], rhs=rw[:, ft, :], start=(ft == 0), stop=(ft == 15))
                        return ins
                    P.add("pe", mr, reads=[("hfs", pb), "rw"], writes=[pk])

            def n2C(tt):
                plg = plgs[tt % 2]
                pk = "plg%d" % (tt % 2)
                S_, Bi, T1, T2, MB, EM = (rt[:, i, :] for i in range(6))
                g3 = lambda ap: ap.rearrange("p (g k) -> p g k", k=8)
                rops = [
                    ("act", lambda e: e.activation(out=S_, in_=plg[:, :], func=AF.Sigmoid), [pk]),
                    ("dve", lambda e: e.tensor_tensor(out=Bi, in0=S_, in1=rb[:], op=ALU.add), ["rb"]),
                    ("dve", lambda e: e.tensor_reduce(out=m8[:, 0:8], in_=g3(Bi), axis=AX.X, op=ALU.max), []),
                    ("dve", lambda e: e.tensor_tensor(out=g3(T1), in0=g3(Bi), in1=m8[:, 0:8].unsqueeze(2).to_broadcast([128, 8, 8]), op=ALU.is_equal), []),
                    ("dve", lambda e: e.scalar_tensor_tensor(out=T1, in0=T1, scalar=-1e9, in1=Bi, op0=ALU.mult, op1=ALU.add), []),
                    ("dve", lambda e: e.tensor_reduce(out=m8[:, 8:16], in_=g3(T1), axis=AX.X, op=ALU.max), []),
                    ("dve", lambda e: e.tensor_tensor(out=m8[:, 0:8], in0=m8[:, 0:8], in1=m8[:, 8:16], op=ALU.add), []),
                    ("dve", lambda e: e.max(out=m8[:, 8:16], in_=m8[:, 0:8]), []),
                    ("dve", lambda e: e.tensor_scalar(out=m8[:, 0:8], in0=m8[:, 0:8], scalar1=m8[:, 11:12], scalar2=None, op0=ALU.is_ge), []),
                    ("dve", lambda e: e.tensor_tensor(out=g3(MB), in0=g3(Bi), in1=m8[:, 0:8].unsqueeze(2).to_broadcast([128, 8, 8]), op=ALU.mult), []),
                    ("dve", lambda e: e.tensor_scalar(out=m8[:, 0:8], in0=m8[:, 0:8], scalar1=-1.0, scalar2=1e9, op0=ALU.add, op1=ALU.mult), []),
                    ("dve", lambda e: e.tensor_tensor(out=g3(MB), in0=g3(MB), in1=m8[:, 0:8].unsqueeze(2).to_broadcast([128, 8, 8]), op=ALU.add), []),
                    ("dve", lambda e: e.max(out=m8[:, 8:16], in_=MB), []),
                    ("dve", lambda e: e.tensor_scalar(out=EM, in0=MB, scalar1=m8[:, 15:16], scalar2=None, op0=ALU.is_ge), []),
                    ("dve", lambda e: e.tensor_tensor(out=T2, in0=S_, in1=EM, op=ALU.mult), []),
                    ("dve", lambda e: e.tensor_reduce(out=m8[:, 0:1], in_=T2, axis=AX.X, op=ALU.add), []),
                    ("dve", lambda e: e.reciprocal(out=m8[:, 0:1], in_=m8[:, 0:1]), []),
                    ("dve", lambda e, tt=tt: e.tensor_scalar(out=Wt[:, tt, :], in0=T2, scalar1=m8[:, 0:1], scalar2=2.5, op0=ALU.mult, op1=ALU.mult), []),
                ]
                for (en, f_, rk) in rops:
                    P.add(en, f_, reads=["rt", "m8"] + rk, writes=["rt", "m8", ("Wt", tt)])

            for it in range(10):
                if it < 8:
                    n2A(it)
                if 1 <= it <= 8:
                    n2B(it - 1)
                if it >= 2:
                    n2C(it - 2)
            P.barrier()
        if debug:
            d_Wt = dout("d_Wt", [128, 512])
            P.add("sp", lambda e: e.dma_start(out=d_Wt, in_=Wt[:].rearrange("p a b -> p (a b)")), reads=["Wt"], writes=["d_Wt"], group="dbgW")
            d_hx2T = dout("d_hx2T", [128, 16 * NOWN], BF16)
            P.add("sp", lambda e: e.dma_start(out=d_hx2T, in_=hx2T[:].rearrange("p a b -> p (a b)")), reads=["hx2T"], writes=["d_hx2T"], group="dbgW2")

        with ExitStack() as ph:
            wg = [sb("wg%d" % i, [128, 16, 512], BF16, ph) for i in range(2)]
            wu = [sb("wu%d" % i, [128, 16, 512], BF16, ph) for i in range(2)]
            wd = [sb("wd0", [128, 4, D], BF16, ph)]
            actT = sb("actT", [128, 4, NOWN], BF16, ph)
            sgl = [sb("sgl%d" % i, [128, 512], F32, ph) for i in range(2)]
            pg = [ps("pg%d" % i, [128, 512], F32, ph) for i in range(2)]
            pu = [ps("pu%d" % i, [128, 512], F32, ph) for i in range(2)]
            pd = [ps("pd%d" % i, [128, 512], F32, ph) for i in range(3)]
            P.add("pool", lambda e: e.memset(acc[:], 0.0), writes=["acc"])
            NE = 65
            import os
            DMAONLY = os.environ.get("MOE_DMAONLY", "")
            _Padd = P.add
            if DMAONLY:
                class _PX:
                    @staticmethod
                    def add(eng, fn, reads=(), writes=(), group=None):
                        if group is None:
                            return None
                        q = {"1": "pool", "2": "sp", "3": "act"}[DMAONLY[0]]
                        return _Padd(q if DMAONLY[0] != "4" else eng, fn, reads=reads, writes=writes, group=group)
                PM = _PX
            else:
                PM = P
            for ex in range(NE):
                b_ = ex % 2
                gsrc = ew_gate[ex] if ex < 64 else sw_gate
                usrc = ew_up[ex] if ex < 64 else sw_up
                dsrc = ew_down[ex] if ex < 64 else sw_down
                PM.add("pool", lambda e, b_=b_, gsrc=gsrc: e.dma_start(out=wg[b_][:], in_=gsrc.rearrange("(kt p) c -> p kt c", p=128)),
                      writes=[("wg", b_)], group="wg%d" % b_)
                PM.add("pool", lambda e, b_=b_, usrc=usrc: e.dma_start(out=wu[b_][:], in_=usrc.rearrange("(kt p) c -> p kt c", p=128)),
                      writes=[("wu", b_)], group="wu%d" % b_)
                dv = dsrc.rearrange("(kt p) c -> p kt c", p=128)
                for hc in range(2):
                    PM.add("pool", lambda e, dv=dv, hc=hc: e.dma_start(out=wd[0][:, :, hc * 1024:(hc + 1) * 1024],
                                                                     in_=dv[:, :, hc * 1024:(hc + 1) * 1024]),
                          writes=[("wd", hc)], group="wd")
                n = 0
                for mt in range(4):
                    for half in range(2):
                        pb = n % 2
                        n += 1
                        tk = slice(half * 512, (half + 1) * 512)

                        def mgu(e, w_, pp, mt=mt, tk=tk, pb=pb, b_=b_):
                            ins = None
                            for kt in range(16):
                                ins = e.matmul(pp[pb][:, :], lhsT=w_[b_][:, kt, mt * 128:(mt + 1) * 128], rhs=hx2T[:, kt, tk],
                                               start=(kt == 0), stop=(kt == 15))
                            return ins
                        PM.add("pe", lambda e, f_=mgu: f_(e, wg, pg), reads=[("wg", b_), "hx2T"], writes=["pg%d" % pb])
                        PM.add("pe", lambda e, f_=mgu: f_(e, wu, pu), reads=[("wu", b_), "hx2T"], writes=["pu%d" % pb])
                        PM.add("act", lambda e, pb=pb: e.activation(out=sgl[pb][:], in_=pg[pb][:, :], func=AF.Silu),
                              reads=["pg%d" % pb], writes=[("sgl", pb)])
                        PM.add("dve", lambda e, pb=pb, mt=mt, tk=tk: e.tensor_tensor(out=actT[:, mt, tk], in0=sgl[pb][:], in1=pu[pb][:, :], op=ALU.mult),
                              reads=[("sgl", pb), "pu%d" % pb], writes=[("actT", mt, half)])
                n = 0
                for tt in range(8):
                    for cc in range(4):
                        pb = n % 3
                        n += 1

                        def mdn(e, tt=tt, cc=cc, pb=pb):
                            ins = None
                            for kt in range(4):
                                ins = e.matmul(pd[pb][:, :], lhsT=actT[:, kt, tt * 128:(tt + 1) * 128], rhs=wd[0][:, kt, cc * 512:(cc + 1) * 512],
                                               start=(kt == 0), stop=(kt == 3))
                            return ins
                        PM.add("pe", mdn, reads=["actT", "wd"], writes=["pd%d" % pb])
                        wsc = Wt[:, tt, ex:ex + 1] if ex < 64 else 1.0
                        PM.add("dve", lambda e, tt=tt, cc=cc, pb=pb, wsc=wsc: e.scalar_tensor_tensor(
                            out=acc[:, tt, cc * 512:(cc + 1) * 512], in0=pd[pb][:, :], scalar=wsc, in1=acc[:, tt, cc * 512:(cc + 1) * 512],
                            op0=ALU.mult, op1=ALU.add), reads=["pd%d" % pb, "Wt"], writes=[("acc", tt, cc)])
            P.barrier()

        with ExitStack() as ph:
            g2b = sb("g2b", [128, D], F32, ph)
            fgb = sb("fgb", [128, D], F32, ph)
            with ExitStack() as ph3:
                row_bcast(g2b, lambda ft: modT[:, 80 + ft, 0:1], ["modT"], "g2b", ph3)
                P.barrier()
            with ExitStack() as ph3:
                row_bcast(fgb, lambda ft: colA[:, 64 + ft:65 + ft], ["colA"], "fgb", ph3)
                P.barrier()
            x1f = [sb("x1f%d" % i, [128, D], F32, ph) for i in range(2)]
            fo = [sb("fo%d" % i, [128, D], F32, ph) for i in range(2)]
            fs = sb("fs", [128, 8], F32, ph)
            fj = sb("fj", [128, D], BF16, ph)
            def stageA(tt):
                b_ = tt % 2
                rows = slice(tt * 128, (tt + 1) * 128)
                P.add("sp", lambda e, b_=b_, rows=rows: e.dma_start(out=x1f[b_][:], in_=scrX1[rows, :]), reads=["scrX1"],
                      writes=[("x1f", b_)], group="x1f%d" % b_)
                for hc in range(2):
                    cs = slice(hc * 1024, (hc + 1) * 1024)
                    P.add("dve", lambda e, tt=tt, cs=cs: e.tensor_tensor(out=acc[:, tt, cs], in0=acc[:, tt, cs], in1=g2b[:, cs], op=ALU.mult),
                          reads=[("acc", tt, hc), "g2b"], writes=[("acc", tt, hc)])
                    P.add("dve", lambda e, tt=tt, b_=b_, cs=cs: e.tensor_tensor(out=acc[:, tt, cs], in0=acc[:, tt, cs], in1=x1f[b_][:, cs], op=ALU.add),
                          reads=[("acc", tt, hc), ("x1f", b_)], writes=[("acc", tt, hc)])
                    P.add("act", lambda e, tt=tt, cs=cs, hc=hc: e.activation(out=fj[:, cs], in_=acc[:, tt, cs], func=AF.Square,
                                                                          accum_out=fs2[:, tt, hc:hc + 1]),
                          reads=[("acc", tt, hc)], writes=[("fj", hc), ("fs2", tt, hc)])

            def stageB(tt):
                b_ = tt % 2
                rows = slice(tt * 128, (tt + 1) * 128)
                P.add("dve", lambda e, tt=tt: e.tensor_tensor(out=fs[:, tt:tt + 1], in0=fs2[:, tt, 0:1], in1=fs2[:, tt, 1:2], op=ALU.add),
                      reads=[("fs2", tt)], writes=[("fs", tt)])
                P.add("dve", lambda e, tt=tt: e.tensor_scalar(out=fs[:, tt:tt + 1], in0=fs[:, tt:tt + 1], scalar1=1.0 / D, scalar2=EPS,
                                                              op0=ALU.mult, op1=ALU.add), reads=[("fs", tt)], writes=[("fs", tt)])
                P.add("act", lambda e, tt=tt: e.activation(out=fs[:, tt:tt + 1], in_=fs[:, tt:tt + 1], func=AF.Sqrt), reads=[("fs", tt)], writes=[("fs", tt)])
                P.add("dve", lambda e, tt=tt: e.reciprocal(out=fs[:, tt:tt + 1], in_=fs[:, tt:tt + 1]), reads=[("fs", tt)], writes=[("fs", tt)])
                P.add("act", lambda e, tt=tt, b_=b_: e.activation(out=fo[b_][:], in_=acc[:, tt, :], func=AF.Copy, scale=fs[:, tt:tt + 1]),
                      reads=[("acc", tt), ("fs", tt)], writes=[("fo", b_)])
                P.add("dve", lambda e, b_=b_: e.tensor_tensor(out=fo[b_][:], in0=fo[b_][:], in1=fgb[:], op=ALU.mult),
                      reads=[("fo", b_), "fgb"], writes=[("fo", b_)])
                P.add("sp", lambda e, b_=b_, rows=rows: e.dma_start(out=out[rows, :], in_=fo[b_][:]), reads=[("fo", b_)], writes=["out"],
                      group="fo%d" % b_)

            fs2 = sb("fs2", [128, 8, 2], F32, ph)
            for tt in range(9):
                if tt < 8:
                    stageA(tt)
                if tt >= 1:
                    stageB(tt - 1)

        P.add("sp", None, reads=["out", "scrW1", "scrT", "scrW2", "scrU", "scrX1"] + list(dbg.keys()))
        P.emit()
    return nc, dbg


def prep_inputs(inp):
    f = lambda a: np.ascontiguousarray(a, dtype=np.float32)
    x, ctx, c = inp["x"], inp["ctx"], inp["c"]
    maps = []
    shared = {
        "w_ada": f(inp["w_ada"][0]), "w_in": f(inp["w_in"][0]),
        "b_ada": f(inp["b_ada"][0].reshape(96, 128)),
        "w_glu": f(inp["ssm_w_glu"][0]), "w_out": f(inp["w_out"][0]), "router_w": f(inp["router_w"][0]),
        "rbias_b": f(np.tile(inp["router_bias"][0].reshape(1, 64), (128, 1))),
        "ew_gate": f(inp["exp_w_gate"][0]), "ew_up": f(inp["exp_w_up"][0]), "ew_down": f(inp["exp_w_down"][0]),
        "sw_gate": f(inp["shared_w_gate"][0]), "sw_up": f(inp["shared_w_up"][0]), "sw_down": f(inp["shared_w_down"][0]),
    }
    for core in range(8):
        b, h = core // 2, core % 2
        xb = x[b]
        cb = ctx[b]
        conv_w = inp["conv_w"][0]
        if h == 1:
            xb = xb[::-1]
            cb = cb[::-1]
            conv_w = conv_w[::-1]
        vecsA = np.concatenate([c[b].reshape(16, 128), inp["c_ctx"].reshape(16, 128),
                                inp["norm1_g"][0].reshape(16, 128), inp["norm2_g"][0].reshape(16, 128),
                                inp["final_g"].reshape(16, 128), inp["mix_norm_g"][0].reshape(16, 128)], 0)
        vecsB = np.concatenate([inp["ssm_d"][0].reshape(8, 128), conv_w.reshape(24, 128),
                                inp["conv_b"][0].reshape(8, 128)], 0)
        sl = slice(None, None, -1) if h == 1 else slice(None)
        m = dict(shared)
        m.update(xs=f(xb), ctxs=f(cb), vecsA=f(vecsA), vecsB=f(vecsB),
                 lamre_p=f(inp["ssm_lam_re"][0][sl].reshape(64, 128)), lamim_p=f(inp["ssm_lam_im"][0][sl].reshape(64, 128)),
                 logdt_p=f(inp["ssm_log_dt"][0][sl].reshape(64, 2)),
                 ssm_b_re=f(inp["ssm_b_re"][0][sl]), ssm_b_im=f(inp["ssm_b_im"][0][sl]),
                 ssm_c_re=f(inp["ssm_c_re"][0][sl]), ssm_c_im=f(inp["ssm_c_im"][0][sl]))
        maps.append(m)
    return maps


def kernel(**inputs):
    nc, _ = build_nc(False)
    maps = prep_inputs(inputs)
    res = run_bass_kernel_spmd(nc, maps, core_ids=list(range(8)))
    outs = np.zeros((4, 2048, 2048), np.float32)
    for core in range(8):
        b, h = core // 2, core % 2
        o = res.results[core]["out"]
        if h == 0:
            outs[b, 0:1024] = o
        else:
            outs[b, 1024:2048] = o[::-1]
    return outs
```

```python
from contextlib import ExitStack
import numpy as np
import concourse.bass as bass
import concourse.mybir as mybir
from concourse.bass_utils import run_bass_kernel_spmd

F32 = mybir.dt.float32
BF16 = mybir.dt.bfloat16
I32 = mybir.dt.int32
ALU = mybir.AluOpType
AF = mybir.ActivationFunctionType
AX = mybir.AxisListType

D = 2048
NOWN = 1024
NSEQ = 2304
EPS = 1e-6


class Prog:
    ENG = ("pe", "act", "dve", "pool", "sp")

    def __init__(self, nc, stack):
        self.nc = nc
        self.stack = stack
        self.ops = []
        self.keys = {}
        self.groups = {}
        self.psum_names = set()
        self.gopen = {}

    @staticmethod
    def _norm(k):
        return k if isinstance(k, tuple) else (k,)

    def _related(self, key):
        d = self.keys.setdefault(key[0], {})
        for k2 in list(d.keys()):
            n = min(len(k2), len(key))
            if k2[:n] == key[:n]:
                yield k2, d[k2]

    def add(self, eng, fn, reads=(), writes=(), group=None):
        op = dict(id=len(self.ops), eng=eng, fn=fn, deps=set(), group=group, used=False)
        reads = [self._norm(k) for k in reads] + [("__phase",)]
        writes = [self._norm(k) for k in writes]
        pk = [(k[0],) for k in reads + writes if k[0] in self.psum_names]
        reads = [k for k in reads if k[0] not in self.psum_names]
        writes = [k for k in writes if k[0] not in self.psum_names] + sorted(set(pk))
        for key in reads:
            for k2, st in self._related(key):
                if st[0] is not None:
                    op["deps"].add(st[0])
        for key in writes:
            for k2, st in self._related(key):
                if st[0] is not None:
                    op["deps"].add(st[0])
                op["deps"].update(st[1])
        for key in reads:
            d = self.keys.setdefault(key[0], {})
            st = d.setdefault(key, [None, []])
            st[1].append(op["id"])
        for key in writes:
            d = self.keys.setdefault(key[0], {})
            for k2 in list(d.keys()):
                if len(k2) > len(key) and k2[:len(key)] == key:
                    del d[k2]
            d[key] = [op["id"], []]
        op["deps"].discard(op["id"])
        for d in op["deps"]:
            gg = self.ops[d]["group"]
            if gg is not None and d in self.gopen.get(gg, ()):
                self.gopen[gg] = []
        if group is not None:
            self.gopen.setdefault(group, []).append(op["id"])
            op["batch"] = self.gopen[group]
        self.ops.append(op)
        return op

    def barrier(self):
        scr = self._bar_scr
        self.add("dve", lambda e: e.memset(scr[:, 0:1], 0.0), writes=[("__phase",), "barscr"])

    def emit(self):
        nc = self.nc
        ops = self.ops
        for op in ops:
            for d in op["deps"]:
                ops[d]["used"] = True
        sems = {}
        for e in self.ENG:
            sems[e] = self.stack.enter_context(nc.semaphore("s_" + e))
        gsem = {}
        cnt = {e: 0 for e in self.ENG}
        gcnt = {}
        for op in ops:
            if op["group"] is not None:
                g = op["group"]
                if g not in gsem:
                    gsem[g] = self.stack.enter_context(nc.semaphore("g_" + str(g)))
                    gcnt[g] = 0
                gcnt[g] += 16
                op["sig"] = (gsem[g], gcnt[g], 16)
            elif op["used"]:
                cnt[op["eng"]] += 1
                op["sig"] = (sems[op["eng"]], cnt[op["eng"]], 1)
            else:
                op["sig"] = None
        per = {e: [o for o in ops if o["eng"] == e] for e in self.ENG}

        def replay(ename, eng):
            waited = {}
            for op in per[ename]:
                need = {}
                for d in op["deps"]:
                    dop = ops[d]
                    if dop["eng"] == "pe" and ename == "pe" and dop["group"] is None:
                        continue
                    s = dop["sig"]
                    assert s is not None
                    if dop["group"] is not None:
                        s = ops[dop["batch"][-1]]["sig"]
                    key = id(s[0])
                    if key not in need or need[key][1] < s[1]:
                        need[key] = (s[0], s[1])
                for key, (sem, val) in need.items():
                    if waited.get(key, 0) >= val:
                        continue
                    waited[key] = val
                    eng.wait_ge(sem, val)
                if op["fn"] is None:
                    continue
                ins = op["fn"](eng)
                if op["sig"] is not None:
                    ins.then_inc(op["sig"][0], op["sig"][2])

        block = self.stack.enter_context(nc.Block())

        @block.tensor
        def _(eng):
            replay("pe", eng)

        @block.scalar
        def _(eng):
            replay("act", eng)

        @block.vector
        def _(eng):
            replay("dve", eng)

        @block.gpsimd
        def _(eng):
            replay("pool", eng)

        @block.sync
        def _(eng):
            replay("sp", eng)


def build_nc(debug=False, stop=99):
    nc = bass.Bass("TRN2", target_bir_lowering=False)
    dbg = {}

    def din(name, shape, dt=F32):
        return nc.dram_tensor(name, list(shape), dt, kind="ExternalInput").ap()

    xs = din("xs", [2048, D])
    ctxs = din("ctxs", [256, D])
    vecsA = din("vecsA", [96, 128])
    vecsB = din("vecsB", [40, 128])
    b_ada = din("b_ada", [96, 128])
    w_ada = din("w_ada", [D, 6 * D])
    w_in = din("w_in", [D, 4096])
    lamre_p = din("lamre_p", [64, 128])
    lamim_p = din("lamim_p", [64, 128])
    logdt_p = din("logdt_p", [64, 2])
    ssm_b_re = din("ssm_b_re", [2, 64, 64, 16])
    ssm_b_im = din("ssm_b_im", [2, 64, 64, 16])
    ssm_c_re = din("ssm_c_re", [2, 64, 16, 64])
    ssm_c_im = din("ssm_c_im", [2, 64, 16, 64])
    w_glu = din("w_glu", [1024, 2048])
    w_out = din("w_out", [D, D])
    router_w = din("router_w", [D, 64])
    rbias_b = din("rbias_b", [128, 64])
    ew_gate = din("ew_gate", [64, D, 512])
    ew_up = din("ew_up", [64, D, 512])
    ew_down = din("ew_down", [64, 512, D])
    sw_gate = din("sw_gate", [D, 512])
    sw_up = din("sw_up", [D, 512])
    sw_down = din("sw_down", [512, D])
    out = nc.dram_tensor("out", [NOWN, D], F32, kind="ExternalOutput").ap()
    skind = "ExternalOutput" if debug else "Internal"
    scrW1 = nc.dram_tensor("scrW1", [2, 32, 2, 128, 128], BF16, kind=skind).ap()
    scrT = nc.dram_tensor("scrT", [2, 64, 128, 128], BF16, kind=skind).ap()
    scrW2 = nc.dram_tensor("scrW2", [2, 32, 2, 128, 128], BF16, kind=skind).ap()
    scrX1 = nc.dram_tensor("scrX1", [NOWN, D], F32, kind=skind).ap()
    scrU = nc.dram_tensor("scrU", [8, 128, NSEQ - NOWN], BF16, kind=skind).ap()

    def dout(name, shape, dt=F32):
        t = nc.dram_tensor(name, list(shape), dt, kind="ExternalOutput").ap()
        dbg[name] = t
        return t

    with ExitStack() as top:
        P = Prog(nc, top)

        def sb(name, shape, dt=F32, stack=top):
            return stack.enter_context(nc.sbuf_tensor(name, list(shape), dt))

        def ps(name, shape, dt=F32, stack=top):
            P.psum_names.add(name)
            esz = 4 if dt == F32 else 2
            full = stack.enter_context(nc.psum_tensor(name, [128, 2048 // esz], dt))
            n = int(np.prod(shape[1:]))
            v = full[:, 0:n]
            if len(shape) == 3:
                v = v.rearrange("p (a b) -> p a b", b=shape[2])
            return v

        P._bar_scr = sb("barscr", [128, 4])

        ident_f = sb("ident_f", [128, 128])
        ident_b = sb("ident_b", [128, 128], BF16)
        iot = sb("iot", [128, 128], I32)
        iotf = sb("iotf", [128, 128])
        P.add("pool", lambda e: e.iota(iot[:], [[1, 128]], base=0, channel_multiplier=-1), writes=["iot"])
        P.add("dve", lambda e: e.tensor_copy(iotf[:], iot[:]), reads=["iot"], writes=["iotf"])
        P.add("dve", lambda e: e.tensor_single_scalar(ident_f[:], iotf[:], 0.0, ALU.is_equal),
              reads=["iotf"], writes=["ident_f"])
        P.add("dve", lambda e: e.tensor_copy(ident_b[:], ident_f[:]), reads=["ident_f"], writes=["ident_b"])

        if stop < 1:
            d_i = dout('d_ident', [128, 128])
            P.add('sp', lambda e: e.dma_start(out=d_i, in_=ident_f[:]), reads=['ident_f'], writes=['d_ident'], group='dbgi')
            P.add('sp', None, reads=list(dbg.keys()))
            P.emit()
            return nc, dbg
        ones_b = sb("ones_b", [128, 128], BF16)
        ones_f = sb("ones_f", [128, 128], F32)
        P.add("dve", lambda e: e.memset(ones_b[:], 1.0), writes=["ones_b"])
        P.add("dve", lambda e: e.memset(ones_f[:], 1.0), writes=["ones_f"])
        ss2 = sb("ss2", [128, 8, 4], F32)
        vA = sb("vA", [96, 128])
        vB = sb("vB", [40, 128])
        vC = sb("vC", [96, 128])
        colA = sb("colA", [128, 96])
        colB = sb("colB", [128, 40])
        badaT = sb("badaT", [128, 96])
        P.add("sp", lambda e: e.dma_start(out=vA[:], in_=vecsA), writes=["vA"], group="vA")
        P.add("sp", lambda e: e.dma_start(out=vB[:], in_=vecsB), writes=["vB"], group="vB")
        P.add("sp", lambda e: e.dma_start(out=vC[:], in_=b_ada), writes=["vC"], group="vC")
        with ExitStack() as ph:
            pt = ps("pt_small", [128, 3, 128], F32, ph)
            P.add("pe", lambda e: e.transpose(out=pt[:, 0, 0:96], in_=vA[:], identity=ident_f[0:96, 0:96]),
                  reads=["vA", "ident_f"], writes=["pt_small"])
            P.add("pe", lambda e: e.transpose(out=pt[:, 1, 0:40], in_=vB[:], identity=ident_f[0:40, 0:40]),
                  reads=["vB", "ident_f"], writes=["pt_small"])
            P.add("pe", lambda e: e.transpose(out=pt[:, 2, 0:96], in_=vC[:], identity=ident_f[0:96, 0:96]),
                  reads=["vC", "ident_f"], writes=["pt_small"])
            P.add("dve", lambda e: e.tensor_copy(colA[:], pt[:, 0, 0:96]), reads=["pt_small"], writes=["colA"])
            P.add("dve", lambda e: e.tensor_copy(colB[:], pt[:, 1, 0:40]), reads=["pt_small"], writes=["colB"])
            P.add("dve", lambda e: e.tensor_copy(badaT[:], pt[:, 2, 0:96]), reads=["pt_small"], writes=["badaT"])
            P.barrier()

        if stop < 2:
            d_c = dout('d_colA', [128, 96])
            P.add('sp', lambda e: e.dma_start(out=d_c, in_=colA[:]), reads=['colA'], writes=['d_colA'], group='dbgc')
            P.add('sp', None, reads=list(dbg.keys()))
            P.emit()
            return nc, dbg
        sc = sb("sc", [128, 16, 2])
        for j in range(2):
            P.add("act", lambda e, j=j: e.activation(out=sc[:, :, j], in_=colA[:, 16 * j:16 * j + 16], func=AF.Silu),
                  reads=["colA"], writes=[("sc", j)])
        modT = sb("modT", [128, 96, 2])
        scale1 = sb("scale1", [128, 16, 2])
        mixer = top.enter_context(ExitStack())
        uTown = sb("uTown", [128, 8, NOWN], BF16, mixer)
        convT = sb("convT", [128, 8, NOWN], BF16, mixer)
        ss = sb("ss", [128, 24], F32, mixer)
        Acplx = sb("Acplx", [128, 2, 64], F32, mixer)
        ada_stack = ExitStack()
        REC_A = []
        _real_add = P.add
        P.add = lambda *a, **k: REC_A.append((a, k))
        if True:
            ph = ada_stack
            scb = sb("scb", [128, 16, 4], BF16, ph)
            sch = sb("sch", [128, 16, 2], F32, ph)
            P.add("dve", lambda e: e.tensor_copy(scb[:, :, 0:2], sc[:]), reads=["sc"], writes=["scb"])
            P.add("dve", lambda e: e.tensor_copy(sch[:], scb[:, :, 0:2]), reads=["scb"], writes=["sch"])
            P.add("dve", lambda e: e.tensor_tensor(out=sch[:], in0=sc[:], in1=sch[:], op=ALU.subtract), reads=["sc", "sch"], writes=["sch"])
            P.add("dve", lambda e: e.tensor_copy(scb[:, :, 2:4], sch[:]), reads=["sch", "scb"], writes=["scb"])
            pm = ps("pmod", [128, 96, 4], F32, ph)
            wbuf = [sb("wada%d" % i, [128, 2048], BF16, ph) for i in range(4)]
            n = 0
            for kt in range(16):
                for cc in range(6):
                    b = n % 4
                    n += 1
                    for hc in range(2):
                        P.add("pool", lambda e, b=b, kt=kt, cc=cc, hc=hc: e.dma_start(
                            out=wbuf[b][:, hc * 1024:(hc + 1) * 1024],
                            in_=w_ada[kt * 128:(kt + 1) * 128, cc * 2048 + hc * 1024:cc * 2048 + (hc + 1) * 1024]),
                            writes=[("wada", b, hc)], group="wada%d" % b)

                    def mm(e, b=b, kt=kt, cc=cc):
                        ins = None
                        for t in range(16):
                            ins = e.matmul(pm[:, cc * 16 + t, :], lhsT=wbuf[b][:, t * 128:(t + 1) * 128],
                                           rhs=scb[:, kt, :], start=(kt == 0 and cc == 0 and t == 0), stop=(kt == 15),
                                           skip_group_check=True)
                        return ins
                    P.add("pe", mm, reads=[("wada", b), "scb"], writes=["pmod"])
            P.add("dve", lambda e: e.tensor_tensor(out=modT[:], in0=pm[:, :, 0:2], in1=badaT[:].unsqueeze(2).to_broadcast([128, 96, 2]),
                                                  op=ALU.add), reads=["pmod", "badaT"], writes=["modT"])
            P.add("dve", lambda e: e.tensor_tensor(out=modT[:], in0=modT[:], in1=pm[:, :, 2:4], op=ALU.add),
                  reads=["pmod", "modT"], writes=["modT"])
        P.add = _real_add
        import math
        PI = math.pi
        with ExitStack() as ph:
            REC_S = []
            P.add = lambda *a, **k: REC_S.append((a, k))
            raw = sb("s0raw", [64, 3, 128], F32, ph)
            ldt = sb("s0ldt", [64, 2], F32, ph)
            P.add("sp", lambda e: e.dma_start(out=raw[:, 0, :], in_=lamre_p), writes=[("s0raw", 0)], group="s0raw0")
            P.add("sp", lambda e: e.dma_start(out=raw[:, 1, :], in_=lamim_p), writes=[("s0raw", 1)], group="s0raw1")
            P.add("sp", lambda e: e.dma_start(out=ldt[:], in_=logdt_p), writes=["s0ldt"], group="s0ldt")
            P.add("act", lambda e: e.activation(out=ldt[:], in_=ldt[:], func=AF.Exp), reads=["s0ldt"], writes=["s0ldt"])
            P.add("dve", lambda e: e.tensor_copy(raw[:, 2, :].rearrange("q (a b) -> q a b", a=2),
                                                 ldt[:].unsqueeze(2).to_broadcast([64, 2, 64])),
                  reads=["s0ldt"], writes=[("s0raw", 2)])
            LRI = sb("LRI", [128, 3, 64], F32, ph)
            ptq = ps("s0pt", [128, 4, 128], F32, ph)
            for i in range(3):
                P.add("pe", lambda e, i=i: e.transpose(out=ptq[:, i, 0:64], in_=raw[:, i, :], identity=ident_f[0:64, 0:64]),
                      reads=[("s0raw", i), "ident_f"], writes=["s0pt"])
            P.add("dve", lambda e: e.tensor_copy(LRI[:], ptq[:, 0:3, 0:64]), reads=["s0pt"], writes=["LRI"])
            ath = sb("ath", [128, 2, 64], F32, ph)
            P.add("dve", lambda e: e.tensor_tensor(out=ath[:], in0=LRI[:, 0:2, :],
                                                   in1=LRI[:, 2:3, :].to_broadcast([128, 2, 64]), op=ALU.mult),
                  reads=["LRI"], writes=["ath"])
            io8 = sb("io8", [128, 8], I32, ph)
            io8f = sb("io8f", [128, 8], F32, ph)
            KM = sb("KM", [128, 3, 2, 8], F32, ph)
            P.add("pool", lambda e: e.iota(io8[:], [[1, 8]], base=0, channel_multiplier=0), writes=["io8"])
            P.add("dve", lambda e: e.tensor_copy(io8f[:], io8[:]), reads=["io8"], writes=["io8f"])
            kmab = {(0, 0): (-1.0, 7.0), (0, 1): (1.0, 0.0), (1, 0): (-1.0, -1.0), (1, 1): (1.0, -8.0),
                    (2, 0): (1.0, 1.0), (2, 1): (-1.0, 8.0)}
            for (u_, d_), (ka, kb) in kmab.items():
                P.add("dve", lambda e, u_=u_, d_=d_, ka=ka, kb=kb: e.tensor_scalar(
                    out=KM[:, u_, d_, :], in0=io8f[:], scalar1=ka, scalar2=kb, op0=ALU.mult, op1=ALU.add),
                    reads=["io8f"], writes=[("KM", u_, d_)])

            et_ang = sb("et_ang", [128, 2, 32, 8], F32, ph)
            et_ex = sb("et_ex", [128, 2, 32, 8], F32, ph)
            et_tmp = sb("et_tmp", [128, 2, 32, 8], F32, ph)
            et_ti = sb("et_ti", [128, 2, 32, 8], I32, ph)
            et_tf = sb("et_tf", [128, 2, 32, 8], F32, ph)

            def etab(name, mult_ap, L, dst_re, dst_im, rkeys, wkeys):
                shp = [128, 2, 32, L]
                name = "et"
                ang = et_ang[:, :, :, 0:L]
                ex = et_ex[:, :, :, 0:L]
                tmp = et_tmp[:, :, :, 0:L]
                ti = et_ti[:, :, :, 0:L]
                tf = et_tf[:, :, :, 0:L]
                a_b = ath[:, 0, :].rearrange("p (d g) -> p d g", d=2).unsqueeze(3).to_broadcast(shp)
                t_b = ath[:, 1, :].rearrange("p (d g) -> p d g", d=2).unsqueeze(3).to_broadcast(shp)
                P.add("dve", lambda e: e.tensor_tensor(out=ex[:], in0=a_b, in1=mult_ap, op=ALU.mult),
                      reads=["ath"] + rkeys, writes=[name + "_ex"])
                P.add("act", lambda e: e.activation(out=ex[:], in_=ex[:], func=AF.Exp), reads=[name + "_ex"], writes=[name + "_ex"])
                P.add("dve", lambda e: e.tensor_tensor(out=ang[:], in0=t_b, in1=mult_ap, op=ALU.mult),
                      reads=["ath"] + rkeys, writes=[name + "_ang"])
                for (dst, shift) in ((dst_im, 32.0), (dst_re, 32.25)):
                    P.add("dve", lambda e, shift=shift: e.tensor_scalar(out=tmp[:], in0=ang[:], scalar1=1.0 / (2.0 * PI), scalar2=shift,
                                                                        op0=ALU.mult, op1=ALU.add),
                          reads=[name + "_ang"], writes=[name + "_tmp"])
                    P.add("dve", lambda e: e.tensor_copy(ti[:], tmp[:]), reads=[name + "_tmp"], writes=[name + "_ti"])
                    P.add("dve", lambda e: e.tensor_copy(tf[:], ti[:]), reads=[name + "_ti"], writes=[name + "_tf"])
                    P.add("dve", lambda e: e.tensor_tensor(out=tmp[:], in0=tmp[:], in1=tf[:], op=ALU.subtract),
                          reads=[name + "_tmp", name + "_tf"], writes=[name + "_tmp"])
                    P.add("dve", lambda e: e.tensor_single_scalar(tf[:], tmp[:], 0.5, ALU.is_gt),
                          reads=[name + "_tmp"], writes=[name + "_tf"])
                    P.add("dve", lambda e: e.tensor_tensor(out=tmp[:], in0=tmp[:], in1=tf[:], op=ALU.subtract),
                          reads=[name + "_tmp", name + "_tf"], writes=[name + "_tmp"])
                    P.add("act", lambda e: e.activation(out=tmp[:], in_=tmp[:], func=AF.Sin, scale=2.0 * PI), reads=[name + "_tmp"],
                          writes=[name + "_tmp"])
                    P.add("dve", lambda e, dst=dst: e.tensor_tensor(out=dst, in0=tmp[:], in1=ex[:], op=ALU.mult),
                          reads=[name + "_tmp", name + "_ex"], writes=wkeys)

            one1 = sb("one1", [128, 1], F32, ph)
            P.add("dve", lambda e: e.memset(one1[:], 1.0), writes=["one1"])
            E1 = sb("E1", [128, 2, 2, 32, 1], F32, ph)
            etab("e1", one1[:].unsqueeze(2).unsqueeze(3).to_broadcast([128, 2, 32, 1]), 1, E1[:, 0], E1[:, 1], ["one1"], ["E1"])
            eight = sb("eight", [128, 1], F32, ph)
            P.add("dve", lambda e: e.memset(eight[:], 8.0), writes=["eight"])
            etab("e8", eight[:].unsqueeze(2).unsqueeze(3).to_broadcast([128, 2, 32, 1]), 1,
                 Acplx[:, 0, :].rearrange("p (d g o) -> p d g o", d=2, o=1),
                 Acplx[:, 1, :].rearrange("p (d g o) -> p d g o", d=2, o=1), ["eight"], ["Acplx"])
            ET = [sb("ET%d" % u_, [128, 2, 2, 32, 8], F32, ph) for u_ in range(3)]
            for u_ in range(3):
                etab("et%d" % u_, KM[:, u_, :, :].unsqueeze(2).to_broadcast([128, 2, 32, 8]), 8,
                     ET[u_][:, 0], ET[u_][:, 1], ["KM"], ["ET%d" % u_])

            LR = LRI[:, 0, :]
            LI = LRI[:, 1, :]
            e1r = E1[:, 0].rearrange("p d g o -> p (d g o)")
            e1i = E1[:, 1].rearrange("p d g o -> p (d g o)")
            cf = sb("cf", [128, 6, 64], F32, ph)
            seq_ops = [
                lambda e: e.tensor_scalar(out=cf[:, 0, :], in0=e1r, scalar1=-1.0, scalar2=None, op0=ALU.add),
                lambda e: e.tensor_tensor(out=cf[:, 1, :], in0=LR, in1=LR, op=ALU.mult),
                lambda e: e.tensor_tensor(out=cf[:, 2, :], in0=LI, in1=LI, op=ALU.mult),
                lambda e: e.tensor_tensor(out=cf[:, 1, :], in0=cf[:, 1, :], in1=cf[:, 2, :], op=ALU.add),
                lambda e: e.reciprocal(out=cf[:, 1, :], in_=cf[:, 1, :]),
                lambda e: e.tensor_tensor(out=cf[:, 2, :], in0=cf[:, 0, :], in1=LR, op=ALU.mult),
                lambda e: e.tensor_tensor(out=cf[:, 3, :], in0=e1i, in1=LI, op=ALU.mult),
                lambda e: e.tensor_tensor(out=cf[:, 2, :], in0=cf[:, 2, :], in1=cf[:, 3, :], op=ALU.add),
                lambda e: e.tensor_tensor(out=cf[:, 4, :], in0=cf[:, 2, :], in1=cf[:, 1, :], op=ALU.mult),
                lambda e: e.tensor_tensor(out=cf[:, 2, :], in0=e1i, in1=LR, op=ALU.mult),
                lambda e: e.tensor_tensor(out=cf[:, 3, :], in0=cf[:, 0, :], in1=LI, op=ALU.mult),
                lambda e: e.tensor_tensor(out=cf[:, 2, :], in0=cf[:, 2, :], in1=cf[:, 3, :], op=ALU.subtract),
                lambda e: e.tensor_tensor(out=cf[:, 5, :], in0=cf[:, 2, :], in1=cf[:, 1, :], op=ALU.mult),
            ]
            for f_ in seq_ops:
                P.add("dve", f_, reads=["E1", "LRI", "cf"], writes=["cf"])
            Braw = sb("Braw", [128, 2, 64, 16], F32, ph)
            for i, src_ in enumerate((ssm_b_re, ssm_b_im)):
                for d_ in range(2):
                    v = src_[d_].rearrange("(g2 gp) p m -> gp p g2 m", gp=2)
                    for gp in range(2):
                        P.add("sp", lambda e, i=i, d_=d_, gp=gp, v=v: e.dma_start(
                            out=Braw[gp * 64:(gp + 1) * 64, i, d_ * 32:(d_ + 1) * 32, :], in_=v[gp]),
                            writes=[("Braw", i, d_, gp)], group="Braw")
            bbar = sb("bbar", [128, 2, 64, 16], F32, ph)
            tA = sb("s0tA", [128, 64, 16], F32, ph)
            cre_b = cf[:, 4, :].unsqueeze(2).to_broadcast([128, 64, 16])
            cim_b = cf[:, 5, :].unsqueeze(2).to_broadcast([128, 64, 16])
            P.add("dve", lambda e: e.tensor_tensor(out=bbar[:, 0], in0=Braw[:, 0], in1=cre_b, op=ALU.mult), reads=["Braw", "cf"], writes=[("bbar", 0)])
            P.add("dve", lambda e: e.tensor_tensor(out=tA[:], in0=Braw[:, 1], in1=cim_b, op=ALU.mult), reads=["Braw", "cf"], writes=["s0tA"])
            P.add("dve", lambda e: e.tensor_tensor(out=bbar[:, 0], in0=bbar[:, 0], in1=tA[:], op=ALU.subtract), reads=["s0tA", ("bbar", 0)], writes=[("bbar", 0)])
            P.add("dve", lambda e: e.tensor_tensor(out=bbar[:, 1], in0=Braw[:, 1], in1=cre_b, op=ALU.mult), reads=["Braw", "cf"], writes=[("bbar", 1)])
            P.add("dve", lambda e: e.tensor_tensor(out=tA[:], in0=Braw[:, 0], in1=cim_b, op=ALU.mult), reads=["Braw", "cf", ("bbar", 0)], writes=["s0tA"])
            P.add("dve", lambda e: e.tensor_tensor(out=bbar[:, 1], in0=bbar[:, 1], in1=tA[:], op=ALU.add), reads=["s0tA", ("bbar", 1)], writes=[("bbar", 1)])

            CT = sb("CT", [128, 2, 64, 16], F32, ph)
            craw = [sb("craw%d" % i, [128, 128], F32, ph) for i in range(2)]
            nb = 0
            for i, src_ in enumerate((ssm_c_re, ssm_c_im)):
                for d_ in range(2):
                    for q in range(4):
                        b_ = nb % 2
                        nb += 1
                        for g2l in range(8):
                            g2 = q * 8 + g2l
                            P.add("sp", lambda e, b_=b_, g2l=g2l, g2=g2, d_=d_, src_=src_: e.dma_start(
                                out=craw[b_][g2l * 16:(g2l + 1) * 16, :].rearrange("n (gp p) -> n gp p", gp=2),
                                in_=src_[d_, 2 * g2:2 * g2 + 2, :, :].rearrange("gp n p -> n gp p")),
                                writes=[("craw", b_, g2l)], group="craw%d" % b_)
                        P.add("pe", lambda e, b_=b_: e.transpose(out=ptq[:, 3, :], in_=craw[b_][:], identity=ident_f[:]),
                              reads=[("craw", b_), "ident_f"], writes=["s0pt"])
                        P.add("dve", lambda e, i=i, d_=d_, q=q: e.tensor_copy(
                            CT[:, i, d_ * 32 + q * 8:d_ * 32 + (q + 1) * 8, :], ptq[:, 3, :].rearrange("p (a n) -> p a n", n=16)),
                            reads=["s0pt"], writes=[("CT", i, d_, q)])

            mk = sb("mk", [128, 2, 128], F32, ph)
            mi = sb("mki", [128, 2, 128], I32, ph)
            mf = sb("mkf", [128, 2, 128], F32, ph)
            P.add("pool", lambda e: e.iota(mi[:, 0, :], [[1, 128]], base=0, channel_multiplier=0), writes=[("mki", 0)])
            P.add("pool", lambda e: e.iota(mi[:, 1, :], [[0, 128]], base=0, channel_multiplier=1), writes=[("mki", 1)])
            P.add("dve", lambda e: e.tensor_single_scalar(mi[:], mi[:], 4, ALU.arith_shift_right), reads=["mki"], writes=["mki"])
            P.add("dve", lambda e: e.tensor_copy(mf[:], mi[:]), reads=["mki"], writes=["mkf"])
            P.add("dve", lambda e: e.tensor_tensor(out=mk[:, 0, :], in0=mf[:, 0, :], in1=mf[:, 1, :], op=ALU.is_ge), reads=["mkf"], writes=[("mk", 0)])
            P.add("dve", lambda e: e.tensor_tensor(out=mk[:, 1, :], in0=mf[:, 1, :], in1=mf[:, 0, :], op=ALU.is_ge), reads=["mkf"], writes=[("mk", 1)])


            oR = sb("oR", [128, 32, 8, 16], F32, ph)
            oI = sb("oI", [128, 32, 8, 16], F32, ph)
            o2R = sb("o2R", [128, 32, 8, 16], F32, ph)
            o2I = sb("o2I", [128, 32, 8, 16], F32, ph)
            t1 = sb("s0t1", [128, 32, 8, 16], F32, ph)
            stg = sb("s0stg", [128, 4, 128], BF16, ph)
            stg2 = [sb("s0stg2", [128, 32, 128], BF16, ph)] * 2
            pT = ps("s0pT", [128, 4, 128], F32, ph)
            pT2 = ps("s0pT2", [128, 4, 128], F32, ph)

            def couter(u_, d_, Br, Bi, dR, dI, neg_im, rk, tag):
                shp = [128, 32, 8, 16]
                Er = ET[u_][:, 0, d_].unsqueeze(3).to_broadcast(shp)
                Ei = ET[u_][:, 1, d_].unsqueeze(3).to_broadcast(shp)
                Brb = Br.unsqueeze(2).to_broadcast(shp)
                Bib = Bi.unsqueeze(2).to_broadcast(shp)
                rk = rk + ["ET%d" % u_]
                P.add("dve", lambda e: e.tensor_tensor(out=dR[:], in0=Er, in1=Brb, op=ALU.mult), reads=rk, writes=[tag + "R"])
                P.add("dve", lambda e: e.tensor_tensor(out=t1[:], in0=Ei, in1=Bib, op=ALU.mult), reads=rk, writes=["s0t1"])
                P.add("dve", lambda e: e.tensor_tensor(out=dR[:], in0=dR[:], in1=t1[:], op=ALU.subtract), reads=[tag + "R", "s0t1"], writes=[tag + "R"])
                P.add("dve", lambda e: e.tensor_tensor(out=dI[:], in0=Er, in1=Bib, op=ALU.mult), reads=rk, writes=[tag + "I"])
                P.add("dve", lambda e: e.tensor_tensor(out=t1[:], in0=Ei, in1=Brb, op=ALU.mult), reads=rk + [tag + "R"], writes=["s0t1"])
                if neg_im:
                    P.add("dve", lambda e: e.scalar_tensor_tensor(out=dI[:], in0=dI[:], scalar=-1.0, in1=t1[:], op0=ALU.mult, op1=ALU.subtract),
                          reads=[tag + "I", "s0t1"], writes=[tag + "I"])
                else:
                    P.add("dve", lambda e: e.tensor_tensor(out=dI[:], in0=dI[:], in1=t1[:], op=ALU.add), reads=[tag + "I", "s0t1"], writes=[tag + "I"])

            for d_ in range(2):
                gs = slice(d_ * 32, (d_ + 1) * 32)
                couter(0, d_, bbar[:, 0, gs, :], bbar[:, 1, gs, :], oR, oI, False, ["bbar"], "o")
                for part, src_t, skey in ((0, oR, "oR"), (1, oI, "oI")):
                    for q in range(8):
                        def trw(e, src_t=src_t, q=q):
                            ins = None
                            for j in range(4):
                                ins = e.transpose(out=pT[:, j, :], in_=src_t[:, q * 4 + j].rearrange("p s m -> p (s m)"), identity=ident_f[:])
                            return ins
                        P.add("pe", trw, reads=[skey, "ident_f"], writes=["s0pT"])
                        P.add("act", lambda e: e.activation(out=stg[:], in_=pT[:], func=AF.Copy), reads=["s0pT"], writes=["s0stg"])
                        P.add("sp", lambda e, d_=d_, q=q, part=part: e.dma_start(
                            out=scrW1[d_, q * 4:(q + 1) * 4, part].rearrange("g r c -> r g c"), in_=stg[:]),
                            reads=["s0stg"], writes=["scrW1"], group="s0st")

                couter(1, d_, bbar[:, 0, gs, :], bbar[:, 1, gs, :], oR, oI, False, ["bbar"], "o")
                couter(2, d_, CT[:, 0, gs, :], CT[:, 1, gs, :], o2R, o2I, True, ["CT"], "o2")
                for part, src_t, skey in ((0, o2R, "o2R"), (1, o2I, "o2I")):
                    P.add("act", lambda e, part=part, src_t=src_t: e.activation(
                        out=stg2[part][:], in_=src_t[:].rearrange("p g t n -> p g (t n)"), func=AF.Copy),
                        reads=[skey], writes=["s0stg2"])
                    P.add("sp", lambda e, d_=d_, part=part: e.dma_start(
                        out=scrW2[d_, :, part].rearrange("g r c -> r g c"), in_=stg2[part][:]),
                        reads=["s0stg2"], writes=["scrW2"], group="s0st2")

                for q in range(8):
                    for gp in range(2):
                        pTx = pT if gp == 0 else pT2
                        pkey = "s0pT" if gp == 0 else "s0pT2"
                        rs = slice(gp * 64, (gp + 1) * 64)

                        def mmT(e, q=q, gp=gp, pTx=pTx, rs=rs):
                            ins = None
                            for j in range(4):
                                g2 = q * 4 + j
                                e.matmul(pTx[:, j, :], lhsT=oR[rs, g2].rearrange("p s m -> p (s m)"),
                                         rhs=o2R[rs, g2].rearrange("p t n -> p (t n)"), start=True, stop=False)
                                ins = e.matmul(pTx[:, j, :], lhsT=oI[rs, g2].rearrange("p s m -> p (s m)"),
                                               rhs=o2I[rs, g2].rearrange("p t n -> p (t n)"), start=False, stop=True)
                            return ins
                        P.add("pe", mmT, reads=["oR", "oI", "o2R", "o2I"], writes=[pkey])
                        P.add("dve", lambda e, d_=d_, pTx=pTx: e.tensor_tensor(
                            out=stg[:], in0=pTx[:], in1=mk[:, d_:d_ + 1, :].to_broadcast([128, 4, 128]), op=ALU.mult),
                            reads=[pkey, "mk"], writes=["s0stg"])
                        P.add("sp", lambda e, d_=d_, q=q, gp=gp: e.dma_start(
                            out=scrT[d_, q * 8:(q + 1) * 8].rearrange("(j gp) r c -> gp r j c", gp=2)[gp], in_=stg[:]),
                            reads=["s0stg"], writes=["scrT"], group="s0st")
            P.add = _real_add
            na, ns = len(REC_A), len(REC_S)
            ia = isx = 0
            while ia < na or isx < ns:
                if isx >= ns or (ia < na and ia * ns <= isx * na):
                    a_, k_ = REC_A[ia]; ia += 1
                else:
                    a_, k_ = REC_S[isx]; isx += 1
                P.add(*a_, **k_)
            P.barrier()
        ada_stack.close()
        if debug:
            d_A = dout("d_A", [128, 128])
            P.add("sp", lambda e: e.dma_start(out=d_A, in_=Acplx[:].rearrange("p a b -> p (a b)")), reads=["Acplx"],
                  writes=["d_A"], group="dbgA")

        if debug:
            d_mod = dout("d_mod", [128, 192])
            P.add("sp", lambda e: e.dma_start(out=d_mod, in_=modT[:].rearrange("p a b -> p (a b)")), reads=["modT"],
                  writes=["d_mod"], group="dbg0")

        P.add("dve", lambda e: e.scalar_tensor_tensor(out=scale1[:], in0=modT[:, 16:32, :], scalar=1.0,
                                                      in1=colA[:, 32:48].unsqueeze(2).to_broadcast([128, 16, 2]),
                                                      op0=ALU.add, op1=ALU.mult),
              reads=["modT", "colA"], writes=["scale1"])

        with ExitStack() as ph:
            w_u = sb("w_u", [128, 16, 1024], BF16, ph)
            ustage = [sb("ustage%d" % i, [128, 512], BF16, ph) for i in range(2)]
            w_in_v = w_in.rearrange("(kt p) c -> p kt c", p=128)
            for kt in range(16):
                P.add("pool", lambda e, kt=kt: e.dma_start(out=w_u[:, kt, :], in_=w_in_v[:, kt, 0:1024]),
                      writes=[("w_u", kt)], group="w_u")
            xt = [sb("xt%d" % i, [128, D], F32, ph) for i in range(2)]
            xn = [sb("xn%d" % i, [128, 4, D], BF16, ph) for i in range(2)]
            hxT = [sb("hxT%d" % i, [128, 16, 512], BF16, ph) for i in range(2)]
            ptr = [ps("ptr%d" % i, [128, 512], BF16, ph) for i in range(2)]
            pmm = [ps("pmm%d" % i, [128, 512], F32, ph) for i in range(6)]
            groups = [("x", 1024, 4, 0, 1024, 0), ("x", 1536, 4, 0, 1536, 1), ("c", 0, 2, 1, 2048, 0),
                      ("x", 0, 4, 0, 0, 1), ("x", 512, 4, 0, 512, 0)]
            nxc = [0]

            def stA1(gi):
                (src, r0, nt, mj, soff, xb) = groups[gi]
                xnb = xn[gi % 2]
                for t in range(nt):
                    nx = nxc[0]
                    b = nx % 2
                    tix = nx % 24
                    nxc[0] += 1
                    srcap = (xs if src == "x" else ctxs)[r0 + t * 128:r0 + (t + 1) * 128, :]
                    P.add("sp", lambda e, b=b, srcap=srcap: e.dma_start(out=xt[b][:], in_=srcap),
                          writes=[("xt", b)], group="xt%d" % b)
                    P.add("act", lambda e, b=b, tix=tix, t=t: e.activation(out=xnb[:, t, :], in_=xt[b][:], func=AF.Square,
                                                                      accum_out=ss[:, tix:tix + 1]),
                          reads=[("xt", b)], writes=[("xn", gi % 2, t), ("ss", tix)])
                    P.add("dve", lambda e, tix=tix: e.tensor_scalar(out=ss[:, tix:tix + 1], in0=ss[:, tix:tix + 1],
                                                                    scalar1=1.0 / D, scalar2=EPS, op0=ALU.mult, op1=ALU.add),
                          reads=[("ss", tix)], writes=[("ss", tix)])
                    P.add("act", lambda e, tix=tix: e.activation(out=ss[:, tix:tix + 1], in_=ss[:, tix:tix + 1], func=AF.Sqrt),
                          reads=[("ss", tix)], writes=[("ss", tix)])
                    P.add("dve", lambda e, tix=tix: e.reciprocal(out=ss[:, tix:tix + 1], in_=ss[:, tix:tix + 1]),
                          reads=[("ss", tix)], writes=[("ss", tix)])
                    P.add("act", lambda e, b=b, tix=tix, t=t: e.activation(
                        out=xnb[:, t, :], in_=xt[b][:], func=AF.Copy, scale=ss[:, tix:tix + 1]),
                        reads=[("xt", b), ("ss", tix)], writes=[("xn", gi % 2, t)])

            def stA2(gi):
                (src, r0, nt, mj, soff, xb) = groups[gi]
                xnb = xn[gi % 2]
                ntok = nt * 128
                for ft in range(16):
                    pb = ft % 2

                    def tr(e, ft=ft, nt=nt, pb=pb):
                        ins = None
                        for t in range(nt):
                            ins = e.transpose(out=ptr[pb][:, t * 128:(t + 1) * 128],
                                              in_=xnb[:, t, ft * 128:(ft + 1) * 128], identity=ident_b[:])
                        return ins
                    P.add("pe", tr, reads=[("xn", gi % 2), "ident_b"], writes=["ptr%d" % pb])
                    if ft % 2 == 0:
                        P.add("dve", lambda e, xb=xb, ft=ft, pb=pb, ntok=ntok, mj=mj: e.tensor_scalar(
                            out=hxT[xb][:, ft, 0:ntok], in0=ptr[pb][:, 0:ntok], scalar1=scale1[:, ft, mj:mj + 1],
                            scalar2=modT[:, ft, mj:mj + 1], op0=ALU.mult, op1=ALU.add),
                            reads=["ptr%d" % pb, "scale1", "modT"], writes=[("hxT", xb, ft)])
                    else:
                        P.add("act", lambda e, xb=xb, ft=ft, pb=pb, ntok=ntok, mj=mj: e.activation(
                            out=hxT[xb][:, ft, 0:ntok], in_=ptr[pb][:, 0:ntok], func=AF.Identity,
                            scale=scale1[:, ft, mj:mj + 1], bias=modT[:, ft, mj:mj + 1]),
                            reads=["ptr%d" % pb, "scale1", "modT"], writes=[("hxT", xb, ft)])

            def stB(gi):
                (src, r0, nt, mj, soff, xb) = groups[gi]
                ntok = nt * 128
                for ct in range(8):
                    pq = ct % 4

                    def mmu(e, xb=xb, ct=ct, ntok=ntok, pq=pq):
                        ins = None
                        for kt in range(16):
                            ins = e.matmul(pmm[pq][:, 0:ntok], lhsT=w_u[:, kt, ct * 128:(ct + 1) * 128],
                                           rhs=hxT[xb][:, kt, 0:ntok], start=(kt == 0), stop=(kt == 15))
                        return ins
                    P.add("pe", mmu, reads=["w_u", ("hxT", xb)], writes=["pmm%d" % pq])
                    nj = ntok // 8
                    if soff < NOWN:
                        dst = uTown[:, ct, :].rearrange("p (s j) -> p s j", s=8)[:, :, soff // 8:soff // 8 + nj]
                        wk = [("uT", ct, soff)]
                    else:
                        sg = ct % 2
                        dst = ustage[sg][:, 0:ntok].rearrange("p (s j) -> p s j", s=8)
                        wk = [("ustage", sg)]
                    srcv = pmm[pq][:, 0:ntok].rearrange("p (j s) -> p s j", s=8)
                    if ct % 2 == 0:
                        P.add("dve", lambda e, dst=dst, srcv=srcv: e.tensor_copy(dst, srcv),
                              reads=["pmm%d" % pq], writes=wk)
                    else:
                        P.add("act", lambda e, dst=dst, srcv=srcv: e.activation(out=dst, in_=srcv, func=AF.Copy),
                              reads=["pmm%d" % pq], writes=wk)
                    if soff >= NOWN:
                        j0r = (soff - NOWN) // 8
                        P.add("sp", lambda e, ct=ct, sg=sg, ntok=ntok, j0r=j0r, nj=nj: e.dma_start(
                            out=scrU[ct].rearrange("p (s j) -> p s j", s=8)[:, :, j0r:j0r + nj],
                            in_=ustage[sg][:, 0:ntok].rearrange("p (s j) -> p s j", s=8)),
                            reads=[("ustage", sg)], writes=["scrU"], group="ustage%d" % sg)

            stA1(0)
            stA2(0)
            for gi in range(5):
                if gi + 1 < 5:
                    stA1(gi + 1)
                stB(gi)
                if gi + 1 < 5:
                    stA2(gi + 1)
            cvs = ph.enter_context(ExitStack())
            wch = [[sb("wch%d_%d" % (s_, i), [128, 16, 128], BF16, cvs) for i in range(3)] for s_ in range(2)]
            zc = sb("zc", [128, 512], F32, cvs)
            zz = sb("zz", [128, 512], F32, cvs)
            yy = sb("yy", [128, 512], F32, cvs)
            for ct in range(8):
                s_ = ct % 2
                for i in range(3):
                    P.add("pool", lambda e, s_=s_, i=i, ct=ct: e.dma_start(
                        out=wch[s_][i][:], in_=w_in_v[:, :, 1024 * (i + 1) + ct * 128:1024 * (i + 1) + (ct + 1) * 128]),
                        writes=[("wch", s_, i)], group="wch%d_%d" % (s_, i))
                for og, (xb, soff) in enumerate([(1, 0), (0, 512)]):
                    pset = 3 * ((ct * 2 + og) % 2)
                    if True:
                        for i in range(3):
                            def mmb(e, xb=xb, i=i, s_=s_, pset=pset):
                                ins = None
                                for kt in range(16):
                                    ins = e.matmul(pmm[pset + i][:, :], lhsT=wch[s_][i][:, kt, :],
                                                   rhs=hxT[xb][:, kt, :], start=(kt == 0), stop=(kt == 15))
                                return ins
                            P.add("pe", mmb, reads=[("wch", s_, i), ("hxT", xb)], writes=["pmm%d" % (pset + i)])
                        P.add("act", lambda e, pset=pset: e.activation(out=zc[:], in_=pmm[pset + 1][:], func=AF.Copy),
                              reads=["pmm%d" % (pset + 1)], writes=["zc"])
                        P.add("dve", lambda e, pset=pset: e.tensor_tensor(out=zz[:], in0=zc[:], in1=pmm[pset + 2][:], op=ALU.mult),
                              reads=["zc", "pmm%d" % (pset + 2)], writes=["zz"])
                        P.add("dve", lambda e, ct=ct: e.tensor_scalar(
                            out=yy[:], in0=zz[:], scalar1=colB[:, 16 + ct:17 + ct], scalar2=colB[:, 32 + ct:33 + ct],
                            op0=ALU.mult, op1=ALU.add), reads=["zz", "colB"], writes=["yy"])
                        yv = yy[:].rearrange("p (r w) -> p r w", w=64)
                        zv = zz[:].rearrange("p (r w) -> p r w", w=64)
                        P.add("dve", lambda e, ct=ct, yv=yv, zv=zv: e.scalar_tensor_tensor(
                            out=yv[:, :, 1:64], in0=zv[:, :, 0:63], scalar=colB[:, 8 + ct:9 + ct], in1=yv[:, :, 1:64],
                            op0=ALU.mult, op1=ALU.add), reads=["zz", "yy", "colB"], writes=["yy"])
                        P.add("dve", lambda e, ct=ct, yv=yv, zv=zv: e.scalar_tensor_tensor(
                            out=yv[:, :, 0:63], in0=zv[:, :, 1:64], scalar=colB[:, 24 + ct:25 + ct], in1=yv[:, :, 0:63],
                            op0=ALU.mult, op1=ALU.add), reads=["zz", "yy", "colB"], writes=["yy"])
                        P.add("dve", lambda e, ct=ct, soff=soff, pset=pset: e.tensor_tensor(
                            out=convT[:, ct, soff:soff + 512], in0=yy[:], in1=pmm[pset][:], op=ALU.mult),
                            reads=["yy", "pmm%d" % pset], writes=[("convT", ct, soff)])
            P.barrier()
        if debug:
            d_uT = dout("d_uT", [128, 8 * NOWN], BF16)
            d_convT = dout("d_convT", [128, 8 * NOWN], BF16)
            P.add("sp", lambda e: e.dma_start(out=d_uT, in_=uTown[:].rearrange("p a b -> p (a b)")), reads=["uT"],
                  writes=["d_uT"], group="dbg1")
            P.add("sp", lambda e: e.dma_start(out=d_convT, in_=convT[:].rearrange("p a b -> p (a b)")), reads=["convT"],
                  writes=["d_convT"], group="dbg2")

        Z = sb("Z", [128, 8, 8, 128], BF16, mixer)
        P.add("pool", lambda e: e.memset(Z[:], 0.0), writes=["Z"])
        for a_ in range(8):
            for b_ in range(8):
                P.add("dve" if (a_ + b_) % 2 else "pool", lambda e, a_=a_, b_=b_: e.tensor_single_scalar(
                    Z[:, a_, b_, 16 * b_:16 * b_ + 16], iotf[:, 16 * b_:16 * b_ + 16], float(16 * (b_ - a_)), ALU.is_equal),
                    reads=["iotf"], writes=[("Z", a_, b_)])
        mix = mixer.enter_context(ExitStack())
        gT = sb("gT", [128, 8, NOWN], BF16, mix)
        ssm = mix.enter_context(ExitStack())
        U = sb("U", [128, 64, 128], BF16, ssm)
        Pt = sb("Pt", [128, 2, 2, 32, 288], BF16, ssm)
        s12 = ssm.enter_context(ExitStack())
        Ur = sb("Ur", [128, 64, 160], BF16, s12)
        with ExitStack() as ph:
            pU = [ps("pU%d" % i, [128, 288], F32, ph) for i in range(3)]
            ucat = [sb("ucat%d" % i, [128, NSEQ], BF16, ph) for i in range(2)]
            for g in range(64):
                ct, gl = g // 8, g % 8
                pb = g % 3
                if gl == 0:
                    P.add("sp", lambda e, ct=ct: e.dma_start(
                        out=ucat[ct % 2][:].rearrange("p (s j) -> p s j", s=8)[:, :, 128:288],
                        in_=scrU[ct].rearrange("p (s j) -> p s j", s=8)), reads=["scrU"],
                        writes=[("ucat", ct % 2, 1)], group="ucat%d" % (ct % 2))
                    P.add("pool", lambda e, ct=ct: e.tensor_copy(
                        ucat[ct % 2][:].rearrange("p (s j) -> p s j", s=8)[:, :, 0:128],
                        uTown[:, ct, :].rearrange("p (s j) -> p s j", s=8)), reads=["uT"],
                        writes=[("ucat", ct % 2, 0)])

                def shf(e, ct=ct, gl=gl, pb=pb):
                    ins = None
                    for s_ in range(8):
                        src = ucat[ct % 2][:, s_ * 288:(s_ + 1) * 288]
                        ins = e.matmul(pU[pb][:, 0:288], lhsT=Z[:, gl, s_, :], rhs=src, start=(s_ == 0), stop=(s_ == 7))
                    return ins
                P.add("pe", shf, reads=["Z", ("ucat", ct % 2)], writes=["pU%d" % pb])
                P.add("dve", lambda e, g=g, pb=pb: e.tensor_copy(U[:, g, :], pU[pb][:, 0:128]), reads=["pU%d" % pb], writes=[("U", g)])
                P.add("act", lambda e, g=g, pb=pb: e.activation(out=Ur[:, g, :], in_=pU[pb][:, 128:288], func=AF.Copy),
                      reads=["pU%d" % pb], writes=[("U", g)])
            P.barrier()
        with ExitStack() as ph:
            w1c = [sb("w1c%d" % i, [128, 8, 2, 128], BF16, ph) for i in range(2)]
            pP = [ps("pP%d" % i, [128, 288], F32, ph) for i in range(3)]
            nn = 0
            for d_ in range(2):
                for q in range(4):
                    wb_ = (d_ * 4 + q) % 2
                    P.add("sp", lambda e, d_=d_, q=q, wb_=wb_: e.dma_start(
                        out=w1c[wb_][:], in_=scrW1[d_, q * 8:(q + 1) * 8].rearrange("g part r c -> r g part c")),
                        reads=["scrW1"], writes=[("w1c", wb_)], group="w1c%d" % wb_)
                    for g2l in range(8):
                        g2 = q * 8 + g2l
                        for part in range(2):
                            pb = nn % 3
                            nn += 1

                            def mmP(e, d_=d_, g2=g2, g2l=g2l, part=part, pb=pb, wb_=wb_):
                                ins = None
                                for gp in range(2):
                                    g = 2 * g2 + gp
                                    lw = w1c[wb_][:, g2l, part, gp * 64:(gp + 1) * 64]
                                    rows = slice(gp * 64, (gp + 1) * 64)
                                    if d_ == 0:
                                        e.matmul(pP[pb][rows, 0:32], lhsT=lw, rhs=Ur[:, g, 128:160], start=True, stop=True)
                                        ins = e.matmul(pP[pb][rows, 32:160], lhsT=lw, rhs=U[:, g, 0:128], start=True, stop=True)
                                    else:
                                        e.matmul(pP[pb][rows, 0:128], lhsT=lw, rhs=U[:, g, 0:128], start=True, stop=True)
                                        ins = e.matmul(pP[pb][rows, 128:288], lhsT=lw, rhs=Ur[:, g, 0:160], start=True, stop=True)
                                return ins
                            P.add("pe", mmP, reads=[("w1c", wb_), "U"], writes=["pP%d" % pb])
                            ncol = 160 if d_ == 0 else 288
                            if nn % 2 == 0:
                                P.add("dve", lambda e, d_=d_, g2=g2, part=part, pb=pb, ncol=ncol: e.tensor_copy(
                                    Pt[:, part, d_, g2, 0:ncol], pP[pb][:, 0:ncol]), reads=["pP%d" % pb], writes=[("Pt", part, d_, g2)])
                            else:
                                P.add("act", lambda e, d_=d_, g2=g2, part=part, pb=pb, ncol=ncol: e.activation(
                                    out=Pt[:, part, d_, g2, 0:ncol], in_=pP[pb][:, 0:ncol], func=AF.Copy),
                                    reads=["pP%d" % pb], writes=[("Pt", part, d_, g2)])
            P.barrier()
        s12.close()
        with ExitStack() as ph:
            St = [sb("St%d" % i, [128, 4, 64], F32, ph) for i in range(2)]
            C4 = sb("C4", [128, 4, 64], F32, ph)
            rt1 = sb("rt1", [128, 4, 64], F32, ph)
            rt2 = sb("rt2", [128, 2, 64], F32, ph)
            P.add("dve", lambda e: e.memset(St[0][:], 0.0), writes=["St0"])
            P.add("dve", lambda e: e.tensor_copy(C4[:, 0:2, :], Acplx[:, 0:1, :].to_broadcast([128, 2, 64])), reads=["Acplx"], writes=["C4"])
            P.add("dve", lambda e: e.tensor_scalar(out=C4[:, 2, :], in0=Acplx[:, 1, :], scalar1=-1.0, scalar2=None, op0=ALU.mult),
                  reads=["Acplx", "C4"], writes=["C4"])
            P.add("dve", lambda e: e.tensor_copy(C4[:, 3, :], Acplx[:, 1, :]), reads=["Acplx", "C4"], writes=["C4"])
            Pt_full = Pt[:]
            pstep = Pt_full.ap[0][0]
            PART = 2 * 32 * 288
            for i in range(288):
                cur, nxt = St[i % 2], St[(i + 1) % 2]
                ck, nk = "St%d" % (i % 2), "St%d" % ((i + 1) % 2)
                qF, qB = i, 287 - i
                if i < 160:
                    cs = slice(0, 64)
                    dd = [[32 * 288 + qB - qF, 2], [288, 32]]
                    off = Pt_full.offset + qF
                    vv = lambda t_, a, b: t_[:, a:b, :].rearrange("p a (d g) -> p a d g", d=2)
                else:
                    cs = slice(32, 64)
                    dd = [[288, 32]]
                    off = Pt_full.offset + 32 * 288 + qB
                    vv = lambda t_, a, b: t_[:, a:b, 32:64]
                pap = bass.AP(Pt_full.tensor, off, [[pstep, 128], [PART, 2]] + dd)
                pap_sw = bass.AP(Pt_full.tensor, off + PART, [[pstep, 128], [-PART, 2]] + dd)
                P.add("dve", lambda e, cur=cur, cs=cs: e.tensor_tensor(out=rt1[:, :, cs], in0=C4[:, :, cs], in1=cur[:, :, cs], op=ALU.mult),
                      reads=[ck, "C4"], writes=["rt1"])
                P.add("dve", lambda e, cs=cs: e.tensor_tensor(out=rt2[:, :, cs], in0=rt1[:, 0:2, cs], in1=rt1[:, 2:4, cs], op=ALU.add),
                      reads=["rt1"], writes=["rt2"])
                P.add("dve", lambda e, nxt=nxt, vv=vv, pap=pap: e.tensor_tensor(out=vv(nxt, 0, 2), in0=vv(rt2, 0, 2), in1=pap, op=ALU.add),
                      reads=["rt2", ("PtS", i)], writes=[(nk, 0)])
                sw_in = bass.AP(rt2[:].tensor, rt2[:].offset + 64 + (32 if i >= 160 else 0), [[rt2[:].ap[0][0], 128], [-64, 2]] + ([[32, 2], [1, 32]] if i < 160 else [[1, 32]]))
                P.add("dve", lambda e, nxt=nxt, vv=vv, pap_sw=pap_sw, sw_in=sw_in: e.tensor_tensor(out=vv(nxt, 2, 4), in0=sw_in, in1=pap_sw, op=ALU.add),
                      reads=["rt2", ("PtS", i)], writes=[(nk, 1)])
                P.add("act", lambda e, nxt=nxt, vv=vv, pap=pap: e.activation(out=pap, in_=vv(nxt, 0, 2), func=AF.Copy),
                      reads=[(nk, 0)], writes=[("PtS", i)])
            P.barrier()
        if debug:
            d_H = dout("d_H", [128, 2 * 2 * 32 * 288], BF16)
            P.add("sp", lambda e: e.dma_start(out=d_H, in_=Pt[:].rearrange("p a b c d -> p (a b c d)")), reads=["Pt", "PtS"],
                  writes=["d_H"], group="dbgH")

        with ExitStack() as ph:
            Ysb = sb("Ysb", [128, 64, 128], BF16, ph)
            tch = [sb("tch%d" % i, [128, 2, 8, 128], BF16, ph) for i in range(1)] * 2
            w2ch = [sb("w2ch%d" % i, [128, 2, 4, 2, 128], BF16, ph) for i in range(1)] * 2
            pY = [ps("pY%d" % i, [128, 4, 128], F32, ph) for i in range(2)]
            for q in range(8):
                b_ = 0
                for d_ in range(2):
                    P.add("sp", lambda e, q=q, b_=b_, d_=d_: e.dma_start(
                        out=tch[b_][:, d_], in_=scrT[d_, q * 8:(q + 1) * 8].rearrange("g r c -> r g c")),
                        reads=["scrT"], writes=[("tch", b_, d_)], group="tch%d" % b_)
                    P.add("sp", lambda e, q=q, b_=b_, d_=d_: e.dma_start(
                        out=w2ch[b_][:, d_], in_=scrW2[d_, q * 4:(q + 1) * 4].rearrange("g part r c -> r g part c")),
                        reads=["scrW2"], writes=[("w2ch", b_, d_)], group="w2ch%d" % b_)
                for hh in range(2):
                    pb = (q * 2 + hh) % 2

                    def mmY(e, q=q, hh=hh, pb=pb, b_=b_):
                        ins = None
                        for j in range(4):
                            gl = hh * 4 + j
                            g = q * 8 + gl
                            g2, gp = g // 2, g % 2
                            g2l = g2 - q * 4
                            rows = slice(gp * 64, (gp + 1) * 64)
                            o = pY[pb][:, j, :]
                            e.matmul(o, lhsT=tch[b_][:, 0, gl, :], rhs=U[:, g, 0:128], start=True, stop=False)
                            e.matmul(o, lhsT=w2ch[b_][rows, 0, g2l, 0, :], rhs=Pt[rows, 0, 0, g2, 31:159], start=False, stop=False)
                            e.matmul(o, lhsT=w2ch[b_][rows, 0, g2l, 1, :], rhs=Pt[rows, 1, 0, g2, 31:159], start=False, stop=False)
                            e.matmul(o, lhsT=tch[b_][:, 1, gl, :], rhs=U[:, g, 0:128], start=False, stop=False)
                            e.matmul(o, lhsT=w2ch[b_][rows, 1, g2l, 0, :], rhs=Pt[rows, 0, 1, g2, 1:129], start=False, stop=False)
                            ins = e.matmul(o, lhsT=w2ch[b_][rows, 1, g2l, 1, :], rhs=Pt[rows, 1, 1, g2, 1:129], start=False, stop=True)
                        return ins
                    P.add("pe", mmY, reads=[("tch", b_), ("w2ch", b_), "U", "Pt", "PtS"], writes=["pY%d" % pb])
                    g0 = q * 8 + hh * 4
                    if hh == 0:
                        P.add("dve", lambda e, g0=g0, pb=pb: e.tensor_copy(Ysb[:, g0:g0 + 4, :], pY[pb][:]), reads=["pY%d" % pb],
                              writes=[("Ysb", g0)])
                    else:
                        P.add("act", lambda e, g0=g0, pb=pb: e.activation(out=Ysb[:, g0:g0 + 4, :], in_=pY[pb][:], func=AF.Copy),
                              reads=["pY%d" % pb], writes=[("Ysb", g0)])
            pZ = [ps("pZ%d" % i, [128, 512], F32, ph) for i in range(2)]
            yf = [sb("yf%d" % i, [128, 512], F32, ph) for i in range(2)]
            ya = [sb("ya%d" % i, [128, 512], F32, ph) for i in range(2)]
            for ct in range(8):
                chains = []
                for half in range(2):
                    pb = half

                    def uns(e, ct=ct, half=half, pb=pb):
                        ins = None
                        ov = pZ[pb][:, :].rearrange("p (j t) -> p t j", t=8)
                        for t_ in range(8):
                            for gl in range(8):
                                ins = e.matmul(ov[:, t_, :], lhsT=Z[:, t_, gl, :], rhs=Ysb[:, ct * 8 + gl, half * 64:(half + 1) * 64],
                                               start=(gl == 0), stop=(gl == 7))
                        return ins
                    P.add("pe", uns, reads=["Z", "Ysb"], writes=["pZ%d" % pb])
                    tk = slice(half * 512, (half + 1) * 512)
                    yk, ak = "yf%d" % pb, "ya%d" % pb
                    chains.append([
                        ("dve", lambda e, ct=ct, pb=pb, half=half: e.scalar_tensor_tensor(
                            out=yf[pb][:].rearrange("p (j s) -> p j s", s=8),
                            in0=uTown[:, ct, :].rearrange("p (s j) -> p j s", s=8)[:, half * 64:(half + 1) * 64, :],
                            scalar=colB[:, ct:ct + 1], in1=pZ[pb][:, :].rearrange("p (j s) -> p j s", s=8), op0=ALU.mult, op1=ALU.add),
                         ["uT", "colB", "pZ%d" % pb], [yk]),
                        ("act", lambda e, pb=pb: e.activation(out=ya[pb][:], in_=yf[pb][:], func=AF.Square), [yk], [ak]),
                        ("dve", lambda e, pb=pb: e.tensor_scalar(out=ya[pb][:], in0=ya[pb][:], scalar1=0.044715, scalar2=1.0,
                                                                 op0=ALU.mult, op1=ALU.add), [ak], [ak]),
                        ("dve", lambda e, pb=pb: e.tensor_tensor(out=ya[pb][:], in0=ya[pb][:], in1=yf[pb][:], op=ALU.mult), [ak, yk], [ak]),
                        ("act", lambda e, pb=pb: e.activation(out=ya[pb][:], in_=ya[pb][:], func=AF.Sigmoid, scale=1.5957691216057308),
                         [ak], [ak]),
                        ("dve", lambda e, pb=pb, ct=ct, tk=tk: e.tensor_tensor(out=gT[:, ct, tk], in0=ya[pb][:], in1=yf[pb][:], op=ALU.mult),
                         [ak, yk], [("gT", ct, half)]),
                    ])
                for k in range(6):
                    for ch in chains:
                        en, f_, rk, wk = ch[k]
                        P.add(en, f_, reads=rk, writes=wk)
            P.barrier()
        ssm.close()
        if debug:
            d_gT = dout("d_gT", [128, 8 * NOWN], BF16)
            P.add("sp", lambda e: e.dma_start(out=d_gT, in_=gT[:].rearrange("p a b -> p (a b)")), reads=["gT"],
                  writes=["d_gT"], group="dbgG")

        ssmT = sb("ssmT", [128, 8, NOWN], BF16, mix)
        rstd = sb("rstdSC", [128, 2, NOWN], F32, mix)
        with ExitStack() as ph:
            wglu = sb("wglu", [128, 8, 2048], BF16, ph)
            wgv = w_glu.rearrange("(kt p) c -> p kt c", p=128)
            for kt in range(8):
                for hc in range(2):
                    P.add("pool", lambda e, kt=kt, hc=hc: e.dma_start(out=wglu[:, kt, hc * 1024:(hc + 1) * 1024],
                                                                      in_=wgv[:, kt, hc * 1024:(hc + 1) * 1024]),
                          writes=[("wglu", kt, hc)], group="wglu")
            pga = [ps("pga%d" % i, [128, 512], F32, ph) for i in range(2)]
            pgb = [ps("pgb%d" % i, [128, 512], F32, ph) for i in range(2)]
            pss = ps("pss", [128, 512], F32, ph)
            sig = [sb("sig%d" % i, [128, 512], F32, ph) for i in range(2)]
            sq = [sb("sq%d" % i, [128, 512], BF16, ph) for i in range(2)]
            pend = []
            for which in range(2):
                for half in range(2):
                    tk = slice(half * 512, (half + 1) * 512)
                    for ot in range(8):
                        b_ = ot % 2
                        if which == 0:
                            def mg(e, ot=ot, b_=b_, tk=tk, off=0, pp=pga):
                                ins = None
                                for kt in range(8):
                                    ins = e.matmul(pp[b_][:, :], lhsT=wglu[:, kt, off + ot * 128:off + (ot + 1) * 128], rhs=gT[:, kt, tk],
                                                   start=(kt == 0), stop=(kt == 7))
                                return ins
                            P.add("pe", mg, reads=["wglu", "gT"], writes=["pga%d" % b_])
                            P.add("pe", lambda e, ot=ot, b_=b_, tk=tk: mg(e, ot, b_, tk, 1024, pgb), reads=["wglu", "gT"], writes=["pgb%d" % b_])
                            P.add("act", lambda e, b_=b_: e.activation(out=sig[b_][:], in_=pgb[b_][:], func=AF.Sigmoid),
                                  reads=["pgb%d" % b_], writes=["sig%d" % b_])
                            P.add("dve", lambda e, b_=b_, ot=ot, tk=tk: e.tensor_tensor(out=ssmT[:, ot, tk], in0=pga[b_][:], in1=sig[b_][:], op=ALU.mult),
                                  reads=["pga%d" % b_, "sig%d" % b_], writes=[("ssmT", ot, half)])
                            srcT, skey = ssmT, ("ssmT", ot, half)
                        else:
                            srcT, skey = convT, "convT"
                        def back(b_=b_, ot=ot, tk=tk, srcT=srcT, skey=skey):
                            P.add("act", lambda e: e.activation(out=sq[b_][:], in_=srcT[:, ot, tk], func=AF.Square),
                                  reads=[skey], writes=["sq%d" % b_])
                            P.add("pe", lambda e: e.matmul(pss[:, :], lhsT=ones_b[:], rhs=sq[b_][:], start=(ot == 0), stop=(ot == 7)),
                                  reads=["ones_b", "sq%d" % b_], writes=["pss"])
                        if pend:
                            pend.pop()()
                        pend.append(back)
                    if pend:
                        pend.pop()()
                    rk = ("rstd", which, half)
                    P.add("dve", lambda e, which=which, tk=tk: e.tensor_scalar(out=rstd[:, which, tk], in0=pss[:, :], scalar1=1.0 / 1024, scalar2=EPS,
                                                                              op0=ALU.mult, op1=ALU.add), reads=["pss"], writes=[rk])
                    P.add("act", lambda e, which=which, tk=tk: e.activation(out=rstd[:, which, tk], in_=rstd[:, which, tk], func=AF.Sqrt),
                          reads=[rk], writes=[rk])
                    P.add("dve", lambda e, which=which, tk=tk: e.reciprocal(out=rstd[:, which, tk], in_=rstd[:, which, tk]), reads=[rk], writes=[rk])
            for ot in range(8):
                P.add("dve", lambda e, ot=ot: e.scalar_tensor_tensor(out=ssmT[:, ot, :], in0=ssmT[:, ot, :], scalar=colA[:, 80 + ot:81 + ot],
                                                                     in1=rstd[:, 0, :], op0=ALU.mult, op1=ALU.mult),
                      reads=["ssmT", "rstd", "colA"], writes=[("ssmT", ot)])
                P.add("dve", lambda e, ot=ot: e.scalar_tensor_tensor(out=convT[:, ot, :], in0=convT[:, ot, :], scalar=colA[:, 88 + ot:89 + ot],
                                                                      in1=rstd[:, 1, :], op0=ALU.mult, op1=ALU.mult),
                      reads=["convT", "rstd", "colA"], writes=[("convT", ot)])
            P.barrier()

        def row_bcast(dst, col_of_ft, rkeys, wkey, stack_ps):
            dgs = [sb(wkey + "_dg%d" % i, [128, 128], F32, stack_ps) for i in range(2)]
            prb = ps(wkey + "_prb", [128, 512], F32, stack_ps)
            for c4 in range(4):
                for j in range(4):
                    ft = c4 * 4 + j
                    b_ = ft % 2
                    P.add("dve", lambda e, ft=ft, b_=b_: e.tensor_scalar(out=dgs[b_][:], in0=ident_f[:], scalar1=col_of_ft(ft), scalar2=None,
                                                                        op0=ALU.mult), reads=["ident_f"] + rkeys, writes=[wkey + "_dg%d" % b_])
                    P.add("pe", lambda e, j=j, b_=b_: e.matmul(prb[:, j * 128:(j + 1) * 128], lhsT=ones_f[:], rhs=dgs[b_][:], start=True, stop=True),
                          reads=["ones_f", wkey + "_dg%d" % b_], writes=[wkey + "_prb"])
                P.add("act", lambda e, c4=c4: e.activation(out=dst[:, c4 * 512:(c4 + 1) * 512], in_=prb[:, :], func=AF.Copy),
                      reads=[wkey + "_prb"], writes=[wkey])

        with ExitStack() as ph:
            g1b = sb("g1b", [128, D], F32, ph)
            with ExitStack() as ph3:
                row_bcast(g1b, lambda ft: modT[:, 32 + ft, 0:1], ["modT"], "g1b", ph3)
                P.barrier()
            woc = [sb("woc%d" % i, [128, 16, 512], BF16, ph) for i in range(2)]
            wov = w_out.rearrange("(kt p) c -> p kt c", p=128)
            po = [ps("po%d" % i, [128, 512], F32, ph) for i in range(3)]
            xp = [sb("xp%d" % i, [128, 512], F32, ph) for i in range(3)]
            x1p = [sb("x1p%d" % i, [128, 512], F32, ph) for i in range(3)]
            jk = sb("jk", [128, 512], BF16, ph)
            n = 0
            for cc in range(4):
                wb_ = cc % 2
                for kt in range(16):
                    P.add("pool", lambda e, kt=kt, cc=cc, wb_=wb_: e.dma_start(out=woc[wb_][:, kt, :], in_=wov[:, kt, cc * 512:(cc + 1) * 512]),
                          writes=[("woc", wb_, kt)], group="woc%d" % wb_)
                for tt in range(8):
                    b_ = n % 3
                    n += 1
                    rows = slice(tt * 128, (tt + 1) * 128)
                    cols = slice(cc * 512, (cc + 1) * 512)
                    P.add("sp", lambda e, b_=b_, rows=rows, cols=cols: e.dma_start(out=xp[b_][:], in_=xs[rows, cols]), writes=[("xp", b_)],
                          group="xp%d" % b_)

                    def mo(e, tt=tt, b_=b_, wb_=wb_):
                        ins = None
                        for ht in range(16):
                            hsrc = ssmT if ht < 8 else convT
                            ins = e.matmul(po[b_][:, :], lhsT=hsrc[:, ht % 8, tt * 128:(tt + 1) * 128], rhs=woc[wb_][:, ht, :],
                                           start=(ht == 0), stop=(ht == 15))
                        return ins
                    P.add("pe", mo, reads=["ssmT", "convT", ("woc", wb_)], writes=["po%d" % b_])
                    P.add("dve", lambda e, b_=b_, cols=cols: e.tensor_tensor(out=x1p[b_][:], in0=po[b_][:, :], in1=g1b[:, cols], op=ALU.mult),
                          reads=["po%d" % b_, "g1b"], writes=[("x1p", b_)])
                    P.add("dve", lambda e, b_=b_: e.tensor_tensor(out=x1p[b_][:], in0=x1p[b_][:], in1=xp[b_][:], op=ALU.add),
                          reads=[("x1p", b_), ("xp", b_)], writes=[("x1p", b_)])
                    P.add("act", lambda e, b_=b_, tt=tt, cc=cc: e.activation(out=jk[:], in_=x1p[b_][:], func=AF.Square,
                                                                            accum_out=ss2[:, tt, cc:cc + 1]),
                          reads=[("x1p", b_)], writes=["jk", ("ss2", tt, cc)])
                    P.add("sp", lambda e, b_=b_, rows=rows, cols=cols: e.dma_start(out=scrX1[rows, cols], in_=x1p[b_][:]),
                          reads=[("x1p", b_)], writes=["scrX1"], group="x1p%d" % b_)
            P.barrier()
        mix.close()
        mixer.close()

        moe = top.enter_context(ExitStack())
        acc = sb("acc", [128, 8, D], F32, moe)
        hx2T = sb("hx2T", [128, 16, NOWN], BF16, moe)
        Wt = sb("Wt", [128, 8, 64], F32, moe)
        with ExitStack() as ph:
            sc2b = sb("sc2b", [128, D], F32, ph)
            sh2b = sb("sh2b", [128, D], F32, ph)
            scale2 = sb("scale2", [128, 16], F32, ph)
            P.add("dve", lambda e: e.scalar_tensor_tensor(out=scale2[:], in0=modT[:, 64:80, 0], scalar=1.0, in1=colA[:, 48:64],
                                                          op0=ALU.add, op1=ALU.mult), reads=["modT", "colA"], writes=["scale2"])
            with ExitStack() as ph3:
                row_bcast(sc2b, lambda ft: scale2[:, ft:ft + 1], ["scale2"], "sc2b", ph3)
                P.barrier()
            with ExitStack() as ph3:
                row_bcast(sh2b, lambda ft: modT[:, 48 + ft, 0:1], ["modT"], "sh2b", ph3)
                P.barrier()
            rw = sb("rw", [128, 16, 64], F32, ph)
            P.add("sp", lambda e: e.dma_start(out=rw[:], in_=router_w.rearrange("(kt p) c -> p kt c", p=128)), writes=["rw"], group="rw")
            rb = sb("rb", [128, 64], F32, ph)
            P.add("sp", lambda e: e.dma_start(out=rb[:], in_=rbias_b), writes=["rb"], group="rb")
            rs2 = sb("rs2", [128, 8], F32, ph)
            P.add("dve", lambda e: e.tensor_reduce(out=rs2[:], in_=ss2[:], axis=AX.X, op=ALU.add), reads=["ss2"], writes=["rs2"])
            P.add("dve", lambda e: e.tensor_scalar(out=rs2[:], in0=rs2[:], scalar1=1.0 / D, scalar2=EPS, op0=ALU.mult, op1=ALU.add),
                  reads=["rs2"], writes=["rs2"])
            P.add("act", lambda e: e.activation(out=rs2[:], in_=rs2[:], func=AF.Sqrt), reads=["rs2"], writes=["rs2"])
            P.add("dve", lambda e: e.reciprocal(out=rs2[:], in_=rs2[:]), reads=["rs2"], writes=["rs2"])
            x1t = [sb("x1t%d" % i, [128, D], F32, ph) for i in range(2)]
            hf = [sb("hf%d" % i, [128, D], F32, ph) for i in range(2)]
            pth = [ps("pth%d" % i, [128, 4, 128], F32, ph) for i in range(2)]
            hfs = [sb("hfs%d" % i, [128, 4, 128], F32, ph) for i in range(2)]
            plgs = [ps("plg%d" % i, [128, 64], F32, ph) for i in range(2)]
            rt = sb("rt", [128, 12, 64], F32, ph)
            m8 = sb("m8", [128, 16], F32, ph)

            def n2A(tt):
                b_ = tt % 2
                rows = slice(tt * 128, (tt + 1) * 128)
                P.add("sp", lambda e, b_=b_, rows=rows: e.dma_start(out=x1t[b_][:], in_=scrX1[rows, :]), reads=["scrX1"],
                      writes=[("x1t", b_)], group="x1t%d" % b_)
                P.add("act", lambda e, b_=b_, tt=tt: e.activation(out=hf[b_][:], in_=x1t[b_][:], func=AF.Copy, scale=rs2[:, tt:tt + 1]),
                      reads=[("x1t", b_), "rs2"], writes=[("hf", b_)])
                P.add("dve", lambda e, b_=b_: e.tensor_tensor(out=hf[b_][:], in0=hf[b_][:], in1=sc2b[:], op=ALU.mult),
                      reads=[("hf", b_), "sc2b"], writes=[("hf", b_)])
                P.add("pool", lambda e, b_=b_: e.tensor_tensor(out=hf[b_][:], in0=hf[b_][:], in1=sh2b[:], op=ALU.add),
                      reads=[("hf", b_), "sh2b"], writes=[("hf", b_)])

            def n2B(tt):
                b_ = tt % 2
                plg = plgs[tt % 2]
                pk = "plg%d" % (tt % 2)
                for f4 in range(4):
                    pb = f4 % 2

                    def trh(e, b_=b_, f4=f4, pb=pb):
                        ins = None
                        for j in range(4):
                            ft = f4 * 4 + j
                            ins = e.transpose(out=pth[pb][:, j, :], in_=hf[b_][:, ft * 128:(ft + 1) * 128], identity=ident_f[:])
                        return ins
                    P.add("pe", trh, reads=[("hf", b_), "ident_f"], writes=["pth%d" % pb])
                    P.add("act", lambda e, pb=pb, f4=f4, tt=tt: e.activation(out=hx2T[:, f4 * 4:(f4 + 1) * 4, tt * 128:(tt + 1) * 128],
                                                                            in_=pth[pb][:], func=AF.Copy),
                          reads=["pth%d" % pb], writes=[("hx2T", f4, tt)])
                    P.add("dve", lambda e, pb=pb: e.tensor_copy(hfs[pb][:], pth[pb][:]), reads=["pth%d" % pb], writes=[("hfs", pb)])

                    def mr(e, pb=pb, f4=f4, plg=plg):
                        ins = None
                        for j in range(4):
                            ft = f4 * 4 + j
                            ins = e.matmul(plg[:, :], lhsT=hfs[pb][:, j, :], rhs=rw[:, ft, :], start=(ft == 0), stop=(ft == 15))
                        return ins
                    P.add("pe", mr, reads=[("hfs", pb), "rw"], writes=[pk])

            def n2C(tt):
                plg = plgs[tt % 2]
                pk = "plg%d" % (tt % 2)
                S_, Bi, T1, T2, MB, EM = (rt[:, i, :] for i in range(6))
                g3 = lambda ap: ap.rearrange("p (g k) -> p g k", k=8)
                rops = [
                    ("act", lambda e: e.activation(out=S_, in_=plg[:, :], func=AF.Sigmoid), [pk]),
                    ("dve", lambda e: e.tensor_tensor(out=Bi, in0=S_, in1=rb[:], op=ALU.add), ["rb"]),
                    ("dve", lambda e: e.tensor_reduce(out=m8[:, 0:8], in_=g3(Bi), axis=AX.X, op=ALU.max), []),
                    ("dve", lambda e: e.tensor_tensor(out=g3(T1), in0=g3(Bi), in1=m8[:, 0:8].unsqueeze(2).to_broadcast([128, 8, 8]), op=ALU.is_equal), []),
                    ("dve", lambda e: e.scalar_tensor_tensor(out=T1, in0=T1, scalar=-1e9, in1=Bi, op0=ALU.mult, op1=ALU.add), []),
                    ("dve", lambda e: e.tensor_reduce(out=m8[:, 8:16], in_=g3(T1), axis=AX.X, op=ALU.max), []),
                    ("dve", lambda e: e.tensor_tensor(out=m8[:, 0:8], in0=m8[:, 0:8], in1=m8[:, 8:16], op=ALU.add), []),
                    ("dve", lambda e: e.max(out=m8[:, 8:16], in_=m8[:, 0:8]), []),
                    ("dve", lambda e: e.tensor_scalar(out=m8[:, 0:8], in0=m8[:, 0:8], scalar1=m8[:, 11:12], scalar2=None, op0=ALU.is_ge), []),
                    ("dve", lambda e: e.tensor_tensor(out=g3(MB), in0=g3(Bi), in1=m8[:, 0:8].unsqueeze(2).to_broadcast([128, 8, 8]), op=ALU.mult), []),
                    ("dve", lambda e: e.tensor_scalar(out=m8[:, 0:8], in0=m8[:, 0:8], scalar1=-1.0, scalar2=1e9, op0=ALU.add, op1=ALU.mult), []),
                    ("dve", lambda e: e.tensor_tensor(out=g3(MB), in0=g3(MB), in1=m8[:, 0:8].unsqueeze(2).to_broadcast([128, 8, 8]), op=ALU.add), []),
                    ("dve", lambda e: e.max(out=m8[:, 8:16], in_=MB), []),
                    ("dve", lambda e: e.tensor_scalar(out=EM, in0=MB, scalar1=m8[:, 15:16], scalar2=None, op0=ALU.is_ge), []),
                    ("dve", lambda e: e.tensor_tensor(out=T2, in0=S_, in1=EM, op=ALU.mult), []),
                    ("dve", lambda e: e.tensor_reduce(out=m8[:, 0:1], in_=T2, axis=AX.X, op=ALU.add), []),
                    ("dve", lambda e: e.reciprocal(out=m8[:, 0:1], in_=m8[:, 0:1]), []),
                    ("dve", lambda e, tt=tt: e.tensor_scalar(out=Wt[:, tt, :], in0=T2, scalar1=m8[:, 0:1], scalar2=2.5, op0=ALU.mult, op1=ALU.mult), []),
                ]
                for (en, f_, rk) in rops:
                    P.add(en, f_, reads=["rt", "m8"] + rk, writes=["rt", "m8", ("Wt", tt)])

            for it in range(10):
                if it < 8:
                    n2A(it)
                if 1 <= it <= 8:
                    n2B(it - 1)
                if it >= 2:
                    n2C(it - 2)
            P.barrier()
        if debug:
            d_Wt = dout("d_Wt", [128, 512])
            P.add("sp", lambda e: e.dma_start(out=d_Wt, in_=Wt[:].rearrange("p a b -> p (a b)")), reads=["Wt"], writes=["d_Wt"], group="dbgW")
            d_hx2T = dout("d_hx2T", [128, 16 * NOWN], BF16)
            P.add("sp", lambda e: e.dma_start(out=d_hx2T, in_=hx2T[:].rearrange("p a b -> p (a b)")), reads=["hx2T"], writes=["d_hx2T"], group="dbgW2")

        with ExitStack() as ph:
            wg = [sb("wg%d" % i, [128, 16, 512], BF16, ph) for i in range(2)]
            wu = [sb("wu%d" % i, [128, 16, 512], BF16, ph) for i in range(2)]
            wd = [sb("wd0", [128, 4, D], BF16, ph)]
            actT = sb("actT", [128, 4, NOWN], BF16, ph)
            sgl = [sb("sgl%d" % i, [128, 512], F32, ph) for i in range(2)]
            pg = [ps("pg%d" % i, [128, 512], F32, ph) for i in range(2)]
            pu = [ps("pu%d" % i, [128, 512], F32, ph) for i in range(2)]
            pd = [ps("pd%d" % i, [128, 512], F32, ph) for i in range(3)]
            P.add("pool", lambda e: e.memset(acc[:], 0.0), writes=["acc"])
            NE = 65
            import os
            DMAONLY = os.environ.get("MOE_DMAONLY", "")
            _Padd = P.add
            if DMAONLY:
                class _PX:
                    @staticmethod
                    def add(eng, fn, reads=(), writes=(), group=None):
                        if group is None:
                            return None
                        q = {"1": "pool", "2": "sp", "3": "act"}[DMAONLY[0]]
                        return _Padd(q if DMAONLY[0] != "4" else eng, fn, reads=reads, writes=writes, group=group)
                PM = _PX
            else:
                PM = P
            for ex in range(NE):
                b_ = ex % 2
                gsrc = ew_gate[ex] if ex < 64 else sw_gate
                usrc = ew_up[ex] if ex < 64 else sw_up
                dsrc = ew_down[ex] if ex < 64 else sw_down
                PM.add("pool", lambda e, b_=b_, gsrc=gsrc: e.dma_start(out=wg[b_][:], in_=gsrc.rearrange("(kt p) c -> p kt c", p=128)),
                      writes=[("wg", b_)], group="wg%d" % b_)
                PM.add("pool", lambda e, b_=b_, usrc=usrc: e.dma_start(out=wu[b_][:], in_=usrc.rearrange("(kt p) c -> p kt c", p=128)),
                      writes=[("wu", b_)], group="wu%d" % b_)
                dv = dsrc.rearrange("(kt p) c -> p kt c", p=128)
                for hc in range(2):
                    PM.add("pool", lambda e, dv=dv, hc=hc: e.dma_start(out=wd[0][:, :, hc * 1024:(hc + 1) * 1024],
                                                                     in_=dv[:, :, hc * 1024:(hc + 1) * 1024]),
                          writes=[("wd", hc)], group="wd")
                n = 0
                for mt in range(4):
                    for half in range(2):
                        pb = n % 2
                        n += 1
                        tk = slice(half * 512, (half + 1) * 512)

                        def mgu(e, w_, pp, mt=mt, tk=tk, pb=pb, b_=b_):
                            ins = None
                            for kt in range(16):
                                ins = e.matmul(pp[pb][:, :], lhsT=w_[b_][:, kt, mt * 128:(mt + 1) * 128], rhs=hx2T[:, kt, tk],
                                               start=(kt == 0), stop=(kt == 15))
                            return ins
                        PM.add("pe", lambda e, f_=mgu: f_(e, wg, pg), reads=[("wg", b_), "hx2T"], writes=["pg%d" % pb])
                        PM.add("pe", lambda e, f_=mgu: f_(e, wu, pu), reads=[("wu", b_), "hx2T"], writes=["pu%d" % pb])
                        PM.add("act", lambda e, pb=pb: e.activation(out=sgl[pb][:], in_=pg[pb][:, :], func=AF.Silu),
                              reads=["pg%d" % pb], writes=[("sgl", pb)])
                        PM.add("dve", lambda e, pb=pb, mt=mt, tk=tk: e.tensor_tensor(out=actT[:, mt, tk], in0=sgl[pb][:], in1=pu[pb][:, :], op=ALU.mult),
                              reads=[("sgl", pb), "pu%d" % pb], writes=[("actT", mt, half)])
                n = 0
                for tt in range(8):
                    for cc in range(4):
                        pb = n % 3
                        n += 1

                        def mdn(e, tt=tt, cc=cc, pb=pb):
                            ins = None
                            for kt in range(4):
                                ins = e.matmul(pd[pb][:, :], lhsT=actT[:, kt, tt * 128:(tt + 1) * 128], rhs=wd[0][:, kt, cc * 512:(cc + 1) * 512],
                                               start=(kt == 0), stop=(kt == 3))
                            return ins
                        PM.add("pe", mdn, reads=["actT", "wd"], writes=["pd%d" % pb])
                        wsc = Wt[:, tt, ex:ex + 1] if ex < 64 else 1.0
                        PM.add("dve", lambda e, tt=tt, cc=cc, pb=pb, wsc=wsc: e.scalar_tensor_tensor(
                            out=acc[:, tt, cc * 512:(cc + 1) * 512], in0=pd[pb][:, :], scalar=wsc, in1=acc[:, tt, cc * 512:(cc + 1) * 512],
                            op0=ALU.mult, op1=ALU.add), reads=["pd%d" % pb, "Wt"], writes=[("acc", tt, cc)])
            P.barrier()

        with ExitStack() as ph:
            g2b = sb("g2b", [128, D], F32, ph)
            fgb = sb("fgb", [128, D], F32, ph)
            with ExitStack() as ph3:
                row_bcast(g2b, lambda ft: modT[:, 80 + ft, 0:1], ["modT"], "g2b", ph3)
                P.barrier()
            with ExitStack() as ph3:
                row_bcast(fgb, lambda ft: colA[:, 64 + ft:65 + ft], ["colA"], "fgb", ph3)
                P.barrier()
            x1f = [sb("x1f%d" % i, [128, D], F32, ph) for i in range(2)]
            fo = [sb("fo%d" % i, [128, D], F32, ph) for i in range(2)]
            fs = sb("fs", [128, 8], F32, ph)
            fj = sb("fj", [128, D], BF16, ph)
            def stageA(tt):
                b_ = tt % 2
                rows = slice(tt * 128, (tt + 1) * 128)
                P.add("sp", lambda e, b_=b_, rows=rows: e.dma_start(out=x1f[b_][:], in_=scrX1[rows, :]), reads=["scrX1"],
                      writes=[("x1f", b_)], group="x1f%d" % b_)
                for hc in range(2):
                    cs = slice(hc * 1024, (hc + 1) * 1024)
                    P.add("dve", lambda e, tt=tt, cs=cs: e.tensor_tensor(out=acc[:, tt, cs], in0=acc[:, tt, cs], in1=g2b[:, cs], op=ALU.mult),
                          reads=[("acc", tt, hc), "g2b"], writes=[("acc", tt, hc)])
                    P.add("dve", lambda e, tt=tt, b_=b_, cs=cs: e.tensor_tensor(out=acc[:, tt, cs], in0=acc[:, tt, cs], in1=x1f[b_][:, cs], op=ALU.add),
                          reads=[("acc", tt, hc), ("x1f", b_)], writes=[("acc", tt, hc)])
                    P.add("act", lambda e, tt=tt, cs=cs, hc=hc: e.activation(out=fj[:, cs], in_=acc[:, tt, cs], func=AF.Square,
                                                                          accum_out=fs2[:, tt, hc:hc + 1]),
                          reads=[("acc", tt, hc)], writes=[("fj", hc), ("fs2", tt, hc)])

            def stageB(tt):
                b_ = tt % 2
                rows = slice(tt * 128, (tt + 1) * 128)
                P.add("dve", lambda e, tt=tt: e.tensor_tensor(out=fs[:, tt:tt + 1], in0=fs2[:, tt, 0:1], in1=fs2[:, tt, 1:2], op=ALU.add),
                      reads=[("fs2", tt)], writes=[("fs", tt)])
                P.add("dve", lambda e, tt=tt: e.tensor_scalar(out=fs[:, tt:tt + 1], in0=fs[:, tt:tt + 1], scalar1=1.0 / D, scalar2=EPS,
                                                              op0=ALU.mult, op1=ALU.add), reads=[("fs", tt)], writes=[("fs", tt)])
                P.add("act", lambda e, tt=tt: e.activation(out=fs[:, tt:tt + 1], in_=fs[:, tt:tt + 1], func=AF.Sqrt), reads=[("fs", tt)], writes=[("fs", tt)])
                P.add("dve", lambda e, tt=tt: e.reciprocal(out=fs[:, tt:tt + 1], in_=fs[:, tt:tt + 1]), reads=[("fs", tt)], writes=[("fs", tt)])
                P.add("act", lambda e, tt=tt, b_=b_: e.activation(out=fo[b_][:], in_=acc[:, tt, :], func=AF.Copy, scale=fs[:, tt:tt + 1]),
                      reads=[("acc", tt), ("fs", tt)], writes=[("fo", b_)])
                P.add("dve", lambda e, b_=b_: e.tensor_tensor(out=fo[b_][:], in0=fo[b_][:], in1=fgb[:], op=ALU.mult),
                      reads=[("fo", b_), "fgb"], writes=[("fo", b_)])
                P.add("sp", lambda e, b_=b_, rows=rows: e.dma_start(out=out[rows, :], in_=fo[b_][:]), reads=[("fo", b_)], writes=["out"],
                      group="fo%d" % b_)

            fs2 = sb("fs2", [128, 8, 2], F32, ph)
            for tt in range(9):
                if tt < 8:
                    stageA(tt)
                if tt >= 1:
                    stageB(tt - 1)

        P.add("sp", None, reads=["out", "scrW1", "scrT", "scrW2", "scrU", "scrX1"] + list(dbg.keys()))
        P.emit()
    return nc, dbg


def prep_inputs(inp):
    f = lambda a: np.ascontiguousarray(a, dtype=np.float32)
    x, ctx, c = inp["x"], inp["ctx"], inp["c"]
    maps = []
    shared = {
        "w_ada": f(inp["w_ada"][0]), "w_in": f(inp["w_in"][0]),
        "b_ada": f(inp["b_ada"][0].reshape(96, 128)),
        "w_glu": f(inp["ssm_w_glu"][0]), "w_out": f(inp["w_out"][0]), "router_w": f(inp["router_w"][0]),
        "rbias_b": f(np.tile(inp["router_bias"][0].reshape(1, 64), (128, 1))),
        "ew_gate": f(inp["exp_w_gate"][0]), "ew_up": f(inp["exp_w_up"][0]), "ew_down": f(inp["exp_w_down"][0]),
        "sw_gate": f(inp["shared_w_gate"][0]), "sw_up": f(inp["shared_w_up"][0]), "sw_down": f(inp["shared_w_down"][0]),
    }
    for core in range(8):
        b, h = core // 2, core % 2
        xb = x[b]
        cb = ctx[b]
        conv_w = inp["conv_w"][0]
        if h == 1:
            xb = xb[::-1]
            cb = cb[::-1]
            conv_w = conv_w[::-1]
        vecsA = np.concatenate([c[b].reshape(16, 128), inp["c_ctx"].reshape(16, 128),
                                inp["norm1_g"][0].reshape(16, 128), inp["norm2_g"][0].reshape(16, 128),
                                inp["final_g"].reshape(16, 128), inp["mix_norm_g"][0].reshape(16, 128)], 0)
        vecsB = np.concatenate([inp["ssm_d"][0].reshape(8, 128), conv_w.reshape(24, 128),
                                inp["conv_b"][0].reshape(8, 128)], 0)
        sl = slice(None, None, -1) if h == 1 else slice(None)
        m = dict(shared)
        m.update(xs=f(xb), ctxs=f(cb), vecsA=f(vecsA), vecsB=f(vecsB),
                 lamre_p=f(inp["ssm_lam_re"][0][sl].reshape(64, 128)), lamim_p=f(inp["ssm_lam_im"][0][sl].reshape(64, 128)),
                 logdt_p=f(inp["ssm_log_dt"][0][sl].reshape(64, 2)),
                 ssm_b_re=f(inp["ssm_b_re"][0][sl]), ssm_b_im=f(inp["ssm_b_im"][0][sl]),
                 ssm_c_re=f(inp["ssm_c_re"][0][sl]), ssm_c_im=f(inp["ssm_c_im"][0][sl]))
        maps.append(m)
    return maps


def kernel(**inputs):
    nc, _ = build_nc(False)
    maps = prep_inputs(inputs)
    res = run_bass_kernel_spmd(nc, maps, core_ids=list(range(8)))
    outs = np.zeros((4, 2048, 2048), np.float32)
    for core in range(8):
        b, h = core // 2, core % 2
        o = res.results[core]["out"]
        if h == 0:
            outs[b, 0:1024] = o
        else:
            outs[b, 1024:2048] = o[::-1]
    return outs
```

```python
from contextlib import ExitStack
import numpy as np
import concourse.bass as bass
import concourse.mybir as mybir
from concourse.bass_utils import run_bass_kernel_spmd

F32 = mybir.dt.float32
BF16 = mybir.dt.bfloat16
I32 = mybir.dt.int32
ALU = mybir.AluOpType
AF = mybir.ActivationFunctionType
AX = mybir.AxisListType

D = 2048
NOWN = 1024
NSEQ = 2304
EPS = 1e-6


class Prog:
    ENG = ("pe", "act", "dve", "pool", "sp")

    def __init__(self, nc, stack):
        self.nc = nc
        self.stack = stack
        self.ops = []
        self.keys = {}
        self.groups = {}
        self.psum_names = set()
        self.gopen = {}

    @staticmethod
    def _norm(k):
        return k if isinstance(k, tuple) else (k,)

    def _related(self, key):
        d = self.keys.setdefault(key[0], {})
        for k2 in list(d.keys()):
            n = min(len(k2), len(key))
            if k2[:n] == key[:n]:
                yield k2, d[k2]

    def add(self, eng, fn, reads=(), writes=(), group=None):
        op = dict(id=len(self.ops), eng=eng, fn=fn, deps=set(), group=group, used=False)
        reads = [self._norm(k) for k in reads] + [("__phase",)]
        writes = [self._norm(k) for k in writes]
        pk = [(k[0],) for k in reads + writes if k[0] in self.psum_names]
        reads = [k for k in reads if k[0] not in self.psum_names]
        writes = [k for k in writes if k[0] not in self.psum_names] + sorted(set(pk))
        for key in reads:
            for k2, st in self._related(key):
                if st[0] is not None:
                    op["deps"].add(st[0])
        for key in writes:
            for k2, st in self._related(key):
                if st[0] is not None:
                    op["deps"].add(st[0])
                op["deps"].update(st[1])
        for key in reads:
            d = self.keys.setdefault(key[0], {})
            st = d.setdefault(key, [None, []])
            st[1].append(op["id"])
        for key in writes:
            d = self.keys.setdefault(key[0], {})
            for k2 in list(d.keys()):
                if len(k2) > len(key) and k2[:len(key)] == key:
                    del d[k2]
            d[key] = [op["id"], []]
        op["deps"].discard(op["id"])
        for d in op["deps"]:
            gg = self.ops[d]["group"]
            if gg is not None and d in self.gopen.get(gg, ()):
                self.gopen[gg] = []
        if group is not None:
            self.gopen.setdefault(group, []).append(op["id"])
            op["batch"] = self.gopen[group]
        self.ops.append(op)
        return op

    def barrier(self):
        scr = self._bar_scr
        self.add("dve", lambda e: e.memset(scr[:, 0:1], 0.0), writes=[("__phase",), "barscr"])

    def emit(self):
        nc = self.nc
        ops = self.ops
        for op in ops:
            for d in op["deps"]:
                ops[d]["used"] = True
        sems = {}
        for e in self.ENG:
            sems[e] = self.stack.enter_context(nc.semaphore("s_" + e))
        gsem = {}
        cnt = {e: 0 for e in self.ENG}
        gcnt = {}
        for op in ops:
            if op["group"] is not None:
                g = op["group"]
                if g not in gsem:
                    gsem[g] = self.stack.enter_context(nc.semaphore("g_" + str(g)))
                    gcnt[g] = 0
                gcnt[g] += 16
                op["sig"] = (gsem[g], gcnt[g], 16)
            elif op["used"]:
                cnt[op["eng"]] += 1
                op["sig"] = (sems[op["eng"]], cnt[op["eng"]], 1)
            else:
                op["sig"] = None
        per = {e: [o for o in ops if o["eng"] == e] for e in self.ENG}

        def replay(ename, eng):
            waited = {}
            for op in per[ename]:
                need = {}
                for d in op["deps"]:
                    dop = ops[d]
                    if dop["eng"] == "pe" and ename == "pe" and dop["group"] is None:
                        continue
                    s = dop["sig"]
                    assert s is not None
                    if dop["group"] is not None:
                        s = ops[dop["batch"][-1]]["sig"]
                    key = id(s[0])
                    if key not in need or need[key][1] < s[1]:
                        need[key] = (s[0], s[1])
                for key, (sem, val) in need.items():
                    if waited.get(key, 0) >= val:
                        continue
                    waited[key] = val
                    eng.wait_ge(sem, val)
                if op["fn"] is None:
                    continue
                ins = op["fn"](eng)
                if op["sig"] is not None:
                    ins.then_inc(op["sig"][0], op["sig"][2])

        block = self.stack.enter_context(nc.Block())

        @block.tensor
        def _(eng):
            replay("pe", eng)

        @block.scalar
        def _(eng):
            replay("act", eng)

        @block.vector
        def _(eng):
            replay("dve", eng)

        @block.gpsimd
        def _(eng):
            replay("pool", eng)

        @block.sync
        def _(eng):
            replay("sp", eng)


def build_nc(debug=False, stop=99):
    nc = bass.Bass("TRN2", target_bir_lowering=False)
    dbg = {}

    def din(name, shape, dt=F32):
        return nc.dram_tensor(name, list(shape), dt, kind="ExternalInput").ap()

    xs = din("xs", [2048, D])
    ctxs = din("ctxs", [256, D])
    vecsA = din("vecsA", [96, 128])
    vecsB = din("vecsB", [40, 128])
    b_ada = din("b_ada", [96, 128])
    w_ada = din("w_ada", [D, 6 * D])
    w_in = din("w_in", [D, 4096])
    lamre_p = din("lamre_p", [64, 128])
    lamim_p = din("lamim_p", [64, 128])
    logdt_p = din("logdt_p", [64, 2])
    ssm_b_re = din("ssm_b_re", [2, 64, 64, 16])
    ssm_b_im = din("ssm_b_im", [2, 64, 64, 16])
    ssm_c_re = din("ssm_c_re", [2, 64, 16, 64])
    ssm_c_im = din("ssm_c_im", [2, 64, 16, 64])
    w_glu = din("w_glu", [1024, 2048])
    w_out = din("w_out", [D, D])
    router_w = din("router_w", [D, 64])
    rbias_b = din("rbias_b", [128, 64])
    ew_gate = din("ew_gate", [64, D, 512])
    ew_up = din("ew_up", [64, D, 512])
    ew_down = din("ew_down", [64, 512, D])
    sw_gate = din("sw_gate", [D, 512])
    sw_up = din("sw_up", [D, 512])
    sw_down = din("sw_down", [512, D])
    out = nc.dram_tensor("out", [NOWN, D], F32, kind="ExternalOutput").ap()
    skind = "ExternalOutput" if debug else "Internal"
    scrW1 = nc.dram_tensor("scrW1", [2, 32, 2, 128, 128], BF16, kind=skind).ap()
    scrT = nc.dram_tensor("scrT", [2, 64, 128, 128], BF16, kind=skind).ap()
    scrW2 = nc.dram_tensor("scrW2", [2, 32, 2, 128, 128], BF16, kind=skind).ap()
    scrX1 = nc.dram_tensor("scrX1", [NOWN, D], F32, kind=skind).ap()
    scrU = nc.dram_tensor("scrU", [8, 128, NSEQ - NOWN], BF16, kind=skind).ap()

    def dout(name, shape, dt=F32):
        t = nc.dram_tensor(name, list(shape), dt, kind="ExternalOutput").ap()
        dbg[name] = t
        return t

    with ExitStack() as top:
        P = Prog(nc, top)

        def sb(name, shape, dt=F32, stack=top):
            return stack.enter_context(nc.sbuf_tensor(name, list(shape), dt))

        def ps(name, shape, dt=F32, stack=top):
            P.psum_names.add(name)
            esz = 4 if dt == F32 else 2
            full = stack.enter_context(nc.psum_tensor(name, [128, 2048 // esz], dt))
            n = int(np.prod(shape[1:]))
            v = full[:, 0:n]
            if len(shape) == 3:
                v = v.rearrange("p (a b) -> p a b", b=shape[2])
            return v

        P._bar_scr = sb("barscr", [128, 4])

        ident_f = sb("ident_f", [128, 128])
        ident_b = sb("ident_b", [128, 128], BF16)
        iot = sb("iot", [128, 128], I32)
        iotf = sb("iotf", [128, 128])
        P.add("pool", lambda e: e.iota(iot[:], [[1, 128]], base=0, channel_multiplier=-1), writes=["iot"])
        P.add("dve", lambda e: e.tensor_copy(iotf[:], iot[:]), reads=["iot"], writes=["iotf"])
        P.add("dve", lambda e: e.tensor_single_scalar(ident_f[:], iotf[:], 0.0, ALU.is_equal),
              reads=["iotf"], writes=["ident_f"])
        P.add("dve", lambda e: e.tensor_copy(ident_b[:], ident_f[:]), reads=["ident_f"], writes=["ident_b"])

        if stop < 1:
            d_i = dout('d_ident', [128, 128])
            P.add('sp', lambda e: e.dma_start(out=d_i, in_=ident_f[:]), reads=['ident_f'], writes=['d_ident'], group='dbgi')
            P.add('sp', None, reads=list(dbg.keys()))
            P.emit()
            return nc, dbg
        ones_b = sb("ones_b", [128, 128], BF16)
        ones_f = sb("ones_f", [128, 128], F32)
        P.add("dve", lambda e: e.memset(ones_b[:], 1.0), writes=["ones_b"])
        P.add("dve", lambda e: e.memset(ones_f[:], 1.0), writes=["ones_f"])
        ss2 = sb("ss2", [128, 8, 4], F32)
        vA = sb("vA", [96, 128])
        vB = sb("vB", [40, 128])
        vC = sb("vC", [96, 128])
        colA = sb("colA", [128, 96])
        colB = sb("colB", [128, 40])
        badaT = sb("badaT", [128, 96])
        P.add("sp", lambda e: e.dma_start(out=vA[:], in_=vecsA), writes=["vA"], group="vA")
        P.add("sp", lambda e: e.dma_start(out=vB[:], in_=vecsB), writes=["vB"], group="vB")
        P.add("sp", lambda e: e.dma_start(out=vC[:], in_=b_ada), writes=["vC"], group="vC")
        with ExitStack() as ph:
            pt = ps("pt_small", [128, 3, 128], F32, ph)
            P.add("pe", lambda e: e.transpose(out=pt[:, 0, 0:96], in_=vA[:], identity=ident_f[0:96, 0:96]),
                  reads=["vA", "ident_f"], writes=["pt_small"])
            P.add("pe", lambda e: e.transpose(out=pt[:, 1, 0:40], in_=vB[:], identity=ident_f[0:40, 0:40]),
                  reads=["vB", "ident_f"], writes=["pt_small"])
            P.add("pe", lambda e: e.transpose(out=pt[:, 2, 0:96], in_=vC[:], identity=ident_f[0:96, 0:96]),
                  reads=["vC", "ident_f"], writes=["pt_small"])
            P.add("dve", lambda e: e.tensor_copy(colA[:], pt[:, 0, 0:96]), reads=["pt_small"], writes=["colA"])
            P.add("dve", lambda e: e.tensor_copy(colB[:], pt[:, 1, 0:40]), reads=["pt_small"], writes=["colB"])
            P.add("dve", lambda e: e.tensor_copy(badaT[:], pt[:, 2, 0:96]), reads=["pt_small"], writes=["badaT"])
            P.barrier()

        if stop < 2:
            d_c = dout('d_colA', [128, 96])
            P.add('sp', lambda e: e.dma_start(out=d_c, in_=colA[:]), reads=['colA'], writes=['d_colA'], group='dbgc')
            P.add('sp', None, reads=list(dbg.keys()))
            P.emit()
            return nc, dbg
        sc = sb("sc", [128, 16, 2])
        for j in range(2):
            P.add("act", lambda e, j=j: e.activation(out=sc[:, :, j], in_=colA[:, 16 * j:16 * j + 16], func=AF.Silu),
                  reads=["colA"], writes=[("sc", j)])
        modT = sb("modT", [128, 96, 2])
        scale1 = sb("scale1", [128, 16, 2])
        mixer = top.enter_context(ExitStack())
        uTown = sb("uTown", [128, 8, NOWN], BF16, mixer)
        convT = sb("convT", [128, 8, NOWN], BF16, mixer)
        ss = sb("ss", [128, 24], F32, mixer)
        Acplx = sb("Acplx", [128, 2, 64], F32, mixer)
        ada_stack = ExitStack()
        REC_A = []
        _real_add = P.add
        P.add = lambda *a, **k: REC_A.append((a, k))
        if True:
            ph = ada_stack
            scb = sb("scb", [128, 16, 4], BF16, ph)
            sch = sb("sch", [128, 16, 2], F32, ph)
            P.add("dve", lambda e: e.tensor_copy(scb[:, :, 0:2], sc[:]), reads=["sc"], writes=["scb"])
            P.add("dve", lambda e: e.tensor_copy(sch[:], scb[:, :, 0:2]), reads=["scb"], writes=["sch"])
            P.add("dve", lambda e: e.tensor_tensor(out=sch[:], in0=sc[:], in1=sch[:], op=ALU.subtract), reads=["sc", "sch"], writes=["sch"])
            P.add("dve", lambda e: e.tensor_copy(scb[:, :, 2:4], sch[:]), reads=["sch", "scb"], writes=["scb"])
            pm = ps("pmod", [128, 96, 4], F32, ph)
            wbuf = [sb("wada%d" % i, [128, 2048], BF16, ph) for i in range(4)]
            n = 0
            for kt in range(16):
                for cc in range(6):
                    b = n % 4
                    n += 1
                    for hc in range(2):
                        P.add("pool", lambda e, b=b, kt=kt, cc=cc, hc=hc: e.dma_start(
                            out=wbuf[b][:, hc * 1024:(hc + 1) * 1024],
                            in_=w_ada[kt * 128:(kt + 1) * 128, cc * 2048 + hc * 1024:cc * 2048 + (hc + 1) * 1024]),
                            writes=[("wada", b, hc)], group="wada%d" % b)

                    def mm(e, b=b, kt=kt, cc=cc):
                        ins = None
                        for t in range(16):
                            ins = e.matmul(pm[:, cc * 16 + t, :], lhsT=wbuf[b][:, t * 128:(t + 1) * 128],
                                           rhs=scb[:, kt, :], start=(kt == 0 and cc == 0 and t == 0), stop=(kt == 15),
                                           skip_group_check=True)
                        return ins
                    P.add("pe", mm, reads=[("wada", b), "scb"], writes=["pmod"])
            P.add("dve", lambda e: e.tensor_tensor(out=modT[:], in0=pm[:, :, 0:2], in1=badaT[:].unsqueeze(2).to_broadcast([128, 96, 2]),
                                                  op=ALU.add), reads=["pmod", "badaT"], writes=["modT"])
            P.add("dve", lambda e: e.tensor_tensor(out=modT[:], in0=modT[:], in1=pm[:, :, 2:4], op=ALU.add),
                  reads=["pmod", "modT"], writes=["modT"])
        P.add = _real_add
        import math
        PI = math.pi
        with ExitStack() as ph:
            REC_S = []
            P.add = lambda *a, **k: REC_S.append((a, k))
            raw = sb("s0raw", [64, 3, 128], F32, ph)
            ldt = sb("s0ldt", [64, 2], F32, ph)
            P.add("sp", lambda e: e.dma_start(out=raw[:, 0, :], in_=lamre_p), writes=[("s0raw", 0)], group="s0raw0")
            P.add("sp", lambda e: e.dma_start(out=raw[:, 1, :], in_=lamim_p), writes=[("s0raw", 1)], group="s0raw1")
            P.add("sp", lambda e: e.dma_start(out=ldt[:], in_=logdt_p), writes=["s0ldt"], group="s0ldt")
            P.add("act", lambda e: e.activation(out=ldt[:], in_=ldt[:], func=AF.Exp), reads=["s0ldt"], writes=["s0ldt"])
            P.add("dve", lambda e: e.tensor_copy(raw[:, 2, :].rearrange("q (a b) -> q a b", a=2),
                                                 ldt[:].unsqueeze(2).to_broadcast([64, 2, 64])),
                  reads=["s0ldt"], writes=[("s0raw", 2)])
            LRI = sb("LRI", [128, 3, 64], F32, ph)
            ptq = ps("s0pt", [128, 4, 128], F32, ph)
            for i in range(3):
                P.add("pe", lambda e, i=i: e.transpose(out=ptq[:, i, 0:64], in_=raw[:, i, :], identity=ident_f[0:64, 0:64]),
                      reads=[("s0raw", i), "ident_f"], writes=["s0pt"])
            P.add("dve", lambda e: e.tensor_copy(LRI[:], ptq[:, 0:3, 0:64]), reads=["s0pt"], writes=["LRI"])
            ath = sb("ath", [128, 2, 64], F32, ph)
            P.add("dve", lambda e: e.tensor_tensor(out=ath[:], in0=LRI[:, 0:2, :],
                                                   in1=LRI[:, 2:3, :].to_broadcast([128, 2, 64]), op=ALU.mult),
                  reads=["LRI"], writes=["ath"])
            io8 = sb("io8", [128, 8], I32, ph)
            io8f = sb("io8f", [128, 8], F32, ph)
            KM = sb("KM", [128, 3, 2, 8], F32, ph)
            P.add("pool", lambda e: e.iota(io8[:], [[1, 8]], base=0, channel_multiplier=0), writes=["io8"])
            P.add("dve", lambda e: e.tensor_copy(io8f[:], io8[:]), reads=["io8"], writes=["io8f"])
            kmab = {(0, 0): (-1.0, 7.0), (0, 1): (1.0, 0.0), (1, 0): (-1.0, -1.0), (1, 1): (1.0, -8.0),
                    (2, 0): (1.0, 1.0), (2, 1): (-1.0, 8.0)}
            for (u_, d_), (ka, kb) in kmab.items():
                P.add("dve", lambda e, u_=u_, d_=d_, ka=ka, kb=kb: e.tensor_scalar(
                    out=KM[:, u_, d_, :], in0=io8f[:], scalar1=ka, scalar2=kb, op0=ALU.mult, op1=ALU.add),
                    reads=["io8f"], writes=[("KM", u_, d_)])

            et_ang = sb("et_ang", [128, 2, 32, 8], F32, ph)
            et_ex = sb("et_ex", [128, 2, 32, 8], F32, ph)
            et_tmp = sb("et_tmp", [128, 2, 32, 8], F32, ph)
            et_ti = sb("et_ti", [128, 2, 32, 8], I32, ph)
            et_tf = sb("et_tf", [128, 2, 32, 8], F32, ph)

            def etab(name, mult_ap, L, dst_re, dst_im, rkeys, wkeys):
                shp = [128, 2, 32, L]
                name = "et"
                ang = et_ang[:, :, :, 0:L]
                ex = et_ex[:, :, :, 0:L]
                tmp = et_tmp[:, :, :, 0:L]
                ti = et_ti[:, :, :, 0:L]
                tf = et_tf[:, :, :, 0:L]
                a_b = ath[:, 0, :].rearrange("p (d g) -> p d g", d=2).unsqueeze(3).to_broadcast(shp)
                t_b = ath[:, 1, :].rearrange("p (d g) -> p d g", d=2).unsqueeze(3).to_broadcast(shp)
                P.add("dve", lambda e: e.tensor_tensor(out=ex[:], in0=a_b, in1=mult_ap, op=ALU.mult),
                      reads=["ath"] + rkeys, writes=[name + "_ex"])
                P.add("act", lambda e: e.activation(out=ex[:], in_=ex[:], func=AF.Exp), reads=[name + "_ex"], writes=[name + "_ex"])
                P.add("dve", lambda e: e.tensor_tensor(out=ang[:], in0=t_b, in1=mult_ap, op=ALU.mult),
                      reads=["ath"] + rkeys, writes=[name + "_ang"])
                for (dst, shift) in ((dst_im, 32.0), (dst_re, 32.25)):
                    P.add("dve", lambda e, shift=shift: e.tensor_scalar(out=tmp[:], in0=ang[:], scalar1=1.0 / (2.0 * PI), scalar2=shift,
                                                                        op0=ALU.mult, op1=ALU.add),
                          reads=[name + "_ang"], writes=[name + "_tmp"])
                    P.add("dve", lambda e: e.tensor_copy(ti[:], tmp[:]), reads=[name + "_tmp"], writes=[name + "_ti"])
                    P.add("dve", lambda e: e.tensor_copy(tf[:], ti[:]), reads=[name + "_ti"], writes=[name + "_tf"])
                    P.add("dve", lambda e: e.tensor_tensor(out=tmp[:], in0=tmp[:], in1=tf[:], op=ALU.subtract),
                          reads=[name + "_tmp", name + "_tf"], writes=[name + "_tmp"])
                    P.add("dve", lambda e: e.tensor_single_scalar(tf[:], tmp[:], 0.5, ALU.is_gt),
                          reads=[name + "_tmp"], writes=[name + "_tf"])
                    P.add("dve", lambda e: e.tensor_tensor(out=tmp[:], in0=tmp[:], in1=tf[:], op=ALU.subtract),
                          reads=[name + "_tmp", name + "_tf"], writes=[name + "_tmp"])
                    P.add("act", lambda e: e.activation(out=tmp[:], in_=tmp[:], func=AF.Sin, scale=2.0 * PI), reads=[name + "_tmp"],
                          writes=[name + "_tmp"])
                    P.add("dve", lambda e, dst=dst: e.tensor_tensor(out=dst, in0=tmp[:], in1=ex[:], op=ALU.mult),
                          reads=[name + "_tmp", name + "_ex"], writes=wkeys)

            one1 = sb("one1", [128, 1], F32, ph)
            P.add("dve", lambda e: e.memset(one1[:], 1.0), writes=["one1"])
            E1 = sb("E1", [128, 2, 2, 32, 1], F32, ph)
            etab("e1", one1[:].unsqueeze(2).unsqueeze(3).to_broadcast([128, 2, 32, 1]), 1, E1[:, 0], E1[:, 1], ["one1"], ["E1"])
            eight = sb("eight", [128, 1], F32, ph)
            P.add("dve", lambda e: e.memset(eight[:], 8.0), writes=["eight"])
            etab("e8", eight[:].unsqueeze(2).unsqueeze(3).to_broadcast([128, 2, 32, 1]), 1,
                 Acplx[:, 0, :].rearrange("p (d g o) -> p d g o", d=2, o=1),
                 Acplx[:, 1, :].rearrange("p (d g o) -> p d g o", d=2, o=1), ["eight"], ["Acplx"])
            ET = [sb("ET%d" % u_, [128, 2, 2, 32, 8], F32, ph) for u_ in range(3)]
            for u_ in range(3):
                etab("et%d" % u_, KM[:, u_, :, :].unsqueeze(2).to_broadcast([128, 2, 32, 8]), 8,
                     ET[u_][:, 0], ET[u_][:, 1], ["KM"], ["ET%d" % u_])

            LR = LRI[:, 0, :]
            LI = LRI[:, 1, :]
            e1r = E1[:, 0].rearrange("p d g o -> p (d g o)")
            e1i = E1[:, 1].rearrange("p d g o -> p (d g o)")
            cf = sb("cf", [128, 6, 64], F32, ph)
            seq_ops = [
                lambda e: e.tensor_scalar(out=cf[:, 0, :], in0=e1r, scalar1=-1.0, scalar2=None, op0=ALU.add),
                lambda e: e.tensor_tensor(out=cf[:, 1, :], in0=LR, in1=LR, op=ALU.mult),
                lambda e: e.tensor_tensor(out=cf[:, 2, :], in0=LI, in1=LI, op=ALU.mult),
                lambda e: e.tensor_tensor(out=cf[:, 1, :], in0=cf[:, 1, :], in1=cf[:, 2, :], op=ALU.add),
                lambda e: e.reciprocal(out=cf[:, 1, :], in_=cf[:, 1, :]),
                lambda e: e.tensor_tensor(out=cf[:, 2, :], in0=cf[:, 0, :], in1=LR, op=ALU.mult),
                lambda e: e.tensor_tensor(out=cf[:, 3, :], in0=e1i, in1=LI, op=ALU.mult),
                lambda e: e.tensor_tensor(out=cf[:, 2, :], in0=cf[:, 2, :], in1=cf[:, 3, :], op=ALU.add),
                lambda e: e.tensor_tensor(out=cf[:, 4, :], in0=cf[:, 2, :], in1=cf[:, 1, :], op=ALU.mult),
                lambda e: e.tensor_tensor(out=cf[:, 2, :], in0=e1i, in1=LR, op=ALU.mult),
                lambda e: e.tensor_tensor(out=cf[:, 3, :], in0=cf[:, 0, :], in1=LI, op=ALU.mult),
                lambda e: e.tensor_tensor(out=cf[:, 2, :], in0=cf[:, 2, :], in1=cf[:, 3, :], op=ALU.subtract),
                lambda e: e.tensor_tensor(out=cf[:, 5, :], in0=cf[:, 2, :], in1=cf[:, 1, :], op=ALU.mult),
            ]
            for f_ in seq_ops:
                P.add("dve", f_, reads=["E1", "LRI", "cf"], writes=["cf"])
            Braw = sb("Braw", [128, 2, 64, 16], F32, ph)
            for i, src_ in enumerate((ssm_b_re, ssm_b_im)):
                for d_ in range(2):
                    v = src_[d_].rearrange("(g2 gp) p m -> gp p g2 m", gp=2)
                    for gp in range(2):
                        P.add("sp", lambda e, i=i, d_=d_, gp=gp, v=v: e.dma_start(
                            out=Braw[gp * 64:(gp + 1) * 64, i, d_ * 32:(d_ + 1) * 32, :], in_=v[gp]),
                            writes=[("Braw", i, d_, gp)], group="Braw")
            bbar = sb("bbar", [128, 2, 64, 16], F32, ph)
            tA = sb("s0tA", [128, 64, 16], F32, ph)
            cre_b = cf[:, 4, :].unsqueeze(2).to_broadcast([128, 64, 16])
            cim_b = cf[:, 5, :].unsqueeze(2).to_broadcast([128, 64, 16])
            P.add("dve", lambda e: e.tensor_tensor(out=bbar[:, 0], in0=Braw[:, 0], in1=cre_b, op=ALU.mult), reads=["Braw", "cf"], writes=[("bbar", 0)])
            P.add("dve", lambda e: e.tensor_tensor(out=tA[:], in0=Braw[:, 1], in1=cim_b, op=ALU.mult), reads=["Braw", "cf"], writes=["s0tA"])
            P.add("dve", lambda e: e.tensor_tensor(out=bbar[:, 0], in0=bbar[:, 0], in1=tA[:], op=ALU.subtract), reads=["s0tA", ("bbar", 0)], writes=[("bbar", 0)])
            P.add("dve", lambda e: e.tensor_tensor(out=bbar[:, 1], in0=Braw[:, 1], in1=cre_b, op=ALU.mult), reads=["Braw", "cf"], writes=[("bbar", 1)])
            P.add("dve", lambda e: e.tensor_tensor(out=tA[:], in0=Braw[:, 0], in1=cim_b, op=ALU.mult), reads=["Braw", "cf", ("bbar", 0)], writes=["s0tA"])
            P.add("dve", lambda e: e.tensor_tensor(out=bbar[:, 1], in0=bbar[:, 1], in1=tA[:], op=ALU.add), reads=["s0tA", ("bbar", 1)], writes=[("bbar", 1)])

            CT = sb("CT", [128, 2, 64, 16], F32, ph)
            craw = [sb("craw%d" % i, [128, 128], F32, ph) for i in range(2)]
            nb = 0
            for i, src_ in enumerate((ssm_c_re, ssm_c_im)):
                for d_ in range(2):
                    for q in range(4):
                        b_ = nb % 2
                        nb += 1
                        for g2l in range(8):
                            g2 = q * 8 + g2l
                            P.add("sp", lambda e, b_=b_, g2l=g2l, g2=g2, d_=d_, src_=src_: e.dma_start(
                                out=craw[b_][g2l * 16:(g2l + 1) * 16, :].rearrange("n (gp p) -> n gp p", gp=2),
                                in_=src_[d_, 2 * g2:2 * g2 + 2, :, :].rearrange("gp n p -> n gp p")),
                                writes=[("craw", b_, g2l)], group="craw%d" % b_)
                        P.add("pe", lambda e, b_=b_: e.transpose(out=ptq[:, 3, :], in_=craw[b_][:], identity=ident_f[:]),
                              reads=[("craw", b_), "ident_f"], writes=["s0pt"])
                        P.add("dve", lambda e, i=i, d_=d_, q=q: e.tensor_copy(
                            CT[:, i, d_ * 32 + q * 8:d_ * 32 + (q + 1) * 8, :], ptq[:, 3, :].rearrange("p (a n) -> p a n", n=16)),
                            reads=["s0pt"], writes=[("CT", i, d_, q)])

            mk = sb("mk", [128, 2, 128], F32, ph)
            mi = sb("mki", [128, 2, 128], I32, ph)
            mf = sb("mkf", [128, 2, 128], F32, ph)
            P.add("pool", lambda e: e.iota(mi[:, 0, :], [[1, 128]], base=0, channel_multiplier=0), writes=[("mki", 0)])
            P.add("pool", lambda e: e.iota(mi[:, 1, :], [[0, 128]], base=0, channel_multiplier=1), writes=[("mki", 1)])
            P.add("dve", lambda e: e.tensor_single_scalar(mi[:], mi[:], 4, ALU.arith_shift_right), reads=["mki"], writes=["mki"])
            P.add("dve", lambda e: e.tensor_copy(mf[:], mi[:]), reads=["mki"], writes=["mkf"])
            P.add("dve", lambda e: e.tensor_tensor(out=mk[:, 0, :], in0=mf[:, 0, :], in1=mf[:, 1, :], op=ALU.is_ge), reads=["mkf"], writes=[("mk", 0)])
            P.add("dve", lambda e: e.tensor_tensor(out=mk[:, 1, :], in0=mf[:, 1, :], in1=mf[:, 0, :], op=ALU.is_ge), reads=["mkf"], writes=[("mk", 1)])


            oR = sb("oR", [128, 32, 8, 16], F32, ph)
            oI = sb("oI", [128, 32, 8, 16], F32, ph)
            o2R = sb("o2R", [128, 32, 8, 16], F32, ph)
            o2I = sb("o2I", [128, 32, 8, 16], F32, ph)
            t1 = sb("s0t1", [128, 32, 8, 16], F32, ph)
            stg = sb("s0stg", [128, 4, 128], BF16, ph)
            stg2 = [sb("s0stg2", [128, 32, 128], BF16, ph)] * 2
            pT = ps("s0pT", [128, 4, 128], F32, ph)
            pT2 = ps("s0pT2", [128, 4, 128], F32, ph)

            def couter(u_, d_, Br, Bi, dR, dI, neg_im, rk, tag):
                shp = [128, 32, 8, 16]
                Er = ET[u_][:, 0, d_].unsqueeze(3).to_broadcast(shp)
                Ei = ET[u_][:, 1, d_].unsqueeze(3).to_broadcast(shp)
                Brb = Br.unsqueeze(2).to_broadcast(shp)
                Bib = Bi.unsqueeze(2).to_broadcast(shp)
                rk = rk + ["ET%d" % u_]
                P.add("dve", lambda e: e.tensor_tensor(out=dR[:], in0=Er, in1=Brb, op=ALU.mult), reads=rk, writes=[tag + "R"])
                P.add("dve", lambda e: e.tensor_tensor(out=t1[:], in0=Ei, in1=Bib, op=ALU.mult), reads=rk, writes=["s0t1"])
                P.add("dve", lambda e: e.tensor_tensor(out=dR[:], in0=dR[:], in1=t1[:], op=ALU.subtract), reads=[tag + "R", "s0t1"], writes=[tag + "R"])
                P.add("dve", lambda e: e.tensor_tensor(out=dI[:], in0=Er, in1=Bib, op=ALU.mult), reads=rk, writes=[tag + "I"])
                P.add("dve", lambda e: e.tensor_tensor(out=t1[:], in0=Ei, in1=Brb, op=ALU.mult), reads=rk + [tag + "R"], writes=["s0t1"])
                if neg_im:
                    P.add("dve", lambda e: e.scalar_tensor_tensor(out=dI[:], in0=dI[:], scalar=-1.0, in1=t1[:], op0=ALU.mult, op1=ALU.subtract),
                          reads=[tag + "I", "s0t1"], writes=[tag + "I"])
                else:
                    P.add("dve", lambda e: e.tensor_tensor(out=dI[:], in0=dI[:], in1=t1[:], op=ALU.add), reads=[tag + "I", "s0t1"], writes=[tag + "I"])

            for d_ in range(2):
                gs = slice(d_ * 32, (d_ + 1) * 32)
                couter(0, d_, bbar[:, 0, gs, :], bbar[:, 1, gs, :], oR, oI, False, ["bbar"], "o")
                for part, src_t, skey in ((0, oR, "oR"), (1, oI, "oI")):
                    for q in range(8):
                        def trw(e, src_t=src_t, q=q):
                            ins = None
                            for j in range(4):
                                ins = e.transpose(out=pT[:, j, :], in_=src_t[:, q * 4 + j].rearrange("p s m -> p (s m)"), identity=ident_f[:])
                            return ins
                        P.add("pe", trw, reads=[skey, "ident_f"], writes=["s0pT"])
                        P.add("act", lambda e: e.activation(out=stg[:], in_=pT[:], func=AF.Copy), reads=["s0pT"], writes=["s0stg"])
                        P.add("sp", lambda e, d_=d_, q=q, part=part: e.dma_start(
                            out=scrW1[d_, q * 4:(q + 1) * 4, part].rearrange("g r c -> r g c"), in_=stg[:]),
                            reads=["s0stg"], writes=["scrW1"], group="s0st")

                couter(1, d_, bbar[:, 0, gs, :], bbar[:, 1, gs, :], oR, oI, False, ["bbar"], "o")
                couter(2, d_, CT[:, 0, gs, :], CT[:, 1, gs, :], o2R, o2I, True, ["CT"], "o2")
                for part, src_t, skey in ((0, o2R, "o2R"), (1, o2I, "o2I")):
                    P.add("act", lambda e, part=part, src_t=src_t: e.activation(
                        out=stg2[part][:], in_=src_t[:].rearrange("p g t n -> p g (t n)"), func=AF.Copy),
                        reads=[skey], writes=["s0stg2"])
                    P.add("sp", lambda e, d_=d_, part=part: e.dma_start(
                        out=scrW2[d_, :, part].rearrange("g r c -> r g c"), in_=stg2[part][:]),
                        reads=["s0stg2"], writes=["scrW2"], group="s0st2")

                for q in range(8):
                    for gp in range(2):
                        pTx = pT if gp == 0 else pT2
                        pkey = "s0pT" if gp == 0 else "s0pT2"
                        rs = slice(gp * 64, (gp + 1) * 64)

                        def mmT(e, q=q, gp=gp, pTx=pTx, rs=rs):
                            ins = None
                            for j in range(4):
                                g2 = q * 4 + j
                                e.matmul(pTx[:, j, :], lhsT=oR[rs, g2].rearrange("p s m -> p (s m)"),
                                         rhs=o2R[rs, g2].rearrange("p t n -> p (t n)"), start=True, stop=False)
                                ins = e.matmul(pTx[:, j, :], lhsT=oI[rs, g2].rearrange("p s m -> p (s m)"),
                                               rhs=o2I[rs, g2].rearrange("p t n -> p (t n)"), start=False, stop=True)
                            return ins
                        P.add("pe", mmT, reads=["oR", "oI", "o2R", "o2I"], writes=[pkey])
                        P.add("dve", lambda e, d_=d_, pTx=pTx: e.tensor_tensor(
                            out=stg[:], in0=pTx[:], in1=mk[:, d_:d_ + 1, :].to_broadcast([128, 4, 128]), op=ALU.mult),
                            reads=[pkey, "mk"], writes=["s0stg"])
                        P.add("sp", lambda e, d_=d_, q=q, gp=gp: e.dma_start(
                            out=scrT[d_, q * 8:(q + 1) * 8].rearrange("(j gp) r c -> gp r j c", gp=2)[gp], in_=stg[:]),
                            reads=["s0stg"], writes=["scrT"], group="s0st")
            P.add = _real_add
            na, ns = len(REC_A), len(REC_S)
            ia = isx = 0
            while ia < na or isx < ns:
                if isx >= ns or (ia < na and ia * ns <= isx * na):
                    a_, k_ = REC_A[ia]; ia += 1
                else:
                    a_, k_ = REC_S[isx]; isx += 1
                P.add(*a_, **k_)
            P.barrier()
        ada_stack.close()
        if debug:
            d_A = dout("d_A", [128, 128])
            P.add("sp", lambda e: e.dma_start(out=d_A, in_=Acplx[:].rearrange("p a b -> p (a b)")), reads=["Acplx"],
                  writes=["d_A"], group="dbgA")

        if debug:
            d_mod = dout("d_mod", [128, 192])
            P.add("sp", lambda e: e.dma_start(out=d_mod, in_=modT[:].rearrange("p a b -> p (a b)")), reads=["modT"],
                  writes=["d_mod"], group="dbg0")

        P.add("dve", lambda e: e.scalar_tensor_tensor(out=scale1[:], in0=modT[:, 16:32, :], scalar=1.0,
                                                      in1=colA[:, 32:48].unsqueeze(2).to_broadcast([128, 16, 2]),
                                                      op0=ALU.add, op1=ALU.mult),
              reads=["modT", "colA"], writes=["scale1"])

        with ExitStack() as ph:
            w_u = sb("w_u", [128, 16, 1024], BF16, ph)
            ustage = [sb("ustage%d" % i, [128, 512], BF16, ph) for i in range(2)]
            w_in_v = w_in.rearrange("(kt p) c -> p kt c", p=128)
            for kt in range(16):
                P.add("pool", lambda e, kt=kt: e.dma_start(out=w_u[:, kt, :], in_=w_in_v[:, kt, 0:1024]),
                      writes=[("w_u", kt)], group="w_u")
            xt = [sb("xt%d" % i, [128, D], F32, ph) for i in range(2)]
            xn = [sb("xn%d" % i, [128, 4, D], BF16, ph) for i in range(2)]
            hxT = [sb("hxT%d" % i, [128, 16, 512], BF16, ph) for i in range(2)]
            ptr = [ps("ptr%d" % i, [128, 512], BF16, ph) for i in range(2)]
            pmm = [ps("pmm%d" % i, [128, 512], F32, ph) for i in range(6)]
            groups = [("x", 1024, 4, 0, 1024, 0), ("x", 1536, 4, 0, 1536, 1), ("c", 0, 2, 1, 2048, 0),
                      ("x", 0, 4, 0, 0, 1), ("x", 512, 4, 0, 512, 0)]
            nxc = [0]

            def stA1(gi):
                (src, r0, nt, mj, soff, xb) = groups[gi]
                xnb = xn[gi % 2]
                for t in range(nt):
                    nx = nxc[0]
                    b = nx % 2
                    tix = nx % 24
                    nxc[0] += 1
                    srcap = (xs if src == "x" else ctxs)[r0 + t * 128:r0 + (t + 1) * 128, :]
                    P.add("sp", lambda e, b=b, srcap=srcap: e.dma_start(out=xt[b][:], in_=srcap),
                          writes=[("xt", b)], group="xt%d" % b)
                    P.add("act", lambda e, b=b, tix=tix, t=t: e.activation(out=xnb[:, t, :], in_=xt[b][:], func=AF.Square,
                                                                      accum_out=ss[:, tix:tix + 1]),
                          reads=[("xt", b)], writes=[("xn", gi % 2, t), ("ss", tix)])
                    P.add("dve", lambda e, tix=tix: e.tensor_scalar(out=ss[:, tix:tix + 1], in0=ss[:, tix:tix + 1],
                                                                    scalar1=1.0 / D, scalar2=EPS, op0=ALU.mult, op1=ALU.add),
                          reads=[("ss", tix)], writes=[("ss", tix)])
                    P.add("act", lambda e, tix=tix: e.activation(out=ss[:, tix:tix + 1], in_=ss[:, tix:tix + 1], func=AF.Sqrt),
                          reads=[("ss", tix)], writes=[("ss", tix)])
                    P.add("dve", lambda e, tix=tix: e.reciprocal(out=ss[:, tix:tix + 1], in_=ss[:, tix:tix + 1]),
                          reads=[("ss", tix)], writes=[("ss", tix)])
                    P.add("act", lambda e, b=b, tix=tix, t=t: e.activation(
                        out=xnb[:, t, :], in_=xt[b][:], func=AF.Copy, scale=ss[:, tix:tix + 1]),
                        reads=[("xt", b), ("ss", tix)], writes=[("xn", gi % 2, t)])

            def stA2(gi):
                (src, r0, nt, mj, soff, xb) = groups[gi]
                xnb = xn[gi % 2]
                ntok = nt * 128
                for ft in range(16):
                    pb = ft % 2

                    def tr(e, ft=ft, nt=nt, pb=pb):
                        ins = None
                        for t in range(nt):
                            ins = e.transpose(out=ptr[pb][:, t * 128:(t + 1) * 128],
                                              in_=xnb[:, t, ft * 128:(ft + 1) * 128], identity=ident_b[:])
                        return ins
                    P.add("pe", tr, reads=[("xn", gi % 2), "ident_b"], writes=["ptr%d" % pb])
                    if ft % 2 == 0:
                        P.add("dve", lambda e, xb=xb, ft=ft, pb=pb, ntok=ntok, mj=mj: e.tensor_scalar(
                            out=hxT[xb][:, ft, 0:ntok], in0=ptr[pb][:, 0:ntok], scalar1=scale1[:, ft, mj:mj + 1],
                            scalar2=modT[:, ft, mj:mj + 1], op0=ALU.mult, op1=ALU.add),
                            reads=["ptr%d" % pb, "scale1", "modT"], writes=[("hxT", xb, ft)])
                    else:
                        P.add("act", lambda e, xb=xb, ft=ft, pb=pb, ntok=ntok, mj=mj: e.activation(
                            out=hxT[xb][:, ft, 0:ntok], in_=ptr[pb][:, 0:ntok], func=AF.Identity,
                            scale=scale1[:, ft, mj:mj + 1], bias=modT[:, ft, mj:mj + 1]),
                            reads=["ptr%d" % pb, "scale1", "modT"], writes=[("hxT", xb, ft)])

            def stB(gi):
                (src, r0, nt, mj, soff, xb) = groups[gi]
                ntok = nt * 128
                for ct in range(8):
                    pq = ct % 4

                    def mmu(e, xb=xb, ct=ct, ntok=ntok, pq=pq):
                        ins = None
                        for kt in range(16):
                            ins = e.matmul(pmm[pq][:, 0:ntok], lhsT=w_u[:, kt, ct * 128:(ct + 1) * 128],
                                           rhs=hxT[xb][:, kt, 0:ntok], start=(kt == 0), stop=(kt == 15))
                        return ins
                    P.add("pe", mmu, reads=["w_u", ("hxT", xb)], writes=["pmm%d" % pq])
                    nj = ntok // 8
                    if soff < NOWN:
                        dst = uTown[:, ct, :].rearrange("p (s j) -> p s j", s=8)[:, :, soff // 8:soff // 8 + nj]
                        wk = [("uT", ct, soff)]
                    else:
                        sg = ct % 2
                        dst = ustage[sg][:, 0:ntok].rearrange("p (s j) -> p s j", s=8)
                        wk = [("ustage", sg)]
                    srcv = pmm[pq][:, 0:ntok].rearrange("p (j s) -> p s j", s=8)
                    if ct % 2 == 0:
                        P.add("dve", lambda e, dst=dst, srcv=srcv: e.tensor_copy(dst, srcv),
                              reads=["pmm%d" % pq], writes=wk)
                    else:
                        P.add("act", lambda e, dst=dst, srcv=srcv: e.activation(out=dst, in_=srcv, func=AF.Copy),
                              reads=["pmm%d" % pq], writes=wk)
                    if soff >= NOWN:
                        j0r = (soff - NOWN) // 8
                        P.add("sp", lambda e, ct=ct, sg=sg, ntok=ntok, j0r=j0r, nj=nj: e.dma_start(
                            out=scrU[ct].rearrange("p (s j) -> p s j", s=8)[:, :, j0r:j0r + nj],
                            in_=ustage[sg][:, 0:ntok].rearrange("p (s j) -> p s j", s=8)),
                            reads=[("ustage", sg)], writes=["scrU"], group="ustage%d" % sg)

            stA1(0)
            stA2(0)
            for gi in range(5):
                if gi + 1 < 5:
                    stA1(gi + 1)
                stB(gi)
                if gi + 1 < 5:
                    stA2(gi + 1)
            cvs = ph.enter_context(ExitStack())
            wch = [[sb("wch%d_%d" % (s_, i), [128, 16, 128], BF16, cvs) for i in range(3)] for s_ in range(2)]
            zc = sb("zc", [128, 512], F32, cvs)
            zz = sb("zz", [128, 512], F32, cvs)
            yy = sb("yy", [128, 512], F32, cvs)
            for ct in range(8):
                s_ = ct % 2
                for i in range(3):
                    P.add("pool", lambda e, s_=s_, i=i, ct=ct: e.dma_start(
                        out=wch[s_][i][:], in_=w_in_v[:, :, 1024 * (i + 1) + ct * 128:1024 * (i + 1) + (ct + 1) * 128]),
                        writes=[("wch", s_, i)], group="wch%d_%d" % (s_, i))
                for og, (xb, soff) in enumerate([(1, 0), (0, 512)]):
                    pset = 3 * ((ct * 2 + og) % 2)
                    if True:
                        for i in range(3):
                            def mmb(e, xb=xb, i=i, s_=s_, pset=pset):
                                ins = None
                                for kt in range(16):
                                    ins = e.matmul(pmm[pset + i][:, :], lhsT=wch[s_][i][:, kt, :],
                                                   rhs=hxT[xb][:, kt, :], start=(kt == 0), stop=(kt == 15))
                                return ins
                            P.add("pe", mmb, reads=[("wch", s_, i), ("hxT", xb)], writes=["pmm%d" % (pset + i)])
                        P.add("act", lambda e, pset=pset: e.activation(out=zc[:], in_=pmm[pset + 1][:], func=AF.Copy),
                              reads=["pmm%d" % (pset + 1)], writes=["zc"])
                        P.add("dve", lambda e, pset=pset: e.tensor_tensor(out=zz[:], in0=zc[:], in1=pmm[pset + 2][:], op=ALU.mult),
                              reads=["zc", "pmm%d" % (pset + 2)], writes=["zz"])
                        P.add("dve", lambda e, ct=ct: e.tensor_scalar(
                            out=yy[:], in0=zz[:], scalar1=colB[:, 16 + ct:17 + ct], scalar2=colB[:, 32 + ct:33 + ct],
                            op0=ALU.mult, op1=ALU.add), reads=["zz", "colB"], writes=["yy"])
                        yv = yy[:].rearrange("p (r w) -> p r w", w=64)
                        zv = zz[:].rearrange("p (r w) -> p r w", w=64)
                        P.add("dve", lambda e, ct=ct, yv=yv, zv=zv: e.scalar_tensor_tensor(
                            out=yv[:, :, 1:64], in0=zv[:, :, 0:63], scalar=colB[:, 8 + ct:9 + ct], in1=yv[:, :, 1:64],
                            op0=ALU.mult, op1=ALU.add), reads=["zz", "yy", "colB"], writes=["yy"])
                        P.add("dve", lambda e, ct=ct, yv=yv, zv=zv: e.scalar_tensor_tensor(
                            out=yv[:, :, 0:63], in0=zv[:, :, 1:64], scalar=colB[:, 24 + ct:25 + ct], in1=yv[:, :, 0:63],
                            op0=ALU.mult, op1=ALU.add), reads=["zz", "yy", "colB"], writes=["yy"])
                        P.add("dve", lambda e, ct=ct, soff=soff, pset=pset: e.tensor_tensor(
                            out=convT[:, ct, soff:soff + 512], in0=yy[:], in1=pmm[pset][:], op=ALU.mult),
                            reads=["yy", "pmm%d" % pset], writes=[("convT", ct, soff)])
            P.barrier()
        if debug:
            d_uT = dout("d_uT", [128, 8 * NOWN], BF16)
            d_convT = dout("d_convT", [128, 8 * NOWN], BF16)
            P.add("sp", lambda e: e.dma_start(out=d_uT, in_=uTown[:].rearrange("p a b -> p (a b)")), reads=["uT"],
                  writes=["d_uT"], group="dbg1")
            P.add("sp", lambda e: e.dma_start(out=d_convT, in_=convT[:].rearrange("p a b -> p (a b)")), reads=["convT"],
                  writes=["d_convT"], group="dbg2")

        Z = sb("Z", [128, 8, 8, 128], BF16, mixer)
        P.add("pool", lambda e: e.memset(Z[:], 0.0), writes=["Z"])
        for a_ in range(8):
            for b_ in range(8):
                P.add("dve" if (a_ + b_) % 2 else "pool", lambda e, a_=a_, b_=b_: e.tensor_single_scalar(
                    Z[:, a_, b_, 16 * b_:16 * b_ + 16], iotf[:, 16 * b_:16 * b_ + 16], float(16 * (b_ - a_)), ALU.is_equal),
                    reads=["iotf"], writes=[("Z", a_, b_)])
        mix = mixer.enter_context(ExitStack())
        gT = sb("gT", [128, 8, NOWN], BF16, mix)
        ssm = mix.enter_context(ExitStack())
        U = sb("U", [128, 64, 128], BF16, ssm)
        Pt = sb("Pt", [128, 2, 2, 32, 288], BF16, ssm)
        s12 = ssm.enter_context(ExitStack())
        Ur = sb("Ur", [128, 64, 160], BF16, s12)
        with ExitStack() as ph:
            pU = [ps("pU%d" % i, [128, 288], F32, ph) for i in range(3)]
            ucat = [sb("ucat%d" % i, [128, NSEQ], BF16, ph) for i in range(2)]
            for g in range(64):
                ct, gl = g // 8, g % 8
                pb = g % 3
                if gl == 0:
                    P.add("sp", lambda e, ct=ct: e.dma_start(
                        out=ucat[ct % 2][:].rearrange("p (s j) -> p s j", s=8)[:, :, 128:288],
                        in_=scrU[ct].rearrange("p (s j) -> p s j", s=8)), reads=["scrU"],
                        writes=[("ucat", ct % 2, 1)], group="ucat%d" % (ct % 2))
                    P.add("pool", lambda e, ct=ct: e.tensor_copy(
                        ucat[ct % 2][:].rearrange("p (s j) -> p s j", s=8)[:, :, 0:128],
                        uTown[:, ct, :].rearrange("p (s j) -> p s j", s=8)), reads=["uT"],
                        writes=[("ucat", ct % 2, 0)])

                def shf(e, ct=ct, gl=gl, pb=pb):
                    ins = None
                    for s_ in range(8):
                        src = ucat[ct % 2][:, s_ * 288:(s_ + 1) * 288]
                        ins = e.matmul(pU[pb][:, 0:288], lhsT=Z[:, gl, s_, :], rhs=src, start=(s_ == 0), stop=(s_ == 7))
                    return ins
                P.add("pe", shf, reads=["Z", ("ucat", ct % 2)], writes=["pU%d" % pb])
                P.add("dve", lambda e, g=g, pb=pb: e.tensor_copy(U[:, g, :], pU[pb][:, 0:128]), reads=["pU%d" % pb], writes=[("U", g)])
                P.add("act", lambda e, g=g, pb=pb: e.activation(out=Ur[:, g, :], in_=pU[pb][:, 128:288], func=AF.Copy),
                      reads=["pU%d" % pb], writes=[("U", g)])
            P.barrier()
        with ExitStack() as ph:
            w1c = [sb("w1c%d" % i, [128, 8, 2, 128], BF16, ph) for i in range(2)]
            pP = [ps("pP%d" % i, [128, 288], F32, ph) for i in range(3)]
            nn = 0
            for d_ in range(2):
                for q in range(4):
                    wb_ = (d_ * 4 + q) % 2
                    P.add("sp", lambda e, d_=d_, q=q, wb_=wb_: e.dma_start(
                        out=w1c[wb_][:], in_=scrW1[d_, q * 8:(q + 1) * 8].rearrange("g part r c -> r g part c")),
                        reads=["scrW1"], writes=[("w1c", wb_)], group="w1c%d" % wb_)
                    for g2l in range(8):
                        g2 = q * 8 + g2l
                        for part in range(2):
                            pb = nn % 3
                            nn += 1

                            def mmP(e, d_=d_, g2=g2, g2l=g2l, part=part, pb=pb, wb_=wb_):
                                ins = None
                                for gp in range(2):
                                    g = 2 * g2 + gp
                                    lw = w1c[wb_][:, g2l, part, gp * 64:(gp + 1) * 64]
                                    rows = slice(gp * 64, (gp + 1) * 64)
                                    if d_ == 0:
                                        e.matmul(pP[pb][rows, 0:32], lhsT=lw, rhs=Ur[:, g, 128:160], start=True, stop=True)
                                        ins = e.matmul(pP[pb][rows, 32:160], lhsT=lw, rhs=U[:, g, 0:128], start=True, stop=True)
                                    else:
                                        e.matmul(pP[pb][rows, 0:128], lhsT=lw, rhs=U[:, g, 0:128], start=True, stop=True)
                                        ins = e.matmul(pP[pb][rows, 128:288], lhsT=lw, rhs=Ur[:, g, 0:160], start=True, stop=True)
                                return ins
                            P.add("pe", mmP, reads=[("w1c", wb_), "U"], writes=["pP%d" % pb])
                            ncol = 160 if d_ == 0 else 288
                            if nn % 2 == 0:
                                P.add("dve", lambda e, d_=d_, g2=g2, part=part, pb=pb, ncol=ncol: e.tensor_copy(
                                    Pt[:, part, d_, g2, 0:ncol], pP[pb][:, 0:ncol]), reads=["pP%d" % pb], writes=[("Pt", part, d_, g2)])
                            else:
                                P.add("act", lambda e, d_=d_, g2=g2, part=part, pb=pb, ncol=ncol: e.activation(
                                    out=Pt[:, part, d_, g2, 0:ncol], in_=pP[pb][:, 0:ncol], func=AF.Copy),
                                    reads=["pP%d" % pb], writes=[("Pt", part, d_, g2)])
            P.barrier()
        s12.close()
        with ExitStack() as ph:
            St = [sb("St%d" % i, [128, 4, 64], F32, ph) for i in range(2)]
            C4 = sb("C4", [128, 4, 64], F32, ph)
            rt1 = sb("rt1", [128, 4, 64], F32, ph)
            rt2 = sb("rt2", [128, 2, 64], F32, ph)
            P.add("dve", lambda e: e.memset(St[0][:], 0.0), writes=["St0"])
            P.add("dve", lambda e: e.tensor_copy(C4[:, 0:2, :], Acplx[:, 0:1, :].to_broadcast([128, 2, 64])), reads=["Acplx"], writes=["C4"])
            P.add("dve", lambda e: e.tensor_scalar(out=C4[:, 2, :], in0=Acplx[:, 1, :], scalar1=-1.0, scalar2=None, op0=ALU.mult),
                  reads=["Acplx", "C4"], writes=["C4"])
            P.add("dve", lambda e: e.tensor_copy(C4[:, 3, :], Acplx[:, 1, :]), reads=["Acplx", "C4"], writes=["C4"])
            Pt_full = Pt[:]
            pstep = Pt_full.ap[0][0]
            PART = 2 * 32 * 288
            for i in range(288):
                cur, nxt = St[i % 2], St[(i + 1) % 2]
                ck, nk = "St%d" % (i % 2), "St%d" % ((i + 1) % 2)
                qF, qB = i, 287 - i
                if i < 160:
                    cs = slice(0, 64)
                    dd = [[32 * 288 + qB - qF, 2], [288, 32]]
                    off = Pt_full.offset + qF
                    vv = lambda t_, a, b: t_[:, a:b, :].rearrange("p a (d g) -> p a d g", d=2)
                else:
                    cs = slice(32, 64)
                    dd = [[288, 32]]
                    off = Pt_full.offset + 32 * 288 + qB
                    vv = lambda t_, a, b: t_[:, a:b, 32:64]
                pap = bass.AP(Pt_full.tensor, off, [[pstep, 128], [PART, 2]] + dd)
                pap_sw = bass.AP(Pt_full.tensor, off + PART, [[pstep, 128], [-PART, 2]] + dd)
                P.add("dve", lambda e, cur=cur, cs=cs: e.tensor_tensor(out=rt1[:, :, cs], in0=C4[:, :, cs], in1=cur[:, :, cs], op=ALU.mult),
                      reads=[ck, "C4"], writes=["rt1"])
                P.add("dve", lambda e, cs=cs: e.tensor_tensor(out=rt2[:, :, cs], in0=rt1[:, 0:2, cs], in1=rt1[:, 2:4, cs], op=ALU.add),
                      reads=["rt1"], writes=["rt2"])
                P.add("dve", lambda e, nxt=nxt, vv=vv, pap=pap: e.tensor_tensor(out=vv(nxt, 0, 2), in0=vv(rt2, 0, 2), in1=pap, op=ALU.add),
                      reads=["rt2", ("PtS", i)], writes=[(nk, 0)])
                sw_in = bass.AP(rt2[:].tensor, rt2[:].offset + 64 + (32 if i >= 160 else 0), [[rt2[:].ap[0][0], 128], [-64, 2]] + ([[32, 2], [1, 32]] if i < 160 else [[1, 32]]))
                P.add("dve", lambda e, nxt=nxt, vv=vv, pap_sw=pap_sw, sw_in=sw_in: e.tensor_tensor(out=vv(nxt, 2, 4), in0=sw_in, in1=pap_sw, op=ALU.add),
                      reads=["rt2", ("PtS", i)], writes=[(nk, 1)])
                P.add("act", lambda e, nxt=nxt, vv=vv, pap=pap: e.activation(out=pap, in_=vv(nxt, 0, 2), func=AF.Copy),
                      reads=[(nk, 0)], writes=[("PtS", i)])
            P.barrier()
        if debug:
            d_H = dout("d_H", [128, 2 * 2 * 32 * 288], BF16)
            P.add("sp", lambda e: e.dma_start(out=d_H, in_=Pt[:].rearrange("p a b c d -> p (a b c d)")), reads=["Pt", "PtS"],
                  writes=["d_H"], group="dbgH")

        with ExitStack() as ph:
            Ysb = sb("Ysb", [128, 64, 128], BF16, ph)
            tch = [sb("tch%d" % i, [128, 2, 8, 128], BF16, ph) for i in range(1)] * 2
            w2ch = [sb("w2ch%d" % i, [128, 2, 4, 2, 128], BF16, ph) for i in range(1)] * 2
            pY = [ps("pY%d" % i, [128, 4, 128], F32, ph) for i in range(2)]
            for q in range(8):
                b_ = 0
                for d_ in range(2):
                    P.add("sp", lambda e, q=q, b_=b_, d_=d_: e.dma_start(
                        out=tch[b_][:, d_], in_=scrT[d_, q * 8:(q + 1) * 8].rearrange("g r c -> r g c")),
                        reads=["scrT"], writes=[("tch", b_, d_)], group="tch%d" % b_)
                    P.add("sp", lambda e, q=q, b_=b_, d_=d_: e.dma_start(
                        out=w2ch[b_][:, d_], in_=scrW2[d_, q * 4:(q + 1) * 4].rearrange("g part r c -> r g part c")),
                        reads=["scrW2"], writes=[("w2ch", b_, d_)], group="w2ch%d" % b_)
                for hh in range(2):
                    pb = (q * 2 + hh) % 2

                    def mmY(e, q=q, hh=hh, pb=pb, b_=b_):
                        ins = None
                        for j in range(4):
                            gl = hh * 4 + j
                            g = q * 8 + gl
                            g2, gp = g // 2, g % 2
                            g2l = g2 - q * 4
                            rows = slice(gp * 64, (gp + 1) * 64)
                            o = pY[pb][:, j, :]
                            e.matmul(o, lhsT=tch[b_][:, 0, gl, :], rhs=U[:, g, 0:128], start=True, stop=False)
                            e.matmul(o, lhsT=w2ch[b_][rows, 0, g2l, 0, :], rhs=Pt[rows, 0, 0, g2, 31:159], start=False, stop=False)
                            e.matmul(o, lhsT=w2ch[b_][rows, 0, g2l, 1, :], rhs=Pt[rows, 1, 0, g2, 31:159], start=False, stop=False)
                            e.matmul(o, lhsT=tch[b_][:, 1, gl, :], rhs=U[:, g, 0:128], start=False, stop=False)
                            e.matmul(o, lhsT=w2ch[b_][rows, 1, g2l, 0, :], rhs=Pt[rows, 0, 1, g2, 1:129], start=False, stop=False)
                            ins = e.matmul(o, lhsT=w2ch[b_][rows, 1, g2l, 1, :], rhs=Pt[rows, 1, 1, g2, 1:129], start=False, stop=True)
                        return ins
                    P.add("pe", mmY, reads=[("tch", b_), ("w2ch", b_), "U", "Pt", "PtS"], writes=["pY%d" % pb])
                    g0 = q * 8 + hh * 4
                    if hh == 0:
                        P.add("dve", lambda e, g0=g0, pb=pb: e.tensor_copy(Ysb[:, g0:g0 + 4, :], pY[pb][:]), reads=["pY%d" % pb],
                              writes=[("Ysb", g0)])
                    else:
                        P.add("act", lambda e, g0=g0, pb=pb: e.activation(out=Ysb[:, g0:g0 + 4, :], in_=pY[pb][:], func=AF.Copy),
                              reads=["pY%d" % pb], writes=[("Ysb", g0)])
            pZ = [ps("pZ%d" % i, [128, 512], F32, ph) for i in range(2)]
            yf = [sb("yf%d" % i, [128, 512], F32, ph) for i in range(2)]
            ya = [sb("ya%d" % i, [128, 512], F32, ph) for i in range(2)]
            for ct in range(8):
                chains = []
                for half in range(2):
                    pb = half

                    def uns(e, ct=ct, half=half, pb=pb):
                        ins = None
                        ov = pZ[pb][:, :].rearrange("p (j t) -> p t j", t=8)
                        for t_ in range(8):
                            for gl in range(8):
                                ins = e.matmul(ov[:, t_, :], lhsT=Z[:, t_, gl, :], rhs=Ysb[:, ct * 8 + gl, half * 64:(half + 1) * 64],
                                               start=(gl == 0), stop=(gl == 7))
                        return ins
                    P.add("pe", uns, reads=["Z", "Ysb"], writes=["pZ%d" % pb])
                    tk = slice(half * 512, (half + 1) * 512)
                    yk, ak = "yf%d" % pb, "ya%d" % pb
                    chains.append([
                        ("dve", lambda e, ct=ct, pb=pb, half=half: e.scalar_tensor_tensor(
                            out=yf[pb][:].rearrange("p (j s) -> p j s", s=8),
                            in0=uTown[:, ct, :].rearrange("p (s j) -> p j s", s=8)[:, half * 64:(half + 1) * 64, :],
                            scalar=colB[:, ct:ct + 1], in1=pZ[pb][:, :].rearrange("p (j s) -> p j s", s=8), op0=ALU.mult, op1=ALU.add),
                         ["uT", "colB", "pZ%d" % pb], [yk]),
                        ("act", lambda e, pb=pb: e.activation(out=ya[pb][:], in_=yf[pb][:], func=AF.Square), [yk], [ak]),
                        ("dve", lambda e, pb=pb: e.tensor_scalar(out=ya[pb][:], in0=ya[pb][:], scalar1=0.044715, scalar2=1.0,
                                                                 op0=ALU.mult, op1=ALU.add), [ak], [ak]),
                        ("dve", lambda e, pb=pb: e.tensor_tensor(out=ya[pb][:], in0=ya[pb][:], in1=yf[pb][:], op=ALU.mult), [ak, yk], [ak]),
                        ("act", lambda e, pb=pb: e.activation(out=ya[pb][:], in_=ya[pb][:], func=AF.Sigmoid, scale=1.5957691216057308),
                         [ak], [ak]),
                        ("dve", lambda e, pb=pb, ct=ct, tk=tk: e.tensor_tensor(out=gT[:, ct, tk], in0=ya[pb][:], in1=yf[pb][:], op=ALU.mult),
                         [ak, yk], [("gT", ct, half)]),
                    ])
                for k in range(6):
                    for ch in chains:
                        en, f_, rk, wk = ch[k]
                        P.add(en, f_, reads=rk, writes=wk)
            P.barrier()
        ssm.close()
        if debug:
            d_gT = dout("d_gT", [128, 8 * NOWN], BF16)
            P.add("sp", lambda e: e.dma_start(out=d_gT, in_=gT[:].rearrange("p a b -> p (a b)")), reads=["gT"],
                  writes=["d_gT"], group="dbgG")

        ssmT = sb("ssmT", [128, 8, NOWN], BF16, mix)
        rstd = sb("rstdSC", [128, 2, NOWN], F32, mix)
        with ExitStack() as ph:
            wglu = sb("wglu", [128, 8, 2048], BF16, ph)
            wgv = w_glu.rearrange("(kt p) c -> p kt c", p=128)
            for kt in range(8):
                for hc in range(2):
                    P.add("pool", lambda e, kt=kt, hc=hc: e.dma_start(out=wglu[:, kt, hc * 1024:(hc + 1) * 1024],
                                                                      in_=wgv[:, kt, hc * 1024:(hc + 1) * 1024]),
                          writes=[("wglu", kt, hc)], group="wglu")
            pga = [ps("pga%d" % i, [128, 512], F32, ph) for i in range(2)]
            pgb = [ps("pgb%d" % i, [128, 512], F32, ph) for i in range(2)]
            pss = ps("pss", [128, 512], F32, ph)
            sig = [sb("sig%d" % i, [128, 512], F32, ph) for i in range(2)]
            sq = [sb("sq%d" % i, [128, 512], BF16, ph) for i in range(2)]
            pend = []
            for which in range(2):
                for half in range(2):
                    tk = slice(half * 512, (half + 1) * 512)
                    for ot in range(8):
                        b_ = ot % 2
                        if which == 0:
                            def mg(e, ot=ot, b_=b_, tk=tk, off=0, pp=pga):
                                ins = None
                                for kt in range(8):
                                    ins = e.matmul(pp[b_][:, :], lhsT=wglu[:, kt, off + ot * 128:off + (ot + 1) * 128], rhs=gT[:, kt, tk],
                                                   start=(kt == 0), stop=(kt == 7))
                                return ins
                            P.add("pe", mg, reads=["wglu", "gT"], writes=["pga%d" % b_])
                            P.add("pe", lambda e, ot=ot, b_=b_, tk=tk: mg(e, ot, b_, tk, 1024, pgb), reads=["wglu", "gT"], writes=["pgb%d" % b_])
                            P.add("act", lambda e, b_=b_: e.activation(out=sig[b_][:], in_=pgb[b_][:], func=AF.Sigmoid),
                                  reads=["pgb%d" % b_], writes=["sig%d" % b_])
                            P.add("dve", lambda e, b_=b_, ot=ot, tk=tk: e.tensor_tensor(out=ssmT[:, ot, tk], in0=pga[b_][:], in1=sig[b_][:], op=ALU.mult),
                                  reads=["pga%d" % b_, "sig%d" % b_], writes=[("ssmT", ot, half)])
                            srcT, skey = ssmT, ("ssmT", ot, half)
                        else:
                            srcT, skey = convT, "convT"
                        def back(b_=b_, ot=ot, tk=tk, srcT=srcT, skey=skey):
                            P.add("act", lambda e: e.activation(out=sq[b_][:], in_=srcT[:, ot, tk], func=AF.Square),
                                  reads=[skey], writes=["sq%d" % b_])
                            P.add("pe", lambda e: e.matmul(pss[:, :], lhsT=ones_b[:], rhs=sq[b_][:], start=(ot == 0), stop=(ot == 7)),
                                  reads=["ones_b", "sq%d" % b_], writes=["pss"])
                        if pend:
                            pend.pop()()
                        pend.append(back)
                    if pend:
                        pend.pop()()
                    rk = ("rstd", which, half)
                    P.add("dve", lambda e, which=which, tk=tk: e.tensor_scalar(out=rstd[:, which, tk], in0=pss[:, :], scalar1=1.0 / 1024, scalar2=EPS,
                                                                              op0=ALU.mult, op1=ALU.add), reads=["pss"], writes=[rk])
                    P.add("act", lambda e, which=which, tk=tk: e.activation(out=rstd[:, which, tk], in_=rstd[:, which, tk], func=AF.Sqrt),
                          reads=[rk], writes=[rk])
                    P.add("dve", lambda e, which=which, tk=tk: e.reciprocal(out=rstd[:, which, tk], in_=rstd[:, which, tk]), reads=[rk], writes=[rk])
            for ot in range(8):
                P.add("dve", lambda e, ot=ot: e.scalar_tensor_tensor(out=ssmT[:, ot, :], in0=ssmT[:, ot, :], scalar=colA[:, 80 + ot:81 + ot],
                                                                     in1=rstd[:, 0, :], op0=ALU.mult, op1=ALU.mult),
                      reads=["ssmT", "rstd", "colA"], writes=[("ssmT", ot)])
                P.add("dve", lambda e, ot=ot: e.scalar_tensor_tensor(out=convT[:, ot, :], in0=convT[:, ot, :], scalar=colA[:, 88 + ot:89 + ot],
                                                                      in1=rstd[:, 1, :], op0=ALU.mult, op1=ALU.mult),
                      reads=["convT", "rstd", "colA"], writes=[("convT", ot)])
            P.barrier()

        def row_bcast(dst, col_of_ft, rkeys, wkey, stack_ps):
            dgs = [sb(wkey + "_dg%d" % i, [128, 128], F32, stack_ps) for i in range(2)]
            prb = ps(wkey + "_prb", [128, 512], F32, stack_ps)
            for c4 in range(4):
                for j in range(4):
                    ft = c4 * 4 + j
                    b_ = ft % 2
                    P.add("dve", lambda e, ft=ft, b_=b_: e.tensor_scalar(out=dgs[b_][:], in0=ident_f[:], scalar1=col_of_ft(ft), scalar2=None,
                                                                        op0=ALU.mult), reads=["ident_f"] + rkeys, writes=[wkey + "_dg%d" % b_])
                    P.add("pe", lambda e, j=j, b_=b_: e.matmul(prb[:, j * 128:(j + 1) * 128], lhsT=ones_f[:], rhs=dgs[b_][:], start=True, stop=True),
                          reads=["ones_f", wkey + "_dg%d" % b_], writes=[wkey + "_prb"])
                P.add("act", lambda e, c4=c4: e.activation(out=dst[:, c4 * 512:(c4 + 1) * 512], in_=prb[:, :], func=AF.Copy),
                      reads=[wkey + "_prb"], writes=[wkey])

        with ExitStack() as ph:
            g1b = sb("g1b", [128, D], F32, ph)
            with ExitStack() as ph3:
                row_bcast(g1b, lambda ft: modT[:, 32 + ft, 0:1], ["modT"], "g1b", ph3)
                P.barrier()
            woc = [sb("woc%d" % i, [128, 16, 512], BF16, ph) for i in range(2)]
            wov = w_out.rearrange("(kt p) c -> p kt c", p=128)
            po = [ps("po%d" % i, [128, 512], F32, ph) for i in range(6)]
            xp = [sb("xp%d" % i, [128, 512], F32, ph) for i in range(8)]
            x1p = [sb("x1p%d" % i, [128, 512], F32, ph) for i in range(4)]
            jk = sb("jk", [128, 512], BF16, ph)
            its = [(cc, tt) for cc in range(4) for tt in range(8)]

            def ld(n):
                cc, tt = its[n]
                P.add("sp", lambda e, n=n, cc=cc, tt=tt: e.dma_start(out=xp[n % 8][:], in_=xs[tt * 128:(tt + 1) * 128, cc * 512:(cc + 1) * 512]),
                      writes=[("xp", n % 8)], group="xp%d" % (n % 8))
            for n in range(6):
                ld(n)
            for n, (cc, tt) in enumerate(its):
                wb_ = cc % 2
                if tt == 0:
                    for kt in range(16):
                        P.add("pool", lambda e, kt=kt, cc=cc, wb_=wb_: e.dma_start(out=woc[wb_][:, kt, :], in_=wov[:, kt, cc * 512:(cc + 1) * 512]),
                              writes=[("woc", wb_, kt)], group="woc%d" % wb_)
                if n + 6 < len(its):
                    ld(n + 6)
                rows = slice(tt * 128, (tt + 1) * 128)
                cols = slice(cc * 512, (cc + 1) * 512)
                pb, xb_, sb_ = n % 6, n % 8, n % 4

                def mo(e, tt=tt, pb=pb, wb_=wb_):
                    ins = None
                    for ht in range(16):
                        hsrc = ssmT if ht < 8 else convT
                        ins = e.matmul(po[pb][:, :], lhsT=hsrc[:, ht % 8, tt * 128:(tt + 1) * 128], rhs=woc[wb_][:, ht, :],
                                       start=(ht == 0), stop=(ht == 15))
                    return ins
                P.add("pe", mo, reads=["ssmT", "convT", ("woc", wb_)], writes=["po%d" % pb])
                P.add("dve", lambda e, pb=pb, sb_=sb_, cols=cols: e.tensor_tensor(out=x1p[sb_][:], in0=po[pb][:, :], in1=g1b[:, cols], op=ALU.mult),
                      reads=["po%d" % pb, "g1b"], writes=[("x1p", sb_)])
                P.add("dve", lambda e, sb_=sb_, xb_=xb_: e.tensor_tensor(out=x1p[sb_][:], in0=x1p[sb_][:], in1=xp[xb_][:], op=ALU.add),
                      reads=[("x1p", sb_), ("xp", xb_)], writes=[("x1p", sb_)])
                P.add("act", lambda e, sb_=sb_, tt=tt, cc=cc: e.activation(out=jk[:], in_=x1p[sb_][:], func=AF.Square,
                                                                        accum_out=ss2[:, tt, cc:cc + 1]),
                      reads=[("x1p", sb_)], writes=["jk", ("ss2", tt, cc)])
                P.add("act", lambda e, sb_=sb_, rows=rows, cols=cols: e.dma_start(out=scrX1[rows, cols], in_=x1p[sb_][:]),
                      reads=[("x1p", sb_)], writes=["scrX1"], group="x1p%d" % sb_)
            P.barrier()
        mix.close()
        mixer.close()

        moe = top.enter_context(ExitStack())
        acc = sb("acc", [128, 8, D], F32, moe)
        hx2T = sb("hx2T", [128, 16, NOWN], BF16, moe)
        Wt = sb("Wt", [128, 8, 64], F32, moe)
        with ExitStack() as ph:
            sc2b = sb("sc2b", [128, D], F32, ph)
            sh2b = sb("sh2b", [128, D], F32, ph)
            scale2 = sb("scale2", [128, 16], F32, ph)
            P.add("dve", lambda e: e.scalar_tensor_tensor(out=scale2[:], in0=modT[:, 64:80, 0], scalar=1.0, in1=colA[:, 48:64],
                                                          op0=ALU.add, op1=ALU.mult), reads=["modT", "colA"], writes=["scale2"])
            with ExitStack() as ph3:
                row_bcast(sc2b, lambda ft: scale2[:, ft:ft + 1], ["scale2"], "sc2b", ph3)
                P.barrier()
            with ExitStack() as ph3:
                row_bcast(sh2b, lambda ft: modT[:, 48 + ft, 0:1], ["modT"], "sh2b", ph3)
                P.barrier()
            rw = sb("rw", [128, 16, 64], F32, ph)
            P.add("sp", lambda e: e.dma_start(out=rw[:], in_=router_w.rearrange("(kt p) c -> p kt c", p=128)), writes=["rw"], group="rw")
            rb = sb("rb", [128, 64], F32, ph)
            P.add("sp", lambda e: e.dma_start(out=rb[:], in_=rbias_b), writes=["rb"], group="rb")
            rs2 = sb("rs2", [128, 8], F32, ph)
            P.add("dve", lambda e: e.tensor_reduce(out=rs2[:], in_=ss2[:], axis=AX.X, op=ALU.add), reads=["ss2"], writes=["rs2"])
            P.add("dve", lambda e: e.tensor_scalar(out=rs2[:], in0=rs2[:], scalar1=1.0 / D, scalar2=EPS, op0=ALU.mult, op1=ALU.add),
                  reads=["rs2"], writes=["rs2"])
            P.add("act", lambda e: e.activation(out=rs2[:], in_=rs2[:], func=AF.Sqrt), reads=["rs2"], writes=["rs2"])
            P.add("dve", lambda e: e.reciprocal(out=rs2[:], in_=rs2[:]), reads=["rs2"], writes=["rs2"])
            x1t = [sb("x1t%d" % i, [128, D], F32, ph) for i in range(3)]
            hf = [sb("hf%d" % i, [128, D], F32, ph) for i in range(3)]
            pth = [ps("pth%d" % i, [128, 4, 128], F32, ph) for i in range(2)]
            hfs = [sb("hfs%d" % i, [128, 4, 128], F32, ph) for i in range(2)]
            plgs = [ps("plg%d" % i, [128, 64], F32, ph) for i in range(2)]
            rt = sb("rt", [128, 12, 64], F32, ph)
            m8 = sb("m8", [128, 16], F32, ph)

            def n2A(tt):
                b_ = tt % 3
                rows = slice(tt * 128, (tt + 1) * 128)
                P.add("sp", lambda e, b_=b_, rows=rows: e.dma_start(out=x1t[b_][:], in_=scrX1[rows, :]), reads=["scrX1"],
                      writes=[("x1t", b_)], group="x1t%d" % b_)
                P.add("act", lambda e, b_=b_, tt=tt: e.activation(out=hf[b_][:], in_=x1t[b_][:], func=AF.Copy, scale=rs2[:, tt:tt + 1]),
                      reads=[("x1t", b_), "rs2"], writes=[("hf", b_)])
                P.add("dve", lambda e, b_=b_: e.tensor_tensor(out=hf[b_][:], in0=hf[b_][:], in1=sc2b[:], op=ALU.mult),
                      reads=[("hf", b_), "sc2b"], writes=[("hf", b_)])
                P.add("dve", lambda e, b_=b_: e.tensor_tensor(out=hf[b_][:, 0:1280], in0=hf[b_][:, 0:1280], in1=sh2b[:, 0:1280], op=ALU.add),
                      reads=[("hf", b_), "sh2b"], writes=[("hf", b_, 0)])
                P.add("pool", lambda e, b_=b_: e.tensor_tensor(out=hf[b_][:, 1280:2048], in0=hf[b_][:, 1280:2048], in1=sh2b[:, 1280:2048], op=ALU.add),
                      reads=[("hf", b_), "sh2b"], writes=[("hf", b_, 1)])

            def n2B(tt):
                b_ = tt % 3
                plg = plgs[tt % 2]
                pk = "plg%d" % (tt % 2)

                def tr_blk(f4):
                    pb = f4 % 2

                    def trh(e, b_=b_, f4=f4, pb=pb):
                        ins = None
                        for j in range(4):
                            ft = f4 * 4 + j
                            ins = e.transpose(out=pth[pb][:, j, :], in_=hf[b_][:, ft * 128:(ft + 1) * 128], identity=ident_f[:])
                        return ins
                    P.add("pe", trh, reads=[("hf", b_), "ident_f"], writes=["pth%d" % pb])
                    P.add("act", lambda e, pb=pb, f4=f4, tt=tt: e.activation(out=hx2T[:, f4 * 4:(f4 + 1) * 4, tt * 128:(tt + 1) * 128],
                                                                            in_=pth[pb][:], func=AF.Copy),
                          reads=["pth%d" % pb], writes=[("hx2T", f4, tt)])
                    P.add("dve", lambda e, pb=pb: e.tensor_copy(hfs[pb][:], pth[pb][:]), reads=["pth%d" % pb], writes=[("hfs", pb)])

                def mr_blk(f4):
                    pb = f4 % 2

                    def mr(e, pb=pb, f4=f4, plg=plg):
                        ins = None
                        for j in range(4):
                            ft = f4 * 4 + j
                            ins = e.matmul(plg[:, :], lhsT=hfs[pb][:, j, :], rhs=rw[:, ft, :], start=(ft == 0), stop=(ft == 15))
                        return ins
                    P.add("pe", mr, reads=[("hfs", pb), "rw"], writes=[pk])
                tr_blk(0)
                for f4 in range(4):
                    if f4 + 1 < 4:
                        tr_blk(f4 + 1)
                    mr_blk(f4)

            def n2C(tt):
                plg = plgs[tt % 2]
                pk = "plg%d" % (tt % 2)
                S_, Bi, T1, T2, MB, EM = (rt[:, i, :] for i in range(6))
                g3 = lambda ap: ap.rearrange("p (g k) -> p g k", k=8)
                rops = [
                    ("act", lambda e: e.activation(out=S_, in_=plg[:, :], func=AF.Sigmoid), [pk]),
                    ("dve", lambda e: e.tensor_tensor(out=Bi, in0=S_, in1=rb[:], op=ALU.add), ["rb"]),
                    ("dve", lambda e: e.tensor_reduce(out=m8[:, 0:8], in_=g3(Bi), axis=AX.X, op=ALU.max), []),
                    ("dve", lambda e: e.tensor_tensor(out=g3(T1), in0=g3(Bi), in1=m8[:, 0:8].unsqueeze(2).to_broadcast([128, 8, 8]), op=ALU.is_equal), []),
                    ("dve", lambda e: e.scalar_tensor_tensor(out=T1, in0=T1, scalar=-1e9, in1=Bi, op0=ALU.mult, op1=ALU.add), []),
                    ("dve", lambda e: e.tensor_reduce(out=m8[:, 8:16], in_=g3(T1), axis=AX.X, op=ALU.max), []),
                    ("dve", lambda e: e.tensor_tensor(out=m8[:, 0:8], in0=m8[:, 0:8], in1=m8[:, 8:16], op=ALU.add), []),
                    ("dve", lambda e: e.max(out=m8[:, 8:16], in_=m8[:, 0:8]), []),
                    ("dve", lambda e: e.tensor_scalar(out=m8[:, 0:8], in0=m8[:, 0:8], scalar1=m8[:, 11:12], scalar2=None, op0=ALU.is_ge), []),
                    ("dve", lambda e: e.tensor_tensor(out=g3(MB), in0=g3(Bi), in1=m8[:, 0:8].unsqueeze(2).to_broadcast([128, 8, 8]), op=ALU.mult), []),
                    ("dve", lambda e: e.tensor_scalar(out=m8[:, 0:8], in0=m8[:, 0:8], scalar1=-1.0, scalar2=1e9, op0=ALU.add, op1=ALU.mult), []),
                    ("dve", lambda e: e.tensor_tensor(out=g3(MB), in0=g3(MB), in1=m8[:, 0:8].unsqueeze(2).to_broadcast([128, 8, 8]), op=ALU.add), []),
                    ("dve", lambda e: e.max(out=m8[:, 8:16], in_=MB), []),
                    ("dve", lambda e: e.tensor_scalar(out=EM, in0=MB, scalar1=m8[:, 15:16], scalar2=None, op0=ALU.is_ge), []),
                    ("dve", lambda e: e.tensor_tensor(out=T2, in0=S_, in1=EM, op=ALU.mult), []),
                    ("dve", lambda e: e.tensor_reduce(out=m8[:, 0:1], in_=T2, axis=AX.X, op=ALU.add), []),
                    ("dve", lambda e: e.reciprocal(out=m8[:, 0:1], in_=m8[:, 0:1]), []),
                    ("dve", lambda e, tt=tt: e.tensor_scalar(out=Wt[:, tt, :], in0=T2, scalar1=m8[:, 0:1], scalar2=2.5, op0=ALU.mult, op1=ALU.mult), []),
                ]
                for (en, f_, rk) in rops:
                    P.add(en, f_, reads=["rt", "m8"] + rk, writes=["rt", "m8", ("Wt", tt)])

            for it in range(10):
                if it < 8:
                    n2A(it)
                if 1 <= it <= 8:
                    n2B(it - 1)
                if it >= 2:
                    n2C(it - 2)
            P.barrier()
        if debug:
            d_Wt = dout("d_Wt", [128, 512])
            P.add("sp", lambda e: e.dma_start(out=d_Wt, in_=Wt[:].rearrange("p a b -> p (a b)")), reads=["Wt"], writes=["d_Wt"], group="dbgW")
            d_hx2T = dout("d_hx2T", [128, 16 * NOWN], BF16)
            P.add("sp", lambda e: e.dma_start(out=d_hx2T, in_=hx2T[:].rearrange("p a b -> p (a b)")), reads=["hx2T"], writes=["d_hx2T"], group="dbgW2")

        with ExitStack() as ph:
            wg = [sb("wg%d" % i, [128, 16, 512], BF16, ph) for i in range(2)]
            wu = [sb("wu%d" % i, [128, 16, 512], BF16, ph) for i in range(2)]
            wd = [sb("wd0", [128, 4, D], BF16, ph)]
            actT = sb("actT", [128, 4, NOWN], BF16, ph)
            sgl = [sb("sgl%d" % i, [128, 512], F32, ph) for i in range(2)]
            pg = [ps("pg%d" % i, [128, 512], F32, ph) for i in range(2)]
            pu = [ps("pu%d" % i, [128, 512], F32, ph) for i in range(2)]
            pd = [ps("pd%d" % i, [128, 512], F32, ph) for i in range(3)]
            P.add("pool", lambda e: e.memset(acc[:], 0.0), writes=["acc"])
            NE = 65
            import os
            DMAONLY = os.environ.get("MOE_DMAONLY", "")
            _Padd = P.add
            if DMAONLY:
                class _PX:
                    @staticmethod
                    def add(eng, fn, reads=(), writes=(), group=None):
                        if group is None:
                            return None
                        q = {"1": "pool", "2": "sp", "3": "act"}[DMAONLY[0]]
                        return _Padd(q if DMAONLY[0] != "4" else eng, fn, reads=reads, writes=writes, group=group)
                PM = _PX
            else:
                PM = P
            for ex in range(NE):
                b_ = ex % 2
                gsrc = ew_gate[ex] if ex < 64 else sw_gate
                usrc = ew_up[ex] if ex < 64 else sw_up
                dsrc = ew_down[ex] if ex < 64 else sw_down
                PM.add("pool", lambda e, b_=b_, gsrc=gsrc: e.dma_start(out=wg[b_][:], in_=gsrc.rearrange("(kt p) c -> p kt c", p=128)),
                      writes=[("wg", b_)], group="wg%d" % b_)
                PM.add("pool", lambda e, b_=b_, usrc=usrc: e.dma_start(out=wu[b_][:], in_=usrc.rearrange("(kt p) c -> p kt c", p=128)),
                      writes=[("wu", b_)], group="wu%d" % b_)
                dv = dsrc.rearrange("(kt p) c -> p kt c", p=128)
                for hc in range(2):
                    PM.add("pool", lambda e, dv=dv, hc=hc: e.dma_start(out=wd[0][:, :, hc * 1024:(hc + 1) * 1024],
                                                                     in_=dv[:, :, hc * 1024:(hc + 1) * 1024]),
                          writes=[("wd", hc)], group="wd")
                n = 0
                for mt in range(4):
                    for half in range(2):
                        pb = n % 2
                        n += 1
                        tk = slice(half * 512, (half + 1) * 512)

                        def mgu(e, w_, pp, mt=mt, tk=tk, pb=pb, b_=b_):
                            ins = None
                            for kt in range(16):
                                ins = e.matmul(pp[pb][:, :], lhsT=w_[b_][:, kt, mt * 128:(mt + 1) * 128], rhs=hx2T[:, kt, tk],
                                               start=(kt == 0), stop=(kt == 15))
                            return ins
                        PM.add("pe", lambda e, f_=mgu: f_(e, wg, pg), reads=[("wg", b_), "hx2T"], writes=["pg%d" % pb])
                        PM.add("pe", lambda e, f_=mgu: f_(e, wu, pu), reads=[("wu", b_), "hx2T"], writes=["pu%d" % pb])
                        PM.add("act", lambda e, pb=pb: e.activation(out=sgl[pb][:], in_=pg[pb][:, :], func=AF.Silu),
                              reads=["pg%d" % pb], writes=[("sgl", pb)])
                        PM.add("dve", lambda e, pb=pb, mt=mt, tk=tk: e.tensor_tensor(out=actT[:, mt, tk], in0=sgl[pb][:], in1=pu[pb][:, :], op=ALU.mult),
                              reads=[("sgl", pb), "pu%d" % pb], writes=[("actT", mt, half)])
                n = 0
                for tt in range(8):
                    for cc in range(4):
                        pb = n % 3
                        n += 1

                        def mdn(e, tt=tt, cc=cc, pb=pb):
                            ins = None
                            for kt in range(4):
                                ins = e.matmul(pd[pb][:, :], lhsT=actT[:, kt, tt * 128:(tt + 1) * 128], rhs=wd[0][:, kt, cc * 512:(cc + 1) * 512],
                                               start=(kt == 0), stop=(kt == 3))
                            return ins
                        PM.add("pe", mdn, reads=["actT", "wd"], writes=["pd%d" % pb])
                        wsc = Wt[:, tt, ex:ex + 1] if ex < 64 else 1.0
                        PM.add("dve", lambda e, tt=tt, cc=cc, pb=pb, wsc=wsc: e.scalar_tensor_tensor(
                            out=acc[:, tt, cc * 512:(cc + 1) * 512], in0=pd[pb][:, :], scalar=wsc, in1=acc[:, tt, cc * 512:(cc + 1) * 512],
                            op0=ALU.mult, op1=ALU.add), reads=["pd%d" % pb, "Wt"], writes=[("acc", tt, cc)])
            P.barrier()

        with ExitStack() as ph:
            g2b = sb("g2b", [128, D], F32, ph)
            fgb = sb("fgb", [128, D], F32, ph)
            with ExitStack() as ph3:
                row_bcast(g2b, lambda ft: modT[:, 80 + ft, 0:1], ["modT"], "g2b", ph3)
                P.barrier()
            with ExitStack() as ph3:
                row_bcast(fgb, lambda ft: colA[:, 64 + ft:65 + ft], ["colA"], "fgb", ph3)
                P.barrier()
            x1f = [sb("x1f%d" % i, [128, D], F32, ph) for i in range(3)]
            fo = [sb("fo%d" % i, [128, D], F32, ph) for i in range(3)]
            fs = sb("fs", [128, 8], F32, ph)
            fj = sb("fj", [128, D], BF16, ph)
            def stageA(tt):
                b_ = tt % 3
                rows = slice(tt * 128, (tt + 1) * 128)
                for hc in range(2):
                    cs = slice(hc * 1024, (hc + 1) * 1024)
                    P.add("dve", lambda e, tt=tt, cs=cs: e.tensor_tensor(out=acc[:, tt, cs], in0=acc[:, tt, cs], in1=g2b[:, cs], op=ALU.mult),
                          reads=[("acc", tt, hc), "g2b"], writes=[("acc", tt, hc)])
                    P.add("dve", lambda e, tt=tt, b_=b_, cs=cs: e.tensor_tensor(out=acc[:, tt, cs], in0=acc[:, tt, cs], in1=x1f[b_][:, cs], op=ALU.add),
                          reads=[("acc", tt, hc), ("x1f", b_)], writes=[("acc", tt, hc)])
                    P.add("act", lambda e, tt=tt, cs=cs, hc=hc: e.activation(out=fj[:, cs], in_=acc[:, tt, cs], func=AF.Square,
                                                                          accum_out=fs2[:, tt, hc:hc + 1]),
                          reads=[("acc", tt, hc)], writes=[("fj", hc), ("fs2", tt, hc)])

            def stageB(tt):
                b_ = tt % 3
                rows = slice(tt * 128, (tt + 1) * 128)
                P.add("dve", lambda e, tt=tt: e.tensor_tensor(out=fs[:, tt:tt + 1], in0=fs2[:, tt, 0:1], in1=fs2[:, tt, 1:2], op=ALU.add),
                      reads=[("fs2", tt)], writes=[("fs", tt)])
                P.add("dve", lambda e, tt=tt: e.tensor_scalar(out=fs[:, tt:tt + 1], in0=fs[:, tt:tt + 1], scalar1=1.0 / D, scalar2=EPS,
                                                              op0=ALU.mult, op1=ALU.add), reads=[("fs", tt)], writes=[("fs", tt)])
                P.add("act", lambda e, tt=tt: e.activation(out=fs[:, tt:tt + 1], in_=fs[:, tt:tt + 1], func=AF.Sqrt), reads=[("fs", tt)], writes=[("fs", tt)])
                P.add("dve", lambda e, tt=tt: e.reciprocal(out=fs[:, tt:tt + 1], in_=fs[:, tt:tt + 1]), reads=[("fs", tt)], writes=[("fs", tt)])
                P.add("act", lambda e, tt=tt, b_=b_: e.activation(out=fo[b_][:], in_=acc[:, tt, :], func=AF.Copy, scale=fs[:, tt:tt + 1]),
                      reads=[("acc", tt), ("fs", tt)], writes=[("fo", b_)])
                P.add("dve", lambda e, b_=b_: e.tensor_tensor(out=fo[b_][:], in0=fo[b_][:], in1=fgb[:], op=ALU.mult),
                      reads=[("fo", b_), "fgb"], writes=[("fo", b_)])
                P.add("pool", lambda e, b_=b_, rows=rows: e.dma_start(out=out[rows, :], in_=fo[b_][:]), reads=[("fo", b_)], writes=["out"],
                      group="fo%d" % b_)

            fs2 = sb("fs2", [128, 8, 2], F32, ph)

            def ldf(tt):
                P.add("sp", lambda e, tt=tt: e.dma_start(out=x1f[tt % 3][:], in_=scrX1[tt * 128:(tt + 1) * 128, :]), reads=["scrX1"],
                      writes=[("x1f", tt % 3)], group="x1f%d" % (tt % 3))
            ldf(0)
            ldf(1)
            for tt in range(9):
                if tt < 8:
                    stageA(tt)
                if tt + 2 < 8:
                    ldf(tt + 2)
                if tt >= 1:
                    stageB(tt - 1)

        P.add("sp", None, reads=["out", "scrW1", "scrT", "scrW2", "scrU", "scrX1"] + list(dbg.keys()))
        P.emit()
    return nc, dbg


def prep_inputs(inp):
    f = lambda a: np.ascontiguousarray(a, dtype=np.float32)
    x, ctx, c = inp["x"], inp["ctx"], inp["c"]
    maps = []
    shared = {
        "w_ada": f(inp["w_ada"][0]), "w_in": f(inp["w_in"][0]),
        "b_ada": f(inp["b_ada"][0].reshape(96, 128)),
        "w_glu": f(inp["ssm_w_glu"][0]), "w_out": f(inp["w_out"][0]), "router_w": f(inp["router_w"][0]),
        "rbias_b": f(np.tile(inp["router_bias"][0].reshape(1, 64), (128, 1))),
        "ew_gate": f(inp["exp_w_gate"][0]), "ew_up": f(inp["exp_w_up"][0]), "ew_down": f(inp["exp_w_down"][0]),
        "sw_gate": f(inp["shared_w_gate"][0]), "sw_up": f(inp["shared_w_up"][0]), "sw_down": f(inp["shared_w_down"][0]),
    }
    for core in range(8):
        b, h = core // 2, core % 2
        xb = x[b]
        cb = ctx[b]
        conv_w = inp["conv_w"][0]
        if h == 1:
            xb = xb[::-1]
            cb = cb[::-1]
            conv_w = conv_w[::-1]
        vecsA = np.concatenate([c[b].reshape(16, 128), inp["c_ctx"].reshape(16, 128),
                                inp["norm1_g"][0].reshape(16, 128), inp["norm2_g"][0].reshape(16, 128),
                                inp["final_g"].reshape(16, 128), inp["mix_norm_g"][0].reshape(16, 128)], 0)
        vecsB = np.concatenate([inp["ssm_d"][0].reshape(8, 128), conv_w.reshape(24, 128),
                                inp["conv_b"][0].reshape(8, 128)], 0)
        sl = slice(None, None, -1) if h == 1 else slice(None)
        m = dict(shared)
        m.update(xs=f(xb), ctxs=f(cb), vecsA=f(vecsA), vecsB=f(vecsB),
                 lamre_p=f(inp["ssm_lam_re"][0][sl].reshape(64, 128)), lamim_p=f(inp["ssm_lam_im"][0][sl].reshape(64, 128)),
                 logdt_p=f(inp["ssm_log_dt"][0][sl].reshape(64, 2)),
                 ssm_b_re=f(inp["ssm_b_re"][0][sl]), ssm_b_im=f(inp["ssm_b_im"][0][sl]),
                 ssm_c_re=f(inp["ssm_c_re"][0][sl]), ssm_c_im=f(inp["ssm_c_im"][0][sl]))
        maps.append(m)
    return maps


def kernel(**inputs):
    nc, _ = build_nc(False)
    maps = prep_inputs(inputs)
    res = run_bass_kernel_spmd(nc, maps, core_ids=list(range(8)))
    outs = np.zeros((4, 2048, 2048), np.float32)
    for core in range(8):
        b, h = core // 2, core % 2
        o = res.results[core]["out"]
        if h == 0:
            outs[b, 0:1024] = o
        else:
            outs[b, 1024:2048] = o[::-1]
    return outs
```

```python
from contextlib import ExitStack
import numpy as np
import concourse.bass as bass
import concourse.mybir as mybir
from concourse.bass_utils import run_bass_kernel_spmd

F32 = mybir.dt.float32
BF16 = mybir.dt.bfloat16
I32 = mybir.dt.int32
ALU = mybir.AluOpType
AF = mybir.ActivationFunctionType
AX = mybir.AxisListType

D = 2048
NOWN = 1024
NSEQ = 2304
EPS = 1e-6


class Prog:
    ENG = ("pe", "act", "dve", "pool", "sp")

    def __init__(self, nc, stack):
        self.nc = nc
        self.stack = stack
        self.ops = []
        self.keys = {}
        self.groups = {}
        self.psum_names = set()
        self.gopen = {}

    @staticmethod
    def _norm(k):
        return k if isinstance(k, tuple) else (k,)

    def _related(self, key):
        d = self.keys.setdefault(key[0], {})
        for k2 in list(d.keys()):
            n = min(len(k2), len(key))
            if k2[:n] == key[:n]:
                yield k2, d[k2]

    def add(self, eng, fn, reads=(), writes=(), group=None):
        op = dict(id=len(self.ops), eng=eng, fn=fn, deps=set(), group=group, used=False)
        reads = [self._norm(k) for k in reads] + [("__phase",)]
        writes = [self._norm(k) for k in writes]
        pk = [(k[0],) for k in reads + writes if k[0] in self.psum_names]
        reads = [k for k in reads if k[0] not in self.psum_names]
        writes = [k for k in writes if k[0] not in self.psum_names] + sorted(set(pk))
        for key in reads:
            for k2, st in self._related(key):
                if st[0] is not None:
                    op["deps"].add(st[0])
        for key in writes:
            for k2, st in self._related(key):
                if st[0] is not None:
                    op["deps"].add(st[0])
                op["deps"].update(st[1])
        for key in reads:
            d = self.keys.setdefault(key[0], {})
            st = d.setdefault(key, [None, []])
            st[1].append(op["id"])
        for key in writes:
            d = self.keys.setdefault(key[0], {})
            for k2 in list(d.keys()):
                if len(k2) > len(key) and k2[:len(key)] == key:
                    del d[k2]
            d[key] = [op["id"], []]
        op["deps"].discard(op["id"])
        for d in op["deps"]:
            gg = self.ops[d]["group"]
            if gg is not None and d in self.gopen.get(gg, ()):
                self.gopen[gg] = []
        if group is not None:
            self.gopen.setdefault(group, []).append(op["id"])
            op["batch"] = self.gopen[group]
        self.ops.append(op)
        return op

    def barrier(self):
        scr = self._bar_scr
        self.add("dve", lambda e: e.memset(scr[:, 0:1], 0.0), writes=[("__phase",), "barscr"])

    def emit(self):
        nc = self.nc
        ops = self.ops
        for op in ops:
            for d in op["deps"]:
                ops[d]["used"] = True
        sems = {}
        for e in self.ENG:
            sems[e] = self.stack.enter_context(nc.semaphore("s_" + e))
        gsem = {}
        cnt = {e: 0 for e in self.ENG}
        gcnt = {}
        for op in ops:
            if op["group"] is not None:
                g = op["group"]
                if g not in gsem:
                    gsem[g] = self.stack.enter_context(nc.semaphore("g_" + str(g)))
                    gcnt[g] = 0
                gcnt[g] += 16
                op["sig"] = (gsem[g], gcnt[g], 16)
            elif op["used"]:
                cnt[op["eng"]] += 1
                op["sig"] = (sems[op["eng"]], cnt[op["eng"]], 1)
            else:
                op["sig"] = None
        per = {e: [o for o in ops if o["eng"] == e] for e in self.ENG}

        def replay(ename, eng):
            waited = {}
            for op in per[ename]:
                need = {}
                for d in op["deps"]:
                    dop = ops[d]
                    if dop["eng"] == "pe" and ename == "pe" and dop["group"] is None:
                        continue
                    s = dop["sig"]
                    assert s is not None
                    if dop["group"] is not None:
                        s = ops[dop["batch"][-1]]["sig"]
                    key = id(s[0])
                    if key not in need or need[key][1] < s[1]:
                        need[key] = (s[0], s[1])
                for key, (sem, val) in need.items():
                    if waited.get(key, 0) >= val:
                        continue
                    waited[key] = val
                    eng.wait_ge(sem, val)
                if op["fn"] is None:
                    continue
                ins = op["fn"](eng)
                if op["sig"] is not None:
                    ins.then_inc(op["sig"][0], op["sig"][2])

        block = self.stack.enter_context(nc.Block())

        @block.tensor
        def _(eng):
            replay("pe", eng)

        @block.scalar
        def _(eng):
            replay("act", eng)

        @block.vector
        def _(eng):
            replay("dve", eng)

        @block.gpsimd
        def _(eng):
            replay("pool", eng)

        @block.sync
        def _(eng):
            replay("sp", eng)


def build_nc(debug=False, stop=99):
    nc = bass.Bass("TRN2", target_bir_lowering=False)
    dbg = {}

    def din(name, shape, dt=F32):
        return nc.dram_tensor(name, list(shape), dt, kind="ExternalInput").ap()

    xs = din("xs", [2048, D])
    ctxs = din("ctxs", [256, D])
    vecsA = din("vecsA", [96, 128])
    vecsB = din("vecsB", [40, 128])
    b_ada = din("b_ada", [96, 128])
    w_ada = din("w_ada", [D, 6 * D])
    w_in = din("w_in", [D, 4096])
    lamre_p = din("lamre_p", [64, 128])
    lamim_p = din("lamim_p", [64, 128])
    logdt_p = din("logdt_p", [64, 2])
    ssm_b_re = din("ssm_b_re", [2, 64, 64, 16])
    ssm_b_im = din("ssm_b_im", [2, 64, 64, 16])
    ssm_c_re = din("ssm_c_re", [2, 64, 16, 64])
    ssm_c_im = din("ssm_c_im", [2, 64, 16, 64])
    w_glu = din("w_glu", [1024, 2048])
    w_out = din("w_out", [D, D])
    router_w = din("router_w", [D, 64])
    rbias_b = din("rbias_b", [128, 64])
    ew_gate = din("ew_gate", [64, D, 512])
    ew_up = din("ew_up", [64, D, 512])
    ew_down = din("ew_down", [64, 512, D])
    sw_gate = din("sw_gate", [D, 512])
    sw_up = din("sw_up", [D, 512])
    sw_down = din("sw_down", [512, D])
    out = nc.dram_tensor("out", [NOWN, D], F32, kind="ExternalOutput").ap()
    skind = "ExternalOutput" if debug else "Internal"
    scrW1 = nc.dram_tensor("scrW1", [2, 32, 2, 128, 128], BF16, kind=skind).ap()
    scrT = nc.dram_tensor("scrT", [2, 64, 128, 128], BF16, kind=skind).ap()
    scrW2 = nc.dram_tensor("scrW2", [2, 32, 2, 128, 128], BF16, kind=skind).ap()
    scrX1 = nc.dram_tensor("scrX1", [NOWN, D], F32, kind=skind).ap()
    scrU = nc.dram_tensor("scrU", [8, 128, NSEQ - NOWN], BF16, kind=skind).ap()

    def dout(name, shape, dt=F32):
        t = nc.dram_tensor(name, list(shape), dt, kind="ExternalOutput").ap()
        dbg[name] = t
        return t

    with ExitStack() as top:
        P = Prog(nc, top)

        def sb(name, shape, dt=F32, stack=top):
            return stack.enter_context(nc.sbuf_tensor(name, list(shape), dt))

        def ps(name, shape, dt=F32, stack=top):
            P.psum_names.add(name)
            esz = 4 if dt == F32 else 2
            full = stack.enter_context(nc.psum_tensor(name, [128, 2048 // esz], dt))
            n = int(np.prod(shape[1:]))
            v = full[:, 0:n]
            if len(shape) == 3:
                v = v.rearrange("p (a b) -> p a b", b=shape[2])
            return v

        P._bar_scr = sb("barscr", [128, 4])

        ident_f = sb("ident_f", [128, 128])
        ident_b = sb("ident_b", [128, 128], BF16)
        iot = sb("iot", [128, 128], I32)
        iotf = sb("iotf", [128, 128])
        P.add("pool", lambda e: e.iota(iot[:], [[1, 128]], base=0, channel_multiplier=-1), writes=["iot"])
        P.add("dve", lambda e: e.tensor_copy(iotf[:], iot[:]), reads=["iot"], writes=["iotf"])
        P.add("dve", lambda e: e.tensor_single_scalar(ident_f[:], iotf[:], 0.0, ALU.is_equal),
              reads=["iotf"], writes=["ident_f"])
        P.add("dve", lambda e: e.tensor_copy(ident_b[:], ident_f[:]), reads=["ident_f"], writes=["ident_b"])

        if stop < 1:
            d_i = dout('d_ident', [128, 128])
            P.add('sp', lambda e: e.dma_start(out=d_i, in_=ident_f[:]), reads=['ident_f'], writes=['d_ident'], group='dbgi')
            P.add('sp', None, reads=list(dbg.keys()))
            P.emit()
            return nc, dbg
        ones_b = sb("ones_b", [128, 128], BF16)
        ones_f = sb("ones_f", [128, 128], F32)
        P.add("dve", lambda e: e.memset(ones_b[:], 1.0), writes=["ones_b"])
        P.add("dve", lambda e: e.memset(ones_f[:], 1.0), writes=["ones_f"])
        ss2 = sb("ss2", [128, 8, 4], F32)
        vA = sb("vA", [96, 128])
        vB = sb("vB", [40, 128])
        vC = sb("vC", [96, 128])
        colA = sb("colA", [128, 96])
        colB = sb("colB", [128, 40])
        badaT = sb("badaT", [128, 96])
        P.add("sp", lambda e: e.dma_start(out=vA[:], in_=vecsA), writes=["vA"], group="vA")
        P.add("sp", lambda e: e.dma_start(out=vB[:], in_=vecsB), writes=["vB"], group="vB")
        P.add("sp", lambda e: e.dma_start(out=vC[:], in_=b_ada), writes=["vC"], group="vC")
        with ExitStack() as ph:
            pt = ps("pt_small", [128, 3, 128], F32, ph)
            P.add("pe", lambda e: e.transpose(out=pt[:, 0, 0:96], in_=vA[:], identity=ident_f[0:96, 0:96]),
                  reads=["vA", "ident_f"], writes=["pt_small"])
            P.add("pe", lambda e: e.transpose(out=pt[:, 1, 0:40], in_=vB[:], identity=ident_f[0:40, 0:40]),
                  reads=["vB", "ident_f"], writes=["pt_small"])
            P.add("pe", lambda e: e.transpose(out=pt[:, 2, 0:96], in_=vC[:], identity=ident_f[0:96, 0:96]),
                  reads=["vC", "ident_f"], writes=["pt_small"])
            P.add("dve", lambda e: e.tensor_copy(colA[:], pt[:, 0, 0:96]), reads=["pt_small"], writes=["colA"])
            P.add("dve", lambda e: e.tensor_copy(colB[:], pt[:, 1, 0:40]), reads=["pt_small"], writes=["colB"])
            P.add("dve", lambda e: e.tensor_copy(badaT[:], pt[:, 2, 0:96]), reads=["pt_small"], writes=["badaT"])
            P.barrier()

        if stop < 2:
            d_c = dout('d_colA', [128, 96])
            P.add('sp', lambda e: e.dma_start(out=d_c, in_=colA[:]), reads=['colA'], writes=['d_colA'], group='dbgc')
            P.add('sp', None, reads=list(dbg.keys()))
            P.emit()
            return nc, dbg
        sc = sb("sc", [128, 16, 2])
        for j in range(2):
            P.add("act", lambda e, j=j: e.activation(out=sc[:, :, j], in_=colA[:, 16 * j:16 * j + 16], func=AF.Silu),
                  reads=["colA"], writes=[("sc", j)])
        modT = sb("modT", [128, 96, 2])
        scale1 = sb("scale1", [128, 16, 2])
        mixer = top.enter_context(ExitStack())
        uTown = sb("uTown", [128, 8, NOWN], BF16, mixer)
        convT = sb("convT", [128, 8, NOWN], BF16, mixer)
        ss = sb("ss", [128, 24], F32, mixer)
        Acplx = sb("Acplx", [128, 2, 64], F32, mixer)
        ada_stack = ExitStack()
        REC_A = []
        _real_add = P.add
        P.add = lambda *a, **k: REC_A.append((a, k))
        if True:
            ph = ada_stack
            scb = sb("scb", [128, 16, 4], BF16, ph)
            sch = sb("sch", [128, 16, 2], F32, ph)
            P.add("dve", lambda e: e.tensor_copy(scb[:, :, 0:2], sc[:]), reads=["sc"], writes=["scb"])
            P.add("dve", lambda e: e.tensor_copy(sch[:], scb[:, :, 0:2]), reads=["scb"], writes=["sch"])
            P.add("dve", lambda e: e.tensor_tensor(out=sch[:], in0=sc[:], in1=sch[:], op=ALU.subtract), reads=["sc", "sch"], writes=["sch"])
            P.add("dve", lambda e: e.tensor_copy(scb[:, :, 2:4], sch[:]), reads=["sch", "scb"], writes=["scb"])
            pm = ps("pmod", [128, 96, 4], F32, ph)
            wbuf = [sb("wada%d" % i, [128, 2048], BF16, ph) for i in range(4)]
            n = 0
            for kt in range(16):
                for cc in range(6):
                    b = n % 4
                    n += 1
                    for hc in range(2):
                        P.add("pool", lambda e, b=b, kt=kt, cc=cc, hc=hc: e.dma_start(
                            out=wbuf[b][:, hc * 1024:(hc + 1) * 1024],
                            in_=w_ada[kt * 128:(kt + 1) * 128, cc * 2048 + hc * 1024:cc * 2048 + (hc + 1) * 1024]),
                            writes=[("wada", b, hc)], group="wada%d" % b)

                    def mm(e, b=b, kt=kt, cc=cc):
                        ins = None
                        for t in range(16):
                            ins = e.matmul(pm[:, cc * 16 + t, :], lhsT=wbuf[b][:, t * 128:(t + 1) * 128],
                                           rhs=scb[:, kt, :], start=(kt == 0 and cc == 0 and t == 0), stop=(kt == 15),
                                           skip_group_check=True)
                        return ins
                    P.add("pe", mm, reads=[("wada", b), "scb"], writes=["pmod"])
            P.add("dve", lambda e: e.tensor_tensor(out=modT[:], in0=pm[:, :, 0:2], in1=badaT[:].unsqueeze(2).to_broadcast([128, 96, 2]),
                                                  op=ALU.add), reads=["pmod", "badaT"], writes=["modT"])
            P.add("dve", lambda e: e.tensor_tensor(out=modT[:], in0=modT[:], in1=pm[:, :, 2:4], op=ALU.add),
                  reads=["pmod", "modT"], writes=["modT"])
        P.add = _real_add
        import math
        PI = math.pi
        with ExitStack() as ph:
            REC_S = []
            P.add = lambda *a, **k: REC_S.append((a, k))
            raw = sb("s0raw", [64, 3, 128], F32, ph)
            ldt = sb("s0ldt", [64, 2], F32, ph)
            P.add("sp", lambda e: e.dma_start(out=raw[:, 0, :], in_=lamre_p), writes=[("s0raw", 0)], group="s0raw0")
            P.add("sp", lambda e: e.dma_start(out=raw[:, 1, :], in_=lamim_p), writes=[("s0raw", 1)], group="s0raw1")
            P.add("sp", lambda e: e.dma_start(out=ldt[:], in_=logdt_p), writes=["s0ldt"], group="s0ldt")
            P.add("act", lambda e: e.activation(out=ldt[:], in_=ldt[:], func=AF.Exp), reads=["s0ldt"], writes=["s0ldt"])
            P.add("dve", lambda e: e.tensor_copy(raw[:, 2, :].rearrange("q (a b) -> q a b", a=2),
                                                 ldt[:].unsqueeze(2).to_broadcast([64, 2, 64])),
                  reads=["s0ldt"], writes=[("s0raw", 2)])
            LRI = sb("LRI", [128, 3, 64], F32, ph)
            ptq = ps("s0pt", [128, 4, 128], F32, ph)
            for i in range(3):
                P.add("pe", lambda e, i=i: e.transpose(out=ptq[:, i, 0:64], in_=raw[:, i, :], identity=ident_f[0:64, 0:64]),
                      reads=[("s0raw", i), "ident_f"], writes=["s0pt"])
            P.add("dve", lambda e: e.tensor_copy(LRI[:], ptq[:, 0:3, 0:64]), reads=["s0pt"], writes=["LRI"])
            ath = sb("ath", [128, 2, 64], F32, ph)
            P.add("dve", lambda e: e.tensor_tensor(out=ath[:], in0=LRI[:, 0:2, :],
                                                   in1=LRI[:, 2:3, :].to_broadcast([128, 2, 64]), op=ALU.mult),
                  reads=["LRI"], writes=["ath"])
            io8 = sb("io8", [128, 8], I32, ph)
            io8f = sb("io8f", [128, 8], F32, ph)
            KM = sb("KM", [128, 3, 2, 8], F32, ph)
            P.add("pool", lambda e: e.iota(io8[:], [[1, 8]], base=0, channel_multiplier=0), writes=["io8"])
            P.add("dve", lambda e: e.tensor_copy(io8f[:], io8[:]), reads=["io8"], writes=["io8f"])
            kmab = {(0, 0): (-1.0, 7.0), (0, 1): (1.0, 0.0), (1, 0): (-1.0, -1.0), (1, 1): (1.0, -8.0),
                    (2, 0): (1.0, 1.0), (2, 1): (-1.0, 8.0)}
            for (u_, d_), (ka, kb) in kmab.items():
                P.add("dve", lambda e, u_=u_, d_=d_, ka=ka, kb=kb: e.tensor_scalar(
                    out=KM[:, u_, d_, :], in0=io8f[:], scalar1=ka, scalar2=kb, op0=ALU.mult, op1=ALU.add),
                    reads=["io8f"], writes=[("KM", u_, d_)])

            et_ang = sb("et_ang", [128, 2, 32, 8], F32, ph)
            et_ex = sb("et_ex", [128, 2, 32, 8], F32, ph)
            et_tmp = sb("et_tmp", [128, 2, 32, 8], F32, ph)
            et_ti = sb("et_ti", [128, 2, 32, 8], I32, ph)
            et_tf = sb("et_tf", [128, 2, 32, 8], F32, ph)

            def etab(name, mult_ap, L, dst_re, dst_im, rkeys, wkeys):
                shp = [128, 2, 32, L]
                name = "et"
                ang = et_ang[:, :, :, 0:L]
                ex = et_ex[:, :, :, 0:L]
                tmp = et_tmp[:, :, :, 0:L]
                ti = et_ti[:, :, :, 0:L]
                tf = et_tf[:, :, :, 0:L]
                a_b = ath[:, 0, :].rearrange("p (d g) -> p d g", d=2).unsqueeze(3).to_broadcast(shp)
                t_b = ath[:, 1, :].rearrange("p (d g) -> p d g", d=2).unsqueeze(3).to_broadcast(shp)
                P.add("dve", lambda e: e.tensor_tensor(out=ex[:], in0=a_b, in1=mult_ap, op=ALU.mult),
                      reads=["ath"] + rkeys, writes=[name + "_ex"])
                P.add("act", lambda e: e.activation(out=ex[:], in_=ex[:], func=AF.Exp), reads=[name + "_ex"], writes=[name + "_ex"])
                P.add("dve", lambda e: e.tensor_tensor(out=ang[:], in0=t_b, in1=mult_ap, op=ALU.mult),
                      reads=["ath"] + rkeys, writes=[name + "_ang"])
                for (dst, shift) in ((dst_im, 32.0), (dst_re, 32.25)):
                    P.add("dve", lambda e, shift=shift: e.tensor_scalar(out=tmp[:], in0=ang[:], scalar1=1.0 / (2.0 * PI), scalar2=shift,
                                                                        op0=ALU.mult, op1=ALU.add),
                          reads=[name + "_ang"], writes=[name + "_tmp"])
                    P.add("dve", lambda e: e.tensor_copy(ti[:], tmp[:]), reads=[name + "_tmp"], writes=[name + "_ti"])
                    P.add("dve", lambda e: e.tensor_copy(tf[:], ti[:]), reads=[name + "_ti"], writes=[name + "_tf"])
                    P.add("dve", lambda e: e.tensor_tensor(out=tmp[:], in0=tmp[:], in1=tf[:], op=ALU.subtract),
                          reads=[name + "_tmp", name + "_tf"], writes=[name + "_tmp"])
                    P.add("dve", lambda e: e.tensor_single_scalar(tf[:], tmp[:], 0.5, ALU.is_gt),
                          reads=[name + "_tmp"], writes=[name + "_tf"])
                    P.add("dve", lambda e: e.tensor_tensor(out=tmp[:], in0=tmp[:], in1=tf[:], op=ALU.subtract),
                          reads=[name + "_tmp", name + "_tf"], writes=[name + "_tmp"])
                    P.add("act", lambda e: e.activation(out=tmp[:], in_=tmp[:], func=AF.Sin, scale=2.0 * PI), reads=[name + "_tmp"],
                          writes=[name + "_tmp"])
                    P.add("dve", lambda e, dst=dst: e.tensor_tensor(out=dst, in0=tmp[:], in1=ex[:], op=ALU.mult),
                          reads=[name + "_tmp", name + "_ex"], writes=wkeys)

            one1 = sb("one1", [128, 1], F32, ph)
            P.add("dve", lambda e: e.memset(one1[:], 1.0), writes=["one1"])
            E1 = sb("E1", [128, 2, 2, 32, 1], F32, ph)
            etab("e1", one1[:].unsqueeze(2).unsqueeze(3).to_broadcast([128, 2, 32, 1]), 1, E1[:, 0], E1[:, 1], ["one1"], ["E1"])
            eight = sb("eight", [128, 1], F32, ph)
            P.add("dve", lambda e: e.memset(eight[:], 8.0), writes=["eight"])
            etab("e8", eight[:].unsqueeze(2).unsqueeze(3).to_broadcast([128, 2, 32, 1]), 1,
                 Acplx[:, 0, :].rearrange("p (d g o) -> p d g o", d=2, o=1),
                 Acplx[:, 1, :].rearrange("p (d g o) -> p d g o", d=2, o=1), ["eight"], ["Acplx"])
            ET = [sb("ET%d" % u_, [128, 2, 2, 32, 8], F32, ph) for u_ in range(3)]
            for u_ in range(3):
                etab("et%d" % u_, KM[:, u_, :, :].unsqueeze(2).to_broadcast([128, 2, 32, 8]), 8,
                     ET[u_][:, 0], ET[u_][:, 1], ["KM"], ["ET%d" % u_])

            LR = LRI[:, 0, :]
            LI = LRI[:, 1, :]
            e1r = E1[:, 0].rearrange("p d g o -> p (d g o)")
            e1i = E1[:, 1].rearrange("p d g o -> p (d g o)")
            cf = sb("cf", [128, 6, 64], F32, ph)
            seq_ops = [
                lambda e: e.tensor_scalar(out=cf[:, 0, :], in0=e1r, scalar1=-1.0, scalar2=None, op0=ALU.add),
                lambda e: e.tensor_tensor(out=cf[:, 1, :], in0=LR, in1=LR, op=ALU.mult),
                lambda e: e.tensor_tensor(out=cf[:, 2, :], in0=LI, in1=LI, op=ALU.mult),
                lambda e: e.tensor_tensor(out=cf[:, 1, :], in0=cf[:, 1, :], in1=cf[:, 2, :], op=ALU.add),
                lambda e: e.reciprocal(out=cf[:, 1, :], in_=cf[:, 1, :]),
                lambda e: e.tensor_tensor(out=cf[:, 2, :], in0=cf[:, 0, :], in1=LR, op=ALU.mult),
                lambda e: e.tensor_tensor(out=cf[:, 3, :], in0=e1i, in1=LI, op=ALU.mult),
                lambda e: e.tensor_tensor(out=cf[:, 2, :], in0=cf[:, 2, :], in1=cf[:, 3, :], op=ALU.add),
                lambda e: e.tensor_tensor(out=cf[:, 4, :], in0=cf[:, 2, :], in1=cf[:, 1, :], op=ALU.mult),
                lambda e: e.tensor_tensor(out=cf[:, 2, :], in0=e1i, in1=LR, op=ALU.mult),
                lambda e: e.tensor_tensor(out=cf[:, 3, :], in0=cf[:, 0, :], in1=LI, op=ALU.mult),
                lambda e: e.tensor_tensor(out=cf[:, 2, :], in0=cf[:, 2, :], in1=cf[:, 3, :], op=ALU.subtract),
                lambda e: e.tensor_tensor(out=cf[:, 5, :], in0=cf[:, 2, :], in1=cf[:, 1, :], op=ALU.mult),
            ]
            for f_ in seq_ops:
                P.add("dve", f_, reads=["E1", "LRI", "cf"], writes=["cf"])
            Braw = sb("Braw", [128, 2, 64, 16], F32, ph)
            for i, src_ in enumerate((ssm_b_re, ssm_b_im)):
                for d_ in range(2):
                    v = src_[d_].rearrange("(g2 gp) p m -> gp p g2 m", gp=2)
                    for gp in range(2):
                        P.add("sp", lambda e, i=i, d_=d_, gp=gp, v=v: e.dma_start(
                            out=Braw[gp * 64:(gp + 1) * 64, i, d_ * 32:(d_ + 1) * 32, :], in_=v[gp]),
                            writes=[("Braw", i, d_, gp)], group="Braw")
            bbar = sb("bbar", [128, 2, 64, 16], F32, ph)
            tA = sb("s0tA", [128, 64, 16], F32, ph)
            cre_b = cf[:, 4, :].unsqueeze(2).to_broadcast([128, 64, 16])
            cim_b = cf[:, 5, :].unsqueeze(2).to_broadcast([128, 64, 16])
            P.add("dve", lambda e: e.tensor_tensor(out=bbar[:, 0], in0=Braw[:, 0], in1=cre_b, op=ALU.mult), reads=["Braw", "cf"], writes=[("bbar", 0)])
            P.add("dve", lambda e: e.tensor_tensor(out=tA[:], in0=Braw[:, 1], in1=cim_b, op=ALU.mult), reads=["Braw", "cf"], writes=["s0tA"])
            P.add("dve", lambda e: e.tensor_tensor(out=bbar[:, 0], in0=bbar[:, 0], in1=tA[:], op=ALU.subtract), reads=["s0tA", ("bbar", 0)], writes=[("bbar", 0)])
            P.add("dve", lambda e: e.tensor_tensor(out=bbar[:, 1], in0=Braw[:, 1], in1=cre_b, op=ALU.mult), reads=["Braw", "cf"], writes=[("bbar", 1)])
            P.add("dve", lambda e: e.tensor_tensor(out=tA[:], in0=Braw[:, 0], in1=cim_b, op=ALU.mult), reads=["Braw", "cf", ("bbar", 0)], writes=["s0tA"])
            P.add("dve", lambda e: e.tensor_tensor(out=bbar[:, 1], in0=bbar[:, 1], in1=tA[:], op=ALU.add), reads=["s0tA", ("bbar", 1)], writes=[("bbar", 1)])

            CT = sb("CT", [128, 2, 64, 16], F32, ph)
            craw = [sb("craw%d" % i, [128, 128], F32, ph) for i in range(2)]
            nb = 0
            for i, src_ in enumerate((ssm_c_re, ssm_c_im)):
                for d_ in range(2):
                    for q in range(4):
                        b_ = nb % 2
                        nb += 1
                        for g2l in range(8):
                            g2 = q * 8 + g2l
                            P.add("sp", lambda e, b_=b_, g2l=g2l, g2=g2, d_=d_, src_=src_: e.dma_start(
                                out=craw[b_][g2l * 16:(g2l + 1) * 16, :].rearrange("n (gp p) -> n gp p", gp=2),
                                in_=src_[d_, 2 * g2:2 * g2 + 2, :, :].rearrange("gp n p -> n gp p")),
                                writes=[("craw", b_, g2l)], group="craw%d" % b_)
                        P.add("pe", lambda e, b_=b_: e.transpose(out=ptq[:, 3, :], in_=craw[b_][:], identity=ident_f[:]),
                              reads=[("craw", b_), "ident_f"], writes=["s0pt"])
                        P.add("dve", lambda e, i=i, d_=d_, q=q: e.tensor_copy(
                            CT[:, i, d_ * 32 + q * 8:d_ * 32 + (q + 1) * 8, :], ptq[:, 3, :].rearrange("p (a n) -> p a n", n=16)),
                            reads=["s0pt"], writes=[("CT", i, d_, q)])

            mk = sb("mk", [128, 2, 128], F32, ph)
            mi = sb("mki", [128, 2, 128], I32, ph)
            mf = sb("mkf", [128, 2, 128], F32, ph)
            P.add("pool", lambda e: e.iota(mi[:, 0, :], [[1, 128]], base=0, channel_multiplier=0), writes=[("mki", 0)])
            P.add("pool", lambda e: e.iota(mi[:, 1, :], [[0, 128]], base=0, channel_multiplier=1), writes=[("mki", 1)])
            P.add("dve", lambda e: e.tensor_single_scalar(mi[:], mi[:], 4, ALU.arith_shift_right), reads=["mki"], writes=["mki"])
            P.add("dve", lambda e: e.tensor_copy(mf[:], mi[:]), reads=["mki"], writes=["mkf"])
            P.add("dve", lambda e: e.tensor_tensor(out=mk[:, 0, :], in0=mf[:, 0, :], in1=mf[:, 1, :], op=ALU.is_ge), reads=["mkf"], writes=[("mk", 0)])
            P.add("dve", lambda e: e.tensor_tensor(out=mk[:, 1, :], in0=mf[:, 1, :], in1=mf[:, 0, :], op=ALU.is_ge), reads=["mkf"], writes=[("mk", 1)])


            oR = sb("oR", [128, 32, 8, 16], F32, ph)
            oI = sb("oI", [128, 32, 8, 16], F32, ph)
            o2R = sb("o2R", [128, 32, 8, 16], F32, ph)
            o2I = sb("o2I", [128, 32, 8, 16], F32, ph)
            t1 = sb("s0t1", [128, 32, 8, 16], F32, ph)
            stg = sb("s0stg", [128, 4, 128], BF16, ph)
            stg2 = [sb("s0stg2", [128, 32, 128], BF16, ph)] * 2
            pT = ps("s0pT", [128, 4, 128], F32, ph)
            pT2 = ps("s0pT2", [128, 4, 128], F32, ph)

            def couter(u_, d_, Br, Bi, dR, dI, neg_im, rk, tag):
                shp = [128, 32, 8, 16]
                Er = ET[u_][:, 0, d_].unsqueeze(3).to_broadcast(shp)
                Ei = ET[u_][:, 1, d_].unsqueeze(3).to_broadcast(shp)
                Brb = Br.unsqueeze(2).to_broadcast(shp)
                Bib = Bi.unsqueeze(2).to_broadcast(shp)
                rk = rk + ["ET%d" % u_]
                P.add("dve", lambda e: e.tensor_tensor(out=dR[:], in0=Er, in1=Brb, op=ALU.mult), reads=rk, writes=[tag + "R"])
                P.add("dve", lambda e: e.tensor_tensor(out=t1[:], in0=Ei, in1=Bib, op=ALU.mult), reads=rk, writes=["s0t1"])
                P.add("dve", lambda e: e.tensor_tensor(out=dR[:], in0=dR[:], in1=t1[:], op=ALU.subtract), reads=[tag + "R", "s0t1"], writes=[tag + "R"])
                P.add("dve", lambda e: e.tensor_tensor(out=dI[:], in0=Er, in1=Bib, op=ALU.mult), reads=rk, writes=[tag + "I"])
                P.add("dve", lambda e: e.tensor_tensor(out=t1[:], in0=Ei, in1=Brb, op=ALU.mult), reads=rk + [tag + "R"], writes=["s0t1"])
                if neg_im:
                    P.add("dve", lambda e: e.scalar_tensor_tensor(out=dI[:], in0=dI[:], scalar=-1.0, in1=t1[:], op0=ALU.mult, op1=ALU.subtract),
                          reads=[tag + "I", "s0t1"], writes=[tag + "I"])
                else:
                    P.add("dve", lambda e: e.tensor_tensor(out=dI[:], in0=dI[:], in1=t1[:], op=ALU.add), reads=[tag + "I", "s0t1"], writes=[tag + "I"])

            for d_ in range(2):
                gs = slice(d_ * 32, (d_ + 1) * 32)
                couter(0, d_, bbar[:, 0, gs, :], bbar[:, 1, gs, :], oR, oI, False, ["bbar"], "o")
                for part, src_t, skey in ((0, oR, "oR"), (1, oI, "oI")):
                    for q in range(8):
                        def trw(e, src_t=src_t, q=q):
                            ins = None
                            for j in range(4):
                                ins = e.transpose(out=pT[:, j, :], in_=src_t[:, q * 4 + j].rearrange("p s m -> p (s m)"), identity=ident_f[:])
                            return ins
                        P.add("pe", trw, reads=[skey, "ident_f"], writes=["s0pT"])
                        P.add("act", lambda e: e.activation(out=stg[:], in_=pT[:], func=AF.Copy), reads=["s0pT"], writes=["s0stg"])
                        P.add("sp", lambda e, d_=d_, q=q, part=part: e.dma_start(
                            out=scrW1[d_, q * 4:(q + 1) * 4, part].rearrange("g r c -> r g c"), in_=stg[:]),
                            reads=["s0stg"], writes=["scrW1"], group="s0st")

                couter(1, d_, bbar[:, 0, gs, :], bbar[:, 1, gs, :], oR, oI, False, ["bbar"], "o")
                couter(2, d_, CT[:, 0, gs, :], CT[:, 1, gs, :], o2R, o2I, True, ["CT"], "o2")
                for part, src_t, skey in ((0, o2R, "o2R"), (1, o2I, "o2I")):
                    P.add("act", lambda e, part=part, src_t=src_t: e.activation(
                        out=stg2[part][:], in_=src_t[:].rearrange("p g t n -> p g (t n)"), func=AF.Copy),
                        reads=[skey], writes=["s0stg2"])
                    P.add("sp", lambda e, d_=d_, part=part: e.dma_start(
                        out=scrW2[d_, :, part].rearrange("g r c -> r g c"), in_=stg2[part][:]),
                        reads=["s0stg2"], writes=["scrW2"], group="s0st2")

                for q in range(8):
                    for gp in range(2):
                        pTx = pT if gp == 0 else pT2
                        pkey = "s0pT" if gp == 0 else "s0pT2"
                        rs = slice(gp * 64, (gp + 1) * 64)

                        def mmT(e, q=q, gp=gp, pTx=pTx, rs=rs):
                            ins = None
                            for j in range(4):
                                g2 = q * 4 + j
                                e.matmul(pTx[:, j, :], lhsT=oR[rs, g2].rearrange("p s m -> p (s m)"),
                                         rhs=o2R[rs, g2].rearrange("p t n -> p (t n)"), start=True, stop=False)
                                ins = e.matmul(pTx[:, j, :], lhsT=oI[rs, g2].rearrange("p s m -> p (s m)"),
                                               rhs=o2I[rs, g2].rearrange("p t n -> p (t n)"), start=False, stop=True)
                            return ins
                        P.add("pe", mmT, reads=["oR", "oI", "o2R", "o2I"], writes=[pkey])
                        P.add("dve", lambda e, d_=d_, pTx=pTx: e.tensor_tensor(
                            out=stg[:], in0=pTx[:], in1=mk[:, d_:d_ + 1, :].to_broadcast([128, 4, 128]), op=ALU.mult),
                            reads=[pkey, "mk"], writes=["s0stg"])
                        P.add("sp", lambda e, d_=d_, q=q, gp=gp: e.dma_start(
                            out=scrT[d_, q * 8:(q + 1) * 8].rearrange("(j gp) r c -> gp r j c", gp=2)[gp], in_=stg[:]),
                            reads=["s0stg"], writes=["scrT"], group="s0st")
            P.add = _real_add
            na, ns = len(REC_A), len(REC_S)
            ia = isx = 0
            while ia < na or isx < ns:
                if isx >= ns or (ia < na and ia * ns <= isx * na):
                    a_, k_ = REC_A[ia]; ia += 1
                else:
                    a_, k_ = REC_S[isx]; isx += 1
                P.add(*a_, **k_)
            P.barrier()
        ada_stack.close()
        if debug:
            d_A = dout("d_A", [128, 128])
            P.add("sp", lambda e: e.dma_start(out=d_A, in_=Acplx[:].rearrange("p a b -> p (a b)")), reads=["Acplx"],
                  writes=["d_A"], group="dbgA")

        if debug:
            d_mod = dout("d_mod", [128, 192])
            P.add("sp", lambda e: e.dma_start(out=d_mod, in_=modT[:].rearrange("p a b -> p (a b)")), reads=["modT"],
                  writes=["d_mod"], group="dbg0")

        P.add("dve", lambda e: e.scalar_tensor_tensor(out=scale1[:], in0=modT[:, 16:32, :], scalar=1.0,
                                                      in1=colA[:, 32:48].unsqueeze(2).to_broadcast([128, 16, 2]),
                                                      op0=ALU.add, op1=ALU.mult),
              reads=["modT", "colA"], writes=["scale1"])

        with ExitStack() as ph:
            w_u = sb("w_u", [128, 16, 1024], BF16, ph)
            ustage = [sb("ustage%d" % i, [128, 512], BF16, ph) for i in range(2)]
            w_in_v = w_in.rearrange("(kt p) c -> p kt c", p=128)
            for kt in range(16):
                P.add("pool", lambda e, kt=kt: e.dma_start(out=w_u[:, kt, :], in_=w_in_v[:, kt, 0:1024]),
                      writes=[("w_u", kt)], group="w_u")
            xt = [sb("xt%d" % i, [128, D], F32, ph) for i in range(2)]
            xn = [sb("xn%d" % i, [128, 4, D], BF16, ph) for i in range(2)]
            hxT = [sb("hxT%d" % i, [128, 16, 512], BF16, ph) for i in range(2)]
            ptr = [ps("ptr%d" % i, [128, 512], BF16, ph) for i in range(2)]
            pmm = [ps("pmm%d" % i, [128, 512], F32, ph) for i in range(6)]
            groups = [("x", 1024, 4, 0, 1024, 0), ("x", 1536, 4, 0, 1536, 1), ("c", 0, 2, 1, 2048, 0),
                      ("x", 0, 4, 0, 0, 1), ("x", 512, 4, 0, 512, 0)]
            nxc = [0]

            def stA1(gi):
                (src, r0, nt, mj, soff, xb) = groups[gi]
                xnb = xn[gi % 2]
                for t in range(nt):
                    nx = nxc[0]
                    b = nx % 2
                    tix = nx % 24
                    nxc[0] += 1
                    srcap = (xs if src == "x" else ctxs)[r0 + t * 128:r0 + (t + 1) * 128, :]
                    P.add("sp", lambda e, b=b, srcap=srcap: e.dma_start(out=xt[b][:], in_=srcap),
                          writes=[("xt", b)], group="xt%d" % b)
                    P.add("act", lambda e, b=b, tix=tix, t=t: e.activation(out=xnb[:, t, :], in_=xt[b][:], func=AF.Square,
                                                                      accum_out=ss[:, tix:tix + 1]),
                          reads=[("xt", b)], writes=[("xn", gi % 2, t), ("ss", tix)])
                    P.add("dve", lambda e, tix=tix: e.tensor_scalar(out=ss[:, tix:tix + 1], in0=ss[:, tix:tix + 1],
                                                                    scalar1=1.0 / D, scalar2=EPS, op0=ALU.mult, op1=ALU.add),
                          reads=[("ss", tix)], writes=[("ss", tix)])
                    P.add("act", lambda e, tix=tix: e.activation(out=ss[:, tix:tix + 1], in_=ss[:, tix:tix + 1], func=AF.Sqrt),
                          reads=[("ss", tix)], writes=[("ss", tix)])
                    P.add("dve", lambda e, tix=tix: e.reciprocal(out=ss[:, tix:tix + 1], in_=ss[:, tix:tix + 1]),
                          reads=[("ss", tix)], writes=[("ss", tix)])
                    P.add("act", lambda e, b=b, tix=tix, t=t: e.activation(
                        out=xnb[:, t, :], in_=xt[b][:], func=AF.Copy, scale=ss[:, tix:tix + 1]),
                        reads=[("xt", b), ("ss", tix)], writes=[("xn", gi % 2, t)])

            def stA2(gi):
                (src, r0, nt, mj, soff, xb) = groups[gi]
                xnb = xn[gi % 2]
                ntok = nt * 128
                for ft in range(16):
                    pb = ft % 2

                    def tr(e, ft=ft, nt=nt, pb=pb):
                        ins = None
                        for t in range(nt):
                            ins = e.transpose(out=ptr[pb][:, t * 128:(t + 1) * 128],
                                              in_=xnb[:, t, ft * 128:(ft + 1) * 128], identity=ident_b[:])
                        return ins
                    P.add("pe", tr, reads=[("xn", gi % 2), "ident_b"], writes=["ptr%d" % pb])
                    if ft % 2 == 0:
                        P.add("dve", lambda e, xb=xb, ft=ft, pb=pb, ntok=ntok, mj=mj: e.tensor_scalar(
                            out=hxT[xb][:, ft, 0:ntok], in0=ptr[pb][:, 0:ntok], scalar1=scale1[:, ft, mj:mj + 1],
                            scalar2=modT[:, ft, mj:mj + 1], op0=ALU.mult, op1=ALU.add),
                            reads=["ptr%d" % pb, "scale1", "modT"], writes=[("hxT", xb, ft)])
                    else:
                        P.add("act", lambda e, xb=xb, ft=ft, pb=pb, ntok=ntok, mj=mj: e.activation(
                            out=hxT[xb][:, ft, 0:ntok], in_=ptr[pb][:, 0:ntok], func=AF.Identity,
                            scale=scale1[:, ft, mj:mj + 1], bias=modT[:, ft, mj:mj + 1]),
                            reads=["ptr%d" % pb, "scale1", "modT"], writes=[("hxT", xb, ft)])

            def stB(gi):
                (src, r0, nt, mj, soff, xb) = groups[gi]
                ntok = nt * 128
                for ct in range(8):
                    pq = ct % 4

                    def mmu(e, xb=xb, ct=ct, ntok=ntok, pq=pq):
                        ins = None
                        for kt in range(16):
                            ins = e.matmul(pmm[pq][:, 0:ntok], lhsT=w_u[:, kt, ct * 128:(ct + 1) * 128],
                                           rhs=hxT[xb][:, kt, 0:ntok], start=(kt == 0), stop=(kt == 15))
                        return ins
                    P.add("pe", mmu, reads=["w_u", ("hxT", xb)], writes=["pmm%d" % pq])
                    nj = ntok // 8
                    if soff < NOWN:
                        dst = uTown[:, ct, :].rearrange("p (s j) -> p s j", s=8)[:, :, soff // 8:soff // 8 + nj]
                        wk = [("uT", ct, soff)]
                    else:
                        sg = ct % 2
                        dst = ustage[sg][:, 0:ntok].rearrange("p (s j) -> p s j", s=8)
                        wk = [("ustage", sg)]
                    srcv = pmm[pq][:, 0:ntok].rearrange("p (j s) -> p s j", s=8)
                    if ct % 2 == 0:
                        P.add("dve", lambda e, dst=dst, srcv=srcv: e.tensor_copy(dst, srcv),
                              reads=["pmm%d" % pq], writes=wk)
                    else:
                        P.add("act", lambda e, dst=dst, srcv=srcv: e.activation(out=dst, in_=srcv, func=AF.Copy),
                              reads=["pmm%d" % pq], writes=wk)
                    if soff >= NOWN:
                        j0r = (soff - NOWN) // 8
                        P.add("sp", lambda e, ct=ct, sg=sg, ntok=ntok, j0r=j0r, nj=nj: e.dma_start(
                            out=scrU[ct].rearrange("p (s j) -> p s j", s=8)[:, :, j0r:j0r + nj],
                            in_=ustage[sg][:, 0:ntok].rearrange("p (s j) -> p s j", s=8)),
                            reads=[("ustage", sg)], writes=["scrU"], group="ustage%d" % sg)

            stA1(0)
            stA2(0)
            for gi in range(5):
                if gi + 1 < 5:
                    stA1(gi + 1)
                stB(gi)
                if gi + 1 < 5:
                    stA2(gi + 1)
            cvs = ph.enter_context(ExitStack())
            wch = [[sb("wch%d_%d" % (s_, i), [128, 16, 128], BF16, cvs) for i in range(3)] for s_ in range(2)]
            zc = sb("zc", [128, 512], F32, cvs)
            zz = sb("zz", [128, 512], F32, cvs)
            yy = sb("yy", [128, 512], F32, cvs)
            for ct in range(8):
                s_ = ct % 2
                for i in range(3):
                    P.add("pool", lambda e, s_=s_, i=i, ct=ct: e.dma_start(
                        out=wch[s_][i][:], in_=w_in_v[:, :, 1024 * (i + 1) + ct * 128:1024 * (i + 1) + (ct + 1) * 128]),
                        writes=[("wch", s_, i)], group="wch%d_%d" % (s_, i))
                for og, (xb, soff) in enumerate([(1, 0), (0, 512)]):
                    pset = 3 * ((ct * 2 + og) % 2)
                    if True:
                        for i in range(3):
                            def mmb(e, xb=xb, i=i, s_=s_, pset=pset):
                                ins = None
                                for kt in range(16):
                                    ins = e.matmul(pmm[pset + i][:, :], lhsT=wch[s_][i][:, kt, :],
                                                   rhs=hxT[xb][:, kt, :], start=(kt == 0), stop=(kt == 15))
                                return ins
                            P.add("pe", mmb, reads=[("wch", s_, i), ("hxT", xb)], writes=["pmm%d" % (pset + i)])
                        P.add("act", lambda e, pset=pset: e.activation(out=zc[:], in_=pmm[pset + 1][:], func=AF.Copy),
                              reads=["pmm%d" % (pset + 1)], writes=["zc"])
                        P.add("dve", lambda e, pset=pset: e.tensor_tensor(out=zz[:], in0=zc[:], in1=pmm[pset + 2][:], op=ALU.mult),
                              reads=["zc", "pmm%d" % (pset + 2)], writes=["zz"])
                        P.add("dve", lambda e, ct=ct: e.tensor_scalar(
                            out=yy[:], in0=zz[:], scalar1=colB[:, 16 + ct:17 + ct], scalar2=colB[:, 32 + ct:33 + ct],
                            op0=ALU.mult, op1=ALU.add), reads=["zz", "colB"], writes=["yy"])
                        yv = yy[:].rearrange("p (r w) -> p r w", w=64)
                        zv = zz[:].rearrange("p (r w) -> p r w", w=64)
                        P.add("dve", lambda e, ct=ct, yv=yv, zv=zv: e.scalar_tensor_tensor(
                            out=yv[:, :, 1:64], in0=zv[:, :, 0:63], scalar=colB[:, 8 + ct:9 + ct], in1=yv[:, :, 1:64],
                            op0=ALU.mult, op1=ALU.add), reads=["zz", "yy", "colB"], writes=["yy"])
                        P.add("dve", lambda e, ct=ct, yv=yv, zv=zv: e.scalar_tensor_tensor(
                            out=yv[:, :, 0:63], in0=zv[:, :, 1:64], scalar=colB[:, 24 + ct:25 + ct], in1=yv[:, :, 0:63],
                            op0=ALU.mult, op1=ALU.add), reads=["zz", "yy", "colB"], writes=["yy"])
                        P.add("dve", lambda e, ct=ct, soff=soff, pset=pset: e.tensor_tensor(
                            out=convT[:, ct, soff:soff + 512], in0=yy[:], in1=pmm[pset][:], op=ALU.mult),
                            reads=["yy", "pmm%d" % pset], writes=[("convT", ct, soff)])
            P.barrier()
        if debug:
            d_uT = dout("d_uT", [128, 8 * NOWN], BF16)
            d_convT = dout("d_convT", [128, 8 * NOWN], BF16)
            P.add("sp", lambda e: e.dma_start(out=d_uT, in_=uTown[:].rearrange("p a b -> p (a b)")), reads=["uT"],
                  writes=["d_uT"], group="dbg1")
            P.add("sp", lambda e: e.dma_start(out=d_convT, in_=convT[:].rearrange("p a b -> p (a b)")), reads=["convT"],
                  writes=["d_convT"], group="dbg2")

        Z = sb("Z", [128, 8, 8, 128], BF16, mixer)
        P.add("pool", lambda e: e.memset(Z[:], 0.0), writes=["Z"])
        for a_ in range(8):
            for b_ in range(8):
                P.add("dve" if (a_ + b_) % 2 else "pool", lambda e, a_=a_, b_=b_: e.tensor_single_scalar(
                    Z[:, a_, b_, 16 * b_:16 * b_ + 16], iotf[:, 16 * b_:16 * b_ + 16], float(16 * (b_ - a_)), ALU.is_equal),
                    reads=["iotf"], writes=[("Z", a_, b_)])
        mix = mixer.enter_context(ExitStack())
        gT = sb("gT", [128, 8, NOWN], BF16, mix)
        ssm = mix.enter_context(ExitStack())
        U = sb("U", [128, 64, 128], BF16, ssm)
        Pt = sb("Pt", [128, 2, 2, 32, 288], BF16, ssm)
        s12 = ssm.enter_context(ExitStack())
        Ur = sb("Ur", [128, 64, 160], BF16, s12)
        with ExitStack() as ph:
            pU = [ps("pU%d" % i, [128, 288], F32, ph) for i in range(3)]
            ucat = [sb("ucat%d" % i, [128, NSEQ], BF16, ph) for i in range(2)]
            for g in range(64):
                ct, gl = g // 8, g % 8
                pb = g % 3
                if gl == 0:
                    P.add("sp", lambda e, ct=ct: e.dma_start(
                        out=ucat[ct % 2][:].rearrange("p (s j) -> p s j", s=8)[:, :, 128:288],
                        in_=scrU[ct].rearrange("p (s j) -> p s j", s=8)), reads=["scrU"],
                        writes=[("ucat", ct % 2, 1)], group="ucat%d" % (ct % 2))
                    P.add("pool", lambda e, ct=ct: e.tensor_copy(
                        ucat[ct % 2][:].rearrange("p (s j) -> p s j", s=8)[:, :, 0:128],
                        uTown[:, ct, :].rearrange("p (s j) -> p s j", s=8)), reads=["uT"],
                        writes=[("ucat", ct % 2, 0)])

                def shf(e, ct=ct, gl=gl, pb=pb):
                    ins = None
                    for s_ in range(8):
                        src = ucat[ct % 2][:, s_ * 288:(s_ + 1) * 288]
                        ins = e.matmul(pU[pb][:, 0:288], lhsT=Z[:, gl, s_, :], rhs=src, start=(s_ == 0), stop=(s_ == 7))
                    return ins
                P.add("pe", shf, reads=["Z", ("ucat", ct % 2)], writes=["pU%d" % pb])
                P.add("dve", lambda e, g=g, pb=pb: e.tensor_copy(U[:, g, :], pU[pb][:, 0:128]), reads=["pU%d" % pb], writes=[("U", g)])
                P.add("act", lambda e, g=g, pb=pb: e.activation(out=Ur[:, g, :], in_=pU[pb][:, 128:288], func=AF.Copy),
                      reads=["pU%d" % pb], writes=[("U", g)])
            P.barrier()
        with ExitStack() as ph:
            w1c = [sb("w1c%d" % i, [128, 8, 2, 128], BF16, ph) for i in range(2)]
            pP = [ps("pP%d" % i, [128, 288], F32, ph) for i in range(3)]
            nn = 0
            for d_ in range(2):
                for q in range(4):
                    wb_ = (d_ * 4 + q) % 2
                    P.add("sp", lambda e, d_=d_, q=q, wb_=wb_: e.dma_start(
                        out=w1c[wb_][:], in_=scrW1[d_, q * 8:(q + 1) * 8].rearrange("g part r c -> r g part c")),
                        reads=["scrW1"], writes=[("w1c", wb_)], group="w1c%d" % wb_)
                    for g2l in range(8):
                        g2 = q * 8 + g2l
                        for part in range(2):
                            pb = nn % 3
                            nn += 1

                            def mmP(e, d_=d_, g2=g2, g2l=g2l, part=part, pb=pb, wb_=wb_):
                                ins = None
                                for gp in range(2):
                                    g = 2 * g2 + gp
                                    lw = w1c[wb_][:, g2l, part, gp * 64:(gp + 1) * 64]
                                    rows = slice(gp * 64, (gp + 1) * 64)
                                    if d_ == 0:
                                        e.matmul(pP[pb][rows, 0:32], lhsT=lw, rhs=Ur[:, g, 128:160], start=True, stop=True)
                                        ins = e.matmul(pP[pb][rows, 32:160], lhsT=lw, rhs=U[:, g, 0:128], start=True, stop=True)
                                    else:
                                        e.matmul(pP[pb][rows, 0:128], lhsT=lw, rhs=U[:, g, 0:128], start=True, stop=True)
                                        ins = e.matmul(pP[pb][rows, 128:288], lhsT=lw, rhs=Ur[:, g, 0:160], start=True, stop=True)
                                return ins
                            P.add("pe", mmP, reads=[("w1c", wb_), "U"], writes=["pP%d" % pb])
                            ncol = 160 if d_ == 0 else 288
                            if nn % 2 == 0:
                                P.add("dve", lambda e, d_=d_, g2=g2, part=part, pb=pb, ncol=ncol: e.tensor_copy(
                                    Pt[:, part, d_, g2, 0:ncol], pP[pb][:, 0:ncol]), reads=["pP%d" % pb], writes=[("Pt", part, d_, g2)])
                            else:
                                P.add("act", lambda e, d_=d_, g2=g2, part=part, pb=pb, ncol=ncol: e.activation(
                                    out=Pt[:, part, d_, g2, 0:ncol], in_=pP[pb][:, 0:ncol], func=AF.Copy),
                                    reads=["pP%d" % pb], writes=[("Pt", part, d_, g2)])
            P.barrier()
        s12.close()
        with ExitStack() as ph:
            St = [sb("St%d" % i, [128, 4, 64], F32, ph) for i in range(2)]
            C4 = sb("C4", [128, 4, 64], F32, ph)
            rt1 = sb("rt1", [128, 4, 64], F32, ph)
            rt2 = sb("rt2", [128, 2, 64], F32, ph)
            P.add("dve", lambda e: e.memset(St[0][:], 0.0), writes=["St0"])
            P.add("dve", lambda e: e.tensor_copy(C4[:, 0:2, :], Acplx[:, 0:1, :].to_broadcast([128, 2, 64])), reads=["Acplx"], writes=["C4"])
            P.add("dve", lambda e: e.tensor_scalar(out=C4[:, 2, :], in0=Acplx[:, 1, :], scalar1=-1.0, scalar2=None, op0=ALU.mult),
                  reads=["Acplx", "C4"], writes=["C4"])
            P.add("dve", lambda e: e.tensor_copy(C4[:, 3, :], Acplx[:, 1, :]), reads=["Acplx", "C4"], writes=["C4"])
            P.add("dve", lambda e: e.memset(St[1][:], 0.0), writes=["St1"])
            AR = Acplx[:, 0, 32:64]
            AI = Acplx[:, 1, 32:64]
            E16 = sb("E16", [128, 2, 32, 16], F32, ph)
            Am = sb("Am", [128, 2, 2, 32], F32, ph)
            ct_ = sb("cmt", [128, 2, 32, 8], F32, ph)

            def cmul(o_re, o_im, a_re, a_im, b_re, b_im, shp, rk, wk):
                t1 = ct_[:, 0].rearrange("p g k -> p (g k)")[:, 0:shp[1] * (shp[2] if len(shp) > 2 else 1)]
                t2 = ct_[:, 1].rearrange("p g k -> p (g k)")[:, 0:shp[1] * (shp[2] if len(shp) > 2 else 1)]
                if len(shp) > 2:
                    t1 = t1.rearrange("p (g k) -> p g k", k=shp[2])
                    t2 = t2.rearrange("p (g k) -> p g k", k=shp[2])
                seq = [
                    lambda e: e.tensor_tensor(out=t1, in0=a_re, in1=b_re, op=ALU.mult),
                    lambda e: e.tensor_tensor(out=t2, in0=a_im, in1=b_im, op=ALU.mult),
                    lambda e: e.tensor_tensor(out=o_re, in0=t1, in1=t2, op=ALU.subtract),
                    lambda e: e.tensor_tensor(out=t1, in0=a_re, in1=b_im, op=ALU.mult),
                    lambda e: e.tensor_tensor(out=t2, in0=a_im, in1=b_re, op=ALU.mult),
                    lambda e: e.tensor_tensor(out=o_im, in0=t1, in1=t2, op=ALU.add),
                ]
                for f_ in seq:
                    P.add("dve", f_, reads=["cmt"] + rk, writes=["cmt"] + wk)

            P.add("dve", lambda e: e.memset(E16[:, 0, :, 0:1], 1.0), writes=["E16"])
            P.add("dve", lambda e: e.memset(E16[:, 1, :, 0:1], 0.0), reads=["E16"], writes=["E16"])
            P.add("dve", lambda e: e.tensor_copy(E16[:, :, :, 1], Acplx[:, :, 32:64]), reads=["Acplx", "E16"], writes=["E16"])
            P.add("dve", lambda e: e.tensor_copy(Am[:, 0], Acplx[:, :, 32:64]), reads=["Acplx"], writes=["Am"])
            cur_a = 0
            m = 1
            while m < 16:
                src_, dst_ = Am[:, cur_a], Am[:, 1 - cur_a]
                if m >= 2:
                    pass
                m2 = m * 2 if m > 1 else 2
                if m == 1:
                    cmul(dst_[:, 0], dst_[:, 1], src_[:, 0], src_[:, 1], src_[:, 0], src_[:, 1], [128, 32], ["Am"], ["Am"])
                    cur_a = 1 - cur_a
                    mm_ = 2
                    am = Am[:, cur_a]
                    cmul(E16[:, 0, :, 2:4], E16[:, 1, :, 2:4], E16[:, 0, :, 0:2], E16[:, 1, :, 0:2],
                         am[:, 0].unsqueeze(2).to_broadcast([128, 32, 2]), am[:, 1].unsqueeze(2).to_broadcast([128, 32, 2]),
                         [128, 32, 2], ["Am", "E16"], ["E16"])
                    m = 2
                    continue
                src_, dst_ = Am[:, cur_a], Am[:, 1 - cur_a]
                cmul(dst_[:, 0], dst_[:, 1], src_[:, 0], src_[:, 1], src_[:, 0], src_[:, 1], [128, 32], ["Am"], ["Am"])
                cur_a = 1 - cur_a
                am = Am[:, cur_a]
                w_ = 2 * m
                if w_ < 16:
                    cmul(E16[:, 0, :, w_:2 * w_], E16[:, 1, :, w_:2 * w_], E16[:, 0, :, 0:w_], E16[:, 1, :, 0:w_],
                         am[:, 0].unsqueeze(2).to_broadcast([128, 32, w_]), am[:, 1].unsqueeze(2).to_broadcast([128, 32, w_]),
                         [128, 32, w_], ["Am", "E16"], ["E16"])
                m = w_
            A16 = Am[:, cur_a]
            ptmp = sb("ptmp", [128, 32, 10, 16], F32, ph)
            pr = sb("pr", [128, 4, 32, 10], F32, ph)
            Sb = sb("Sb", [128, 2, 32, 10], F32, ph)
            for idx, (pp, ep) in enumerate([(0, 0), (1, 1), (0, 1), (1, 0)]):
                Pv = Pt[:, pp, 1, :, 128:288].rearrange("p g (b k) -> p g b k", k=16)
                Eb = E16[:, ep].unsqueeze(2).to_broadcast([128, 32, 10, 16])
                P.add("dve", lambda e, Pv=Pv, Eb=Eb: e.tensor_tensor(out=ptmp[:], in0=Pv, in1=Eb, op=ALU.mult),
                      reads=["Pt", "E16"], writes=["ptmp"])
                P.add("dve", lambda e, idx=idx: e.tensor_reduce(out=pr[:, idx], in_=ptmp[:], axis=AX.X, op=ALU.add),
                      reads=["ptmp"], writes=[("pr", idx)])
            P.add("dve", lambda e: e.tensor_tensor(out=Sb[:, 0], in0=pr[:, 0], in1=pr[:, 1], op=ALU.subtract), reads=["pr"], writes=[("Sb", 0)])
            P.add("dve", lambda e: e.tensor_tensor(out=Sb[:, 1], in0=pr[:, 2], in1=pr[:, 3], op=ALU.add), reads=["pr"], writes=[("Sb", 1)])
            Hh = sb("Hh", [128, 2, 2, 32], F32, ph)
            P.add("dve", lambda e: e.tensor_copy(Hh[:, 0], Sb[:, :, :, 9]), reads=["Sb"], writes=["Hh"])
            hc_ = 0
            for b_ in range(8, -1, -1):
                hs, hd = Hh[:, hc_], Hh[:, 1 - hc_]
                cmul(hd[:, 0], hd[:, 1], A16[:, 0], A16[:, 1], hs[:, 0], hs[:, 1], [128, 32], ["Am", "Hh"], ["Hh"])
                P.add("dve", lambda e, hd=hd, b_=b_: e.tensor_tensor(out=hd, in0=hd, in1=Sb[:, :, :, b_], op=ALU.add),
                      reads=["Hh", "Sb"], writes=["Hh"])
                hc_ = 1 - hc_
            Hf = Hh[:, hc_]
            for slot, part in ((0, 0), (1, 1), (2, 1), (3, 0)):
                P.add("dve", lambda e, slot=slot, part=part: e.tensor_copy(St[0][:, slot, 32:64], Hf[:, part]), reads=["Hh", "St0"], writes=["St0"])
            P.add("act", lambda e: e.activation(out=Pt[:, :, 1, :, 128], in_=Hf, func=AF.Copy), reads=["Hh"], writes=[("PtS", "bnd")])

            Pt_full = Pt[:]
            pstep = Pt_full.ap[0][0]
            PART = 2 * 32 * 288
            for i in range(160):
                cur, nxt = St[i % 2], St[(i + 1) % 2]
                ck, nk = "St%d" % (i % 2), "St%d" % ((i + 1) % 2)
                qF, qB = i, 159 - i
                if i >= 32:
                    cs = slice(0, 64)
                    dd = [[32 * 288 + qB - qF, 2], [288, 32]]
                    off = Pt_full.offset + qF
                    vv = lambda t_, a, b: t_[:, a:b, :].rearrange("p a (d g) -> p a d g", d=2)
                    swp = [[32, 2], [1, 32]]
                else:
                    cs = slice(0, 32)
                    dd = [[288, 32]]
                    off = Pt_full.offset + qF
                    vv = lambda t_, a, b: t_[:, a:b, 0:32]
                    swp = [[1, 32]]
                pap = bass.AP(Pt_full.tensor, off, [[pstep, 128], [PART, 2]] + dd)
                pap_sw = bass.AP(Pt_full.tensor, off + PART, [[pstep, 128], [-PART, 2]] + dd)
                P.add("dve", lambda e, cur=cur, cs=cs: e.tensor_tensor(out=rt1[:, :, cs], in0=C4[:, :, cs], in1=cur[:, :, cs], op=ALU.mult),
                      reads=[ck, "C4"], writes=["rt1"])
                P.add("dve", lambda e, cs=cs: e.tensor_tensor(out=rt2[:, :, cs], in0=rt1[:, 0:2, cs], in1=rt1[:, 2:4, cs], op=ALU.add),
                      reads=["rt1"], writes=["rt2"])
                P.add("dve", lambda e, nxt=nxt, vv=vv, pap=pap: e.tensor_tensor(out=vv(nxt, 0, 2), in0=vv(rt2, 0, 2), in1=pap, op=ALU.add),
                      reads=["rt2", ("PtS", i)], writes=[(nk, 0)])
                sw_in = bass.AP(rt2[:].tensor, rt2[:].offset + 64, [[rt2[:].ap[0][0], 128], [-64, 2]] + swp)
                P.add("dve", lambda e, nxt=nxt, vv=vv, pap_sw=pap_sw, sw_in=sw_in: e.tensor_tensor(out=vv(nxt, 2, 4), in0=sw_in, in1=pap_sw, op=ALU.add),
                      reads=["rt2", ("PtS", i)], writes=[(nk, 1)])
                P.add("act", lambda e, nxt=nxt, vv=vv, pap=pap: e.activation(out=pap, in_=vv(nxt, 0, 2), func=AF.Copy),
                      reads=[(nk, 0)], writes=[("PtS", i)])
            P.barrier()
        if debug:
            d_H = dout("d_H", [128, 2 * 2 * 32 * 288], BF16)
            P.add("sp", lambda e: e.dma_start(out=d_H, in_=Pt[:].rearrange("p a b c d -> p (a b c d)")), reads=["Pt", "PtS"],
                  writes=["d_H"], group="dbgH")

        with ExitStack() as ph:
            Ysb = sb("Ysb", [128, 64, 128], BF16, ph)
            tch = [sb("tch%d" % i, [128, 2, 8, 128], BF16, ph) for i in range(1)] * 2
            w2ch = [sb("w2ch%d" % i, [128, 2, 4, 2, 128], BF16, ph) for i in range(1)] * 2
            pY = [ps("pY%d" % i, [128, 4, 128], F32, ph) for i in range(2)]
            for q in range(8):
                b_ = 0
                for d_ in range(2):
                    P.add("sp", lambda e, q=q, b_=b_, d_=d_: e.dma_start(
                        out=tch[b_][:, d_], in_=scrT[d_, q * 8:(q + 1) * 8].rearrange("g r c -> r g c")),
                        reads=["scrT"], writes=[("tch", b_, d_)], group="tch%d" % b_)
                    P.add("sp", lambda e, q=q, b_=b_, d_=d_: e.dma_start(
                        out=w2ch[b_][:, d_], in_=scrW2[d_, q * 4:(q + 1) * 4].rearrange("g part r c -> r g part c")),
                        reads=["scrW2"], writes=[("w2ch", b_, d_)], group="w2ch%d" % b_)
                for hh in range(2):
                    pb = (q * 2 + hh) % 2

                    def mmY(e, q=q, hh=hh, pb=pb, b_=b_):
                        ins = None
                        for j in range(4):
                            gl = hh * 4 + j
                            g = q * 8 + gl
                            g2, gp = g // 2, g % 2
                            g2l = g2 - q * 4
                            rows = slice(gp * 64, (gp + 1) * 64)
                            o = pY[pb][:, j, :]
                            e.matmul(o, lhsT=tch[b_][:, 0, gl, :], rhs=U[:, g, 0:128], start=True, stop=False)
                            e.matmul(o, lhsT=w2ch[b_][rows, 0, g2l, 0, :], rhs=Pt[rows, 0, 0, g2, 31:159], start=False, stop=False)
                            e.matmul(o, lhsT=w2ch[b_][rows, 0, g2l, 1, :], rhs=Pt[rows, 1, 0, g2, 31:159], start=False, stop=False)
                            e.matmul(o, lhsT=tch[b_][:, 1, gl, :], rhs=U[:, g, 0:128], start=False, stop=False)
                            e.matmul(o, lhsT=w2ch[b_][rows, 1, g2l, 0, :], rhs=Pt[rows, 0, 1, g2, 1:129], start=False, stop=False)
                            ins = e.matmul(o, lhsT=w2ch[b_][rows, 1, g2l, 1, :], rhs=Pt[rows, 1, 1, g2, 1:129], start=False, stop=True)
                        return ins
                    P.add("pe", mmY, reads=[("tch", b_), ("w2ch", b_), "U", "Pt", "PtS"], writes=["pY%d" % pb])
                    g0 = q * 8 + hh * 4
                    if hh == 0:
                        P.add("dve", lambda e, g0=g0, pb=pb: e.tensor_copy(Ysb[:, g0:g0 + 4, :], pY[pb][:]), reads=["pY%d" % pb],
                              writes=[("Ysb", g0)])
                    else:
                        P.add("act", lambda e, g0=g0, pb=pb: e.activation(out=Ysb[:, g0:g0 + 4, :], in_=pY[pb][:], func=AF.Copy),
                              reads=["pY%d" % pb], writes=[("Ysb", g0)])
            pZ = [ps("pZ%d" % i, [128, 512], F32, ph) for i in range(2)]
            yf = [sb("yf%d" % i, [128, 512], F32, ph) for i in range(2)]
            ya = [sb("ya%d" % i, [128, 512], F32, ph) for i in range(2)]
            for ct in range(8):
                chains = []
                for half in range(2):
                    pb = half

                    def uns(e, ct=ct, half=half, pb=pb):
                        ins = None
                        ov = pZ[pb][:, :].rearrange("p (j t) -> p t j", t=8)
                        for t_ in range(8):
                            for gl in range(8):
                                ins = e.matmul(ov[:, t_, :], lhsT=Z[:, t_, gl, :], rhs=Ysb[:, ct * 8 + gl, half * 64:(half + 1) * 64],
                                               start=(gl == 0), stop=(gl == 7))
                        return ins
                    P.add("pe", uns, reads=["Z", "Ysb"], writes=["pZ%d" % pb])
                    tk = slice(half * 512, (half + 1) * 512)
                    yk, ak = "yf%d" % pb, "ya%d" % pb
                    chains.append([
                        ("dve", lambda e, ct=ct, pb=pb, half=half: e.scalar_tensor_tensor(
                            out=yf[pb][:].rearrange("p (j s) -> p j s", s=8),
                            in0=uTown[:, ct, :].rearrange("p (s j) -> p j s", s=8)[:, half * 64:(half + 1) * 64, :],
                            scalar=colB[:, ct:ct + 1], in1=pZ[pb][:, :].rearrange("p (j s) -> p j s", s=8), op0=ALU.mult, op1=ALU.add),
                         ["uT", "colB", "pZ%d" % pb], [yk]),
                        ("act", lambda e, pb=pb: e.activation(out=ya[pb][:], in_=yf[pb][:], func=AF.Square), [yk], [ak]),
                        ("dve", lambda e, pb=pb: e.tensor_scalar(out=ya[pb][:], in0=ya[pb][:], scalar1=0.044715, scalar2=1.0,
                                                                 op0=ALU.mult, op1=ALU.add), [ak], [ak]),
                        ("dve", lambda e, pb=pb: e.tensor_tensor(out=ya[pb][:], in0=ya[pb][:], in1=yf[pb][:], op=ALU.mult), [ak, yk], [ak]),
                        ("act", lambda e, pb=pb: e.activation(out=ya[pb][:], in_=ya[pb][:], func=AF.Sigmoid, scale=1.5957691216057308),
                         [ak], [ak]),
                        ("dve", lambda e, pb=pb, ct=ct, tk=tk: e.tensor_tensor(out=gT[:, ct, tk], in0=ya[pb][:], in1=yf[pb][:], op=ALU.mult),
                         [ak, yk], [("gT", ct, half)]),
                    ])
                for k in range(6):
                    for ch in chains:
                        en, f_, rk, wk = ch[k]
                        P.add(en, f_, reads=rk, writes=wk)
            P.barrier()
        ssm.close()
        if debug:
            d_gT = dout("d_gT", [128, 8 * NOWN], BF16)
            P.add("sp", lambda e: e.dma_start(out=d_gT, in_=gT[:].rearrange("p a b -> p (a b)")), reads=["gT"],
                  writes=["d_gT"], group="dbgG")

        ssmT = sb("ssmT", [128, 8, NOWN], BF16, mix)
        rstd = sb("rstdSC", [128, 2, NOWN], F32, mix)
        with ExitStack() as ph:
            wglu = sb("wglu", [128, 8, 2048], BF16, ph)
            wgv = w_glu.rearrange("(kt p) c -> p kt c", p=128)
            for kt in range(8):
                for hc in range(2):
                    P.add("pool", lambda e, kt=kt, hc=hc: e.dma_start(out=wglu[:, kt, hc * 1024:(hc + 1) * 1024],
                                                                      in_=wgv[:, kt, hc * 1024:(hc + 1) * 1024]),
                          writes=[("wglu", kt, hc)], group="wglu")
            pga = [ps("pga%d" % i, [128, 512], F32, ph) for i in range(2)]
            pgb = [ps("pgb%d" % i, [128, 512], F32, ph) for i in range(2)]
            pss = ps("pss", [128, 512], F32, ph)
            sig = [sb("sig%d" % i, [128, 512], F32, ph) for i in range(2)]
            sq = [sb("sq%d" % i, [128, 512], BF16, ph) for i in range(2)]
            pend = []
            for which in range(2):
                for half in range(2):
                    tk = slice(half * 512, (half + 1) * 512)
                    for ot in range(8):
                        b_ = ot % 2
                        if which == 0:
                            def mg(e, ot=ot, b_=b_, tk=tk, off=0, pp=pga):
                                ins = None
                                for kt in range(8):
                                    ins = e.matmul(pp[b_][:, :], lhsT=wglu[:, kt, off + ot * 128:off + (ot + 1) * 128], rhs=gT[:, kt, tk],
                                                   start=(kt == 0), stop=(kt == 7))
                                return ins
                            P.add("pe", mg, reads=["wglu", "gT"], writes=["pga%d" % b_])
                            P.add("pe", lambda e, ot=ot, b_=b_, tk=tk: mg(e, ot, b_, tk, 1024, pgb), reads=["wglu", "gT"], writes=["pgb%d" % b_])
                            P.add("act", lambda e, b_=b_: e.activation(out=sig[b_][:], in_=pgb[b_][:], func=AF.Sigmoid),
                                  reads=["pgb%d" % b_], writes=["sig%d" % b_])
                            P.add("dve", lambda e, b_=b_, ot=ot, tk=tk: e.tensor_tensor(out=ssmT[:, ot, tk], in0=pga[b_][:], in1=sig[b_][:], op=ALU.mult),
                                  reads=["pga%d" % b_, "sig%d" % b_], writes=[("ssmT", ot, half)])
                            srcT, skey = ssmT, ("ssmT", ot, half)
                        else:
                            srcT, skey = convT, "convT"
                        def back(b_=b_, ot=ot, tk=tk, srcT=srcT, skey=skey):
                            P.add("act", lambda e: e.activation(out=sq[b_][:], in_=srcT[:, ot, tk], func=AF.Square),
                                  reads=[skey], writes=["sq%d" % b_])
                            P.add("pe", lambda e: e.matmul(pss[:, :], lhsT=ones_b[:], rhs=sq[b_][:], start=(ot == 0), stop=(ot == 7)),
                                  reads=["ones_b", "sq%d" % b_], writes=["pss"])
                        if pend:
                            pend.pop()()
                        pend.append(back)
                    if pend:
                        pend.pop()()
                    rk = ("rstd", which, half)
                    P.add("dve", lambda e, which=which, tk=tk: e.tensor_scalar(out=rstd[:, which, tk], in0=pss[:, :], scalar1=1.0 / 1024, scalar2=EPS,
                                                                              op0=ALU.mult, op1=ALU.add), reads=["pss"], writes=[rk])
                    P.add("act", lambda e, which=which, tk=tk: e.activation(out=rstd[:, which, tk], in_=rstd[:, which, tk], func=AF.Sqrt),
                          reads=[rk], writes=[rk])
                    P.add("dve", lambda e, which=which, tk=tk: e.reciprocal(out=rstd[:, which, tk], in_=rstd[:, which, tk]), reads=[rk], writes=[rk])
            for ot in range(8):
                P.add("dve", lambda e, ot=ot: e.scalar_tensor_tensor(out=ssmT[:, ot, :], in0=ssmT[:, ot, :], scalar=colA[:, 80 + ot:81 + ot],
                                                                     in1=rstd[:, 0, :], op0=ALU.mult, op1=ALU.mult),
                      reads=["ssmT", "rstd", "colA"], writes=[("ssmT", ot)])
                P.add("dve", lambda e, ot=ot: e.scalar_tensor_tensor(out=convT[:, ot, :], in0=convT[:, ot, :], scalar=colA[:, 88 + ot:89 + ot],
                                                                      in1=rstd[:, 1, :], op0=ALU.mult, op1=ALU.mult),
                      reads=["convT", "rstd", "colA"], writes=[("convT", ot)])
            P.barrier()

        def row_bcast(dst, col_of_ft, rkeys, wkey, stack_ps):
            dgs = [sb(wkey + "_dg%d" % i, [128, 128], F32, stack_ps) for i in range(2)]
            prb = ps(wkey + "_prb", [128, 512], F32, stack_ps)
            for c4 in range(4):
                for j in range(4):
                    ft = c4 * 4 + j
                    b_ = ft % 2
                    P.add("dve", lambda e, ft=ft, b_=b_: e.tensor_scalar(out=dgs[b_][:], in0=ident_f[:], scalar1=col_of_ft(ft), scalar2=None,
                                                                        op0=ALU.mult), reads=["ident_f"] + rkeys, writes=[wkey + "_dg%d" % b_])
                    P.add("pe", lambda e, j=j, b_=b_: e.matmul(prb[:, j * 128:(j + 1) * 128], lhsT=ones_f[:], rhs=dgs[b_][:], start=True, stop=True),
                          reads=["ones_f", wkey + "_dg%d" % b_], writes=[wkey + "_prb"])
                P.add("act", lambda e, c4=c4: e.activation(out=dst[:, c4 * 512:(c4 + 1) * 512], in_=prb[:, :], func=AF.Copy),
                      reads=[wkey + "_prb"], writes=[wkey])

        with ExitStack() as ph:
            g1b = sb("g1b", [128, D], F32, ph)
            with ExitStack() as ph3:
                row_bcast(g1b, lambda ft: modT[:, 32 + ft, 0:1], ["modT"], "g1b", ph3)
                P.barrier()
            woc = [sb("woc%d" % i, [128, 16, 512], BF16, ph) for i in range(2)]
            wov = w_out.rearrange("(kt p) c -> p kt c", p=128)
            po = [ps("po%d" % i, [128, 512], F32, ph) for i in range(6)]
            xp = [sb("xp%d" % i, [128, 512], F32, ph) for i in range(8)]
            x1p = [sb("x1p%d" % i, [128, 512], F32, ph) for i in range(4)]
            jk = sb("jk", [128, 512], BF16, ph)
            its = [(cc, tt) for cc in range(4) for tt in range(8)]

            def ld(n):
                cc, tt = its[n]
                P.add("sp", lambda e, n=n, cc=cc, tt=tt: e.dma_start(out=xp[n % 8][:], in_=xs[tt * 128:(tt + 1) * 128, cc * 512:(cc + 1) * 512]),
                      writes=[("xp", n % 8)], group="xp%d" % (n % 8))
            for n in range(6):
                ld(n)
            for n, (cc, tt) in enumerate(its):
                wb_ = cc % 2
                if tt == 0:
                    for kt in range(16):
                        P.add("pool", lambda e, kt=kt, cc=cc, wb_=wb_: e.dma_start(out=woc[wb_][:, kt, :], in_=wov[:, kt, cc * 512:(cc + 1) * 512]),
                              writes=[("woc", wb_, kt)], group="woc%d" % wb_)
                if n + 6 < len(its):
                    ld(n + 6)
                rows = slice(tt * 128, (tt + 1) * 128)
                cols = slice(cc * 512, (cc + 1) * 512)
                pb, xb_, sb_ = n % 6, n % 8, n % 4

                def mo(e, tt=tt, pb=pb, wb_=wb_):
                    ins = None
                    for ht in range(16):
                        hsrc = ssmT if ht < 8 else convT
                        ins = e.matmul(po[pb][:, :], lhsT=hsrc[:, ht % 8, tt * 128:(tt + 1) * 128], rhs=woc[wb_][:, ht, :],
                                       start=(ht == 0), stop=(ht == 15))
                    return ins
                P.add("pe", mo, reads=["ssmT", "convT", ("woc", wb_)], writes=["po%d" % pb])
                P.add("dve", lambda e, pb=pb, sb_=sb_, cols=cols: e.tensor_tensor(out=x1p[sb_][:], in0=po[pb][:, :], in1=g1b[:, cols], op=ALU.mult),
                      reads=["po%d" % pb, "g1b"], writes=[("x1p", sb_)])
                P.add("dve", lambda e, sb_=sb_, xb_=xb_: e.tensor_tensor(out=x1p[sb_][:], in0=x1p[sb_][:], in1=xp[xb_][:], op=ALU.add),
                      reads=[("x1p", sb_), ("xp", xb_)], writes=[("x1p", sb_)])
                P.add("act", lambda e, sb_=sb_, tt=tt, cc=cc: e.activation(out=jk[:], in_=x1p[sb_][:], func=AF.Square,
                                                                        accum_out=ss2[:, tt, cc:cc + 1]),
                      reads=[("x1p", sb_)], writes=["jk", ("ss2", tt, cc)])
                P.add("act", lambda e, sb_=sb_, rows=rows, cols=cols: e.dma_start(out=scrX1[rows, cols], in_=x1p[sb_][:]),
                      reads=[("x1p", sb_)], writes=["scrX1"], group="x1p%d" % sb_)
            P.barrier()
        mix.close()
        mixer.close()

        moe = top.enter_context(ExitStack())
        acc = sb("acc", [128, 8, D], F32, moe)
        hx2T = sb("hx2T", [128, 16, NOWN], BF16, moe)
        Wt = sb("Wt", [128, 8, 64], F32, moe)
        with ExitStack() as ph:
            sc2b = sb("sc2b", [128, D], F32, ph)
            sh2b = sb("sh2b", [128, D], F32, ph)
            scale2 = sb("scale2", [128, 16], F32, ph)
            P.add("dve", lambda e: e.scalar_tensor_tensor(out=scale2[:], in0=modT[:, 64:80, 0], scalar=1.0, in1=colA[:, 48:64],
                                                          op0=ALU.add, op1=ALU.mult), reads=["modT", "colA"], writes=["scale2"])
            with ExitStack() as ph3:
                row_bcast(sc2b, lambda ft: scale2[:, ft:ft + 1], ["scale2"], "sc2b", ph3)
                P.barrier()
            with ExitStack() as ph3:
                row_bcast(sh2b, lambda ft: modT[:, 48 + ft, 0:1], ["modT"], "sh2b", ph3)
                P.barrier()
            rw = sb("rw", [128, 16, 64], F32, ph)
            P.add("sp", lambda e: e.dma_start(out=rw[:], in_=router_w.rearrange("(kt p) c -> p kt c", p=128)), writes=["rw"], group="rw")
            rb = sb("rb", [128, 64], F32, ph)
            P.add("sp", lambda e: e.dma_start(out=rb[:], in_=rbias_b), writes=["rb"], group="rb")
            rs2 = sb("rs2", [128, 8], F32, ph)
            P.add("dve", lambda e: e.tensor_reduce(out=rs2[:], in_=ss2[:], axis=AX.X, op=ALU.add), reads=["ss2"], writes=["rs2"])
            P.add("dve", lambda e: e.tensor_scalar(out=rs2[:], in0=rs2[:], scalar1=1.0 / D, scalar2=EPS, op0=ALU.mult, op1=ALU.add),
                  reads=["rs2"], writes=["rs2"])
            P.add("act", lambda e: e.activation(out=rs2[:], in_=rs2[:], func=AF.Sqrt), reads=["rs2"], writes=["rs2"])
            P.add("dve", lambda e: e.reciprocal(out=rs2[:], in_=rs2[:]), reads=["rs2"], writes=["rs2"])
            x1t = [sb("x1t%d" % i, [128, D], F32, ph) for i in range(3)]
            hf = [sb("hf%d" % i, [128, D], F32, ph) for i in range(3)]
            pth = [ps("pth%d" % i, [128, 4, 128], F32, ph) for i in range(2)]
            hfs = [sb("hfs%d" % i, [128, 4, 128], F32, ph) for i in range(2)]
            plgs = [ps("plg%d" % i, [128, 64], F32, ph) for i in range(2)]
            rt = sb("rt", [128, 12, 64], F32, ph)
            m8 = sb("m8", [128, 16], F32, ph)

            def n2A(tt):
                b_ = tt % 3
                rows = slice(tt * 128, (tt + 1) * 128)
                P.add("sp", lambda e, b_=b_, rows=rows: e.dma_start(out=x1t[b_][:], in_=scrX1[rows, :]), reads=["scrX1"],
                      writes=[("x1t", b_)], group="x1t%d" % b_)
                P.add("act", lambda e, b_=b_, tt=tt: e.activation(out=hf[b_][:], in_=x1t[b_][:], func=AF.Copy, scale=rs2[:, tt:tt + 1]),
                      reads=[("x1t", b_), "rs2"], writes=[("hf", b_)])
                P.add("dve", lambda e, b_=b_: e.tensor_tensor(out=hf[b_][:], in0=hf[b_][:], in1=sc2b[:], op=ALU.mult),
                      reads=[("hf", b_), "sc2b"], writes=[("hf", b_)])
                P.add("dve", lambda e, b_=b_: e.tensor_tensor(out=hf[b_][:, 0:1280], in0=hf[b_][:, 0:1280], in1=sh2b[:, 0:1280], op=ALU.add),
                      reads=[("hf", b_), "sh2b"], writes=[("hf", b_, 0)])
                P.add("pool", lambda e, b_=b_: e.tensor_tensor(out=hf[b_][:, 1280:2048], in0=hf[b_][:, 1280:2048], in1=sh2b[:, 1280:2048], op=ALU.add),
                      reads=[("hf", b_), "sh2b"], writes=[("hf", b_, 1)])

            def n2B(tt):
                b_ = tt % 3
                plg = plgs[tt % 2]
                pk = "plg%d" % (tt % 2)

                def tr_blk(f4):
                    pb = f4 % 2

                    def trh(e, b_=b_, f4=f4, pb=pb):
                        ins = None
                        for j in range(4):
                            ft = f4 * 4 + j
                            ins = e.transpose(out=pth[pb][:, j, :], in_=hf[b_][:, ft * 128:(ft + 1) * 128], identity=ident_f[:])
                        return ins
                    P.add("pe", trh, reads=[("hf", b_), "ident_f"], writes=["pth%d" % pb])
                    P.add("act", lambda e, pb=pb, f4=f4, tt=tt: e.activation(out=hx2T[:, f4 * 4:(f4 + 1) * 4, tt * 128:(tt + 1) * 128],
                                                                            in_=pth[pb][:], func=AF.Copy),
                          reads=["pth%d" % pb], writes=[("hx2T", f4, tt)])
                    P.add("dve", lambda e, pb=pb: e.tensor_copy(hfs[pb][:], pth[pb][:]), reads=["pth%d" % pb], writes=[("hfs", pb)])

                def mr_blk(f4):
                    pb = f4 % 2

                    def mr(e, pb=pb, f4=f4, plg=plg):
                        ins = None
                        for j in range(4):
                            ft = f4 * 4 + j
                            ins = e.matmul(plg[:, :], lhsT=hfs[pb][:, j, :], rhs=rw[:, ft, :], start=(ft == 0), stop=(ft == 15))
                        return ins
                    P.add("pe", mr, reads=[("hfs", pb), "rw"], writes=[pk])
                tr_blk(0)
                for f4 in range(4):
                    if f4 + 1 < 4:
                        tr_blk(f4 + 1)
                    mr_blk(f4)

            def n2C(tt):
                plg = plgs[tt % 2]
                pk = "plg%d" % (tt % 2)
                S_, Bi, T1, T2, MB, EM = (rt[:, i, :] for i in range(6))
                g3 = lambda ap: ap.rearrange("p (g k) -> p g k", k=8)
                rops = [
                    ("act", lambda e: e.activation(out=S_, in_=plg[:, :], func=AF.Sigmoid), [pk]),
                    ("dve", lambda e: e.tensor_tensor(out=Bi, in0=S_, in1=rb[:], op=ALU.add), ["rb"]),
                    ("dve", lambda e: e.tensor_reduce(out=m8[:, 0:8], in_=g3(Bi), axis=AX.X, op=ALU.max), []),
                    ("dve", lambda e: e.tensor_tensor(out=g3(T1), in0=g3(Bi), in1=m8[:, 0:8].unsqueeze(2).to_broadcast([128, 8, 8]), op=ALU.is_equal), []),
                    ("dve", lambda e: e.scalar_tensor_tensor(out=T1, in0=T1, scalar=-1e9, in1=Bi, op0=ALU.mult, op1=ALU.add), []),
                    ("dve", lambda e: e.tensor_reduce(out=m8[:, 8:16], in_=g3(T1), axis=AX.X, op=ALU.max), []),
                    ("dve", lambda e: e.tensor_tensor(out=m8[:, 0:8], in0=m8[:, 0:8], in1=m8[:, 8:16], op=ALU.add), []),
                    ("dve", lambda e: e.max(out=m8[:, 8:16], in_=m8[:, 0:8]), []),
                    ("dve", lambda e: e.tensor_scalar(out=m8[:, 0:8], in0=m8[:, 0:8], scalar1=m8[:, 11:12], scalar2=None, op0=ALU.is_ge), []),
                    ("dve", lambda e: e.tensor_tensor(out=g3(MB), in0=g3(Bi), in1=m8[:, 0:8].unsqueeze(2).to_broadcast([128, 8, 8]), op=ALU.mult), []),
                    ("dve", lambda e: e.tensor_scalar(out=m8[:, 0:8], in0=m8[:, 0:8], scalar1=-1.0, scalar2=1e9, op0=ALU.add, op1=ALU.mult), []),
                    ("dve", lambda e: e.tensor_tensor(out=g3(MB), in0=g3(MB), in1=m8[:, 0:8].unsqueeze(2).to_broadcast([128, 8, 8]), op=ALU.add), []),
                    ("dve", lambda e: e.max(out=m8[:, 8:16], in_=MB), []),
                    ("dve", lambda e: e.tensor_scalar(out=EM, in0=MB, scalar1=m8[:, 15:16], scalar2=None, op0=ALU.is_ge), []),
                    ("dve", lambda e: e.tensor_tensor(out=T2, in0=S_, in1=EM, op=ALU.mult), []),
                    ("dve", lambda e: e.tensor_reduce(out=m8[:, 0:1], in_=T2, axis=AX.X, op=ALU.add), []),
                    ("dve", lambda e: e.reciprocal(out=m8[:, 0:1], in_=m8[:, 0:1]), []),
                    ("dve", lambda e, tt=tt: e.tensor_scalar(out=Wt[:, tt, :], in0=T2, scalar1=m8[:, 0:1], scalar2=2.5, op0=ALU.mult, op1=ALU.mult), []),
                ]
                for (en, f_, rk) in rops:
                    P.add(en, f_, reads=["rt", "m8"] + rk, writes=["rt", "m8", ("Wt", tt)])

            for it in range(10):
                if it < 8:
                    n2A(it)
                if 1 <= it <= 8:
                    n2B(it - 1)
                if it >= 2:
                    n2C(it - 2)
            P.barrier()
        if debug:
            d_Wt = dout("d_Wt", [128, 512])
            P.add("sp", lambda e: e.dma_start(out=d_Wt, in_=Wt[:].rearrange("p a b -> p (a b)")), reads=["Wt"], writes=["d_Wt"], group="dbgW")
            d_hx2T = dout("d_hx2T", [128, 16 * NOWN], BF16)
            P.add("sp", lambda e: e.dma_start(out=d_hx2T, in_=hx2T[:].rearrange("p a b -> p (a b)")), reads=["hx2T"], writes=["d_hx2T"], group="dbgW2")

        with ExitStack() as ph:
            wg = [sb("wg%d" % i, [128, 16, 512], BF16, ph) for i in range(2)]
            wu = [sb("wu%d" % i, [128, 16, 512], BF16, ph) for i in range(2)]
            wd = [sb("wd0", [128, 4, D], BF16, ph)]
            actT = sb("actT", [128, 4, NOWN], BF16, ph)
            sgl = [sb("sgl%d" % i, [128, 512], F32, ph) for i in range(2)]
            pg = [ps("pg%d" % i, [128, 512], F32, ph) for i in range(2)]
            pu = [ps("pu%d" % i, [128, 512], F32, ph) for i in range(2)]
            pd = [ps("pd%d" % i, [128, 512], F32, ph) for i in range(3)]
            P.add("pool", lambda e: e.memset(acc[:], 0.0), writes=["acc"])
            NE = 65
            import os
            DMAONLY = os.environ.get("MOE_DMAONLY", "")
            _Padd = P.add
            if DMAONLY:
                class _PX:
                    @staticmethod
                    def add(eng, fn, reads=(), writes=(), group=None):
                        if group is None:
                            return None
                        q = {"1": "pool", "2": "sp", "3": "act"}[DMAONLY[0]]
                        return _Padd(q if DMAONLY[0] != "4" else eng, fn, reads=reads, writes=writes, group=group)
                PM = _PX
            else:
                PM = P
            for ex in range(NE):
                b_ = ex % 2
                gsrc = ew_gate[ex] if ex < 64 else sw_gate
                usrc = ew_up[ex] if ex < 64 else sw_up
                dsrc = ew_down[ex] if ex < 64 else sw_down
                PM.add("pool", lambda e, b_=b_, gsrc=gsrc: e.dma_start(out=wg[b_][:], in_=gsrc.rearrange("(kt p) c -> p kt c", p=128)),
                      writes=[("wg", b_)], group="wg%d" % b_)
                PM.add("pool", lambda e, b_=b_, usrc=usrc: e.dma_start(out=wu[b_][:], in_=usrc.rearrange("(kt p) c -> p kt c", p=128)),
                      writes=[("wu", b_)], group="wu%d" % b_)
                dv = dsrc.rearrange("(kt p) c -> p kt c", p=128)
                for hc in range(2):
                    PM.add("pool", lambda e, dv=dv, hc=hc: e.dma_start(out=wd[0][:, :, hc * 1024:(hc + 1) * 1024],
                                                                     in_=dv[:, :, hc * 1024:(hc + 1) * 1024]),
                          writes=[("wd", hc)], group="wd")
                n = 0
                for mt in range(4):
                    for half in range(2):
                        pb = n % 2
                        n += 1
                        tk = slice(half * 512, (half + 1) * 512)

                        def mgu(e, w_, pp, mt=mt, tk=tk, pb=pb, b_=b_):
                            ins = None
                            for kt in range(16):
                                ins = e.matmul(pp[pb][:, :], lhsT=w_[b_][:, kt, mt * 128:(mt + 1) * 128], rhs=hx2T[:, kt, tk],
                                               start=(kt == 0), stop=(kt == 15))
                            return ins
                        PM.add("pe", lambda e, f_=mgu: f_(e, wg, pg), reads=[("wg", b_), "hx2T"], writes=["pg%d" % pb])
                        PM.add("pe", lambda e, f_=mgu: f_(e, wu, pu), reads=[("wu", b_), "hx2T"], writes=["pu%d" % pb])
                        PM.add("act", lambda e, pb=pb: e.activation(out=sgl[pb][:], in_=pg[pb][:, :], func=AF.Silu),
                              reads=["pg%d" % pb], writes=[("sgl", pb)])
                        PM.add("dve", lambda e, pb=pb, mt=mt, tk=tk: e.tensor_tensor(out=actT[:, mt, tk], in0=sgl[pb][:], in1=pu[pb][:, :], op=ALU.mult),
                              reads=[("sgl", pb), "pu%d" % pb], writes=[("actT", mt, half)])
                n = 0
                for tt in range(8):
                    for cc in range(4):
                        pb = n % 3
                        n += 1

                        def mdn(e, tt=tt, cc=cc, pb=pb):
                            ins = None
                            for kt in range(4):
                                ins = e.matmul(pd[pb][:, :], lhsT=actT[:, kt, tt * 128:(tt + 1) * 128], rhs=wd[0][:, kt, cc * 512:(cc + 1) * 512],
                                               start=(kt == 0), stop=(kt == 3))
                            return ins
                        PM.add("pe", mdn, reads=["actT", "wd"], writes=["pd%d" % pb])
                        wsc = Wt[:, tt, ex:ex + 1] if ex < 64 else 1.0
                        PM.add("dve", lambda e, tt=tt, cc=cc, pb=pb, wsc=wsc: e.scalar_tensor_tensor(
                            out=acc[:, tt, cc * 512:(cc + 1) * 512], in0=pd[pb][:, :], scalar=wsc, in1=acc[:, tt, cc * 512:(cc + 1) * 512],
                            op0=ALU.mult, op1=ALU.add), reads=["pd%d" % pb, "Wt"], writes=[("acc", tt, cc)])
            P.barrier()

        with ExitStack() as ph:
            g2b = sb("g2b", [128, D], F32, ph)
            fgb = sb("fgb", [128, D], F32, ph)
            with ExitStack() as ph3:
                row_bcast(g2b, lambda ft: modT[:, 80 + ft, 0:1], ["modT"], "g2b", ph3)
                P.barrier()
            with ExitStack() as ph3:
                row_bcast(fgb, lambda ft: colA[:, 64 + ft:65 + ft], ["colA"], "fgb", ph3)
                P.barrier()
            x1f = [sb("x1f%d" % i, [128, D], F32, ph) for i in range(3)]
            fo = [sb("fo%d" % i, [128, D], F32, ph) for i in range(3)]
            fs = sb("fs", [128, 8], F32, ph)
            fj = sb("fj", [128, D], BF16, ph)
            def stageA(tt):
                b_ = tt % 3
                rows = slice(tt * 128, (tt + 1) * 128)
                for hc in range(2):
                    cs = slice(hc * 1024, (hc + 1) * 1024)
                    P.add("dve", lambda e, tt=tt, cs=cs: e.tensor_tensor(out=acc[:, tt, cs], in0=acc[:, tt, cs], in1=g2b[:, cs], op=ALU.mult),
                          reads=[("acc", tt, hc), "g2b"], writes=[("acc", tt, hc)])
                    P.add("dve", lambda e, tt=tt, b_=b_, cs=cs: e.tensor_tensor(out=acc[:, tt, cs], in0=acc[:, tt, cs], in1=x1f[b_][:, cs], op=ALU.add),
                          reads=[("acc", tt, hc), ("x1f", b_)], writes=[("acc", tt, hc)])
                    P.add("act", lambda e, tt=tt, cs=cs, hc=hc: e.activation(out=fj[:, cs], in_=acc[:, tt, cs], func=AF.Square,
                                                                          accum_out=fs2[:, tt, hc:hc + 1]),
                          reads=[("acc", tt, hc)], writes=[("fj", hc), ("fs2", tt, hc)])

            def stageB(tt):
                b_ = tt % 3
                rows = slice(tt * 128, (tt + 1) * 128)
                P.add("dve", lambda e, tt=tt: e.tensor_tensor(out=fs[:, tt:tt + 1], in0=fs2[:, tt, 0:1], in1=fs2[:, tt, 1:2], op=ALU.add),
                      reads=[("fs2", tt)], writes=[("fs", tt)])
                P.add("dve", lambda e, tt=tt: e.tensor_scalar(out=fs[:, tt:tt + 1], in0=fs[:, tt:tt + 1], scalar1=1.0 / D, scalar2=EPS,
                                                              op0=ALU.mult, op1=ALU.add), reads=[("fs", tt)], writes=[("fs", tt)])
                P.add("act", lambda e, tt=tt: e.activation(out=fs[:, tt:tt + 1], in_=fs[:, tt:tt + 1], func=AF.Sqrt), reads=[("fs", tt)], writes=[("fs", tt)])
                P.add("dve", lambda e, tt=tt: e.reciprocal(out=fs[:, tt:tt + 1], in_=fs[:, tt:tt + 1]), reads=[("fs", tt)], writes=[("fs", tt)])
                P.add("act", lambda e, tt=tt, b_=b_: e.activation(out=fo[b_][:], in_=acc[:, tt, :], func=AF.Copy, scale=fs[:, tt:tt + 1]),
                      reads=[("acc", tt), ("fs", tt)], writes=[("fo", b_)])
                P.add("dve", lambda e, b_=b_: e.tensor_tensor(out=fo[b_][:], in0=fo[b_][:], in1=fgb[:], op=ALU.mult),
                      reads=[("fo", b_), "fgb"], writes=[("fo", b_)])
                P.add("pool", lambda e, b_=b_, rows=rows: e.dma_start(out=out[rows, :], in_=fo[b_][:]), reads=[("fo", b_)], writes=["out"],
                      group="fo%d" % b_)

            fs2 = sb("fs2", [128, 8, 2], F32, ph)

            def ldf(tt):
                P.add("sp", lambda e, tt=tt: e.dma_start(out=x1f[tt % 3][:], in_=scrX1[tt * 128:(tt + 1) * 128, :]), reads=["scrX1"],
                      writes=[("x1f", tt % 3)], group="x1f%d" % (tt % 3))
            ldf(0)
            ldf(1)
            for tt in range(9):
                if tt < 8:
                    stageA(tt)
                if tt + 2 < 8:
                    ldf(tt + 2)
                if tt >= 1:
                    stageB(tt - 1)

        P.add("sp", None, reads=["out", "scrW1", "scrT", "scrW2", "scrU", "scrX1"] + list(dbg.keys()))
        P.emit()
    return nc, dbg


def prep_inputs(inp):
    f = lambda a: np.ascontiguousarray(a, dtype=np.float32)
    x, ctx, c = inp["x"], inp["ctx"], inp["c"]
    maps = []
    shared = {
        "w_ada": f(inp["w_ada"][0]), "w_in": f(inp["w_in"][0]),
        "b_ada": f(inp["b_ada"][0].reshape(96, 128)),
        "w_glu": f(inp["ssm_w_glu"][0]), "w_out": f(inp["w_out"][0]), "router_w": f(inp["router_w"][0]),
        "rbias_b": f(np.tile(inp["router_bias"][0].reshape(1, 64), (128, 1))),
        "ew_gate": f(inp["exp_w_gate"][0]), "ew_up": f(inp["exp_w_up"][0]), "ew_down": f(inp["exp_w_down"][0]),
        "sw_gate": f(inp["shared_w_gate"][0]), "sw_up": f(inp["shared_w_up"][0]), "sw_down": f(inp["shared_w_down"][0]),
    }
    for core in range(8):
        b, h = core // 2, core % 2
        xb = x[b]
        cb = ctx[b]
        conv_w = inp["conv_w"][0]
        if h == 1:
            xb = xb[::-1]
            cb = cb[::-1]
            conv_w = conv_w[::-1]
        vecsA = np.concatenate([c[b].reshape(16, 128), inp["c_ctx"].reshape(16, 128),
                                inp["norm1_g"][0].reshape(16, 128), inp["norm2_g"][0].reshape(16, 128),
                                inp["final_g"].reshape(16, 128), inp["mix_norm_g"][0].reshape(16, 128)], 0)
        vecsB = np.concatenate([inp["ssm_d"][0].reshape(8, 128), conv_w.reshape(24, 128),
                                inp["conv_b"][0].reshape(8, 128)], 0)
        sl = slice(None, None, -1) if h == 1 else slice(None)
        m = dict(shared)
        m.update(xs=f(xb), ctxs=f(cb), vecsA=f(vecsA), vecsB=f(vecsB),
                 lamre_p=f(inp["ssm_lam_re"][0][sl].reshape(64, 128)), lamim_p=f(inp["ssm_lam_im"][0][sl].reshape(64, 128)),
                 logdt_p=f(inp["ssm_log_dt"][0][sl].reshape(64, 2)),
                 ssm_b_re=f(inp["ssm_b_re"][0][sl]), ssm_b_im=f(inp["ssm_b_im"][0][sl]),
                 ssm_c_re=f(inp["ssm_c_re"][0][sl]), ssm_c_im=f(inp["ssm_c_im"][0][sl]))
        maps.append(m)
    return maps


def kernel(**inputs):
    nc, _ = build_nc(False)
    maps = prep_inputs(inputs)
    res = run_bass_kernel_spmd(nc, maps, core_ids=list(range(8)))
    outs = np.zeros((4, 2048, 2048), np.float32)
    for core in range(8):
        b, h = core // 2, core % 2
        o = res.results[core]["out"]
        if h == 0:
            outs[b, 0:1024] = o
        else:
            outs[b, 1024:2048] = o[::-1]
    return outs
```

```python
from contextlib import ExitStack
import numpy as np
import concourse.bass as bass
import concourse.mybir as mybir
from concourse.bass_utils import run_bass_kernel_spmd

F32 = mybir.dt.float32
BF16 = mybir.dt.bfloat16
I32 = mybir.dt.int32
ALU = mybir.AluOpType
AF = mybir.ActivationFunctionType
AX = mybir.AxisListType

D = 2048
NOWN = 1024
NSEQ = 2304
EPS = 1e-6


class Prog:
    ENG = ("pe", "act", "dve", "pool", "sp")

    def __init__(self, nc, stack):
        self.nc = nc
        self.stack = stack
        self.ops = []
        self.keys = {}
        self.groups = {}
        self.psum_names = set()
        self.gopen = {}

    @staticmethod
    def _norm(k):
        return k if isinstance(k, tuple) else (k,)

    def _related(self, key):
        d = self.keys.setdefault(key[0], {})
        for k2 in list(d.keys()):
            n = min(len(k2), len(key))
            if k2[:n] == key[:n]:
                yield k2, d[k2]

    def add(self, eng, fn, reads=(), writes=(), group=None):
        op = dict(id=len(self.ops), eng=eng, fn=fn, deps=set(), group=group, used=False)
        reads = [self._norm(k) for k in reads] + [("__phase",)]
        writes = [self._norm(k) for k in writes]
        pk = [(k[0],) for k in reads + writes if k[0] in self.psum_names]
        reads = [k for k in reads if k[0] not in self.psum_names]
        writes = [k for k in writes if k[0] not in self.psum_names] + sorted(set(pk))
        for key in reads:
            for k2, st in self._related(key):
                if st[0] is not None:
                    op["deps"].add(st[0])
        for key in writes:
            for k2, st in self._related(key):
                if st[0] is not None:
                    op["deps"].add(st[0])
                op["deps"].update(st[1])
        for key in reads:
            d = self.keys.setdefault(key[0], {})
            st = d.setdefault(key, [None, []])
            st[1].append(op["id"])
        for key in writes:
            d = self.keys.setdefault(key[0], {})
            for k2 in list(d.keys()):
                if len(k2) > len(key) and k2[:len(key)] == key:
                    del d[k2]
            d[key] = [op["id"], []]
        op["deps"].discard(op["id"])
        for d in op["deps"]:
            gg = self.ops[d]["group"]
            if gg is not None and d in self.gopen.get(gg, ()):
                self.gopen[gg] = []
        if group is not None:
            self.gopen.setdefault(group, []).append(op["id"])
            op["batch"] = self.gopen[group]
        self.ops.append(op)
        return op

    def barrier(self):
        scr = self._bar_scr
        self.add("dve", lambda e: e.memset(scr[:, 0:1], 0.0), writes=[("__phase",), "barscr"])

    def emit(self):
        nc = self.nc
        ops = self.ops
        for op in ops:
            for d in op["deps"]:
                ops[d]["used"] = True
        sems = {}
        for e in self.ENG:
            sems[e] = self.stack.enter_context(nc.semaphore("s_" + e))
        gsem = {}
        cnt = {e: 0 for e in self.ENG}
        gcnt = {}
        for op in ops:
            if op["group"] is not None:
                g = op["group"]
                if g not in gsem:
                    gsem[g] = self.stack.enter_context(nc.semaphore("g_" + str(g)))
                    gcnt[g] = 0
                gcnt[g] += 16
                op["sig"] = (gsem[g], gcnt[g], 16)
            elif op["used"]:
                cnt[op["eng"]] += 1
                op["sig"] = (sems[op["eng"]], cnt[op["eng"]], 1)
            else:
                op["sig"] = None
        per = {e: [o for o in ops if o["eng"] == e] for e in self.ENG}

        def replay(ename, eng):
            waited = {}
            for op in per[ename]:
                need = {}
                for d in op["deps"]:
                    dop = ops[d]
                    if dop["eng"] == "pe" and ename == "pe" and dop["group"] is None:
                        continue
                    s = dop["sig"]
                    assert s is not None
                    if dop["group"] is not None:
                        s = ops[dop["batch"][-1]]["sig"]
                    key = id(s[0])
                    if key not in need or need[key][1] < s[1]:
                        need[key] = (s[0], s[1])
                for key, (sem, val) in need.items():
                    if waited.get(key, 0) >= val:
                        continue
                    waited[key] = val
                    eng.wait_ge(sem, val)
                if op["fn"] is None:
                    continue
                ins = op["fn"](eng)
                if op["sig"] is not None:
                    ins.then_inc(op["sig"][0], op["sig"][2])

        block = self.stack.enter_context(nc.Block())

        @block.tensor
        def _(eng):
            replay("pe", eng)

        @block.scalar
        def _(eng):
            replay("act", eng)

        @block.vector
        def _(eng):
            replay("dve", eng)

        @block.gpsimd
        def _(eng):
            replay("pool", eng)

        @block.sync
        def _(eng):
            replay("sp", eng)


def build_nc(debug=False, stop=99):
    nc = bass.Bass("TRN2", target_bir_lowering=False)
    dbg = {}

    def din(name, shape, dt=F32):
        return nc.dram_tensor(name, list(shape), dt, kind="ExternalInput").ap()

    xs = din("xs", [2048, D])
    ctxs = din("ctxs", [256, D])
    vecsA = din("vecsA", [96, 128])
    vecsB = din("vecsB", [40, 128])
    b_ada = din("b_ada", [96, 128])
    w_ada = din("w_ada", [D, 6 * D])
    w_in = din("w_in", [D, 4096])
    lamre_p = din("lamre_p", [64, 128])
    lamim_p = din("lamim_p", [64, 128])
    logdt_p = din("logdt_p", [64, 2])
    ssm_b_re = din("ssm_b_re", [2, 64, 64, 16])
    ssm_b_im = din("ssm_b_im", [2, 64, 64, 16])
    ssm_c_re = din("ssm_c_re", [2, 64, 16, 64])
    ssm_c_im = din("ssm_c_im", [2, 64, 16, 64])
    w_glu = din("w_glu", [1024, 2048])
    w_out = din("w_out", [D, D])
    router_w = din("router_w", [D, 64])
    rbias_b = din("rbias_b", [128, 64])
    ew_gate = din("ew_gate", [64, D, 512])
    ew_up = din("ew_up", [64, D, 512])
    ew_down = din("ew_down", [64, 512, D])
    sw_gate = din("sw_gate", [D, 512])
    sw_up = din("sw_up", [D, 512])
    sw_down = din("sw_down", [512, D])
    out = nc.dram_tensor("out", [NOWN, D], F32, kind="ExternalOutput").ap()
    skind = "ExternalOutput" if debug else "Internal"
    scrW1 = nc.dram_tensor("scrW1", [2, 32, 2, 128, 128], BF16, kind=skind).ap()
    scrT = nc.dram_tensor("scrT", [2, 64, 128, 128], BF16, kind=skind).ap()
    scrW2 = nc.dram_tensor("scrW2", [2, 32, 2, 128, 128], BF16, kind=skind).ap()
    scrX1 = nc.dram_tensor("scrX1", [NOWN, D], F32, kind=skind).ap()
    scrU = nc.dram_tensor("scrU", [8, 128, NSEQ - NOWN], BF16, kind=skind).ap()

    def dout(name, shape, dt=F32):
        t = nc.dram_tensor(name, list(shape), dt, kind="ExternalOutput").ap()
        dbg[name] = t
        return t

    with ExitStack() as top:
        P = Prog(nc, top)

        def sb(name, shape, dt=F32, stack=top):
            return stack.enter_context(nc.sbuf_tensor(name, list(shape), dt))

        def ps(name, shape, dt=F32, stack=top):
            P.psum_names.add(name)
            esz = 4 if dt == F32 else 2
            full = stack.enter_context(nc.psum_tensor(name, [128, 2048 // esz], dt))
            n = int(np.prod(shape[1:]))
            v = full[:, 0:n]
            if len(shape) == 3:
                v = v.rearrange("p (a b) -> p a b", b=shape[2])
            return v

        P._bar_scr = sb("barscr", [128, 4])

        ident_f = sb("ident_f", [128, 128])
        ident_b = sb("ident_b", [128, 128], BF16)
        iot = sb("iot", [128, 128], I32)
        iotf = sb("iotf", [128, 128])
        P.add("pool", lambda e: e.iota(iot[:], [[1, 128]], base=0, channel_multiplier=-1), writes=["iot"])
        P.add("dve", lambda e: e.tensor_copy(iotf[:], iot[:]), reads=["iot"], writes=["iotf"])
        P.add("dve", lambda e: e.tensor_single_scalar(ident_f[:], iotf[:], 0.0, ALU.is_equal),
              reads=["iotf"], writes=["ident_f"])
        P.add("dve", lambda e: e.tensor_copy(ident_b[:], ident_f[:]), reads=["ident_f"], writes=["ident_b"])

        if stop < 1:
            d_i = dout('d_ident', [128, 128])
            P.add('sp', lambda e: e.dma_start(out=d_i, in_=ident_f[:]), reads=['ident_f'], writes=['d_ident'], group='dbgi')
            P.add('sp', None, reads=list(dbg.keys()))
            P.emit()
            return nc, dbg
        ones_b = sb("ones_b", [128, 128], BF16)
        ones_f = sb("ones_f", [128, 128], F32)
        P.add("dve", lambda e: e.memset(ones_b[:], 1.0), writes=["ones_b"])
        P.add("dve", lambda e: e.memset(ones_f[:], 1.0), writes=["ones_f"])
        ss2 = sb("ss2", [128, 8, 4], F32)
        vA = sb("vA", [96, 128])
        vB = sb("vB", [40, 128])
        vC = sb("vC", [96, 128])
        colA = sb("colA", [128, 96])
        colB = sb("colB", [128, 40])
        badaT = sb("badaT", [128, 96])
        P.add("sp", lambda e: e.dma_start(out=vA[:], in_=vecsA), writes=["vA"], group="vA")
        P.add("sp", lambda e: e.dma_start(out=vB[:], in_=vecsB), writes=["vB"], group="vB")
        P.add("sp", lambda e: e.dma_start(out=vC[:], in_=b_ada), writes=["vC"], group="vC")
        with ExitStack() as ph:
            pt = ps("pt_small", [128, 3, 128], F32, ph)
            P.add("pe", lambda e: e.transpose(out=pt[:, 0, 0:96], in_=vA[:], identity=ident_f[0:96, 0:96]),
                  reads=["vA", "ident_f"], writes=["pt_small"])
            P.add("pe", lambda e: e.transpose(out=pt[:, 1, 0:40], in_=vB[:], identity=ident_f[0:40, 0:40]),
                  reads=["vB", "ident_f"], writes=["pt_small"])
            P.add("pe", lambda e: e.transpose(out=pt[:, 2, 0:96], in_=vC[:], identity=ident_f[0:96, 0:96]),
                  reads=["vC", "ident_f"], writes=["pt_small"])
            P.add("dve", lambda e: e.tensor_copy(colA[:], pt[:, 0, 0:96]), reads=["pt_small"], writes=["colA"])
            P.add("dve", lambda e: e.tensor_copy(colB[:], pt[:, 1, 0:40]), reads=["pt_small"], writes=["colB"])
            P.add("dve", lambda e: e.tensor_copy(badaT[:], pt[:, 2, 0:96]), reads=["pt_small"], writes=["badaT"])
            P.barrier()

        if stop < 2:
            d_c = dout('d_colA', [128, 96])
            P.add('sp', lambda e: e.dma_start(out=d_c, in_=colA[:]), reads=['colA'], writes=['d_colA'], group='dbgc')
            P.add('sp', None, reads=list(dbg.keys()))
            P.emit()
            return nc, dbg
        sc = sb("sc", [128, 16, 2])
        for j in range(2):
            P.add("act", lambda e, j=j: e.activation(out=sc[:, :, j], in_=colA[:, 16 * j:16 * j + 16], func=AF.Silu),
                  reads=["colA"], writes=[("sc", j)])
        modT = sb("modT", [128, 96, 2])
        scale1 = sb("scale1", [128, 16, 2])
        mixer = top.enter_context(ExitStack())
        uTown = sb("uTown", [128, 8, NOWN], BF16, mixer)
        convT = sb("convT", [128, 8, NOWN], BF16, mixer)
        ss = sb("ss", [128, 24], F32, mixer)
        Acplx = sb("Acplx", [128, 2, 64], F32, mixer)
        ada_stack = ExitStack()
        REC_A = []
        _real_add = P.add
        P.add = lambda *a, **k: REC_A.append((a, k))
        if True:
            ph = ada_stack
            scb = sb("scb", [128, 16, 4], BF16, ph)
            sch = sb("sch", [128, 16, 2], F32, ph)
            P.add("dve", lambda e: e.tensor_copy(scb[:, :, 0:2], sc[:]), reads=["sc"], writes=["scb"])
            P.add("dve", lambda e: e.tensor_copy(sch[:], scb[:, :, 0:2]), reads=["scb"], writes=["sch"])
            P.add("dve", lambda e: e.tensor_tensor(out=sch[:], in0=sc[:], in1=sch[:], op=ALU.subtract), reads=["sc", "sch"], writes=["sch"])
            P.add("dve", lambda e: e.tensor_copy(scb[:, :, 2:4], sch[:]), reads=["sch", "scb"], writes=["scb"])
            pm = ps("pmod", [128, 96, 4], F32, ph)
            wbuf = [sb("wada%d" % i, [128, 2048], BF16, ph) for i in range(4)]
            n = 0
            for kt in range(16):
                for cc in range(6):
                    b = n % 4
                    n += 1
                    for hc in range(2):
                        P.add("pool", lambda e, b=b, kt=kt, cc=cc, hc=hc: e.dma_start(
                            out=wbuf[b][:, hc * 1024:(hc + 1) * 1024],
                            in_=w_ada[kt * 128:(kt + 1) * 128, cc * 2048 + hc * 1024:cc * 2048 + (hc + 1) * 1024]),
                            writes=[("wada", b, hc)], group="wada%d" % b)

                    def mm(e, b=b, kt=kt, cc=cc):
                        ins = None
                        for t in range(16):
                            ins = e.matmul(pm[:, cc * 16 + t, :], lhsT=wbuf[b][:, t * 128:(t + 1) * 128],
                                           rhs=scb[:, kt, :], start=(kt == 0 and cc == 0 and t == 0), stop=(kt == 15),
                                           skip_group_check=True)
                        return ins
                    P.add("pe", mm, reads=[("wada", b), "scb"], writes=["pmod"])
            P.add("dve", lambda e: e.tensor_tensor(out=modT[:], in0=pm[:, :, 0:2], in1=badaT[:].unsqueeze(2).to_broadcast([128, 96, 2]),
                                                  op=ALU.add), reads=["pmod", "badaT"], writes=["modT"])
            P.add("dve", lambda e: e.tensor_tensor(out=modT[:], in0=modT[:], in1=pm[:, :, 2:4], op=ALU.add),
                  reads=["pmod", "modT"], writes=["modT"])
        P.add = _real_add
        import math
        PI = math.pi
        with ExitStack() as ph:
            REC_S = []
            P.add = lambda *a, **k: REC_S.append((a, k))
            raw = sb("s0raw", [64, 3, 128], F32, ph)
            ldt = sb("s0ldt", [64, 2], F32, ph)
            P.add("sp", lambda e: e.dma_start(out=raw[:, 0, :], in_=lamre_p), writes=[("s0raw", 0)], group="s0raw0")
            P.add("sp", lambda e: e.dma_start(out=raw[:, 1, :], in_=lamim_p), writes=[("s0raw", 1)], group="s0raw1")
            P.add("sp", lambda e: e.dma_start(out=ldt[:], in_=logdt_p), writes=["s0ldt"], group="s0ldt")
            P.add("act", lambda e: e.activation(out=ldt[:], in_=ldt[:], func=AF.Exp), reads=["s0ldt"], writes=["s0ldt"])
            P.add("dve", lambda e: e.tensor_copy(raw[:, 2, :].rearrange("q (a b) -> q a b", a=2),
                                                 ldt[:].unsqueeze(2).to_broadcast([64, 2, 64])),
                  reads=["s0ldt"], writes=[("s0raw", 2)])
            LRI = sb("LRI", [128, 3, 64], F32, ph)
            ptq = ps("s0pt", [128, 4, 128], F32, ph)
            for i in range(3):
                P.add("pe", lambda e, i=i: e.transpose(out=ptq[:, i, 0:64], in_=raw[:, i, :], identity=ident_f[0:64, 0:64]),
                      reads=[("s0raw", i), "ident_f"], writes=["s0pt"])
            P.add("dve", lambda e: e.tensor_copy(LRI[:], ptq[:, 0:3, 0:64]), reads=["s0pt"], writes=["LRI"])
            ath = sb("ath", [128, 2, 64], F32, ph)
            P.add("dve", lambda e: e.tensor_tensor(out=ath[:], in0=LRI[:, 0:2, :],
                                                   in1=LRI[:, 2:3, :].to_broadcast([128, 2, 64]), op=ALU.mult),
                  reads=["LRI"], writes=["ath"])
            io8 = sb("io8", [128, 8], I32, ph)
            io8f = sb("io8f", [128, 8], F32, ph)
            KM = sb("KM", [128, 3, 2, 8], F32, ph)
            P.add("pool", lambda e: e.iota(io8[:], [[1, 8]], base=0, channel_multiplier=0), writes=["io8"])
            P.add("dve", lambda e: e.tensor_copy(io8f[:], io8[:]), reads=["io8"], writes=["io8f"])
            kmab = {(0, 0): (-1.0, 7.0), (0, 1): (1.0, 0.0), (1, 0): (-1.0, -1.0), (1, 1): (1.0, -8.0),
                    (2, 0): (1.0, 1.0), (2, 1): (-1.0, 8.0)}
            for (u_, d_), (ka, kb) in kmab.items():
                P.add("dve", lambda e, u_=u_, d_=d_, ka=ka, kb=kb: e.tensor_scalar(
                    out=KM[:, u_, d_, :], in0=io8f[:], scalar1=ka, scalar2=kb, op0=ALU.mult, op1=ALU.add),
                    reads=["io8f"], writes=[("KM", u_, d_)])

            et_ang = sb("et_ang", [128, 2, 32, 8], F32, ph)
            et_ex = sb("et_ex", [128, 2, 32, 8], F32, ph)
            et_tmp = sb("et_tmp", [128, 2, 32, 8], F32, ph)
            et_ti = sb("et_ti", [128, 2, 32, 8], I32, ph)
            et_tf = sb("et_tf", [128, 2, 32, 8], F32, ph)

            def etab(name, mult_ap, L, dst_re, dst_im, rkeys, wkeys):
                shp = [128, 2, 32, L]
                name = "et"
                ang = et_ang[:, :, :, 0:L]
                ex = et_ex[:, :, :, 0:L]
                tmp = et_tmp[:, :, :, 0:L]
                ti = et_ti[:, :, :, 0:L]
                tf = et_tf[:, :, :, 0:L]
                a_b = ath[:, 0, :].rearrange("p (d g) -> p d g", d=2).unsqueeze(3).to_broadcast(shp)
                t_b = ath[:, 1, :].rearrange("p (d g) -> p d g", d=2).unsqueeze(3).to_broadcast(shp)
                P.add("dve", lambda e: e.tensor_tensor(out=ex[:], in0=a_b, in1=mult_ap, op=ALU.mult),
                      reads=["ath"] + rkeys, writes=[name + "_ex"])
                P.add("act", lambda e: e.activation(out=ex[:], in_=ex[:], func=AF.Exp), reads=[name + "_ex"], writes=[name + "_ex"])
                P.add("dve", lambda e: e.tensor_tensor(out=ang[:], in0=t_b, in1=mult_ap, op=ALU.mult),
                      reads=["ath"] + rkeys, writes=[name + "_ang"])
                for (dst, shift) in ((dst_im, 32.0), (dst_re, 32.25)):
                    P.add("dve", lambda e, shift=shift: e.tensor_scalar(out=tmp[:], in0=ang[:], scalar1=1.0 / (2.0 * PI), scalar2=shift,
                                                                        op0=ALU.mult, op1=ALU.add),
                          reads=[name + "_ang"], writes=[name + "_tmp"])
                    P.add("dve", lambda e: e.tensor_copy(ti[:], tmp[:]), reads=[name + "_tmp"], writes=[name + "_ti"])
                    P.add("dve", lambda e: e.tensor_copy(tf[:], ti[:]), reads=[name + "_ti"], writes=[name + "_tf"])
                    P.add("dve", lambda e: e.tensor_tensor(out=tmp[:], in0=tmp[:], in1=tf[:], op=ALU.subtract),
                          reads=[name + "_tmp", name + "_tf"], writes=[name + "_tmp"])
                    P.add("dve", lambda e: e.tensor_single_scalar(tf[:], tmp[:], 0.5, ALU.is_gt),
                          reads=[name + "_tmp"], writes=[name + "_tf"])
                    P.add("dve", lambda e: e.tensor_tensor(out=tmp[:], in0=tmp[:], in1=tf[:], op=ALU.subtract),
                          reads=[name + "_tmp", name + "_tf"], writes=[name + "_tmp"])
                    P.add("act", lambda e: e.activation(out=tmp[:], in_=tmp[:], func=AF.Sin, scale=2.0 * PI), reads=[name + "_tmp"],
                          writes=[name + "_tmp"])
                    P.add("dve", lambda e, dst=dst: e.tensor_tensor(out=dst, in0=tmp[:], in1=ex[:], op=ALU.mult),
                          reads=[name + "_tmp", name + "_ex"], writes=wkeys)

            one1 = sb("one1", [128, 1], F32, ph)
            P.add("dve", lambda e: e.memset(one1[:], 1.0), writes=["one1"])
            E1 = sb("E1", [128, 2, 2, 32, 1], F32, ph)
            etab("e1", one1[:].unsqueeze(2).unsqueeze(3).to_broadcast([128, 2, 32, 1]), 1, E1[:, 0], E1[:, 1], ["one1"], ["E1"])
            eight = sb("eight", [128, 1], F32, ph)
            P.add("dve", lambda e: e.memset(eight[:], 8.0), writes=["eight"])
            etab("e8", eight[:].unsqueeze(2).unsqueeze(3).to_broadcast([128, 2, 32, 1]), 1,
                 Acplx[:, 0, :].rearrange("p (d g o) -> p d g o", d=2, o=1),
                 Acplx[:, 1, :].rearrange("p (d g o) -> p d g o", d=2, o=1), ["eight"], ["Acplx"])
            ET = [sb("ET%d" % u_, [128, 2, 2, 32, 8], F32, ph) for u_ in range(3)]
            for u_ in range(3):
                etab("et%d" % u_, KM[:, u_, :, :].unsqueeze(2).to_broadcast([128, 2, 32, 8]), 8,
                     ET[u_][:, 0], ET[u_][:, 1], ["KM"], ["ET%d" % u_])

            LR = LRI[:, 0, :]
            LI = LRI[:, 1, :]
            e1r = E1[:, 0].rearrange("p d g o -> p (d g o)")
            e1i = E1[:, 1].rearrange("p d g o -> p (d g o)")
            cf = sb("cf", [128, 6, 64], F32, ph)
            seq_ops = [
                lambda e: e.tensor_scalar(out=cf[:, 0, :], in0=e1r, scalar1=-1.0, scalar2=None, op0=ALU.add),
                lambda e: e.tensor_tensor(out=cf[:, 1, :], in0=LR, in1=LR, op=ALU.mult),
                lambda e: e.tensor_tensor(out=cf[:, 2, :], in0=LI, in1=LI, op=ALU.mult),
                lambda e: e.tensor_tensor(out=cf[:, 1, :], in0=cf[:, 1, :], in1=cf[:, 2, :], op=ALU.add),
                lambda e: e.reciprocal(out=cf[:, 1, :], in_=cf[:, 1, :]),
                lambda e: e.tensor_tensor(out=cf[:, 2, :], in0=cf[:, 0, :], in1=LR, op=ALU.mult),
                lambda e: e.tensor_tensor(out=cf[:, 3, :], in0=e1i, in1=LI, op=ALU.mult),
                lambda e: e.tensor_tensor(out=cf[:, 2, :], in0=cf[:, 2, :], in1=cf[:, 3, :], op=ALU.add),
                lambda e: e.tensor_tensor(out=cf[:, 4, :], in0=cf[:, 2, :], in1=cf[:, 1, :], op=ALU.mult),
                lambda e: e.tensor_tensor(out=cf[:, 2, :], in0=e1i, in1=LR, op=ALU.mult),
                lambda e: e.tensor_tensor(out=cf[:, 3, :], in0=cf[:, 0, :], in1=LI, op=ALU.mult),
                lambda e: e.tensor_tensor(out=cf[:, 2, :], in0=cf[:, 2, :], in1=cf[:, 3, :], op=ALU.subtract),
                lambda e: e.tensor_tensor(out=cf[:, 5, :], in0=cf[:, 2, :], in1=cf[:, 1, :], op=ALU.mult),
            ]
            for f_ in seq_ops:
                P.add("dve", f_, reads=["E1", "LRI", "cf"], writes=["cf"])
            Braw = sb("Braw", [128, 2, 64, 16], F32, ph)
            for i, src_ in enumerate((ssm_b_re, ssm_b_im)):
                for d_ in range(2):
                    v = src_[d_].rearrange("(g2 gp) p m -> gp p g2 m", gp=2)
                    for gp in range(2):
                        P.add("sp", lambda e, i=i, d_=d_, gp=gp, v=v: e.dma_start(
                            out=Braw[gp * 64:(gp + 1) * 64, i, d_ * 32:(d_ + 1) * 32, :], in_=v[gp]),
                            writes=[("Braw", i, d_, gp)], group="Braw")
            bbar = sb("bbar", [128, 2, 64, 16], F32, ph)
            tA = sb("s0tA", [128, 64, 16], F32, ph)
            cre_b = cf[:, 4, :].unsqueeze(2).to_broadcast([128, 64, 16])
            cim_b = cf[:, 5, :].unsqueeze(2).to_broadcast([128, 64, 16])
            P.add("dve", lambda e: e.tensor_tensor(out=bbar[:, 0], in0=Braw[:, 0], in1=cre_b, op=ALU.mult), reads=["Braw", "cf"], writes=[("bbar", 0)])
            P.add("dve", lambda e: e.tensor_tensor(out=tA[:], in0=Braw[:, 1], in1=cim_b, op=ALU.mult), reads=["Braw", "cf"], writes=["s0tA"])
            P.add("dve", lambda e: e.tensor_tensor(out=bbar[:, 0], in0=bbar[:, 0], in1=tA[:], op=ALU.subtract), reads=["s0tA", ("bbar", 0)], writes=[("bbar", 0)])
            P.add("dve", lambda e: e.tensor_tensor(out=bbar[:, 1], in0=Braw[:, 1], in1=cre_b, op=ALU.mult), reads=["Braw", "cf"], writes=[("bbar", 1)])
            P.add("dve", lambda e: e.tensor_tensor(out=tA[:], in0=Braw[:, 0], in1=cim_b, op=ALU.mult), reads=["Braw", "cf", ("bbar", 0)], writes=["s0tA"])
            P.add("dve", lambda e: e.tensor_tensor(out=bbar[:, 1], in0=bbar[:, 1], in1=tA[:], op=ALU.add), reads=["s0tA", ("bbar", 1)], writes=[("bbar", 1)])

            CT = sb("CT", [128, 2, 64, 16], F32, ph)
            craw = [sb("craw%d" % i, [128, 128], F32, ph) for i in range(2)]
            nb = 0
            for i, src_ in enumerate((ssm_c_re, ssm_c_im)):
                for d_ in range(2):
                    for q in range(4):
                        b_ = nb % 2
                        nb += 1
                        for g2l in range(8):
                            g2 = q * 8 + g2l
                            P.add("sp", lambda e, b_=b_, g2l=g2l, g2=g2, d_=d_, src_=src_: e.dma_start(
                                out=craw[b_][g2l * 16:(g2l + 1) * 16, :].rearrange("n (gp p) -> n gp p", gp=2),
                                in_=src_[d_, 2 * g2:2 * g2 + 2, :, :].rearrange("gp n p -> n gp p")),
                                writes=[("craw", b_, g2l)], group="craw%d" % b_)
                        P.add("pe", lambda e, b_=b_: e.transpose(out=ptq[:, 3, :], in_=craw[b_][:], identity=ident_f[:]),
                              reads=[("craw", b_), "ident_f"], writes=["s0pt"])
                        P.add("dve", lambda e, i=i, d_=d_, q=q: e.tensor_copy(
                            CT[:, i, d_ * 32 + q * 8:d_ * 32 + (q + 1) * 8, :], ptq[:, 3, :].rearrange("p (a n) -> p a n", n=16)),
                            reads=["s0pt"], writes=[("CT", i, d_, q)])

            mk = sb("mk", [128, 2, 128], F32, ph)
            mi = sb("mki", [128, 2, 128], I32, ph)
            mf = sb("mkf", [128, 2, 128], F32, ph)
            P.add("pool", lambda e: e.iota(mi[:, 0, :], [[1, 128]], base=0, channel_multiplier=0), writes=[("mki", 0)])
            P.add("pool", lambda e: e.iota(mi[:, 1, :], [[0, 128]], base=0, channel_multiplier=1), writes=[("mki", 1)])
            P.add("dve", lambda e: e.tensor_single_scalar(mi[:], mi[:], 4, ALU.arith_shift_right), reads=["mki"], writes=["mki"])
            P.add("dve", lambda e: e.tensor_copy(mf[:], mi[:]), reads=["mki"], writes=["mkf"])
            P.add("dve", lambda e: e.tensor_tensor(out=mk[:, 0, :], in0=mf[:, 0, :], in1=mf[:, 1, :], op=ALU.is_ge), reads=["mkf"], writes=[("mk", 0)])
            P.add("dve", lambda e: e.tensor_tensor(out=mk[:, 1, :], in0=mf[:, 1, :], in1=mf[:, 0, :], op=ALU.is_ge), reads=["mkf"], writes=[("mk", 1)])


            oR = sb("oR", [128, 32, 8, 16], F32, ph)
            oI = sb("oI", [128, 32, 8, 16], F32, ph)
            o2R = sb("o2R", [128, 32, 8, 16], F32, ph)
            o2I = sb("o2I", [128, 32, 8, 16], F32, ph)
            t1 = sb("s0t1", [128, 32, 8, 16], F32, ph)
            stg = sb("s0stg", [128, 4, 128], BF16, ph)
            stg2 = [sb("s0stg2", [128, 32, 128], BF16, ph)] * 2
            pT = ps("s0pT", [128, 4, 128], F32, ph)
            pT2 = ps("s0pT2", [128, 4, 128], F32, ph)

            def couter(u_, d_, Br, Bi, dR, dI, neg_im, rk, tag):
                shp = [128, 32, 8, 16]
                Er = ET[u_][:, 0, d_].unsqueeze(3).to_broadcast(shp)
                Ei = ET[u_][:, 1, d_].unsqueeze(3).to_broadcast(shp)
                Brb = Br.unsqueeze(2).to_broadcast(shp)
                Bib = Bi.unsqueeze(2).to_broadcast(shp)
                rk = rk + ["ET%d" % u_]
                P.add("dve", lambda e: e.tensor_tensor(out=dR[:], in0=Er, in1=Brb, op=ALU.mult), reads=rk, writes=[tag + "R"])
                P.add("dve", lambda e: e.tensor_tensor(out=t1[:], in0=Ei, in1=Bib, op=ALU.mult), reads=rk, writes=["s0t1"])
                P.add("dve", lambda e: e.tensor_tensor(out=dR[:], in0=dR[:], in1=t1[:], op=ALU.subtract), reads=[tag + "R", "s0t1"], writes=[tag + "R"])
                P.add("dve", lambda e: e.tensor_tensor(out=dI[:], in0=Er, in1=Bib, op=ALU.mult), reads=rk, writes=[tag + "I"])
                P.add("dve", lambda e: e.tensor_tensor(out=t1[:], in0=Ei, in1=Brb, op=ALU.mult), reads=rk + [tag + "R"], writes=["s0t1"])
                if neg_im:
                    P.add("dve", lambda e: e.scalar_tensor_tensor(out=dI[:], in0=dI[:], scalar=-1.0, in1=t1[:], op0=ALU.mult, op1=ALU.subtract),
                          reads=[tag + "I", "s0t1"], writes=[tag + "I"])
                else:
                    P.add("dve", lambda e: e.tensor_tensor(out=dI[:], in0=dI[:], in1=t1[:], op=ALU.add), reads=[tag + "I", "s0t1"], writes=[tag + "I"])

            for d_ in range(2):
                gs = slice(d_ * 32, (d_ + 1) * 32)
                couter(0, d_, bbar[:, 0, gs, :], bbar[:, 1, gs, :], oR, oI, False, ["bbar"], "o")
                for part, src_t, skey in ((0, oR, "oR"), (1, oI, "oI")):
                    for q in range(8):
                        def trw(e, src_t=src_t, q=q):
                            ins = None
                            for j in range(4):
                                ins = e.transpose(out=pT[:, j, :], in_=src_t[:, q * 4 + j].rearrange("p s m -> p (s m)"), identity=ident_f[:])
                            return ins
                        P.add("pe", trw, reads=[skey, "ident_f"], writes=["s0pT"])
                        P.add("act", lambda e: e.activation(out=stg[:], in_=pT[:], func=AF.Copy), reads=["s0pT"], writes=["s0stg"])
                        P.add("sp", lambda e, d_=d_, q=q, part=part: e.dma_start(
                            out=scrW1[d_, q * 4:(q + 1) * 4, part].rearrange("g r c -> r g c"), in_=stg[:]),
                            reads=["s0stg"], writes=["scrW1"], group="s0st")

                couter(1, d_, bbar[:, 0, gs, :], bbar[:, 1, gs, :], oR, oI, False, ["bbar"], "o")
                couter(2, d_, CT[:, 0, gs, :], CT[:, 1, gs, :], o2R, o2I, True, ["CT"], "o2")
                for part, src_t, skey in ((0, o2R, "o2R"), (1, o2I, "o2I")):
                    P.add("act", lambda e, part=part, src_t=src_t: e.activation(
                        out=stg2[part][:], in_=src_t[:].rearrange("p g t n -> p g (t n)"), func=AF.Copy),
                        reads=[skey], writes=["s0stg2"])
                    P.add("sp", lambda e, d_=d_, part=part: e.dma_start(
                        out=scrW2[d_, :, part].rearrange("g r c -> r g c"), in_=stg2[part][:]),
                        reads=["s0stg2"], writes=["scrW2"], group="s0st2")

                for q in range(8):
                    for gp in range(2):
                        pTx = pT if gp == 0 else pT2
                        pkey = "s0pT" if gp == 0 else "s0pT2"
                        rs = slice(gp * 64, (gp + 1) * 64)

                        def mmT(e, q=q, gp=gp, pTx=pTx, rs=rs):
                            ins = None
                            for j in range(4):
                                g2 = q * 4 + j
                                e.matmul(pTx[:, j, :], lhsT=oR[rs, g2].rearrange("p s m -> p (s m)"),
                                         rhs=o2R[rs, g2].rearrange("p t n -> p (t n)"), start=True, stop=False)
                                ins = e.matmul(pTx[:, j, :], lhsT=oI[rs, g2].rearrange("p s m -> p (s m)"),
                                               rhs=o2I[rs, g2].rearrange("p t n -> p (t n)"), start=False, stop=True)
                            return ins
                        P.add("pe", mmT, reads=["oR", "oI", "o2R", "o2I"], writes=[pkey])
                        P.add("dve", lambda e, d_=d_, pTx=pTx: e.tensor_tensor(
                            out=stg[:], in0=pTx[:], in1=mk[:, d_:d_ + 1, :].to_broadcast([128, 4, 128]), op=ALU.mult),
                            reads=[pkey, "mk"], writes=["s0stg"])
                        P.add("sp", lambda e, d_=d_, q=q, gp=gp: e.dma_start(
                            out=scrT[d_, q * 8:(q + 1) * 8].rearrange("(j gp) r c -> gp r j c", gp=2)[gp], in_=stg[:]),
                            reads=["s0stg"], writes=["scrT"], group="s0st")
            P.add = _real_add
            na, ns = len(REC_A), len(REC_S)
            ia = isx = 0
            while ia < na or isx < ns:
                if isx >= ns or (ia < na and ia * ns <= isx * na):
                    a_, k_ = REC_A[ia]; ia += 1
                else:
                    a_, k_ = REC_S[isx]; isx += 1
                P.add(*a_, **k_)
            P.barrier()
        ada_stack.close()
        if debug:
            d_A = dout("d_A", [128, 128])
            P.add("sp", lambda e: e.dma_start(out=d_A, in_=Acplx[:].rearrange("p a b -> p (a b)")), reads=["Acplx"],
                  writes=["d_A"], group="dbgA")

        if debug:
            d_mod = dout("d_mod", [128, 192])
            P.add("sp", lambda e: e.dma_start(out=d_mod, in_=modT[:].rearrange("p a b -> p (a b)")), reads=["modT"],
                  writes=["d_mod"], group="dbg0")

        P.add("dve", lambda e: e.scalar_tensor_tensor(out=scale1[:], in0=modT[:, 16:32, :], scalar=1.0,
                                                      in1=colA[:, 32:48].unsqueeze(2).to_broadcast([128, 16, 2]),
                                                      op0=ALU.add, op1=ALU.mult),
              reads=["modT", "colA"], writes=["scale1"])

        with ExitStack() as ph:
            w_u = sb("w_u", [128, 16, 1024], BF16, ph)
            ustage = [sb("ustage%d" % i, [128, 512], BF16, ph) for i in range(2)]
            w_in_v = w_in.rearrange("(kt p) c -> p kt c", p=128)
            for kt in range(16):
                P.add("pool", lambda e, kt=kt: e.dma_start(out=w_u[:, kt, :], in_=w_in_v[:, kt, 0:1024]),
                      writes=[("w_u", kt)], group="w_u")
            xt = [sb("xt%d" % i, [128, D], F32, ph) for i in range(2)]
            xn = [sb("xn%d" % i, [128, 4, D], BF16, ph) for i in range(2)]
            hxT = [sb("hxT%d" % i, [128, 16, 512], BF16, ph) for i in range(2)]
            ptr = [ps("ptr%d" % i, [128, 512], BF16, ph) for i in range(2)]
            pmm = [ps("pmm%d" % i, [128, 512], F32, ph) for i in range(6)]
            groups = [("x", 1024, 4, 0, 1024, 0), ("x", 1536, 4, 0, 1536, 1), ("c", 0, 2, 1, 2048, 0),
                      ("x", 0, 4, 0, 0, 1), ("x", 512, 4, 0, 512, 0)]
            nxc = [0]

            def stA1(gi):
                (src, r0, nt, mj, soff, xb) = groups[gi]
                xnb = xn[gi % 2]
                for t in range(nt):
                    nx = nxc[0]
                    b = nx % 2
                    tix = nx % 24
                    nxc[0] += 1
                    srcap = (xs if src == "x" else ctxs)[r0 + t * 128:r0 + (t + 1) * 128, :]
                    P.add("sp", lambda e, b=b, srcap=srcap: e.dma_start(out=xt[b][:], in_=srcap),
                          writes=[("xt", b)], group="xt%d" % b)
                    P.add("act", lambda e, b=b, tix=tix, t=t: e.activation(out=xnb[:, t, :], in_=xt[b][:], func=AF.Square,
                                                                      accum_out=ss[:, tix:tix + 1]),
                          reads=[("xt", b)], writes=[("xn", gi % 2, t), ("ss", tix)])
                    P.add("dve", lambda e, tix=tix: e.tensor_scalar(out=ss[:, tix:tix + 1], in0=ss[:, tix:tix + 1],
                                                                    scalar1=1.0 / D, scalar2=EPS, op0=ALU.mult, op1=ALU.add),
                          reads=[("ss", tix)], writes=[("ss", tix)])
                    P.add("act", lambda e, tix=tix: e.activation(out=ss[:, tix:tix + 1], in_=ss[:, tix:tix + 1], func=AF.Sqrt),
                          reads=[("ss", tix)], writes=[("ss", tix)])
                    P.add("dve", lambda e, tix=tix: e.reciprocal(out=ss[:, tix:tix + 1], in_=ss[:, tix:tix + 1]),
                          reads=[("ss", tix)], writes=[("ss", tix)])
                    P.add("act", lambda e, b=b, tix=tix, t=t: e.activation(
                        out=xnb[:, t, :], in_=xt[b][:], func=AF.Copy, scale=ss[:, tix:tix + 1]),
                        reads=[("xt", b), ("ss", tix)], writes=[("xn", gi % 2, t)])

            def stA2(gi):
                (src, r0, nt, mj, soff, xb) = groups[gi]
                xnb = xn[gi % 2]
                ntok = nt * 128
                for ft in range(16):
                    pb = ft % 2

                    def tr(e, ft=ft, nt=nt, pb=pb):
                        ins = None
                        for t in range(nt):
                            ins = e.transpose(out=ptr[pb][:, t * 128:(t + 1) * 128],
                                              in_=xnb[:, t, ft * 128:(ft + 1) * 128], identity=ident_b[:])
                        return ins
                    P.add("pe", tr, reads=[("xn", gi % 2), "ident_b"], writes=["ptr%d" % pb])
                    if ft % 2 == 0:
                        P.add("dve", lambda e, xb=xb, ft=ft, pb=pb, ntok=ntok, mj=mj: e.tensor_scalar(
                            out=hxT[xb][:, ft, 0:ntok], in0=ptr[pb][:, 0:ntok], scalar1=scale1[:, ft, mj:mj + 1],
                            scalar2=modT[:, ft, mj:mj + 1], op0=ALU.mult, op1=ALU.add),
                            reads=["ptr%d" % pb, "scale1", "modT"], writes=[("hxT", xb, ft)])
                    else:
                        P.add("act", lambda e, xb=xb, ft=ft, pb=pb, ntok=ntok, mj=mj: e.activation(
                            out=hxT[xb][:, ft, 0:ntok], in_=ptr[pb][:, 0:ntok], func=AF.Identity,
                            scale=scale1[:, ft, mj:mj + 1], bias=modT[:, ft, mj:mj + 1]),
                            reads=["ptr%d" % pb, "scale1", "modT"], writes=[("hxT", xb, ft)])

            def stB(gi):
                (src, r0, nt, mj, soff, xb) = groups[gi]
                ntok = nt * 128
                for ct in range(8):
                    pq = ct % 4

                    def mmu(e, xb=xb, ct=ct, ntok=ntok, pq=pq):
                        ins = None
                        for kt in range(16):
                            ins = e.matmul(pmm[pq][:, 0:ntok], lhsT=w_u[:, kt, ct * 128:(ct + 1) * 128],
                                           rhs=hxT[xb][:, kt, 0:ntok], start=(kt == 0), stop=(kt == 15))
                        return ins
                    P.add("pe", mmu, reads=["w_u", ("hxT", xb)], writes=["pmm%d" % pq])
                    nj = ntok // 8
                    if soff < NOWN:
                        dst = uTown[:, ct, :].rearrange("p (s j) -> p s j", s=8)[:, :, soff // 8:soff // 8 + nj]
                        wk = [("uT", ct, soff)]
                    else:
                        sg = ct % 2
                        dst = ustage[sg][:, 0:ntok].rearrange("p (s j) -> p s j", s=8)
                        wk = [("ustage", sg)]
                    srcv = pmm[pq][:, 0:ntok].rearrange("p (j s) -> p s j", s=8)
                    if ct % 2 == 0:
                        P.add("dve", lambda e, dst=dst, srcv=srcv: e.tensor_copy(dst, srcv),
                              reads=["pmm%d" % pq], writes=wk)
                    else:
                        P.add("act", lambda e, dst=dst, srcv=srcv: e.activation(out=dst, in_=srcv, func=AF.Copy),
                              reads=["pmm%d" % pq], writes=wk)
                    if soff >= NOWN:
                        j0r = (soff - NOWN) // 8
                        P.add("sp", lambda e, ct=ct, sg=sg, ntok=ntok, j0r=j0r, nj=nj: e.dma_start(
                            out=scrU[ct].rearrange("p (s j) -> p s j", s=8)[:, :, j0r:j0r + nj],
                            in_=ustage[sg][:, 0:ntok].rearrange("p (s j) -> p s j", s=8)),
                            reads=[("ustage", sg)], writes=["scrU"], group="ustage%d" % sg)

            stA1(0)
            stA2(0)
            for gi in range(5):
                if gi + 1 < 5:
                    stA1(gi + 1)
                stB(gi)
                if gi + 1 < 5:
                    stA2(gi + 1)
            cvs = ph.enter_context(ExitStack())
            wch = [[sb("wch%d_%d" % (s_, i), [128, 16, 128], BF16, cvs) for i in range(3)] for s_ in range(2)]
            zc = sb("zc", [128, 512], F32, cvs)
            zz = sb("zz", [128, 512], F32, cvs)
            yy = sb("yy", [128, 512], F32, cvs)
            for ct in range(8):
                s_ = ct % 2
                for i in range(3):
                    P.add("pool", lambda e, s_=s_, i=i, ct=ct: e.dma_start(
                        out=wch[s_][i][:], in_=w_in_v[:, :, 1024 * (i + 1) + ct * 128:1024 * (i + 1) + (ct + 1) * 128]),
                        writes=[("wch", s_, i)], group="wch%d_%d" % (s_, i))
                for og, (xb, soff) in enumerate([(1, 0), (0, 512)]):
                    pset = 3 * ((ct * 2 + og) % 2)
                    if True:
                        for i in range(3):
                            def mmb(e, xb=xb, i=i, s_=s_, pset=pset):
                                ins = None
                                for kt in range(16):
                                    ins = e.matmul(pmm[pset + i][:, :], lhsT=wch[s_][i][:, kt, :],
                                                   rhs=hxT[xb][:, kt, :], start=(kt == 0), stop=(kt == 15))
                                return ins
                            P.add("pe", mmb, reads=[("wch", s_, i), ("hxT", xb)], writes=["pmm%d" % (pset + i)])
                        P.add("act", lambda e, pset=pset: e.activation(out=zc[:], in_=pmm[pset + 1][:], func=AF.Copy),
                              reads=["pmm%d" % (pset + 1)], writes=["zc"])
                        P.add("dve", lambda e, pset=pset: e.tensor_tensor(out=zz[:], in0=zc[:], in1=pmm[pset + 2][:], op=ALU.mult),
                              reads=["zc", "pmm%d" % (pset + 2)], writes=["zz"])
                        P.add("dve", lambda e, ct=ct: e.tensor_scalar(
                            out=yy[:], in0=zz[:], scalar1=colB[:, 16 + ct:17 + ct], scalar2=colB[:, 32 + ct:33 + ct],
                            op0=ALU.mult, op1=ALU.add), reads=["zz", "colB"], writes=["yy"])
                        yv = yy[:].rearrange("p (r w) -> p r w", w=64)
                        zv = zz[:].rearrange("p (r w) -> p r w", w=64)
                        P.add("dve", lambda e, ct=ct, yv=yv, zv=zv: e.scalar_tensor_tensor(
                            out=yv[:, :, 1:64], in0=zv[:, :, 0:63], scalar=colB[:, 8 + ct:9 + ct], in1=yv[:, :, 1:64],
                            op0=ALU.mult, op1=ALU.add), reads=["zz", "yy", "colB"], writes=["yy"])
                        P.add("dve", lambda e, ct=ct, yv=yv, zv=zv: e.scalar_tensor_tensor(
                            out=yv[:, :, 0:63], in0=zv[:, :, 1:64], scalar=colB[:, 24 + ct:25 + ct], in1=yv[:, :, 0:63],
                            op0=ALU.mult, op1=ALU.add), reads=["zz", "yy", "colB"], writes=["yy"])
                        P.add("dve", lambda e, ct=ct, soff=soff, pset=pset: e.tensor_tensor(
                            out=convT[:, ct, soff:soff + 512], in0=yy[:], in1=pmm[pset][:], op=ALU.mult),
                            reads=["yy", "pmm%d" % pset], writes=[("convT", ct, soff)])
            P.barrier()
        if debug:
            d_uT = dout("d_uT", [128, 8 * NOWN], BF16)
            d_convT = dout("d_convT", [128, 8 * NOWN], BF16)
            P.add("sp", lambda e: e.dma_start(out=d_uT, in_=uTown[:].rearrange("p a b -> p (a b)")), reads=["uT"],
                  writes=["d_uT"], group="dbg1")
            P.add("sp", lambda e: e.dma_start(out=d_convT, in_=convT[:].rearrange("p a b -> p (a b)")), reads=["convT"],
                  writes=["d_convT"], group="dbg2")

        Z = sb("Z", [128, 8, 8, 128], BF16, mixer)
        P.add("pool", lambda e: e.memset(Z[:], 0.0), writes=["Z"])
        for a_ in range(8):
            for b_ in range(8):
                P.add("dve" if (a_ + b_) % 2 else "pool", lambda e, a_=a_, b_=b_: e.tensor_single_scalar(
                    Z[:, a_, b_, 16 * b_:16 * b_ + 16], iotf[:, 16 * b_:16 * b_ + 16], float(16 * (b_ - a_)), ALU.is_equal),
                    reads=["iotf"], writes=[("Z", a_, b_)])
        mix = mixer.enter_context(ExitStack())
        gT = sb("gT", [128, 8, NOWN], BF16, mix)
        ssm = mix.enter_context(ExitStack())
        U = sb("U", [128, 64, 128], BF16, ssm)
        Pt = sb("Pt", [128, 2, 2, 32, 288], BF16, ssm)
        s12 = ssm.enter_context(ExitStack())
        Ur = sb("Ur", [128, 64, 160], BF16, s12)
        with ExitStack() as ph:
            pU = [ps("pU%d" % i, [128, 288], F32, ph) for i in range(3)]
            ucat = [sb("ucat%d" % i, [128, NSEQ], BF16, ph) for i in range(2)]
            for g in range(64):
                ct, gl = g // 8, g % 8
                pb = g % 3
                if gl == 0:
                    P.add("sp", lambda e, ct=ct: e.dma_start(
                        out=ucat[ct % 2][:].rearrange("p (s j) -> p s j", s=8)[:, :, 128:288],
                        in_=scrU[ct].rearrange("p (s j) -> p s j", s=8)), reads=["scrU"],
                        writes=[("ucat", ct % 2, 1)], group="ucat%d" % (ct % 2))
                    P.add("pool", lambda e, ct=ct: e.tensor_copy(
                        ucat[ct % 2][:].rearrange("p (s j) -> p s j", s=8)[:, :, 0:128],
                        uTown[:, ct, :].rearrange("p (s j) -> p s j", s=8)), reads=["uT"],
                        writes=[("ucat", ct % 2, 0)])

                def shf(e, ct=ct, gl=gl, pb=pb):
                    ins = None
                    for s_ in range(8):
                        src = ucat[ct % 2][:, s_ * 288:(s_ + 1) * 288]
                        ins = e.matmul(pU[pb][:, 0:288], lhsT=Z[:, gl, s_, :], rhs=src, start=(s_ == 0), stop=(s_ == 7))
                    return ins
                P.add("pe", shf, reads=["Z", ("ucat", ct % 2)], writes=["pU%d" % pb])
                P.add("dve", lambda e, g=g, pb=pb: e.tensor_copy(U[:, g, :], pU[pb][:, 0:128]), reads=["pU%d" % pb], writes=[("U", g)])
                P.add("act", lambda e, g=g, pb=pb: e.activation(out=Ur[:, g, :], in_=pU[pb][:, 128:288], func=AF.Copy),
                      reads=["pU%d" % pb], writes=[("U", g)])
            P.barrier()
        with ExitStack() as ph:
            w1c = [sb("w1c%d" % i, [128, 8, 2, 128], BF16, ph) for i in range(2)]
            pP = [ps("pP%d" % i, [128, 288], F32, ph) for i in range(3)]
            nn = 0
            for d_ in range(2):
                for q in range(4):
                    wb_ = (d_ * 4 + q) % 2
                    P.add("sp", lambda e, d_=d_, q=q, wb_=wb_: e.dma_start(
                        out=w1c[wb_][:], in_=scrW1[d_, q * 8:(q + 1) * 8].rearrange("g part r c -> r g part c")),
                        reads=["scrW1"], writes=[("w1c", wb_)], group="w1c%d" % wb_)
                    for g2l in range(8):
                        g2 = q * 8 + g2l
                        for part in range(2):
                            pb = nn % 3
                            nn += 1

                            def mmP(e, d_=d_, g2=g2, g2l=g2l, part=part, pb=pb, wb_=wb_):
                                ins = None
                                for gp in range(2):
                                    g = 2 * g2 + gp
                                    lw = w1c[wb_][:, g2l, part, gp * 64:(gp + 1) * 64]
                                    rows = slice(gp * 64, (gp + 1) * 64)
                                    if d_ == 0:
                                        e.matmul(pP[pb][rows, 0:32], lhsT=lw, rhs=Ur[:, g, 128:160], start=True, stop=True)
                                        ins = e.matmul(pP[pb][rows, 32:160], lhsT=lw, rhs=U[:, g, 0:128], start=True, stop=True)
                                    else:
                                        e.matmul(pP[pb][rows, 0:128], lhsT=lw, rhs=U[:, g, 0:128], start=True, stop=True)
                                        ins = e.matmul(pP[pb][rows, 128:288], lhsT=lw, rhs=Ur[:, g, 0:160], start=True, stop=True)
                                return ins
                            P.add("pe", mmP, reads=[("w1c", wb_), "U"], writes=["pP%d" % pb])
                            ncol = 160 if d_ == 0 else 288
                            if nn % 2 == 0:
                                P.add("dve", lambda e, d_=d_, g2=g2, part=part, pb=pb, ncol=ncol: e.tensor_copy(
                                    Pt[:, part, d_, g2, 0:ncol], pP[pb][:, 0:ncol]), reads=["pP%d" % pb], writes=[("Pt", part, d_, g2)])
                            else:
                                P.add("act", lambda e, d_=d_, g2=g2, part=part, pb=pb, ncol=ncol: e.activation(
                                    out=Pt[:, part, d_, g2, 0:ncol], in_=pP[pb][:, 0:ncol], func=AF.Copy),
                                    reads=["pP%d" % pb], writes=[("Pt", part, d_, g2)])
            P.barrier()
        s12.close()
        with ExitStack() as ph:
            St = [sb("St%d" % i, [128, 4, 64], F32, ph) for i in range(2)]
            C4 = sb("C4", [128, 4, 64], F32, ph)
            rt1 = sb("rt1", [128, 4, 64], F32, ph)
            rt2 = sb("rt2", [128, 2, 64], F32, ph)
            P.add("dve", lambda e: e.memset(St[0][:], 0.0), writes=["St0"])
            P.add("dve", lambda e: e.tensor_copy(C4[:, 0:2, :], Acplx[:, 0:1, :].to_broadcast([128, 2, 64])), reads=["Acplx"], writes=["C4"])
            P.add("dve", lambda e: e.tensor_scalar(out=C4[:, 2, :], in0=Acplx[:, 1, :], scalar1=-1.0, scalar2=None, op0=ALU.mult),
                  reads=["Acplx", "C4"], writes=["C4"])
            P.add("dve", lambda e: e.tensor_copy(C4[:, 3, :], Acplx[:, 1, :]), reads=["Acplx", "C4"], writes=["C4"])
            P.add("dve", lambda e: e.memset(St[1][:], 0.0), writes=["St1"])
            AR = Acplx[:, 0, 32:64]
            AI = Acplx[:, 1, 32:64]
            E16 = sb("E16", [128, 2, 32, 16], F32, ph)
            Am = sb("Am", [128, 2, 2, 32], F32, ph)
            ct_ = sb("cmt", [128, 2, 32, 8], F32, ph)

            def cmul(o_re, o_im, a_re, a_im, b_re, b_im, shp, rk, wk):
                t1 = ct_[:, 0].rearrange("p g k -> p (g k)")[:, 0:shp[1] * (shp[2] if len(shp) > 2 else 1)]
                t2 = ct_[:, 1].rearrange("p g k -> p (g k)")[:, 0:shp[1] * (shp[2] if len(shp) > 2 else 1)]
                if len(shp) > 2:
                    t1 = t1.rearrange("p (g k) -> p g k", k=shp[2])
                    t2 = t2.rearrange("p (g k) -> p g k", k=shp[2])
                seq = [
                    lambda e: e.tensor_tensor(out=t1, in0=a_re, in1=b_re, op=ALU.mult),
                    lambda e: e.tensor_tensor(out=t2, in0=a_im, in1=b_im, op=ALU.mult),
                    lambda e: e.tensor_tensor(out=o_re, in0=t1, in1=t2, op=ALU.subtract),
                    lambda e: e.tensor_tensor(out=t1, in0=a_re, in1=b_im, op=ALU.mult),
                    lambda e: e.tensor_tensor(out=t2, in0=a_im, in1=b_re, op=ALU.mult),
                    lambda e: e.tensor_tensor(out=o_im, in0=t1, in1=t2, op=ALU.add),
                ]
                for f_ in seq:
                    P.add("dve", f_, reads=["cmt"] + rk, writes=["cmt"] + wk)

            P.add("dve", lambda e: e.memset(E16[:, 0, :, 0:1], 1.0), writes=["E16"])
            P.add("dve", lambda e: e.memset(E16[:, 1, :, 0:1], 0.0), reads=["E16"], writes=["E16"])
            P.add("dve", lambda e: e.tensor_copy(E16[:, :, :, 1], Acplx[:, :, 32:64]), reads=["Acplx", "E16"], writes=["E16"])
            P.add("dve", lambda e: e.tensor_copy(Am[:, 0], Acplx[:, :, 32:64]), reads=["Acplx"], writes=["Am"])
            cur_a = 0
            m = 1
            while m < 16:
                src_, dst_ = Am[:, cur_a], Am[:, 1 - cur_a]
                if m >= 2:
                    pass
                m2 = m * 2 if m > 1 else 2
                if m == 1:
                    cmul(dst_[:, 0], dst_[:, 1], src_[:, 0], src_[:, 1], src_[:, 0], src_[:, 1], [128, 32], ["Am"], ["Am"])
                    cur_a = 1 - cur_a
                    mm_ = 2
                    am = Am[:, cur_a]
                    cmul(E16[:, 0, :, 2:4], E16[:, 1, :, 2:4], E16[:, 0, :, 0:2], E16[:, 1, :, 0:2],
                         am[:, 0].unsqueeze(2).to_broadcast([128, 32, 2]), am[:, 1].unsqueeze(2).to_broadcast([128, 32, 2]),
                         [128, 32, 2], ["Am", "E16"], ["E16"])
                    m = 2
                    continue
                src_, dst_ = Am[:, cur_a], Am[:, 1 - cur_a]
                cmul(dst_[:, 0], dst_[:, 1], src_[:, 0], src_[:, 1], src_[:, 0], src_[:, 1], [128, 32], ["Am"], ["Am"])
                cur_a = 1 - cur_a
                am = Am[:, cur_a]
                w_ = 2 * m
                if w_ < 16:
                    cmul(E16[:, 0, :, w_:2 * w_], E16[:, 1, :, w_:2 * w_], E16[:, 0, :, 0:w_], E16[:, 1, :, 0:w_],
                         am[:, 0].unsqueeze(2).to_broadcast([128, 32, w_]), am[:, 1].unsqueeze(2).to_broadcast([128, 32, w_]),
                         [128, 32, w_], ["Am", "E16"], ["E16"])
                m = w_
            A16 = Am[:, cur_a]
            ptmp = sb("ptmp", [128, 32, 10, 16], F32, ph)
            pr = sb("pr", [128, 4, 32, 10], F32, ph)
            Sb = sb("Sb", [128, 2, 32, 10], F32, ph)
            for idx, (pp, ep) in enumerate([(0, 0), (1, 1), (0, 1), (1, 0)]):
                Pv = Pt[:, pp, 1, :, 128:288].rearrange("p g (b k) -> p g b k", k=16)
                Eb = E16[:, ep].unsqueeze(2).to_broadcast([128, 32, 10, 16])
                P.add("dve", lambda e, Pv=Pv, Eb=Eb: e.tensor_tensor(out=ptmp[:], in0=Pv, in1=Eb, op=ALU.mult),
                      reads=["Pt", "E16"], writes=["ptmp"])
                P.add("dve", lambda e, idx=idx: e.tensor_reduce(out=pr[:, idx], in_=ptmp[:], axis=AX.X, op=ALU.add),
                      reads=["ptmp"], writes=[("pr", idx)])
            P.add("dve", lambda e: e.tensor_tensor(out=Sb[:, 0], in0=pr[:, 0], in1=pr[:, 1], op=ALU.subtract), reads=["pr"], writes=[("Sb", 0)])
            P.add("dve", lambda e: e.tensor_tensor(out=Sb[:, 1], in0=pr[:, 2], in1=pr[:, 3], op=ALU.add), reads=["pr"], writes=[("Sb", 1)])
            Hh = sb("Hh", [128, 2, 2, 32], F32, ph)
            P.add("dve", lambda e: e.tensor_copy(Hh[:, 0], Sb[:, :, :, 9]), reads=["Sb"], writes=["Hh"])
            hc_ = 0
            for b_ in range(8, -1, -1):
                hs, hd = Hh[:, hc_], Hh[:, 1 - hc_]
                cmul(hd[:, 0], hd[:, 1], A16[:, 0], A16[:, 1], hs[:, 0], hs[:, 1], [128, 32], ["Am", "Hh"], ["Hh"])
                P.add("dve", lambda e, hd=hd, b_=b_: e.tensor_tensor(out=hd, in0=hd, in1=Sb[:, :, :, b_], op=ALU.add),
                      reads=["Hh", "Sb"], writes=["Hh"])
                hc_ = 1 - hc_
            Hf = Hh[:, hc_]
            for slot, part in ((0, 0), (1, 1), (2, 1), (3, 0)):
                P.add("dve", lambda e, slot=slot, part=part: e.tensor_copy(St[0][:, slot, 32:64], Hf[:, part]), reads=["Hh", "St0"], writes=["St0"])
            P.add("act", lambda e: e.activation(out=Pt[:, :, 1, :, 128], in_=Hf, func=AF.Copy), reads=["Hh"], writes=[("PtS", "bnd")])

            Pt_full = Pt[:]
            pstep = Pt_full.ap[0][0]
            PART = 2 * 32 * 288
            for i in range(160):
                cur, nxt = St[i % 2], St[(i + 1) % 2]
                ck, nk = "St%d" % (i % 2), "St%d" % ((i + 1) % 2)
                qF, qB = i, 159 - i
                if i >= 32:
                    cs = slice(0, 64)
                    dd = [[32 * 288 + qB - qF, 2], [288, 32]]
                    off = Pt_full.offset + qF
                    vv = lambda t_, a, b: t_[:, a:b, :].rearrange("p a (d g) -> p a d g", d=2)
                    swp = [[32, 2], [1, 32]]
                else:
                    cs = slice(0, 32)
                    dd = [[288, 32]]
                    off = Pt_full.offset + qF
                    vv = lambda t_, a, b: t_[:, a:b, 0:32]
                    swp = [[1, 32]]
                pap = bass.AP(Pt_full.tensor, off, [[pstep, 128], [PART, 2]] + dd)
                pap_sw = bass.AP(Pt_full.tensor, off + PART, [[pstep, 128], [-PART, 2]] + dd)
                P.add("dve", lambda e, cur=cur, cs=cs: e.tensor_tensor(out=rt1[:, :, cs], in0=C4[:, :, cs], in1=cur[:, :, cs], op=ALU.mult),
                      reads=[ck, "C4"], writes=["rt1"])
                P.add("dve", lambda e, cs=cs: e.tensor_tensor(out=rt2[:, :, cs], in0=rt1[:, 0:2, cs], in1=rt1[:, 2:4, cs], op=ALU.add),
                      reads=["rt1"], writes=["rt2"])
                P.add("dve", lambda e, nxt=nxt, vv=vv, pap=pap: e.tensor_tensor(out=vv(nxt, 0, 2), in0=vv(rt2, 0, 2), in1=pap, op=ALU.add),
                      reads=["rt2", ("PtS", i)], writes=[(nk, 0)])
                sw_in = bass.AP(rt2[:].tensor, rt2[:].offset + 64, [[rt2[:].ap[0][0], 128], [-64, 2]] + swp)
                P.add("dve", lambda e, nxt=nxt, vv=vv, pap_sw=pap_sw, sw_in=sw_in: e.tensor_tensor(out=vv(nxt, 2, 4), in0=sw_in, in1=pap_sw, op=ALU.add),
                      reads=["rt2", ("PtS", i)], writes=[(nk, 1)])
                P.add("act", lambda e, nxt=nxt, vv=vv, pap=pap: e.activation(out=pap, in_=vv(nxt, 0, 2), func=AF.Copy),
                      reads=[(nk, 0)], writes=[("PtS", i)])
            P.barrier()
        if debug:
            d_H = dout("d_H", [128, 2 * 2 * 32 * 288], BF16)
            P.add("sp", lambda e: e.dma_start(out=d_H, in_=Pt[:].rearrange("p a b c d -> p (a b c d)")), reads=["Pt", "PtS"],
                  writes=["d_H"], group="dbgH")

        with ExitStack() as ph:
            Ysb = sb("Ysb", [128, 64, 128], BF16, ph)
            tch = [sb("tch%d" % i, [128, 2, 8, 128], BF16, ph) for i in range(1)] * 2
            w2ch = [sb("w2ch%d" % i, [128, 2, 4, 2, 128], BF16, ph) for i in range(1)] * 2
            pY = [ps("pY%d" % i, [128, 4, 128], F32, ph) for i in range(2)]
            for q in range(8):
                b_ = 0
                for d_ in range(2):
                    P.add("sp", lambda e, q=q, b_=b_, d_=d_: e.dma_start(
                        out=tch[b_][:, d_], in_=scrT[d_, q * 8:(q + 1) * 8].rearrange("g r c -> r g c")),
                        reads=["scrT"], writes=[("tch", b_, d_)], group="tch%d" % b_)
                    P.add("sp", lambda e, q=q, b_=b_, d_=d_: e.dma_start(
                        out=w2ch[b_][:, d_], in_=scrW2[d_, q * 4:(q + 1) * 4].rearrange("g part r c -> r g part c")),
                        reads=["scrW2"], writes=[("w2ch", b_, d_)], group="w2ch%d" % b_)
                for hh in range(2):
                    pb = (q * 2 + hh) % 2

                    def mmY(e, q=q, hh=hh, pb=pb, b_=b_):
                        ins = None
                        for j in range(4):
                            gl = hh * 4 + j
                            g = q * 8 + gl
                            g2, gp = g // 2, g % 2
                            g2l = g2 - q * 4
                            rows = slice(gp * 64, (gp + 1) * 64)
                            o = pY[pb][:, j, :]
                            e.matmul(o, lhsT=tch[b_][:, 0, gl, :], rhs=U[:, g, 0:128], start=True, stop=False)
                            e.matmul(o, lhsT=w2ch[b_][rows, 0, g2l, 0, :], rhs=Pt[rows, 0, 0, g2, 31:159], start=False, stop=False)
                            e.matmul(o, lhsT=w2ch[b_][rows, 0, g2l, 1, :], rhs=Pt[rows, 1, 0, g2, 31:159], start=False, stop=False)
                            e.matmul(o, lhsT=tch[b_][:, 1, gl, :], rhs=U[:, g, 0:128], start=False, stop=False)
                            e.matmul(o, lhsT=w2ch[b_][rows, 1, g2l, 0, :], rhs=Pt[rows, 0, 1, g2, 1:129], start=False, stop=False)
                            ins = e.matmul(o, lhsT=w2ch[b_][rows, 1, g2l, 1, :], rhs=Pt[rows, 1, 1, g2, 1:129], start=False, stop=True)
                        return ins
                    P.add("pe", mmY, reads=[("tch", b_), ("w2ch", b_), "U", "Pt", "PtS"], writes=["pY%d" % pb])
                    g0 = q * 8 + hh * 4
                    if hh == 0:
                        P.add("dve", lambda e, g0=g0, pb=pb: e.tensor_copy(Ysb[:, g0:g0 + 4, :], pY[pb][:]), reads=["pY%d" % pb],
                              writes=[("Ysb", g0)])
                    else:
                        P.add("act", lambda e, g0=g0, pb=pb: e.activation(out=Ysb[:, g0:g0 + 4, :], in_=pY[pb][:], func=AF.Copy),
                              reads=["pY%d" % pb], writes=[("Ysb", g0)])
            pZ = [ps("pZ%d" % i, [128, 512], F32, ph) for i in range(2)]
            yf = [sb("yf%d" % i, [128, 512], F32, ph) for i in range(2)]
            ya = [sb("ya%d" % i, [128, 512], F32, ph) for i in range(2)]
            for ct in range(8):
                chains = []
                for half in range(2):
                    pb = half

                    def uns(e, ct=ct, half=half, pb=pb):
                        ins = None
                        ov = pZ[pb][:, :].rearrange("p (j t) -> p t j", t=8)
                        for t_ in range(8):
                            for gl in range(8):
                                ins = e.matmul(ov[:, t_, :], lhsT=Z[:, t_, gl, :], rhs=Ysb[:, ct * 8 + gl, half * 64:(half + 1) * 64],
                                               start=(gl == 0), stop=(gl == 7))
                        return ins
                    P.add("pe", uns, reads=["Z", "Ysb"], writes=["pZ%d" % pb])
                    tk = slice(half * 512, (half + 1) * 512)
                    yk, ak = "yf%d" % pb, "ya%d" % pb
                    chains.append([
                        ("dve", lambda e, ct=ct, pb=pb, half=half: e.scalar_tensor_tensor(
                            out=yf[pb][:].rearrange("p (j s) -> p j s", s=8),
                            in0=uTown[:, ct, :].rearrange("p (s j) -> p j s", s=8)[:, half * 64:(half + 1) * 64, :],
                            scalar=colB[:, ct:ct + 1], in1=pZ[pb][:, :].rearrange("p (j s) -> p j s", s=8), op0=ALU.mult, op1=ALU.add),
                         ["uT", "colB", "pZ%d" % pb], [yk]),
                        ("act", lambda e, pb=pb: e.activation(out=ya[pb][:], in_=yf[pb][:], func=AF.Square), [yk], [ak]),
                        ("dve", lambda e, pb=pb: e.tensor_scalar(out=ya[pb][:], in0=ya[pb][:], scalar1=0.044715, scalar2=1.0,
                                                                 op0=ALU.mult, op1=ALU.add), [ak], [ak]),
                        ("dve", lambda e, pb=pb: e.tensor_tensor(out=ya[pb][:], in0=ya[pb][:], in1=yf[pb][:], op=ALU.mult), [ak, yk], [ak]),
                        ("act", lambda e, pb=pb: e.activation(out=ya[pb][:], in_=ya[pb][:], func=AF.Sigmoid, scale=1.5957691216057308),
                         [ak], [ak]),
                        ("dve", lambda e, pb=pb, ct=ct, tk=tk: e.tensor_tensor(out=gT[:, ct, tk], in0=ya[pb][:], in1=yf[pb][:], op=ALU.mult),
                         [ak, yk], [("gT", ct, half)]),
                    ])
                for k in range(6):
                    for ch in chains:
                        en, f_, rk, wk = ch[k]
                        P.add(en, f_, reads=rk, writes=wk)
            P.barrier()
        ssm.close()
        if debug:
            d_gT = dout("d_gT", [128, 8 * NOWN], BF16)
            P.add("sp", lambda e: e.dma_start(out=d_gT, in_=gT[:].rearrange("p a b -> p (a b)")), reads=["gT"],
                  writes=["d_gT"], group="dbgG")

        ssmT = sb("ssmT", [128, 8, NOWN], BF16, mix)
        rstd = sb("rstdSC", [128, 2, NOWN], F32, mix)
        with ExitStack() as ph:
            wglu = sb("wglu", [128, 8, 2048], BF16, ph)
            wgv = w_glu.rearrange("(kt p) c -> p kt c", p=128)
            for kt in range(8):
                for hc in range(2):
                    P.add("pool", lambda e, kt=kt, hc=hc: e.dma_start(out=wglu[:, kt, hc * 1024:(hc + 1) * 1024],
                                                                      in_=wgv[:, kt, hc * 1024:(hc + 1) * 1024]),
                          writes=[("wglu", kt, hc)], group="wglu")
            pga = [ps("pga%d" % i, [128, 512], F32, ph) for i in range(2)]
            pgb = [ps("pgb%d" % i, [128, 512], F32, ph) for i in range(2)]
            pss = ps("pss", [128, 512], F32, ph)
            sig = [sb("sig%d" % i, [128, 512], F32, ph) for i in range(2)]
            sq = [sb("sq%d" % i, [128, 512], BF16, ph) for i in range(2)]
            pend = []
            for which in range(2):
                for half in range(2):
                    tk = slice(half * 512, (half + 1) * 512)
                    for ot in range(8):
                        b_ = ot % 2
                        if which == 0:
                            def mg(e, ot=ot, b_=b_, tk=tk, off=0, pp=pga):
                                ins = None
                                for kt in range(8):
                                    ins = e.matmul(pp[b_][:, :], lhsT=wglu[:, kt, off + ot * 128:off + (ot + 1) * 128], rhs=gT[:, kt, tk],
                                                   start=(kt == 0), stop=(kt == 7))
                                return ins
                            P.add("pe", mg, reads=["wglu", "gT"], writes=["pga%d" % b_])
                            P.add("pe", lambda e, ot=ot, b_=b_, tk=tk: mg(e, ot, b_, tk, 1024, pgb), reads=["wglu", "gT"], writes=["pgb%d" % b_])
                            P.add("act", lambda e, b_=b_: e.activation(out=sig[b_][:], in_=pgb[b_][:], func=AF.Sigmoid),
                                  reads=["pgb%d" % b_], writes=["sig%d" % b_])
                            P.add("dve", lambda e, b_=b_, ot=ot, tk=tk: e.tensor_tensor(out=ssmT[:, ot, tk], in0=pga[b_][:], in1=sig[b_][:], op=ALU.mult),
                                  reads=["pga%d" % b_, "sig%d" % b_], writes=[("ssmT", ot, half)])
                            srcT, skey = ssmT, ("ssmT", ot, half)
                        else:
                            srcT, skey = convT, "convT"
                        def back(b_=b_, ot=ot, tk=tk, srcT=srcT, skey=skey):
                            P.add("act", lambda e: e.activation(out=sq[b_][:], in_=srcT[:, ot, tk], func=AF.Square),
                                  reads=[skey], writes=["sq%d" % b_])
                            P.add("pe", lambda e: e.matmul(pss[:, :], lhsT=ones_b[:], rhs=sq[b_][:], start=(ot == 0), stop=(ot == 7)),
                                  reads=["ones_b", "sq%d" % b_], writes=["pss"])
                        if pend:
                            pend.pop()()
                        pend.append(back)
                    if pend:
                        pend.pop()()
                    rk = ("rstd", which, half)
                    P.add("dve", lambda e, which=which, tk=tk: e.tensor_scalar(out=rstd[:, which, tk], in0=pss[:, :], scalar1=1.0 / 1024, scalar2=EPS,
                                                                              op0=ALU.mult, op1=ALU.add), reads=["pss"], writes=[rk])
                    P.add("act", lambda e, which=which, tk=tk: e.activation(out=rstd[:, which, tk], in_=rstd[:, which, tk], func=AF.Sqrt),
                          reads=[rk], writes=[rk])
                    P.add("dve", lambda e, which=which, tk=tk: e.reciprocal(out=rstd[:, which, tk], in_=rstd[:, which, tk]), reads=[rk], writes=[rk])
            for ot in range(8):
                P.add("dve", lambda e, ot=ot: e.scalar_tensor_tensor(out=ssmT[:, ot, :], in0=ssmT[:, ot, :], scalar=colA[:, 80 + ot:81 + ot],
                                                                     in1=rstd[:, 0, :], op0=ALU.mult, op1=ALU.mult),
                      reads=["ssmT", "rstd", "colA"], writes=[("ssmT", ot)])
                P.add("dve", lambda e, ot=ot: e.scalar_tensor_tensor(out=convT[:, ot, :], in0=convT[:, ot, :], scalar=colA[:, 88 + ot:89 + ot],
                                                                      in1=rstd[:, 1, :], op0=ALU.mult, op1=ALU.mult),
                      reads=["convT", "rstd", "colA"], writes=[("convT", ot)])
            P.barrier()

        def row_bcast(dst, col_of_ft, rkeys, wkey, stack_ps):
            dgs = [sb(wkey + "_dg%d" % i, [128, 128], F32, stack_ps) for i in range(2)]
            prb = ps(wkey + "_prb", [128, 512], F32, stack_ps)
            for c4 in range(4):
                for j in range(4):
                    ft = c4 * 4 + j
                    b_ = ft % 2
                    P.add("dve", lambda e, ft=ft, b_=b_: e.tensor_scalar(out=dgs[b_][:], in0=ident_f[:], scalar1=col_of_ft(ft), scalar2=None,
                                                                        op0=ALU.mult), reads=["ident_f"] + rkeys, writes=[wkey + "_dg%d" % b_])
                    P.add("pe", lambda e, j=j, b_=b_: e.matmul(prb[:, j * 128:(j + 1) * 128], lhsT=ones_f[:], rhs=dgs[b_][:], start=True, stop=True),
                          reads=["ones_f", wkey + "_dg%d" % b_], writes=[wkey + "_prb"])
                P.add("act", lambda e, c4=c4: e.activation(out=dst[:, c4 * 512:(c4 + 1) * 512], in_=prb[:, :], func=AF.Copy),
                      reads=[wkey + "_prb"], writes=[wkey])

        with ExitStack() as ph:
            g1b = sb("g1b", [128, D], F32, ph)
            with ExitStack() as ph3:
                row_bcast(g1b, lambda ft: modT[:, 32 + ft, 0:1], ["modT"], "g1b", ph3)
                P.barrier()
            woc = [sb("woc%d" % i, [128, 16, 512], BF16, ph) for i in range(2)]
            wov = w_out.rearrange("(kt p) c -> p kt c", p=128)
            po = [ps("po%d" % i, [128, 512], F32, ph) for i in range(6)]
            xp = [sb("xp%d" % i, [128, 512], F32, ph) for i in range(8)]
            x1p = [sb("x1p%d" % i, [128, 512], F32, ph) for i in range(4)]
            jk = sb("jk", [128, 512], BF16, ph)
            its = [(cc, tt) for cc in range(4) for tt in range(8)]

            def ld(n):
                cc, tt = its[n]
                P.add("sp", lambda e, n=n, cc=cc, tt=tt: e.dma_start(out=xp[n % 8][:], in_=xs[tt * 128:(tt + 1) * 128, cc * 512:(cc + 1) * 512]),
                      writes=[("xp", n % 8)], group="xp%d" % (n % 8))
            for n in range(6):
                ld(n)
            for n, (cc, tt) in enumerate(its):
                wb_ = cc % 2
                if tt == 0:
                    for kt in range(16):
                        P.add("pool", lambda e, kt=kt, cc=cc, wb_=wb_: e.dma_start(out=woc[wb_][:, kt, :], in_=wov[:, kt, cc * 512:(cc + 1) * 512]),
                              writes=[("woc", wb_, kt)], group="woc%d" % wb_)
                if n + 6 < len(its):
                    ld(n + 6)
                rows = slice(tt * 128, (tt + 1) * 128)
                cols = slice(cc * 512, (cc + 1) * 512)
                pb, xb_, sb_ = n % 6, n % 8, n % 4

                def mo(e, tt=tt, pb=pb, wb_=wb_):
                    ins = None
                    for ht in range(16):
                        hsrc = ssmT if ht < 8 else convT
                        ins = e.matmul(po[pb][:, :], lhsT=hsrc[:, ht % 8, tt * 128:(tt + 1) * 128], rhs=woc[wb_][:, ht, :],
                                       start=(ht == 0), stop=(ht == 15))
                    return ins
                P.add("pe", mo, reads=["ssmT", "convT", ("woc", wb_)], writes=["po%d" % pb])
                P.add("dve", lambda e, pb=pb, sb_=sb_, cols=cols: e.tensor_tensor(out=x1p[sb_][:], in0=po[pb][:, :], in1=g1b[:, cols], op=ALU.mult),
                      reads=["po%d" % pb, "g1b"], writes=[("x1p", sb_)])
                P.add("dve", lambda e, sb_=sb_, xb_=xb_: e.tensor_tensor(out=x1p[sb_][:], in0=x1p[sb_][:], in1=xp[xb_][:], op=ALU.add),
                      reads=[("x1p", sb_), ("xp", xb_)], writes=[("x1p", sb_)])
                P.add("act", lambda e, sb_=sb_, tt=tt, cc=cc: e.activation(out=jk[:], in_=x1p[sb_][:], func=AF.Square,
                                                                        accum_out=ss2[:, tt, cc:cc + 1]),
                      reads=[("x1p", sb_)], writes=["jk", ("ss2", tt, cc)])
                P.add("act", lambda e, sb_=sb_, rows=rows, cols=cols: e.dma_start(out=scrX1[rows, cols], in_=x1p[sb_][:]),
                      reads=[("x1p", sb_)], writes=["scrX1"], group="x1p%d" % sb_)
            P.barrier()
        mix.close()
        mixer.close()

        moe = top.enter_context(ExitStack())
        acc = sb("acc", [128, 8, D], F32, moe)
        hx2T = sb("hx2T", [128, 16, NOWN], BF16, moe)
        Wt = sb("Wt", [128, 8, 64], F32, moe)
        with ExitStack() as ph:
            sc2b = sb("sc2b", [128, D], F32, ph)
            sh2b = sb("sh2b", [128, D], F32, ph)
            scale2 = sb("scale2", [128, 16], F32, ph)
            P.add("dve", lambda e: e.scalar_tensor_tensor(out=scale2[:], in0=modT[:, 64:80, 0], scalar=1.0, in1=colA[:, 48:64],
                                                          op0=ALU.add, op1=ALU.mult), reads=["modT", "colA"], writes=["scale2"])
            with ExitStack() as ph3:
                row_bcast(sc2b, lambda ft: scale2[:, ft:ft + 1], ["scale2"], "sc2b", ph3)
                P.barrier()
            with ExitStack() as ph3:
                row_bcast(sh2b, lambda ft: modT[:, 48 + ft, 0:1], ["modT"], "sh2b", ph3)
                P.barrier()
            rw = sb("rw", [128, 16, 64], F32, ph)
            P.add("sp", lambda e: e.dma_start(out=rw[:], in_=router_w.rearrange("(kt p) c -> p kt c", p=128)), writes=["rw"], group="rw")
            rb = sb("rb", [128, 64], F32, ph)
            P.add("sp", lambda e: e.dma_start(out=rb[:], in_=rbias_b), writes=["rb"], group="rb")
            rs2 = sb("rs2", [128, 8], F32, ph)
            P.add("dve", lambda e: e.tensor_reduce(out=rs2[:], in_=ss2[:], axis=AX.X, op=ALU.add), reads=["ss2"], writes=["rs2"])
            P.add("dve", lambda e: e.tensor_scalar(out=rs2[:], in0=rs2[:], scalar1=1.0 / D, scalar2=EPS, op0=ALU.mult, op1=ALU.add),
                  reads=["rs2"], writes=["rs2"])
            P.add("act", lambda e: e.activation(out=rs2[:], in_=rs2[:], func=AF.Sqrt), reads=["rs2"], writes=["rs2"])
            P.add("dve", lambda e: e.reciprocal(out=rs2[:], in_=rs2[:]), reads=["rs2"], writes=["rs2"])
            x1t = [sb("x1t%d" % i, [128, D], F32, ph) for i in range(3)]
            hf = [sb("hf%d" % i, [128, D], F32, ph) for i in range(3)]
            pth = [ps("pth%d" % i, [128, 4, 128], F32, ph) for i in range(2)]
            hfs = [sb("hfs%d" % i, [128, 4, 128], F32, ph) for i in range(2)]
            plgs = [ps("plg%d" % i, [128, 64], F32, ph) for i in range(2)]
            rt = sb("rt", [128, 12, 64], F32, ph)
            m8 = sb("m8", [128, 16], F32, ph)

            def n2A(tt):
                b_ = tt % 3
                rows = slice(tt * 128, (tt + 1) * 128)
                P.add("sp", lambda e, b_=b_, rows=rows: e.dma_start(out=x1t[b_][:], in_=scrX1[rows, :]), reads=["scrX1"],
                      writes=[("x1t", b_)], group="x1t%d" % b_)
                P.add("act", lambda e, b_=b_, tt=tt: e.activation(out=hf[b_][:], in_=x1t[b_][:], func=AF.Copy, scale=rs2[:, tt:tt + 1]),
                      reads=[("x1t", b_), "rs2"], writes=[("hf", b_)])
                P.add("dve", lambda e, b_=b_: e.tensor_tensor(out=hf[b_][:], in0=hf[b_][:], in1=sc2b[:], op=ALU.mult),
                      reads=[("hf", b_), "sc2b"], writes=[("hf", b_)])
                P.add("dve", lambda e, b_=b_: e.tensor_tensor(out=hf[b_][:, 0:1280], in0=hf[b_][:, 0:1280], in1=sh2b[:, 0:1280], op=ALU.add),
                      reads=[("hf", b_), "sh2b"], writes=[("hf", b_, 0)])
                P.add("pool", lambda e, b_=b_: e.tensor_tensor(out=hf[b_][:, 1280:2048], in0=hf[b_][:, 1280:2048], in1=sh2b[:, 1280:2048], op=ALU.add),
                      reads=[("hf", b_), "sh2b"], writes=[("hf", b_, 1)])

            def n2B(tt):
                b_ = tt % 3
                plg = plgs[tt % 2]
                pk = "plg%d" % (tt % 2)

                def tr_blk(f4):
                    pb = f4 % 2

                    def trh(e, b_=b_, f4=f4, pb=pb):
                        ins = None
                        for j in range(4):
                            ft = f4 * 4 + j
                            ins = e.transpose(out=pth[pb][:, j, :], in_=hf[b_][:, ft * 128:(ft + 1) * 128], identity=ident_f[:])
                        return ins
                    P.add("pe", trh, reads=[("hf", b_), "ident_f"], writes=["pth%d" % pb])
                    P.add("act", lambda e, pb=pb, f4=f4, tt=tt: e.activation(out=hx2T[:, f4 * 4:(f4 + 1) * 4, tt * 128:(tt + 1) * 128],
                                                                            in_=pth[pb][:], func=AF.Copy),
                          reads=["pth%d" % pb], writes=[("hx2T", f4, tt)])
                    P.add("dve", lambda e, pb=pb: e.tensor_copy(hfs[pb][:], pth[pb][:]), reads=["pth%d" % pb], writes=[("hfs", pb)])

                def mr_blk(f4):
                    pb = f4 % 2

                    def mr(e, pb=pb, f4=f4, plg=plg):
                        ins = None
                        for j in range(4):
                            ft = f4 * 4 + j
                            ins = e.matmul(plg[:, :], lhsT=hfs[pb][:, j, :], rhs=rw[:, ft, :], start=(ft == 0), stop=(ft == 15))
                        return ins
                    P.add("pe", mr, reads=[("hfs", pb), "rw"], writes=[pk])
                tr_blk(0)
                for f4 in range(4):
                    if f4 + 1 < 4:
                        tr_blk(f4 + 1)
                    mr_blk(f4)

            def n2C(tt):
                plg = plgs[tt % 2]
                pk = "plg%d" % (tt % 2)
                S_, Bi, T1, T2, MB, EM = (rt[:, i, :] for i in range(6))
                g3 = lambda ap: ap.rearrange("p (g k) -> p g k", k=8)
                rops = [
                    ("act", lambda e: e.activation(out=S_, in_=plg[:, :], func=AF.Sigmoid), [pk]),
                    ("dve", lambda e: e.tensor_tensor(out=Bi, in0=S_, in1=rb[:], op=ALU.add), ["rb"]),
                    ("dve", lambda e: e.tensor_reduce(out=m8[:, 0:8], in_=g3(Bi), axis=AX.X, op=ALU.max), []),
                    ("dve", lambda e: e.tensor_tensor(out=g3(T1), in0=g3(Bi), in1=m8[:, 0:8].unsqueeze(2).to_broadcast([128, 8, 8]), op=ALU.is_equal), []),
                    ("dve", lambda e: e.scalar_tensor_tensor(out=T1, in0=T1, scalar=-1e9, in1=Bi, op0=ALU.mult, op1=ALU.add), []),
                    ("dve", lambda e: e.tensor_reduce(out=m8[:, 8:16], in_=g3(T1), axis=AX.X, op=ALU.max), []),
                    ("dve", lambda e: e.tensor_tensor(out=m8[:, 0:8], in0=m8[:, 0:8], in1=m8[:, 8:16], op=ALU.add), []),
                    ("dve", lambda e: e.max(out=m8[:, 8:16], in_=m8[:, 0:8]), []),
                    ("dve", lambda e: e.tensor_scalar(out=m8[:, 0:8], in0=m8[:, 0:8], scalar1=m8[:, 11:12], scalar2=None, op0=ALU.is_ge), []),
                    ("dve", lambda e: e.tensor_tensor(out=g3(MB), in0=g3(Bi), in1=m8[:, 0:8].unsqueeze(2).to_broadcast([128, 8, 8]), op=ALU.mult), []),
                    ("dve", lambda e: e.tensor_scalar(out=m8[:, 0:8], in0=m8[:, 0:8], scalar1=-1.0, scalar2=1e9, op0=ALU.add, op1=ALU.mult), []),
                    ("dve", lambda e: e.tensor_tensor(out=g3(MB), in0=g3(MB), in1=m8[:, 0:8].unsqueeze(2).to_broadcast([128, 8, 8]), op=ALU.add), []),
                    ("dve", lambda e: e.max(out=m8[:, 8:16], in_=MB), []),
                    ("dve", lambda e: e.tensor_scalar(out=EM, in0=MB, scalar1=m8[:, 15:16], scalar2=None, op0=ALU.is_ge), []),
                    ("dve", lambda e: e.tensor_tensor(out=T2, in0=S_, in1=EM, op=ALU.mult), []),
                    ("dve", lambda e: e.tensor_reduce(out=m8[:, 0:1], in_=T2, axis=AX.X, op=ALU.add), []),
                    ("dve", lambda e: e.reciprocal(out=m8[:, 0:1], in_=m8[:, 0:1]), []),
                    ("dve", lambda e, tt=tt: e.tensor_scalar(out=Wt[:, tt, :], in0=T2, scalar1=m8[:, 0:1], scalar2=2.5, op0=ALU.mult, op1=ALU.mult), []),
                ]
                for (en, f_, rk) in rops:
                    P.add(en, f_, reads=["rt", "m8"] + rk, writes=["rt", "m8", ("Wt", tt)])

            for it in range(10):
                if it < 8:
                    n2A(it)
                if 1 <= it <= 8:
                    n2B(it - 1)
                if it >= 2:
                    n2C(it - 2)
            P.barrier()
        if debug:
            d_Wt = dout("d_Wt", [128, 512])
            P.add("sp", lambda e: e.dma_start(out=d_Wt, in_=Wt[:].rearrange("p a b -> p (a b)")), reads=["Wt"], writes=["d_Wt"], group="dbgW")
            d_hx2T = dout("d_hx2T", [128, 16 * NOWN], BF16)
            P.add("sp", lambda e: e.dma_start(out=d_hx2T, in_=hx2T[:].rearrange("p a b -> p (a b)")), reads=["hx2T"], writes=["d_hx2T"], group="dbgW2")

        with ExitStack() as ph:
            wg = [sb("wg%d" % i, [128, 16, 512], BF16, ph) for i in range(2)]
            wu = [sb("wu%d" % i, [128, 16, 512], BF16, ph) for i in range(2)]
            wd = [sb("wd0", [128, 4, D], BF16, ph)]
            actT = sb("actT", [128, 4, NOWN], BF16, ph)
            sgl = [sb("sgl%d" % i, [128, 512], F32, ph) for i in range(2)]
            pg = [ps("pg%d" % i, [128, 512], F32, ph) for i in range(2)]
            pu = [ps("pu%d" % i, [128, 512], F32, ph) for i in range(2)]
            pd = [ps("pd%d" % i, [128, 512], F32, ph) for i in range(3)]
            P.add("pool", lambda e: e.memset(acc[:], 0.0), writes=["acc"])
            NE = 65
            import os
            DMAONLY = os.environ.get("MOE_DMAONLY", "")
            _Padd = P.add
            if DMAONLY:
                class _PX:
                    @staticmethod
                    def add(eng, fn, reads=(), writes=(), group=None):
                        if group is None:
                            return None
                        q = {"1": "pool", "2": "sp", "3": "act"}[DMAONLY[0]]
                        return _Padd(q if DMAONLY[0] != "4" else eng, fn, reads=reads, writes=writes, group=group)
                PM = _PX
            else:
                PM = P
            for ex in range(NE):
                b_ = ex % 2
                gsrc = ew_gate[ex] if ex < 64 else sw_gate
                usrc = ew_up[ex] if ex < 64 else sw_up
                dsrc = ew_down[ex] if ex < 64 else sw_down
                PM.add("pool", lambda e, b_=b_, gsrc=gsrc: e.dma_start(out=wg[b_][:], in_=gsrc.rearrange("(kt p) c -> p kt c", p=128)),
                      writes=[("wg", b_)], group="wg%d" % b_)
                PM.add("pool", lambda e, b_=b_, usrc=usrc: e.dma_start(out=wu[b_][:], in_=usrc.rearrange("(kt p) c -> p kt c", p=128)),
                      writes=[("wu", b_)], group="wu%d" % b_)
                dv = dsrc.rearrange("(kt p) c -> p kt c", p=128)
                for hc in range(2):
                    PM.add("pool", lambda e, dv=dv, hc=hc: e.dma_start(out=wd[0][:, :, hc * 1024:(hc + 1) * 1024],
                                                                     in_=dv[:, :, hc * 1024:(hc + 1) * 1024]),
                          writes=[("wd", hc)], group="wd")
                n = 0
                for half in range(2):
                    for mt in range(4):
                        pb = n % 2
                        n += 1
                        tk = slice(half * 512, (half + 1) * 512)

                        def mgu(e, w_, pp, mt=mt, tk=tk, pb=pb, b_=b_):
                            ins = None
                            for kt in range(16):
                                ins = e.matmul(pp[pb][:, :], lhsT=w_[b_][:, kt, mt * 128:(mt + 1) * 128], rhs=hx2T[:, kt, tk],
                                               start=(kt == 0), stop=(kt == 15))
                            return ins
                        PM.add("pe", lambda e, f_=mgu: f_(e, wg, pg), reads=[("wg", b_), "hx2T"], writes=["pg%d" % pb])
                        PM.add("pe", lambda e, f_=mgu: f_(e, wu, pu), reads=[("wu", b_), "hx2T"], writes=["pu%d" % pb])
                        PM.add("act", lambda e, pb=pb: e.activation(out=sgl[pb][:], in_=pg[pb][:, :], func=AF.Silu),
                              reads=["pg%d" % pb], writes=[("sgl", pb)])
                        PM.add("dve", lambda e, pb=pb, mt=mt, tk=tk: e.tensor_tensor(out=actT[:, mt, tk], in0=sgl[pb][:], in1=pu[pb][:, :], op=ALU.mult),
                              reads=[("sgl", pb), "pu%d" % pb], writes=[("actT", mt, half)])
                n = 0
                for tt in range(8):
                    for cc in range(4):
                        pb = n % 3
                        n += 1

                        def mdn(e, tt=tt, cc=cc, pb=pb):
                            ins = None
                            for kt in range(4):
                                ins = e.matmul(pd[pb][:, :], lhsT=actT[:, kt, tt * 128:(tt + 1) * 128], rhs=wd[0][:, kt, cc * 512:(cc + 1) * 512],
                                               start=(kt == 0), stop=(kt == 3))
                            return ins
                        PM.add("pe", mdn, reads=[("actT", m_, tt // 4) for m_ in range(4)] + ["wd"], writes=["pd%d" % pb])
                        wsc = Wt[:, tt, ex:ex + 1] if ex < 64 else 1.0
                        PM.add("dve", lambda e, tt=tt, cc=cc, pb=pb, wsc=wsc: e.scalar_tensor_tensor(
                            out=acc[:, tt, cc * 512:(cc + 1) * 512], in0=pd[pb][:, :], scalar=wsc, in1=acc[:, tt, cc * 512:(cc + 1) * 512],
                            op0=ALU.mult, op1=ALU.add), reads=["pd%d" % pb, "Wt"], writes=[("acc", tt, cc)])
            P.barrier()

        with ExitStack() as ph:
            g2b = sb("g2b", [128, D], F32, ph)
            fgb = sb("fgb", [128, D], F32, ph)
            with ExitStack() as ph3:
                row_bcast(g2b, lambda ft: modT[:, 80 + ft, 0:1], ["modT"], "g2b", ph3)
                P.barrier()
            with ExitStack() as ph3:
                row_bcast(fgb, lambda ft: colA[:, 64 + ft:65 + ft], ["colA"], "fgb", ph3)
                P.barrier()
            x1f = [sb("x1f%d" % i, [128, D], F32, ph) for i in range(3)]
            fo = [sb("fo%d" % i, [128, D], F32, ph) for i in range(3)]
            fs = sb("fs", [128, 8], F32, ph)
            fj = sb("fj", [128, D], BF16, ph)
            def stageA(tt):
                b_ = tt % 3
                rows = slice(tt * 128, (tt + 1) * 128)
                for hc in range(2):
                    cs = slice(hc * 1024, (hc + 1) * 1024)
                    P.add("dve", lambda e, tt=tt, cs=cs: e.tensor_tensor(out=acc[:, tt, cs], in0=acc[:, tt, cs], in1=g2b[:, cs], op=ALU.mult),
                          reads=[("acc", tt, hc), "g2b"], writes=[("acc", tt, hc)])
                    P.add("dve", lambda e, tt=tt, b_=b_, cs=cs: e.tensor_tensor(out=acc[:, tt, cs], in0=acc[:, tt, cs], in1=x1f[b_][:, cs], op=ALU.add),
                          reads=[("acc", tt, hc), ("x1f", b_)], writes=[("acc", tt, hc)])
                    P.add("act", lambda e, tt=tt, cs=cs, hc=hc: e.activation(out=fj[:, cs], in_=acc[:, tt, cs], func=AF.Square,
                                                                          accum_out=fs2[:, tt, hc:hc + 1]),
                          reads=[("acc", tt, hc)], writes=[("fj", hc), ("fs2", tt, hc)])

            def stageB(tt):
                b_ = tt % 3
                rows = slice(tt * 128, (tt + 1) * 128)
                P.add("dve", lambda e, tt=tt: e.tensor_tensor(out=fs[:, tt:tt + 1], in0=fs2[:, tt, 0:1], in1=fs2[:, tt, 1:2], op=ALU.add),
                      reads=[("fs2", tt)], writes=[("fs", tt)])
                P.add("dve", lambda e, tt=tt: e.tensor_scalar(out=fs[:, tt:tt + 1], in0=fs[:, tt:tt + 1], scalar1=1.0 / D, scalar2=EPS,
                                                              op0=ALU.mult, op1=ALU.add), reads=[("fs", tt)], writes=[("fs", tt)])
                P.add("act", lambda e, tt=tt: e.activation(out=fs[:, tt:tt + 1], in_=fs[:, tt:tt + 1], func=AF.Sqrt), reads=[("fs", tt)], writes=[("fs", tt)])
                P.add("dve", lambda e, tt=tt: e.reciprocal(out=fs[:, tt:tt + 1], in_=fs[:, tt:tt + 1]), reads=[("fs", tt)], writes=[("fs", tt)])
                P.add("act", lambda e, tt=tt, b_=b_: e.activation(out=fo[b_][:], in_=acc[:, tt, :], func=AF.Copy, scale=fs[:, tt:tt + 1]),
                      reads=[("acc", tt), ("fs", tt)], writes=[("fo", b_)])
                P.add("dve", lambda e, b_=b_: e.tensor_tensor(out=fo[b_][:], in0=fo[b_][:], in1=fgb[:], op=ALU.mult),
                      reads=[("fo", b_), "fgb"], writes=[("fo", b_)])
                P.add("pool", lambda e, b_=b_, rows=rows: e.dma_start(out=out[rows, :], in_=fo[b_][:]), reads=[("fo", b_)], writes=["out"],
                      group="fo%d" % b_)

            fs2 = sb("fs2", [128, 8, 2], F32, ph)

            def ldf(tt):
                P.add("sp", lambda e, tt=tt: e.dma_start(out=x1f[tt % 3][:], in_=scrX1[tt * 128:(tt + 1) * 128, :]), reads=["scrX1"],
                      writes=[("x1f", tt % 3)], group="x1f%d" % (tt % 3))
            ldf(0)
            ldf(1)
            for tt in range(9):
                if tt < 8:
                    stageA(tt)
                if tt + 2 < 8:
                    ldf(tt + 2)
                if tt >= 1:
                    stageB(tt - 1)

        P.add("sp", None, reads=["out", "scrW1", "scrT", "scrW2", "scrU", "scrX1"] + list(dbg.keys()))
        P.emit()
    return nc, dbg


def prep_inputs(inp):
    f = lambda a: np.ascontiguousarray(a, dtype=np.float32)
    x, ctx, c = inp["x"], inp["ctx"], inp["c"]
    maps = []
    shared = {
        "w_ada": f(inp["w_ada"][0]), "w_in": f(inp["w_in"][0]),
        "b_ada": f(inp["b_ada"][0].reshape(96, 128)),
        "w_glu": f(inp["ssm_w_glu"][0]), "w_out": f(inp["w_out"][0]), "router_w": f(inp["router_w"][0]),
        "rbias_b": f(np.tile(inp["router_bias"][0].reshape(1, 64), (128, 1))),
        "ew_gate": f(inp["exp_w_gate"][0]), "ew_up": f(inp["exp_w_up"][0]), "ew_down": f(inp["exp_w_down"][0]),
        "sw_gate": f(inp["shared_w_gate"][0]), "sw_up": f(inp["shared_w_up"][0]), "sw_down": f(inp["shared_w_down"][0]),
    }
    for core in range(8):
        b, h = core // 2, core % 2
        xb = x[b]
        cb = ctx[b]
        conv_w = inp["conv_w"][0]
        if h == 1:
            xb = xb[::-1]
            cb = cb[::-1]
            conv_w = conv_w[::-1]
        vecsA = np.concatenate([c[b].reshape(16, 128), inp["c_ctx"].reshape(16, 128),
                                inp["norm1_g"][0].reshape(16, 128), inp["norm2_g"][0].reshape(16, 128),
                                inp["final_g"].reshape(16, 128), inp["mix_norm_g"][0].reshape(16, 128)], 0)
        vecsB = np.concatenate([inp["ssm_d"][0].reshape(8, 128), conv_w.reshape(24, 128),
                                inp["conv_b"][0].reshape(8, 128)], 0)
        sl = slice(None, None, -1) if h == 1 else slice(None)
        m = dict(shared)
        m.update(xs=f(xb), ctxs=f(cb), vecsA=f(vecsA), vecsB=f(vecsB),
                 lamre_p=f(inp["ssm_lam_re"][0][sl].reshape(64, 128)), lamim_p=f(inp["ssm_lam_im"][0][sl].reshape(64, 128)),
                 logdt_p=f(inp["ssm_log_dt"][0][sl].reshape(64, 2)),
                 ssm_b_re=f(inp["ssm_b_re"][0][sl]), ssm_b_im=f(inp["ssm_b_im"][0][sl]),
                 ssm_c_re=f(inp["ssm_c_re"][0][sl]), ssm_c_im=f(inp["ssm_c_im"][0][sl]))
        maps.append(m)
    return maps


def kernel(**inputs):
    nc, _ = build_nc(False)
    maps = prep_inputs(inputs)
    res = run_bass_kernel_spmd(nc, maps, core_ids=list(range(8)))
    outs = np.zeros((4, 2048, 2048), np.float32)
    for core in range(8):
        b, h = core // 2, core % 2
        o = res.results[core]["out"]
        if h == 0:
            outs[b, 0:1024] = o
        else:
            outs[b, 1024:2048] = o[::-1]
    return outs
```

```python
from contextlib import ExitStack
import numpy as np
import concourse.bass as bass
import concourse.mybir as mybir
from concourse.bass_utils import run_bass_kernel_spmd

F32 = mybir.dt.float32
BF16 = mybir.dt.bfloat16
I32 = mybir.dt.int32
ALU = mybir.AluOpType
AF = mybir.ActivationFunctionType
AX = mybir.AxisListType

D = 2048
NOWN = 1024
NSEQ = 2304
EPS = 1e-6


class Prog:
    ENG = ("pe", "act", "dve", "pool", "sp")

    def __init__(self, nc, stack):
        self.nc = nc
        self.stack = stack
        self.ops = []
        self.keys = {}
        self.groups = {}
        self.psum_names = set()
        self.gopen = {}

    @staticmethod
    def _norm(k):
        return k if isinstance(k, tuple) else (k,)

    def _related(self, key):
        d = self.keys.setdefault(key[0], {})
        for k2 in list(d.keys()):
            n = min(len(k2), len(key))
            if k2[:n] == key[:n]:
                yield k2, d[k2]

    def add(self, eng, fn, reads=(), writes=(), group=None):
        op = dict(id=len(self.ops), eng=eng, fn=fn, deps=set(), group=group, used=False)
        reads = [self._norm(k) for k in reads] + [("__phase",)]
        writes = [self._norm(k) for k in writes]
        pk = [(k[0],) for k in reads + writes if k[0] in self.psum_names]
        reads = [k for k in reads if k[0] not in self.psum_names]
        writes = [k for k in writes if k[0] not in self.psum_names] + sorted(set(pk))
        for key in reads:
            for k2, st in self._related(key):
                if st[0] is not None:
                    op["deps"].add(st[0])
        for key in writes:
            for k2, st in self._related(key):
                if st[0] is not None:
                    op["deps"].add(st[0])
                op["deps"].update(st[1])
        for key in reads:
            d = self.keys.setdefault(key[0], {})
            st = d.setdefault(key, [None, []])
            st[1].append(op["id"])
        for key in writes:
            d = self.keys.setdefault(key[0], {})
            for k2 in list(d.keys()):
                if len(k2) > len(key) and k2[:len(key)] == key:
                    del d[k2]
            d[key] = [op["id"], []]
        op["deps"].discard(op["id"])
        for d in op["deps"]:
            gg = self.ops[d]["group"]
            if gg is not None and d in self.gopen.get(gg, ()):
                self.gopen[gg] = []
        if group is not None:
            self.gopen.setdefault(group, []).append(op["id"])
            op["batch"] = self.gopen[group]
        self.ops.append(op)
        return op

    def barrier(self):
        scr = self._bar_scr
        self.add("dve", lambda e: e.memset(scr[:, 0:1], 0.0), writes=[("__phase",), "barscr"])

    def emit(self):
        nc = self.nc
        ops = self.ops
        for op in ops:
            for d in op["deps"]:
                ops[d]["used"] = True
        sems = {}
        for e in self.ENG:
            sems[e] = self.stack.enter_context(nc.semaphore("s_" + e))
        gsem = {}
        cnt = {e: 0 for e in self.ENG}
        gcnt = {}
        for op in ops:
            if op["group"] is not None:
                g = op["group"]
                if g not in gsem:
                    gsem[g] = self.stack.enter_context(nc.semaphore("g_" + str(g)))
                    gcnt[g] = 0
                gcnt[g] += 16
                op["sig"] = (gsem[g], gcnt[g], 16)
            elif op["used"]:
                cnt[op["eng"]] += 1
                op["sig"] = (sems[op["eng"]], cnt[op["eng"]], 1)
            else:
                op["sig"] = None
        per = {e: [o for o in ops if o["eng"] == e] for e in self.ENG}

        def replay(ename, eng):
            waited = {}
            for op in per[ename]:
                need = {}
                for d in op["deps"]:
                    dop = ops[d]
                    if dop["eng"] == "pe" and ename == "pe" and dop["group"] is None:
                        continue
                    s = dop["sig"]
                    assert s is not None
                    if dop["group"] is not None:
                        s = ops[dop["batch"][-1]]["sig"]
                    key = id(s[0])
                    if key not in need or need[key][1] < s[1]:
                        need[key] = (s[0], s[1])
                for key, (sem, val) in need.items():
                    if waited.get(key, 0) >= val:
                        continue
                    waited[key] = val
                    eng.wait_ge(sem, val)
                if op["fn"] is None:
                    continue
                ins = op["fn"](eng)
                if op["sig"] is not None:
                    ins.then_inc(op["sig"][0], op["sig"][2])

        block = self.stack.enter_context(nc.Block())

        @block.tensor
        def _(eng):
            replay("pe", eng)

        @block.scalar
        def _(eng):
            replay("act", eng)

        @block.vector
        def _(eng):
            replay("dve", eng)

        @block.gpsimd
        def _(eng):
            replay("pool", eng)

        @block.sync
        def _(eng):
            replay("sp", eng)


def build_nc(debug=False, stop=99):
    nc = bass.Bass("TRN2", target_bir_lowering=False)
    dbg = {}

    def din(name, shape, dt=F32):
        return nc.dram_tensor(name, list(shape), dt, kind="ExternalInput").ap()

    xs = din("xs", [2048, D])
    ctxs = din("ctxs", [256, D])
    vecsA = din("vecsA", [96, 128])
    vecsB = din("vecsB", [40, 128])
    b_ada = din("b_ada", [96, 128])
    w_ada = din("w_ada", [D, 6 * D])
    w_in = din("w_in", [D, 4096])
    lamre_p = din("lamre_p", [64, 128])
    lamim_p = din("lamim_p", [64, 128])
    logdt_p = din("logdt_p", [64, 2])
    ssm_b_re = din("ssm_b_re", [2, 64, 64, 16])
    ssm_b_im = din("ssm_b_im", [2, 64, 64, 16])
    ssm_c_re = din("ssm_c_re", [2, 64, 16, 64])
    ssm_c_im = din("ssm_c_im", [2, 64, 16, 64])
    w_glu = din("w_glu", [1024, 2048])
    w_out = din("w_out", [D, D])
    router_w = din("router_w", [D, 64])
    rbias_b = din("rbias_b", [128, 64])
    ew_gate = din("ew_gate", [64, D, 512])
    ew_up = din("ew_up", [64, D, 512])
    ew_down = din("ew_down", [64, 512, D])
    sw_gate = din("sw_gate", [D, 512])
    sw_up = din("sw_up", [D, 512])
    sw_down = din("sw_down", [512, D])
    out = nc.dram_tensor("out", [NOWN, D], F32, kind="ExternalOutput").ap()
    skind = "ExternalOutput" if debug else "Internal"
    scrW1 = nc.dram_tensor("scrW1", [2, 32, 2, 128, 128], BF16, kind=skind).ap()
    scrT = nc.dram_tensor("scrT", [2, 64, 128, 128], BF16, kind=skind).ap()
    scrW2 = nc.dram_tensor("scrW2", [2, 32, 2, 128, 128], BF16, kind=skind).ap()
    scrX1 = nc.dram_tensor("scrX1", [NOWN, D], F32, kind=skind).ap()
    scrU = nc.dram_tensor("scrU", [8, 128, NSEQ - NOWN], BF16, kind=skind).ap()

    def dout(name, shape, dt=F32):
        t = nc.dram_tensor(name, list(shape), dt, kind="ExternalOutput").ap()
        dbg[name] = t
        return t

    with ExitStack() as top:
        P = Prog(nc, top)

        def sb(name, shape, dt=F32, stack=top):
            return stack.enter_context(nc.sbuf_tensor(name, list(shape), dt))

        def ps(name, shape, dt=F32, stack=top):
            P.psum_names.add(name)
            esz = 4 if dt == F32 else 2
            full = stack.enter_context(nc.psum_tensor(name, [128, 2048 // esz], dt))
            n = int(np.prod(shape[1:]))
            v = full[:, 0:n]
            if len(shape) == 3:
                v = v.rearrange("p (a b) -> p a b", b=shape[2])
            return v

        P._bar_scr = sb("barscr", [128, 4])

        ident_f = sb("ident_f", [128, 128])
        ident_b = sb("ident_b", [128, 128], BF16)
        iot = sb("iot", [128, 128], I32)
        iotf = sb("iotf", [128, 128])
        P.add("pool", lambda e: e.iota(iot[:], [[1, 128]], base=0, channel_multiplier=-1), writes=["iot"])
        P.add("dve", lambda e: e.tensor_copy(iotf[:], iot[:]), reads=["iot"], writes=["iotf"])
        P.add("dve", lambda e: e.tensor_single_scalar(ident_f[:], iotf[:], 0.0, ALU.is_equal),
              reads=["iotf"], writes=["ident_f"])
        P.add("dve", lambda e: e.tensor_copy(ident_b[:], ident_f[:]), reads=["ident_f"], writes=["ident_b"])

        if stop < 1:
            d_i = dout('d_ident', [128, 128])
            P.add('sp', lambda e: e.dma_start(out=d_i, in_=ident_f[:]), reads=['ident_f'], writes=['d_ident'], group='dbgi')
            P.add('sp', None, reads=list(dbg.keys()))
            P.emit()
            return nc, dbg
        ones_b = sb("ones_b", [128, 128], BF16)
        ones_f = sb("ones_f", [128, 128], F32)
        P.add("dve", lambda e: e.memset(ones_b[:], 1.0), writes=["ones_b"])
        P.add("dve", lambda e: e.memset(ones_f[:], 1.0), writes=["ones_f"])
        ss2 = sb("ss2", [128, 8, 4], F32)
        vA = sb("vA", [96, 128])
        vB = sb("vB", [40, 128])
        vC = sb("vC", [96, 128])
        colA = sb("colA", [128, 96])
        colB = sb("colB", [128, 40])
        badaT = sb("badaT", [128, 96])
        P.add("sp", lambda e: e.dma_start(out=vA[:], in_=vecsA), writes=["vA"], group="vA")
        P.add("sp", lambda e: e.dma_start(out=vB[:], in_=vecsB), writes=["vB"], group="vB")
        P.add("sp", lambda e: e.dma_start(out=vC[:], in_=b_ada), writes=["vC"], group="vC")
        with ExitStack() as ph:
            pt = ps("pt_small", [128, 3, 128], F32, ph)
            P.add("pe", lambda e: e.transpose(out=pt[:, 0, 0:96], in_=vA[:], identity=ident_f[0:96, 0:96]),
                  reads=["vA", "ident_f"], writes=["pt_small"])
            P.add("pe", lambda e: e.transpose(out=pt[:, 1, 0:40], in_=vB[:], identity=ident_f[0:40, 0:40]),
                  reads=["vB", "ident_f"], writes=["pt_small"])
            P.add("pe", lambda e: e.transpose(out=pt[:, 2, 0:96], in_=vC[:], identity=ident_f[0:96, 0:96]),
                  reads=["vC", "ident_f"], writes=["pt_small"])
            P.add("dve", lambda e: e.tensor_copy(colA[:], pt[:, 0, 0:96]), reads=["pt_small"], writes=["colA"])
            P.add("dve", lambda e: e.tensor_copy(colB[:], pt[:, 1, 0:40]), reads=["pt_small"], writes=["colB"])
            P.add("dve", lambda e: e.tensor_copy(badaT[:], pt[:, 2, 0:96]), reads=["pt_small"], writes=["badaT"])
            P.barrier()

        if stop < 2:
            d_c = dout('d_colA', [128, 96])
            P.add('sp', lambda e: e.dma_start(out=d_c, in_=colA[:]), reads=['colA'], writes=['d_colA'], group='dbgc')
            P.add('sp', None, reads=list(dbg.keys()))
            P.emit()
            return nc, dbg
        sc = sb("sc", [128, 16, 2])
        for j in range(2):
            P.add("act", lambda e, j=j: e.activation(out=sc[:, :, j], in_=colA[:, 16 * j:16 * j + 16], func=AF.Silu),
                  reads=["colA"], writes=[("sc", j)])
        modT = sb("modT", [128, 96, 2])
        scale1 = sb("scale1", [128, 16, 2])
        mixer = top.enter_context(ExitStack())
        uTown = sb("uTown", [128, 8, NOWN], BF16, mixer)
        convT = sb("convT", [128, 8, NOWN], BF16, mixer)
        ss = sb("ss", [128, 24], F32, mixer)
        Acplx = sb("Acplx", [128, 2, 64], F32, mixer)
        ada_stack = ExitStack()
        REC_A = []
        _real_add = P.add
        P.add = lambda *a, **k: REC_A.append((a, k))
        if True:
            ph = ada_stack
            scb = sb("scb", [128, 16, 4], BF16, ph)
            sch = sb("sch", [128, 16, 2], F32, ph)
            P.add("dve", lambda e: e.tensor_copy(scb[:, :, 0:2], sc[:]), reads=["sc"], writes=["scb"])
            P.add("dve", lambda e: e.tensor_copy(sch[:], scb[:, :, 0:2]), reads=["scb"], writes=["sch"])
            P.add("dve", lambda e: e.tensor_tensor(out=sch[:], in0=sc[:], in1=sch[:], op=ALU.subtract), reads=["sc", "sch"], writes=["sch"])
            P.add("dve", lambda e: e.tensor_copy(scb[:, :, 2:4], sch[:]), reads=["sch", "scb"], writes=["scb"])
            pm = ps("pmod", [128, 96, 4], F32, ph)
            wbuf = [sb("wada%d" % i, [128, 2048], BF16, ph) for i in range(4)]
            n = 0
            for kt in range(16):
                for cc in range(6):
                    b = n % 4
                    n += 1
                    for hc in range(2):
                        P.add("pool", lambda e, b=b, kt=kt, cc=cc, hc=hc: e.dma_start(
                            out=wbuf[b][:, hc * 1024:(hc + 1) * 1024],
                            in_=w_ada[kt * 128:(kt + 1) * 128, cc * 2048 + hc * 1024:cc * 2048 + (hc + 1) * 1024]),
                            writes=[("wada", b, hc)], group="wada%d" % b)

                    def mm(e, b=b, kt=kt, cc=cc):
                        ins = None
                        for t in range(16):
                            ins = e.matmul(pm[:, cc * 16 + t, :], lhsT=wbuf[b][:, t * 128:(t + 1) * 128],
                                           rhs=scb[:, kt, :], start=(kt == 0 and cc == 0 and t == 0), stop=(kt == 15),
                                           skip_group_check=True)
                        return ins
                    P.add("pe", mm, reads=[("wada", b), "scb"], writes=["pmod"])
            P.add("dve", lambda e: e.tensor_tensor(out=modT[:], in0=pm[:, :, 0:2], in1=badaT[:].unsqueeze(2).to_broadcast([128, 96, 2]),
                                                  op=ALU.add), reads=["pmod", "badaT"], writes=["modT"])
            P.add("dve", lambda e: e.tensor_tensor(out=modT[:], in0=modT[:], in1=pm[:, :, 2:4], op=ALU.add),
                  reads=["pmod", "modT"], writes=["modT"])
        P.add = _real_add
        import math
        PI = math.pi
        with ExitStack() as ph:
            REC_S = []
            P.add = lambda *a, **k: REC_S.append((a, k))
            raw = sb("s0raw", [64, 3, 128], F32, ph)
            ldt = sb("s0ldt", [64, 2], F32, ph)
            P.add("sp", lambda e: e.dma_start(out=raw[:, 0, :], in_=lamre_p), writes=[("s0raw", 0)], group="s0raw0")
            P.add("sp", lambda e: e.dma_start(out=raw[:, 1, :], in_=lamim_p), writes=[("s0raw", 1)], group="s0raw1")
            P.add("sp", lambda e: e.dma_start(out=ldt[:], in_=logdt_p), writes=["s0ldt"], group="s0ldt")
            P.add("act", lambda e: e.activation(out=ldt[:], in_=ldt[:], func=AF.Exp), reads=["s0ldt"], writes=["s0ldt"])
            P.add("dve", lambda e: e.tensor_copy(raw[:, 2, :].rearrange("q (a b) -> q a b", a=2),
                                                 ldt[:].unsqueeze(2).to_broadcast([64, 2, 64])),
                  reads=["s0ldt"], writes=[("s0raw", 2)])
            LRI = sb("LRI", [128, 3, 64], F32, ph)
            ptq = ps("s0pt", [128, 4, 128], F32, ph)
            for i in range(3):
                P.add("pe", lambda e, i=i: e.transpose(out=ptq[:, i, 0:64], in_=raw[:, i, :], identity=ident_f[0:64, 0:64]),
                      reads=[("s0raw", i), "ident_f"], writes=["s0pt"])
            P.add("dve", lambda e: e.tensor_copy(LRI[:], ptq[:, 0:3, 0:64]), reads=["s0pt"], writes=["LRI"])
            ath = sb("ath", [128, 2, 64], F32, ph)
            P.add("dve", lambda e: e.tensor_tensor(out=ath[:], in0=LRI[:, 0:2, :],
                                                   in1=LRI[:, 2:3, :].to_broadcast([128, 2, 64]), op=ALU.mult),
                  reads=["LRI"], writes=["ath"])
            io8 = sb("io8", [128, 8], I32, ph)
            io8f = sb("io8f", [128, 8], F32, ph)
            KM = sb("KM", [128, 3, 2, 8], F32, ph)
            P.add("pool", lambda e: e.iota(io8[:], [[1, 8]], base=0, channel_multiplier=0), writes=["io8"])
            P.add("dve", lambda e: e.tensor_copy(io8f[:], io8[:]), reads=["io8"], writes=["io8f"])
            kmab = {(0, 0): (-1.0, 7.0), (0, 1): (1.0, 0.0), (1, 0): (-1.0, -1.0), (1, 1): (1.0, -8.0),
                    (2, 0): (1.0, 1.0), (2, 1): (-1.0, 8.0)}
            for (u_, d_), (ka, kb) in kmab.items():
                P.add("dve", lambda e, u_=u_, d_=d_, ka=ka, kb=kb: e.tensor_scalar(
                    out=KM[:, u_, d_, :], in0=io8f[:], scalar1=ka, scalar2=kb, op0=ALU.mult, op1=ALU.add),
                    reads=["io8f"], writes=[("KM", u_, d_)])

            et_ang = sb("et_ang", [128, 2, 32, 8], F32, ph)
            et_ex = sb("et_ex", [128, 2, 32, 8], F32, ph)
            et_tmp = sb("et_tmp", [128, 2, 32, 8], F32, ph)
            et_ti = sb("et_ti", [128, 2, 32, 8], I32, ph)
            et_tf = sb("et_tf", [128, 2, 32, 8], F32, ph)

            def etab(name, mult_ap, L, dst_re, dst_im, rkeys, wkeys):
                shp = [128, 2, 32, L]
                name = "et"
                ang = et_ang[:, :, :, 0:L]
                ex = et_ex[:, :, :, 0:L]
                tmp = et_tmp[:, :, :, 0:L]
                ti = et_ti[:, :, :, 0:L]
                tf = et_tf[:, :, :, 0:L]
                a_b = ath[:, 0, :].rearrange("p (d g) -> p d g", d=2).unsqueeze(3).to_broadcast(shp)
                t_b = ath[:, 1, :].rearrange("p (d g) -> p d g", d=2).unsqueeze(3).to_broadcast(shp)
                P.add("dve", lambda e: e.tensor_tensor(out=ex[:], in0=a_b, in1=mult_ap, op=ALU.mult),
                      reads=["ath"] + rkeys, writes=[name + "_ex"])
                P.add("act", lambda e: e.activation(out=ex[:], in_=ex[:], func=AF.Exp), reads=[name + "_ex"], writes=[name + "_ex"])
                P.add("dve", lambda e: e.tensor_tensor(out=ang[:], in0=t_b, in1=mult_ap, op=ALU.mult),
                      reads=["ath"] + rkeys, writes=[name + "_ang"])
                for (dst, shift) in ((dst_im, 32.0), (dst_re, 32.25)):
                    P.add("dve", lambda e, shift=shift: e.tensor_scalar(out=tmp[:], in0=ang[:], scalar1=1.0 / (2.0 * PI), scalar2=shift,
                                                                        op0=ALU.mult, op1=ALU.add),
                          reads=[name + "_ang"], writes=[name + "_tmp"])
                    P.add("dve", lambda e: e.tensor_copy(ti[:], tmp[:]), reads=[name + "_tmp"], writes=[name + "_ti"])
                    P.add("dve", lambda e: e.tensor_copy(tf[:], ti[:]), reads=[name + "_ti"], writes=[name + "_tf"])
                    P.add("dve", lambda e: e.tensor_tensor(out=tmp[:], in0=tmp[:], in1=tf[:], op=ALU.subtract),
                          reads=[name + "_tmp", name + "_tf"], writes=[name + "_tmp"])
                    P.add("dve", lambda e: e.tensor_single_scalar(tf[:], tmp[:], 0.5, ALU.is_gt),
                          reads=[name + "_tmp"], writes=[name + "_tf"])
                    P.add("dve", lambda e: e.tensor_tensor(out=tmp[:], in0=tmp[:], in1=tf[:], op=ALU.subtract),
                          reads=[name + "_tmp", name + "_tf"], writes=[name + "_tmp"])
                    P.add("act", lambda e: e.activation(out=tmp[:], in_=tmp[:], func=AF.Sin, scale=2.0 * PI), reads=[name + "_tmp"],
                          writes=[name + "_tmp"])
                    P.add("dve", lambda e, dst=dst: e.tensor_tensor(out=dst, in0=tmp[:], in1=ex[:], op=ALU.mult),
                          reads=[name + "_tmp", name + "_ex"], writes=wkeys)

            one1 = sb("one1", [128, 1], F32, ph)
            P.add("dve", lambda e: e.memset(one1[:], 1.0), writes=["one1"])
            E1 = sb("E1", [128, 2, 2, 32, 1], F32, ph)
            etab("e1", one1[:].unsqueeze(2).unsqueeze(3).to_broadcast([128, 2, 32, 1]), 1, E1[:, 0], E1[:, 1], ["one1"], ["E1"])
            eight = sb("eight", [128, 1], F32, ph)
            P.add("dve", lambda e: e.memset(eight[:], 8.0), writes=["eight"])
            etab("e8", eight[:].unsqueeze(2).unsqueeze(3).to_broadcast([128, 2, 32, 1]), 1,
                 Acplx[:, 0, :].rearrange("p (d g o) -> p d g o", d=2, o=1),
                 Acplx[:, 1, :].rearrange("p (d g o) -> p d g o", d=2, o=1), ["eight"], ["Acplx"])
            ET = [sb("ET%d" % u_, [128, 2, 2, 32, 8], F32, ph) for u_ in range(3)]
            for u_ in range(3):
                etab("et%d" % u_, KM[:, u_, :, :].unsqueeze(2).to_broadcast([128, 2, 32, 8]), 8,
                     ET[u_][:, 0], ET[u_][:, 1], ["KM"], ["ET%d" % u_])

            LR = LRI[:, 0, :]
            LI = LRI[:, 1, :]
            e1r = E1[:, 0].rearrange("p d g o -> p (d g o)")
            e1i = E1[:, 1].rearrange("p d g o -> p (d g o)")
            cf = sb("cf", [128, 6, 64], F32, ph)
            seq_ops = [
                lambda e: e.tensor_scalar(out=cf[:, 0, :], in0=e1r, scalar1=-1.0, scalar2=None, op0=ALU.add),
                lambda e: e.tensor_tensor(out=cf[:, 1, :], in0=LR, in1=LR, op=ALU.mult),
                lambda e: e.tensor_tensor(out=cf[:, 2, :], in0=LI, in1=LI, op=ALU.mult),
                lambda e: e.tensor_tensor(out=cf[:, 1, :], in0=cf[:, 1, :], in1=cf[:, 2, :], op=ALU.add),
                lambda e: e.reciprocal(out=cf[:, 1, :], in_=cf[:, 1, :]),
                lambda e: e.tensor_tensor(out=cf[:, 2, :], in0=cf[:, 0, :], in1=LR, op=ALU.mult),
                lambda e: e.tensor_tensor(out=cf[:, 3, :], in0=e1i, in1=LI, op=ALU.mult),
                lambda e: e.tensor_tensor(out=cf[:, 2, :], in0=cf[:, 2, :], in1=cf[:, 3, :], op=ALU.add),
                lambda e: e.tensor_tensor(out=cf[:, 4, :], in0=cf[:, 2, :], in1=cf[:, 1, :], op=ALU.mult),
                lambda e: e.tensor_tensor(out=cf[:, 2, :], in0=e1i, in1=LR, op=ALU.mult),
                lambda e: e.tensor_tensor(out=cf[:, 3, :], in0=cf[:, 0, :], in1=LI, op=ALU.mult),
                lambda e: e.tensor_tensor(out=cf[:, 2, :], in0=cf[:, 2, :], in1=cf[:, 3, :], op=ALU.subtract),
                lambda e: e.tensor_tensor(out=cf[:, 5, :], in0=cf[:, 2, :], in1=cf[:, 1, :], op=ALU.mult),
            ]
            for f_ in seq_ops:
                P.add("dve", f_, reads=["E1", "LRI", "cf"], writes=["cf"])
            Braw = sb("Braw", [128, 2, 64, 16], F32, ph)
            for i, src_ in enumerate((ssm_b_re, ssm_b_im)):
                for d_ in range(2):
                    v = src_[d_].rearrange("(g2 gp) p m -> gp p g2 m", gp=2)
                    for gp in range(2):
                        P.add("sp", lambda e, i=i, d_=d_, gp=gp, v=v: e.dma_start(
                            out=Braw[gp * 64:(gp + 1) * 64, i, d_ * 32:(d_ + 1) * 32, :], in_=v[gp]),
                            writes=[("Braw", i, d_, gp)], group="Braw")
            bbar = sb("bbar", [128, 2, 64, 16], F32, ph)
            tA = sb("s0tA", [128, 64, 16], F32, ph)
            cre_b = cf[:, 4, :].unsqueeze(2).to_broadcast([128, 64, 16])
            cim_b = cf[:, 5, :].unsqueeze(2).to_broadcast([128, 64, 16])
            P.add("dve", lambda e: e.tensor_tensor(out=bbar[:, 0], in0=Braw[:, 0], in1=cre_b, op=ALU.mult), reads=["Braw", "cf"], writes=[("bbar", 0)])
            P.add("dve", lambda e: e.tensor_tensor(out=tA[:], in0=Braw[:, 1], in1=cim_b, op=ALU.mult), reads=["Braw", "cf"], writes=["s0tA"])
            P.add("dve", lambda e: e.tensor_tensor(out=bbar[:, 0], in0=bbar[:, 0], in1=tA[:], op=ALU.subtract), reads=["s0tA", ("bbar", 0)], writes=[("bbar", 0)])
            P.add("dve", lambda e: e.tensor_tensor(out=bbar[:, 1], in0=Braw[:, 1], in1=cre_b, op=ALU.mult), reads=["Braw", "cf"], writes=[("bbar", 1)])
            P.add("dve", lambda e: e.tensor_tensor(out=tA[:], in0=Braw[:, 0], in1=cim_b, op=ALU.mult), reads=["Braw", "cf", ("bbar", 0)], writes=["s0tA"])
            P.add("dve", lambda e: e.tensor_tensor(out=bbar[:, 1], in0=bbar[:, 1], in1=tA[:], op=ALU.add), reads=["s0tA", ("bbar", 1)], writes=[("bbar", 1)])

            CT = sb("CT", [128, 2, 64, 16], F32, ph)
            craw = [sb("craw%d" % i, [128, 128], F32, ph) for i in range(2)]
            nb = 0
            for i, src_ in enumerate((ssm_c_re, ssm_c_im)):
                for d_ in range(2):
                    for q in range(4):
                        b_ = nb % 2
                        nb += 1
                        for g2l in range(8):
                            g2 = q * 8 + g2l
                            P.add("sp", lambda e, b_=b_, g2l=g2l, g2=g2, d_=d_, src_=src_: e.dma_start(
                                out=craw[b_][g2l * 16:(g2l + 1) * 16, :].rearrange("n (gp p) -> n gp p", gp=2),
                                in_=src_[d_, 2 * g2:2 * g2 + 2, :, :].rearrange("gp n p -> n gp p")),
                                writes=[("craw", b_, g2l)], group="craw%d" % b_)
                        P.add("pe", lambda e, b_=b_: e.transpose(out=ptq[:, 3, :], in_=craw[b_][:], identity=ident_f[:]),
                              reads=[("craw", b_), "ident_f"], writes=["s0pt"])
                        P.add("dve", lambda e, i=i, d_=d_, q=q: e.tensor_copy(
                            CT[:, i, d_ * 32 + q * 8:d_ * 32 + (q + 1) * 8, :], ptq[:, 3, :].rearrange("p (a n) -> p a n", n=16)),
                            reads=["s0pt"], writes=[("CT", i, d_, q)])

            mk = sb("mk", [128, 2, 128], F32, ph)
            mi = sb("mki", [128, 2, 128], I32, ph)
            mf = sb("mkf", [128, 2, 128], F32, ph)
            P.add("pool", lambda e: e.iota(mi[:, 0, :], [[1, 128]], base=0, channel_multiplier=0), writes=[("mki", 0)])
            P.add("pool", lambda e: e.iota(mi[:, 1, :], [[0, 128]], base=0, channel_multiplier=1), writes=[("mki", 1)])
            P.add("dve", lambda e: e.tensor_single_scalar(mi[:], mi[:], 4, ALU.arith_shift_right), reads=["mki"], writes=["mki"])
            P.add("dve", lambda e: e.tensor_copy(mf[:], mi[:]), reads=["mki"], writes=["mkf"])
            P.add("dve", lambda e: e.tensor_tensor(out=mk[:, 0, :], in0=mf[:, 0, :], in1=mf[:, 1, :], op=ALU.is_ge), reads=["mkf"], writes=[("mk", 0)])
            P.add("dve", lambda e: e.tensor_tensor(out=mk[:, 1, :], in0=mf[:, 1, :], in1=mf[:, 0, :], op=ALU.is_ge), reads=["mkf"], writes=[("mk", 1)])


            oR = sb("oR", [128, 32, 8, 16], F32, ph)
            oI = sb("oI", [128, 32, 8, 16], F32, ph)
            o2R = sb("o2R", [128, 32, 8, 16], F32, ph)
            o2I = sb("o2I", [128, 32, 8, 16], F32, ph)
            t1 = sb("s0t1", [128, 32, 8, 16], F32, ph)
            stg = sb("s0stg", [128, 4, 128], BF16, ph)
            stg2 = [sb("s0stg2", [128, 32, 128], BF16, ph)] * 2
            pT = ps("s0pT", [128, 4, 128], F32, ph)
            pT2 = ps("s0pT2", [128, 4, 128], F32, ph)

            def couter(u_, d_, Br, Bi, dR, dI, neg_im, rk, tag):
                shp = [128, 32, 8, 16]
                Er = ET[u_][:, 0, d_].unsqueeze(3).to_broadcast(shp)
                Ei = ET[u_][:, 1, d_].unsqueeze(3).to_broadcast(shp)
                Brb = Br.unsqueeze(2).to_broadcast(shp)
                Bib = Bi.unsqueeze(2).to_broadcast(shp)
                rk = rk + ["ET%d" % u_]
                P.add("dve", lambda e: e.tensor_tensor(out=dR[:], in0=Er, in1=Brb, op=ALU.mult), reads=rk, writes=[tag + "R"])
                P.add("dve", lambda e: e.tensor_tensor(out=t1[:], in0=Ei, in1=Bib, op=ALU.mult), reads=rk, writes=["s0t1"])
                P.add("dve", lambda e: e.tensor_tensor(out=dR[:], in0=dR[:], in1=t1[:], op=ALU.subtract), reads=[tag + "R", "s0t1"], writes=[tag + "R"])
                P.add("dve", lambda e: e.tensor_tensor(out=dI[:], in0=Er, in1=Bib, op=ALU.mult), reads=rk, writes=[tag + "I"])
                P.add("dve", lambda e: e.tensor_tensor(out=t1[:], in0=Ei, in1=Brb, op=ALU.mult), reads=rk + [tag + "R"], writes=["s0t1"])
                if neg_im:
                    P.add("dve", lambda e: e.scalar_tensor_tensor(out=dI[:], in0=dI[:], scalar=-1.0, in1=t1[:], op0=ALU.mult, op1=ALU.subtract),
                          reads=[tag + "I", "s0t1"], writes=[tag + "I"])
                else:
                    P.add("dve", lambda e: e.tensor_tensor(out=dI[:], in0=dI[:], in1=t1[:], op=ALU.add), reads=[tag + "I", "s0t1"], writes=[tag + "I"])

            for d_ in range(2):
                gs = slice(d_ * 32, (d_ + 1) * 32)
                couter(0, d_, bbar[:, 0, gs, :], bbar[:, 1, gs, :], oR, oI, False, ["bbar"], "o")
                for part, src_t, skey in ((0, oR, "oR"), (1, oI, "oI")):
                    for q in range(8):
                        def trw(e, src_t=src_t, q=q):
                            ins = None
                            for j in range(4):
                                ins = e.transpose(out=pT[:, j, :], in_=src_t[:, q * 4 + j].rearrange("p s m -> p (s m)"), identity=ident_f[:])
                            return ins
                        P.add("pe", trw, reads=[skey, "ident_f"], writes=["s0pT"])
                        P.add("act", lambda e: e.activation(out=stg[:], in_=pT[:], func=AF.Copy), reads=["s0pT"], writes=["s0stg"])
                        P.add("sp", lambda e, d_=d_, q=q, part=part: e.dma_start(
                            out=scrW1[d_, q * 4:(q + 1) * 4, part].rearrange("g r c -> r g c"), in_=stg[:]),
                            reads=["s0stg"], writes=["scrW1"], group="s0st")

                couter(1, d_, bbar[:, 0, gs, :], bbar[:, 1, gs, :], oR, oI, False, ["bbar"], "o")
                couter(2, d_, CT[:, 0, gs, :], CT[:, 1, gs, :], o2R, o2I, True, ["CT"], "o2")
                for part, src_t, skey in ((0, o2R, "o2R"), (1, o2I, "o2I")):
                    P.add("act", lambda e, part=part, src_t=src_t: e.activation(
                        out=stg2[part][:], in_=src_t[:].rearrange("p g t n -> p g (t n)"), func=AF.Copy),
                        reads=[skey], writes=["s0stg2"])
                    P.add("sp", lambda e, d_=d_, part=part: e.dma_start(
                        out=scrW2[d_, :, part].rearrange("g r c -> r g c"), in_=stg2[part][:]),
                        reads=["s0stg2"], writes=["scrW2"], group="s0st2")

                for q in range(8):
                    for gp in range(2):
                        pTx = pT if gp == 0 else pT2
                        pkey = "s0pT" if gp == 0 else "s0pT2"
                        rs = slice(gp * 64, (gp + 1) * 64)

                        def mmT(e, q=q, gp=gp, pTx=pTx, rs=rs):
                            ins = None
                            for j in range(4):
                                g2 = q * 4 + j
                                e.matmul(pTx[:, j, :], lhsT=oR[rs, g2].rearrange("p s m -> p (s m)"),
                                         rhs=o2R[rs, g2].rearrange("p t n -> p (t n)"), start=True, stop=False)
                                ins = e.matmul(pTx[:, j, :], lhsT=oI[rs, g2].rearrange("p s m -> p (s m)"),
                                               rhs=o2I[rs, g2].rearrange("p t n -> p (t n)"), start=False, stop=True)
                            return ins
                        P.add("pe", mmT, reads=["oR", "oI", "o2R", "o2I"], writes=[pkey])
                        P.add("dve", lambda e, d_=d_, pTx=pTx: e.tensor_tensor(
                            out=stg[:], in0=pTx[:], in1=mk[:, d_:d_ + 1, :].to_broadcast([128, 4, 128]), op=ALU.mult),
                            reads=[pkey, "mk"], writes=["s0stg"])
                        P.add("sp", lambda e, d_=d_, q=q, gp=gp: e.dma_start(
                            out=scrT[d_, q * 8:(q + 1) * 8].rearrange("(j gp) r c -> gp r j c", gp=2)[gp], in_=stg[:]),
                            reads=["s0stg"], writes=["scrT"], group="s0st")
            P.add = _real_add
            na, ns = len(REC_A), len(REC_S)
            ia = isx = 0
            while ia < na or isx < ns:
                if isx >= ns or (ia < na and ia * ns <= isx * na):
                    a_, k_ = REC_A[ia]; ia += 1
                else:
                    a_, k_ = REC_S[isx]; isx += 1
                P.add(*a_, **k_)
            P.barrier()
        ada_stack.close()
        if debug:
            d_A = dout("d_A", [128, 128])
            P.add("sp", lambda e: e.dma_start(out=d_A, in_=Acplx[:].rearrange("p a b -> p (a b)")), reads=["Acplx"],
                  writes=["d_A"], group="dbgA")

        if debug:
            d_mod = dout("d_mod", [128, 192])
            P.add("sp", lambda e: e.dma_start(out=d_mod, in_=modT[:].rearrange("p a b -> p (a b)")), reads=["modT"],
                  writes=["d_mod"], group="dbg0")

        P.add("dve", lambda e: e.scalar_tensor_tensor(out=scale1[:], in0=modT[:, 16:32, :], scalar=1.0,
                                                      in1=colA[:, 32:48].unsqueeze(2).to_broadcast([128, 16, 2]),
                                                      op0=ALU.add, op1=ALU.mult),
              reads=["modT", "colA"], writes=["scale1"])

        with ExitStack() as ph:
            w_u = sb("w_u", [128, 16, 1024], BF16, ph)
            ustage = [sb("ustage%d" % i, [128, 512], BF16, ph) for i in range(2)]
            w_in_v = w_in.rearrange("(kt p) c -> p kt c", p=128)
            for kt in range(16):
                P.add("pool", lambda e, kt=kt: e.dma_start(out=w_u[:, kt, :], in_=w_in_v[:, kt, 0:1024]),
                      writes=[("w_u", kt)], group="w_u")
            xt = [sb("xt%d" % i, [128, D], F32, ph) for i in range(2)]
            xn = [sb("xn%d" % i, [128, 4, D], BF16, ph) for i in range(2)]
            hxT = [sb("hxT%d" % i, [128, 16, 512], BF16, ph) for i in range(2)]
            ptr = [ps("ptr%d" % i, [128, 512], BF16, ph) for i in range(2)]
            pmm = [ps("pmm%d" % i, [128, 512], F32, ph) for i in range(6)]
            groups = [("x", 1024, 4, 0, 1024, 0), ("x", 1536, 4, 0, 1536, 1), ("c", 0, 2, 1, 2048, 0),
                      ("x", 0, 4, 0, 0, 1), ("x", 512, 4, 0, 512, 0)]
            nxc = [0]

            def stA1(gi):
                (src, r0, nt, mj, soff, xb) = groups[gi]
                xnb = xn[gi % 2]
                for t in range(nt):
                    nx = nxc[0]
                    b = nx % 2
                    tix = nx % 24
                    nxc[0] += 1
                    srcap = (xs if src == "x" else ctxs)[r0 + t * 128:r0 + (t + 1) * 128, :]
                    P.add("sp", lambda e, b=b, srcap=srcap: e.dma_start(out=xt[b][:], in_=srcap),
                          writes=[("xt", b)], group="xt%d" % b)
                    P.add("act", lambda e, b=b, tix=tix, t=t: e.activation(out=xnb[:, t, :], in_=xt[b][:], func=AF.Square,
                                                                      accum_out=ss[:, tix:tix + 1]),
                          reads=[("xt", b)], writes=[("xn", gi % 2, t), ("ss", tix)])
                    P.add("dve", lambda e, tix=tix: e.tensor_scalar(out=ss[:, tix:tix + 1], in0=ss[:, tix:tix + 1],
                                                                    scalar1=1.0 / D, scalar2=EPS, op0=ALU.mult, op1=ALU.add),
                          reads=[("ss", tix)], writes=[("ss", tix)])
                    P.add("act", lambda e, tix=tix: e.activation(out=ss[:, tix:tix + 1], in_=ss[:, tix:tix + 1], func=AF.Sqrt),
                          reads=[("ss", tix)], writes=[("ss", tix)])
                    P.add("dve", lambda e, tix=tix: e.reciprocal(out=ss[:, tix:tix + 1], in_=ss[:, tix:tix + 1]),
                          reads=[("ss", tix)], writes=[("ss", tix)])
                    P.add("act", lambda e, b=b, tix=tix, t=t: e.activation(
                        out=xnb[:, t, :], in_=xt[b][:], func=AF.Copy, scale=ss[:, tix:tix + 1]),
                        reads=[("xt", b), ("ss", tix)], writes=[("xn", gi % 2, t)])

            def stA2(gi):
                (src, r0, nt, mj, soff, xb) = groups[gi]
                xnb = xn[gi % 2]
                ntok = nt * 128
                for ft in range(16):
                    pb = ft % 2

                    def tr(e, ft=ft, nt=nt, pb=pb):
                        ins = None
                        for t in range(nt):
                            ins = e.transpose(out=ptr[pb][:, t * 128:(t + 1) * 128],
                                              in_=xnb[:, t, ft * 128:(ft + 1) * 128], identity=ident_b[:])
                        return ins
                    P.add("pe", tr, reads=[("xn", gi % 2), "ident_b"], writes=["ptr%d" % pb])
                    if ft % 2 == 0:
                        P.add("dve", lambda e, xb=xb, ft=ft, pb=pb, ntok=ntok, mj=mj: e.tensor_scalar(
                            out=hxT[xb][:, ft, 0:ntok], in0=ptr[pb][:, 0:ntok], scalar1=scale1[:, ft, mj:mj + 1],
                            scalar2=modT[:, ft, mj:mj + 1], op0=ALU.mult, op1=ALU.add),
                            reads=["ptr%d" % pb, "scale1", "modT"], writes=[("hxT", xb, ft)])
                    else:
                        P.add("act", lambda e, xb=xb, ft=ft, pb=pb, ntok=ntok, mj=mj: e.activation(
                            out=hxT[xb][:, ft, 0:ntok], in_=ptr[pb][:, 0:ntok], func=AF.Identity,
                            scale=scale1[:, ft, mj:mj + 1], bias=modT[:, ft, mj:mj + 1]),
                            reads=["ptr%d" % pb, "scale1", "modT"], writes=[("hxT", xb, ft)])

            def stB(gi):
                (src, r0, nt, mj, soff, xb) = groups[gi]
                ntok = nt * 128
                for ct in range(8):
                    pq = ct % 4

                    def mmu(e, xb=xb, ct=ct, ntok=ntok, pq=pq):
                        ins = None
                        for kt in range(16):
                            ins = e.matmul(pmm[pq][:, 0:ntok], lhsT=w_u[:, kt, ct * 128:(ct + 1) * 128],
                                           rhs=hxT[xb][:, kt, 0:ntok], start=(kt == 0), stop=(kt == 15))
                        return ins
                    P.add("pe", mmu, reads=["w_u", ("hxT", xb)], writes=["pmm%d" % pq])
                    nj = ntok // 8
                    if soff < NOWN:
                        dst = uTown[:, ct, :].rearrange("p (s j) -> p s j", s=8)[:, :, soff // 8:soff // 8 + nj]
                        wk = [("uT", ct, soff)]
                    else:
                        sg = ct % 2
                        dst = ustage[sg][:, 0:ntok].rearrange("p (s j) -> p s j", s=8)
                        wk = [("ustage", sg)]
                    srcv = pmm[pq][:, 0:ntok].rearrange("p (j s) -> p s j", s=8)
                    if ct % 2 == 0:
                        P.add("dve", lambda e, dst=dst, srcv=srcv: e.tensor_copy(dst, srcv),
                              reads=["pmm%d" % pq], writes=wk)
                    else:
                        P.add("act", lambda e, dst=dst, srcv=srcv: e.activation(out=dst, in_=srcv, func=AF.Copy),
                              reads=["pmm%d" % pq], writes=wk)
                    if soff >= NOWN:
                        j0r = (soff - NOWN) // 8
                        P.add("sp", lambda e, ct=ct, sg=sg, ntok=ntok, j0r=j0r, nj=nj: e.dma_start(
                            out=scrU[ct].rearrange("p (s j) -> p s j", s=8)[:, :, j0r:j0r + nj],
                            in_=ustage[sg][:, 0:ntok].rearrange("p (s j) -> p s j", s=8)),
                            reads=[("ustage", sg)], writes=["scrU"], group="ustage%d" % sg)

            stA1(0)
            stA2(0)
            for gi in range(5):
                if gi + 1 < 5:
                    stA1(gi + 1)
                stB(gi)
                if gi + 1 < 5:
                    stA2(gi + 1)
            cvs = ph.enter_context(ExitStack())
            wch = [[sb("wch%d_%d" % (s_, i), [128, 16, 128], BF16, cvs) for i in range(3)] for s_ in range(2)]
            zc = sb("zc", [128, 512], F32, cvs)
            zz = sb("zz", [128, 512], F32, cvs)
            yy = sb("yy", [128, 512], F32, cvs)
            for ct in range(8):
                s_ = ct % 2
                for i in range(3):
                    P.add("pool", lambda e, s_=s_, i=i, ct=ct: e.dma_start(
                        out=wch[s_][i][:], in_=w_in_v[:, :, 1024 * (i + 1) + ct * 128:1024 * (i + 1) + (ct + 1) * 128]),
                        writes=[("wch", s_, i)], group="wch%d_%d" % (s_, i))
                for og, (xb, soff) in enumerate([(1, 0), (0, 512)]):
                    pset = 3 * ((ct * 2 + og) % 2)
                    if True:
                        for i in range(3):
                            def mmb(e, xb=xb, i=i, s_=s_, pset=pset):
                                ins = None
                                for kt in range(16):
                                    ins = e.matmul(pmm[pset + i][:, :], lhsT=wch[s_][i][:, kt, :],
                                                   rhs=hxT[xb][:, kt, :], start=(kt == 0), stop=(kt == 15))
                                return ins
                            P.add("pe", mmb, reads=[("wch", s_, i), ("hxT", xb)], writes=["pmm%d" % (pset + i)])
                        P.add("act", lambda e, pset=pset: e.activation(out=zc[:], in_=pmm[pset + 1][:], func=AF.Copy),
                              reads=["pmm%d" % (pset + 1)], writes=["zc"])
                        P.add("dve", lambda e, pset=pset: e.tensor_tensor(out=zz[:], in0=zc[:], in1=pmm[pset + 2][:], op=ALU.mult),
                              reads=["zc", "pmm%d" % (pset + 2)], writes=["zz"])
                        P.add("dve", lambda e, ct=ct: e.tensor_scalar(
                            out=yy[:], in0=zz[:], scalar1=colB[:, 16 + ct:17 + ct], scalar2=colB[:, 32 + ct:33 + ct],
                            op0=ALU.mult, op1=ALU.add), reads=["zz", "colB"], writes=["yy"])
                        yv = yy[:].rearrange("p (r w) -> p r w", w=64)
                        zv = zz[:].rearrange("p (r w) -> p r w", w=64)
                        P.add("dve", lambda e, ct=ct, yv=yv, zv=zv: e.scalar_tensor_tensor(
                            out=yv[:, :, 1:64], in0=zv[:, :, 0:63], scalar=colB[:, 8 + ct:9 + ct], in1=yv[:, :, 1:64],
                            op0=ALU.mult, op1=ALU.add), reads=["zz", "yy", "colB"], writes=["yy"])
                        P.add("dve", lambda e, ct=ct, yv=yv, zv=zv: e.scalar_tensor_tensor(
                            out=yv[:, :, 0:63], in0=zv[:, :, 1:64], scalar=colB[:, 24 + ct:25 + ct], in1=yv[:, :, 0:63],
                            op0=ALU.mult, op1=ALU.add), reads=["zz", "yy", "colB"], writes=["yy"])
                        P.add("dve", lambda e, ct=ct, soff=soff, pset=pset: e.tensor_tensor(
                            out=convT[:, ct, soff:soff + 512], in0=yy[:], in1=pmm[pset][:], op=ALU.mult),
                            reads=["yy", "pmm%d" % pset], writes=[("convT", ct, soff)])
            P.barrier()
        if debug:
            d_uT = dout("d_uT", [128, 8 * NOWN], BF16)
            d_convT = dout("d_convT", [128, 8 * NOWN], BF16)
            P.add("sp", lambda e: e.dma_start(out=d_uT, in_=uTown[:].rearrange("p a b -> p (a b)")), reads=["uT"],
                  writes=["d_uT"], group="dbg1")
            P.add("sp", lambda e: e.dma_start(out=d_convT, in_=convT[:].rearrange("p a b -> p (a b)")), reads=["convT"],
                  writes=["d_convT"], group="dbg2")

        Z = sb("Z", [128, 8, 8, 128], BF16, mixer)
        P.add("pool", lambda e: e.memset(Z[:], 0.0), writes=["Z"])
        for a_ in range(8):
            for b_ in range(8):
                P.add("dve" if (a_ + b_) % 2 else "pool", lambda e, a_=a_, b_=b_: e.tensor_single_scalar(
                    Z[:, a_, b_, 16 * b_:16 * b_ + 16], iotf[:, 16 * b_:16 * b_ + 16], float(16 * (b_ - a_)), ALU.is_equal),
                    reads=["iotf"], writes=[("Z", a_, b_)])
        mix = mixer.enter_context(ExitStack())
        gT = sb("gT", [128, 8, NOWN], BF16, mix)
        ssm = mix.enter_context(ExitStack())
        U = sb("U", [128, 64, 128], BF16, ssm)
        Pt = sb("Pt", [128, 2, 2, 32, 288], BF16, ssm)
        s12 = ssm.enter_context(ExitStack())
        Ur = sb("Ur", [128, 64, 160], BF16, s12)
        with ExitStack() as ph:
            pU = [ps("pU%d" % i, [128, 288], F32, ph) for i in range(3)]
            ucat = [sb("ucat%d" % i, [128, NSEQ], BF16, ph) for i in range(2)]
            for g in range(64):
                ct, gl = g // 8, g % 8
                pb = g % 3
                if gl == 0:
                    P.add("sp", lambda e, ct=ct: e.dma_start(
                        out=ucat[ct % 2][:].rearrange("p (s j) -> p s j", s=8)[:, :, 128:288],
                        in_=scrU[ct].rearrange("p (s j) -> p s j", s=8)), reads=["scrU"],
                        writes=[("ucat", ct % 2, 1)], group="ucat%d" % (ct % 2))
                    P.add("pool", lambda e, ct=ct: e.tensor_copy(
                        ucat[ct % 2][:].rearrange("p (s j) -> p s j", s=8)[:, :, 0:128],
                        uTown[:, ct, :].rearrange("p (s j) -> p s j", s=8)), reads=["uT"],
                        writes=[("ucat", ct % 2, 0)])

                def shf(e, ct=ct, gl=gl, pb=pb):
                    ins = None
                    for s_ in range(8):
                        src = ucat[ct % 2][:, s_ * 288:(s_ + 1) * 288]
                        ins = e.matmul(pU[pb][:, 0:288], lhsT=Z[:, gl, s_, :], rhs=src, start=(s_ == 0), stop=(s_ == 7))
                    return ins
                P.add("pe", shf, reads=["Z", ("ucat", ct % 2)], writes=["pU%d" % pb])
                P.add("dve", lambda e, g=g, pb=pb: e.tensor_copy(U[:, g, :], pU[pb][:, 0:128]), reads=["pU%d" % pb], writes=[("U", g)])
                P.add("act", lambda e, g=g, pb=pb: e.activation(out=Ur[:, g, :], in_=pU[pb][:, 128:288], func=AF.Copy),
                      reads=["pU%d" % pb], writes=[("U", g)])
            P.barrier()
        with ExitStack() as ph:
            w1c = [sb("w1c%d" % i, [128, 8, 2, 128], BF16, ph) for i in range(2)]
            pP = [ps("pP%d" % i, [128, 288], F32, ph) for i in range(3)]
            nn = 0
            for d_ in range(2):
                for q in range(4):
                    wb_ = (d_ * 4 + q) % 2
                    P.add("sp", lambda e, d_=d_, q=q, wb_=wb_: e.dma_start(
                        out=w1c[wb_][:], in_=scrW1[d_, q * 8:(q + 1) * 8].rearrange("g part r c -> r g part c")),
                        reads=["scrW1"], writes=[("w1c", wb_)], group="w1c%d" % wb_)
                    for g2l in range(8):
                        g2 = q * 8 + g2l
                        for part in range(2):
                            pb = nn % 3
                            nn += 1

                            def mmP(e, d_=d_, g2=g2, g2l=g2l, part=part, pb=pb, wb_=wb_):
                                ins = None
                                for gp in range(2):
                                    g = 2 * g2 + gp
                                    lw = w1c[wb_][:, g2l, part, gp * 64:(gp + 1) * 64]
                                    rows = slice(gp * 64, (gp + 1) * 64)
                                    if d_ == 0:
                                        e.matmul(pP[pb][rows, 0:32], lhsT=lw, rhs=Ur[:, g, 128:160], start=True, stop=True)
                                        ins = e.matmul(pP[pb][rows, 32:160], lhsT=lw, rhs=U[:, g, 0:128], start=True, stop=True)
                                    else:
                                        e.matmul(pP[pb][rows, 0:128], lhsT=lw, rhs=U[:, g, 0:128], start=True, stop=True)
                                        ins = e.matmul(pP[pb][rows, 128:288], lhsT=lw, rhs=Ur[:, g, 0:160], start=True, stop=True)
                                return ins
                            P.add("pe", mmP, reads=[("w1c", wb_), "U"], writes=["pP%d" % pb])
                            ncol = 160 if d_ == 0 else 288
                            if nn % 2 == 0:
                                P.add("dve", lambda e, d_=d_, g2=g2, part=part, pb=pb, ncol=ncol: e.tensor_copy(
                                    Pt[:, part, d_, g2, 0:ncol], pP[pb][:, 0:ncol]), reads=["pP%d" % pb], writes=[("Pt", part, d_, g2)])
                            else:
                                P.add("act", lambda e, d_=d_, g2=g2, part=part, pb=pb, ncol=ncol: e.activation(
                                    out=Pt[:, part, d_, g2, 0:ncol], in_=pP[pb][:, 0:ncol], func=AF.Copy),
                                    reads=["pP%d" % pb], writes=[("Pt", part, d_, g2)])
            P.barrier()
        s12.close()
        with ExitStack() as ph:
            St = [sb("St%d" % i, [128, 4, 64], F32, ph) for i in range(2)]
            C4 = sb("C4", [128, 4, 64], F32, ph)
            rt1 = sb("rt1", [128, 4, 64], F32, ph)
            rt2 = sb("rt2", [128, 2, 64], F32, ph)
            P.add("dve", lambda e: e.memset(St[0][:], 0.0), writes=["St0"])
            P.add("dve", lambda e: e.tensor_copy(C4[:, 0:2, :], Acplx[:, 0:1, :].to_broadcast([128, 2, 64])), reads=["Acplx"], writes=["C4"])
            P.add("dve", lambda e: e.tensor_scalar(out=C4[:, 3, :], in0=Acplx[:, 1, :], scalar1=-1.0, scalar2=None, op0=ALU.mult),
                  reads=["Acplx", "C4"], writes=["C4"])
            P.add("dve", lambda e: e.tensor_copy(C4[:, 2, :], Acplx[:, 1, :]), reads=["Acplx", "C4"], writes=["C4"])
            P.add("dve", lambda e: e.memset(St[1][:], 0.0), writes=["St1"])
            AR = Acplx[:, 0, 32:64]
            AI = Acplx[:, 1, 32:64]
            E16 = sb("E16", [128, 2, 32, 16], F32, ph)
            Am = sb("Am", [128, 2, 2, 32], F32, ph)
            ct_ = sb("cmt", [128, 2, 32, 8], F32, ph)

            def cmul(o_re, o_im, a_re, a_im, b_re, b_im, shp, rk, wk):
                t1 = ct_[:, 0].rearrange("p g k -> p (g k)")[:, 0:shp[1] * (shp[2] if len(shp) > 2 else 1)]
                t2 = ct_[:, 1].rearrange("p g k -> p (g k)")[:, 0:shp[1] * (shp[2] if len(shp) > 2 else 1)]
                if len(shp) > 2:
                    t1 = t1.rearrange("p (g k) -> p g k", k=shp[2])
                    t2 = t2.rearrange("p (g k) -> p g k", k=shp[2])
                seq = [
                    lambda e: e.tensor_tensor(out=t1, in0=a_re, in1=b_re, op=ALU.mult),
                    lambda e: e.tensor_tensor(out=t2, in0=a_im, in1=b_im, op=ALU.mult),
                    lambda e: e.tensor_tensor(out=o_re, in0=t1, in1=t2, op=ALU.subtract),
                    lambda e: e.tensor_tensor(out=t1, in0=a_re, in1=b_im, op=ALU.mult),
                    lambda e: e.tensor_tensor(out=t2, in0=a_im, in1=b_re, op=ALU.mult),
                    lambda e: e.tensor_tensor(out=o_im, in0=t1, in1=t2, op=ALU.add),
                ]
                for f_ in seq:
                    P.add("dve", f_, reads=["cmt"] + rk, writes=["cmt"] + wk)

            P.add("dve", lambda e: e.memset(E16[:, 0, :, 0:1], 1.0), writes=["E16"])
            P.add("dve", lambda e: e.memset(E16[:, 1, :, 0:1], 0.0), reads=["E16"], writes=["E16"])
            P.add("dve", lambda e: e.tensor_copy(E16[:, :, :, 1], Acplx[:, :, 32:64]), reads=["Acplx", "E16"], writes=["E16"])
            P.add("dve", lambda e: e.tensor_copy(Am[:, 0], Acplx[:, :, 32:64]), reads=["Acplx"], writes=["Am"])
            cur_a = 0
            m = 1
            while m < 16:
                src_, dst_ = Am[:, cur_a], Am[:, 1 - cur_a]
                if m >= 2:
                    pass
                m2 = m * 2 if m > 1 else 2
                if m == 1:
                    cmul(dst_[:, 0], dst_[:, 1], src_[:, 0], src_[:, 1], src_[:, 0], src_[:, 1], [128, 32], ["Am"], ["Am"])
                    cur_a = 1 - cur_a
                    mm_ = 2
                    am = Am[:, cur_a]
                    cmul(E16[:, 0, :, 2:4], E16[:, 1, :, 2:4], E16[:, 0, :, 0:2], E16[:, 1, :, 0:2],
                         am[:, 0].unsqueeze(2).to_broadcast([128, 32, 2]), am[:, 1].unsqueeze(2).to_broadcast([128, 32, 2]),
                         [128, 32, 2], ["Am", "E16"], ["E16"])
                    m = 2
                    continue
                src_, dst_ = Am[:, cur_a], Am[:, 1 - cur_a]
                cmul(dst_[:, 0], dst_[:, 1], src_[:, 0], src_[:, 1], src_[:, 0], src_[:, 1], [128, 32], ["Am"], ["Am"])
                cur_a = 1 - cur_a
                am = Am[:, cur_a]
                w_ = 2 * m
                if w_ < 16:
                    cmul(E16[:, 0, :, w_:2 * w_], E16[:, 1, :, w_:2 * w_], E16[:, 0, :, 0:w_], E16[:, 1, :, 0:w_],
                         am[:, 0].unsqueeze(2).to_broadcast([128, 32, w_]), am[:, 1].unsqueeze(2).to_broadcast([128, 32, w_]),
                         [128, 32, w_], ["Am", "E16"], ["E16"])
                m = w_
            A16 = Am[:, cur_a]
            ptmp = sb("ptmp", [128, 32, 10, 16], F32, ph)
            pr = sb("pr", [128, 4, 32, 10], F32, ph)
            Sb = sb("Sb", [128, 2, 32, 10], F32, ph)
            for idx, (pp, ep) in enumerate([(0, 0), (1, 1), (0, 1), (1, 0)]):
                Pv = Pt[:, pp, 1, :, 128:288].rearrange("p g (b k) -> p g b k", k=16)
                Eb = E16[:, ep].unsqueeze(2).to_broadcast([128, 32, 10, 16])
                P.add("dve", lambda e, Pv=Pv, Eb=Eb: e.tensor_tensor(out=ptmp[:], in0=Pv, in1=Eb, op=ALU.mult),
                      reads=["Pt", "E16"], writes=["ptmp"])
                P.add("dve", lambda e, idx=idx: e.tensor_reduce(out=pr[:, idx], in_=ptmp[:], axis=AX.X, op=ALU.add),
                      reads=["ptmp"], writes=[("pr", idx)])
            P.add("dve", lambda e: e.tensor_tensor(out=Sb[:, 0], in0=pr[:, 0], in1=pr[:, 1], op=ALU.subtract), reads=["pr"], writes=[("Sb", 0)])
            P.add("dve", lambda e: e.tensor_tensor(out=Sb[:, 1], in0=pr[:, 2], in1=pr[:, 3], op=ALU.add), reads=["pr"], writes=[("Sb", 1)])
            Hh = sb("Hh", [128, 2, 2, 32], F32, ph)
            P.add("dve", lambda e: e.tensor_copy(Hh[:, 0], Sb[:, :, :, 9]), reads=["Sb"], writes=["Hh"])
            hc_ = 0
            for b_ in range(8, -1, -1):
                hs, hd = Hh[:, hc_], Hh[:, 1 - hc_]
                cmul(hd[:, 0], hd[:, 1], A16[:, 0], A16[:, 1], hs[:, 0], hs[:, 1], [128, 32], ["Am", "Hh"], ["Hh"])
                P.add("dve", lambda e, hd=hd, b_=b_: e.tensor_tensor(out=hd, in0=hd, in1=Sb[:, :, :, b_], op=ALU.add),
                      reads=["Hh", "Sb"], writes=["Hh"])
                hc_ = 1 - hc_
            Hf = Hh[:, hc_]
            for slot, part in ((0, 0), (1, 1), (2, 0), (3, 1)):
                P.add("dve", lambda e, slot=slot, part=part: e.tensor_copy(St[0][:, slot, 32:64], Hf[:, part]), reads=["Hh", "St0"], writes=["St0"])
            P.add("act", lambda e: e.activation(out=Pt[:, :, 1, :, 128], in_=Hf, func=AF.Copy), reads=["Hh"], writes=[("PtS", "bnd")])

            Pt_full = Pt[:]
            pstep = Pt_full.ap[0][0]
            PART = 2 * 32 * 288
            for i in range(160):
                cur, nxt = St[i % 2], St[(i + 1) % 2]
                ck, nk = "St%d" % (i % 2), "St%d" % ((i + 1) % 2)
                qF, qB = i, 159 - i
                if i >= 32:
                    cs = slice(0, 64)
                    dd = [[32 * 288 + qB - qF, 2], [288, 32]]
                    off = Pt_full.offset + qF
                    vv = lambda t_, a, b: t_[:, a:b, :].rearrange("p a (d g) -> p a d g", d=2)
                    swp = [[32, 2], [1, 32]]
                else:
                    cs = slice(0, 32)
                    dd = [[288, 32]]
                    off = Pt_full.offset + qF
                    vv = lambda t_, a, b: t_[:, a:b, 0:32]
                    swp = [[1, 32]]
                pap = bass.AP(Pt_full.tensor, off, [[pstep, 128], [PART, 2]] + dd)
                ncs = cs.stop - cs.start
                r1 = rt1[:]
                c4_ = C4[:]
                cu_ = cur[:]
                o1 = bass.AP(r1.tensor, r1.offset, [[r1.ap[0][0], 128], [128, 2], [64, 2], [1, ncs]])
                a1 = bass.AP(c4_.tensor, c4_.offset, [[c4_.ap[0][0], 128], [128, 2], [64, 2], [1, ncs]])
                b1 = bass.AP(cu_.tensor, cu_.offset, [[cu_.ap[0][0], 128], [0, 2], [64, 2], [1, ncs]])
                P.add("dve", lambda e, o1=o1, a1=a1, b1=b1: e.tensor_tensor(out=o1, in0=a1, in1=b1, op=ALU.mult),
                      reads=[ck, "C4"], writes=["rt1"])
                rev_in = bass.AP(r1.tensor, r1.offset + 3 * 64, [[r1.ap[0][0], 128], [-64, 2]] + swp)
                P.add("dve", lambda e, vv=vv, rev_in=rev_in: e.tensor_tensor(out=vv(rt2, 0, 2), in0=vv(rt1, 0, 2), in1=rev_in, op=ALU.add),
                      reads=["rt1"], writes=["rt2"])
                P.add("dve", lambda e, nxt=nxt, vv=vv, pap=pap: e.tensor_tensor(out=vv(nxt, 0, 2), in0=vv(rt2, 0, 2), in1=pap, op=ALU.add),
                      reads=["rt2", ("PtS", i)], writes=[(nk, 0)])
                P.add("act", lambda e, nxt=nxt, vv=vv, pap=pap: e.activation(out=pap, in_=vv(nxt, 0, 2), func=AF.Copy),
                      reads=[(nk, 0)], writes=[("PtS", i)])
            P.barrier()
        if debug:
            d_H = dout("d_H", [128, 2 * 2 * 32 * 288], BF16)
            P.add("sp", lambda e: e.dma_start(out=d_H, in_=Pt[:].rearrange("p a b c d -> p (a b c d)")), reads=["Pt", "PtS"],
                  writes=["d_H"], group="dbgH")

        with ExitStack() as ph:
            Ysb = sb("Ysb", [128, 64, 128], BF16, ph)
            tch = [sb("tch%d" % i, [128, 2, 8, 128], BF16, ph) for i in range(1)] * 2
            w2ch = [sb("w2ch%d" % i, [128, 2, 4, 2, 128], BF16, ph) for i in range(1)] * 2
            pY = [ps("pY%d" % i, [128, 4, 128], F32, ph) for i in range(2)]
            for q in range(8):
                b_ = 0
                for d_ in range(2):
                    P.add("sp", lambda e, q=q, b_=b_, d_=d_: e.dma_start(
                        out=tch[b_][:, d_], in_=scrT[d_, q * 8:(q + 1) * 8].rearrange("g r c -> r g c")),
                        reads=["scrT"], writes=[("tch", b_, d_)], group="tch%d" % b_)
                    P.add("sp", lambda e, q=q, b_=b_, d_=d_: e.dma_start(
                        out=w2ch[b_][:, d_], in_=scrW2[d_, q * 4:(q + 1) * 4].rearrange("g part r c -> r g part c")),
                        reads=["scrW2"], writes=[("w2ch", b_, d_)], group="w2ch%d" % b_)
                for hh in range(2):
                    pb = (q * 2 + hh) % 2

                    def mmY(e, q=q, hh=hh, pb=pb, b_=b_):
                        ins = None
                        for j in range(4):
                            gl = hh * 4 + j
                            g = q * 8 + gl
                            g2, gp = g // 2, g % 2
                            g2l = g2 - q * 4
                            rows = slice(gp * 64, (gp + 1) * 64)
                            o = pY[pb][:, j, :]
                            e.matmul(o, lhsT=tch[b_][:, 0, gl, :], rhs=U[:, g, 0:128], start=True, stop=False)
                            e.matmul(o, lhsT=w2ch[b_][rows, 0, g2l, 0, :], rhs=Pt[rows, 0, 0, g2, 31:159], start=False, stop=False)
                            e.matmul(o, lhsT=w2ch[b_][rows, 0, g2l, 1, :], rhs=Pt[rows, 1, 0, g2, 31:159], start=False, stop=False)
                            e.matmul(o, lhsT=tch[b_][:, 1, gl, :], rhs=U[:, g, 0:128], start=False, stop=False)
                            e.matmul(o, lhsT=w2ch[b_][rows, 1, g2l, 0, :], rhs=Pt[rows, 0, 1, g2, 1:129], start=False, stop=False)
                            ins = e.matmul(o, lhsT=w2ch[b_][rows, 1, g2l, 1, :], rhs=Pt[rows, 1, 1, g2, 1:129], start=False, stop=True)
                        return ins
                    P.add("pe", mmY, reads=[("tch", b_), ("w2ch", b_), "U", "Pt", "PtS"], writes=["pY%d" % pb])
                    g0 = q * 8 + hh * 4
                    if hh == 0:
                        P.add("dve", lambda e, g0=g0, pb=pb: e.tensor_copy(Ysb[:, g0:g0 + 4, :], pY[pb][:]), reads=["pY%d" % pb],
                              writes=[("Ysb", g0)])
                    else:
                        P.add("act", lambda e, g0=g0, pb=pb: e.activation(out=Ysb[:, g0:g0 + 4, :], in_=pY[pb][:], func=AF.Copy),
                              reads=["pY%d" % pb], writes=[("Ysb", g0)])
            pZ = [ps("pZ%d" % i, [128, 512], F32, ph) for i in range(2)]
            yf = [sb("yf%d" % i, [128, 512], F32, ph) for i in range(2)]
            ya = [sb("ya%d" % i, [128, 512], F32, ph) for i in range(2)]
            for ct in range(8):
                chains = []
                for half in range(2):
                    pb = half

                    def uns(e, ct=ct, half=half, pb=pb):
                        ins = None
                        ov = pZ[pb][:, :].rearrange("p (j t) -> p t j", t=8)
                        for t_ in range(8):
                            for gl in range(8):
                                ins = e.matmul(ov[:, t_, :], lhsT=Z[:, t_, gl, :], rhs=Ysb[:, ct * 8 + gl, half * 64:(half + 1) * 64],
                                               start=(gl == 0), stop=(gl == 7))
                        return ins
                    P.add("pe", uns, reads=["Z", "Ysb"], writes=["pZ%d" % pb])
                    tk = slice(half * 512, (half + 1) * 512)
                    yk, ak = "yf%d" % pb, "ya%d" % pb
                    chains.append([
                        ("dve", lambda e, ct=ct, pb=pb, half=half: e.scalar_tensor_tensor(
                            out=yf[pb][:].rearrange("p (j s) -> p j s", s=8),
                            in0=uTown[:, ct, :].rearrange("p (s j) -> p j s", s=8)[:, half * 64:(half + 1) * 64, :],
                            scalar=colB[:, ct:ct + 1], in1=pZ[pb][:, :].rearrange("p (j s) -> p j s", s=8), op0=ALU.mult, op1=ALU.add),
                         ["uT", "colB", "pZ%d" % pb], [yk]),
                        ("act", lambda e, pb=pb: e.activation(out=ya[pb][:], in_=yf[pb][:], func=AF.Square), [yk], [ak]),
                        ("dve", lambda e, pb=pb: e.tensor_scalar(out=ya[pb][:], in0=ya[pb][:], scalar1=0.044715, scalar2=1.0,
                                                                 op0=ALU.mult, op1=ALU.add), [ak], [ak]),
                        ("dve", lambda e, pb=pb: e.tensor_tensor(out=ya[pb][:], in0=ya[pb][:], in1=yf[pb][:], op=ALU.mult), [ak, yk], [ak]),
                        ("act", lambda e, pb=pb: e.activation(out=ya[pb][:], in_=ya[pb][:], func=AF.Sigmoid, scale=1.5957691216057308),
                         [ak], [ak]),
                        ("dve", lambda e, pb=pb, ct=ct, tk=tk: e.tensor_tensor(out=gT[:, ct, tk], in0=ya[pb][:], in1=yf[pb][:], op=ALU.mult),
                         [ak, yk], [("gT", ct, half)]),
                    ])
                for k in range(6):
                    for ch in chains:
                        en, f_, rk, wk = ch[k]
                        P.add(en, f_, reads=rk, writes=wk)
            P.barrier()
        ssm.close()
        if debug:
            d_gT = dout("d_gT", [128, 8 * NOWN], BF16)
            P.add("sp", lambda e: e.dma_start(out=d_gT, in_=gT[:].rearrange("p a b -> p (a b)")), reads=["gT"],
                  writes=["d_gT"], group="dbgG")

        ssmT = sb("ssmT", [128, 8, NOWN], BF16, mix)
        rstd = sb("rstdSC", [128, 2, NOWN], F32, mix)
        with ExitStack() as ph:
            wglu = sb("wglu", [128, 8, 2048], BF16, ph)
            wgv = w_glu.rearrange("(kt p) c -> p kt c", p=128)
            for kt in range(8):
                for hc in range(2):
                    P.add("pool", lambda e, kt=kt, hc=hc: e.dma_start(out=wglu[:, kt, hc * 1024:(hc + 1) * 1024],
                                                                      in_=wgv[:, kt, hc * 1024:(hc + 1) * 1024]),
                          writes=[("wglu", kt, hc)], group="wglu")
            pga = [ps("pga%d" % i, [128, 512], F32, ph) for i in range(2)]
            pgb = [ps("pgb%d" % i, [128, 512], F32, ph) for i in range(2)]
            pss = ps("pss", [128, 512], F32, ph)
            sig = [sb("sig%d" % i, [128, 512], F32, ph) for i in range(2)]
            sq = [sb("sq%d" % i, [128, 512], BF16, ph) for i in range(2)]
            pend = []
            for which in range(2):
                for half in range(2):
                    tk = slice(half * 512, (half + 1) * 512)
                    for ot in range(8):
                        b_ = ot % 2
                        if which == 0:
                            def mg(e, ot=ot, b_=b_, tk=tk, off=0, pp=pga):
                                ins = None
                                for kt in range(8):
                                    ins = e.matmul(pp[b_][:, :], lhsT=wglu[:, kt, off + ot * 128:off + (ot + 1) * 128], rhs=gT[:, kt, tk],
                                                   start=(kt == 0), stop=(kt == 7))
                                return ins
                            P.add("pe", mg, reads=["wglu", "gT"], writes=["pga%d" % b_])
                            P.add("pe", lambda e, ot=ot, b_=b_, tk=tk: mg(e, ot, b_, tk, 1024, pgb), reads=["wglu", "gT"], writes=["pgb%d" % b_])
                            P.add("act", lambda e, b_=b_: e.activation(out=sig[b_][:], in_=pgb[b_][:], func=AF.Sigmoid),
                                  reads=["pgb%d" % b_], writes=["sig%d" % b_])
                            P.add("dve", lambda e, b_=b_, ot=ot, tk=tk: e.tensor_tensor(out=ssmT[:, ot, tk], in0=pga[b_][:], in1=sig[b_][:], op=ALU.mult),
                                  reads=["pga%d" % b_, "sig%d" % b_], writes=[("ssmT", ot, half)])
                            srcT, skey = ssmT, ("ssmT", ot, half)
                        else:
                            srcT, skey = convT, "convT"
                        def back(b_=b_, ot=ot, tk=tk, srcT=srcT, skey=skey):
                            P.add("act", lambda e: e.activation(out=sq[b_][:], in_=srcT[:, ot, tk], func=AF.Square),
                                  reads=[skey], writes=["sq%d" % b_])
                            P.add("pe", lambda e: e.matmul(pss[:, :], lhsT=ones_b[:], rhs=sq[b_][:], start=(ot == 0), stop=(ot == 7)),
                                  reads=["ones_b", "sq%d" % b_], writes=["pss"])
                        if pend:
                            pend.pop()()
                        pend.append(back)
                    if pend:
                        pend.pop()()
                    rk = ("rstd", which, half)
                    P.add("dve", lambda e, which=which, tk=tk: e.tensor_scalar(out=rstd[:, which, tk], in0=pss[:, :], scalar1=1.0 / 1024, scalar2=EPS,
                                                                              op0=ALU.mult, op1=ALU.add), reads=["pss"], writes=[rk])
                    P.add("act", lambda e, which=which, tk=tk: e.activation(out=rstd[:, which, tk], in_=rstd[:, which, tk], func=AF.Sqrt),
                          reads=[rk], writes=[rk])
                    P.add("dve", lambda e, which=which, tk=tk: e.reciprocal(out=rstd[:, which, tk], in_=rstd[:, which, tk]), reads=[rk], writes=[rk])
            for ot in range(8):
                P.add("dve", lambda e, ot=ot: e.scalar_tensor_tensor(out=ssmT[:, ot, :], in0=ssmT[:, ot, :], scalar=colA[:, 80 + ot:81 + ot],
                                                                     in1=rstd[:, 0, :], op0=ALU.mult, op1=ALU.mult),
                      reads=["ssmT", "rstd", "colA"], writes=[("ssmT", ot)])
                P.add("dve", lambda e, ot=ot: e.scalar_tensor_tensor(out=convT[:, ot, :], in0=convT[:, ot, :], scalar=colA[:, 88 + ot:89 + ot],
                                                                      in1=rstd[:, 1, :], op0=ALU.mult, op1=ALU.mult),
                      reads=["convT", "rstd", "colA"], writes=[("convT", ot)])
            P.barrier()

        def row_bcast(dst, col_of_ft, rkeys, wkey, stack_ps):
            dgs = [sb(wkey + "_dg%d" % i, [128, 128], F32, stack_ps) for i in range(2)]
            prb = ps(wkey + "_prb", [128, 512], F32, stack_ps)
            for c4 in range(4):
                for j in range(4):
                    ft = c4 * 4 + j
                    b_ = ft % 2
                    P.add("dve", lambda e, ft=ft, b_=b_: e.tensor_scalar(out=dgs[b_][:], in0=ident_f[:], scalar1=col_of_ft(ft), scalar2=None,
                                                                        op0=ALU.mult), reads=["ident_f"] + rkeys, writes=[wkey + "_dg%d" % b_])
                    P.add("pe", lambda e, j=j, b_=b_: e.matmul(prb[:, j * 128:(j + 1) * 128], lhsT=ones_f[:], rhs=dgs[b_][:], start=True, stop=True),
                          reads=["ones_f", wkey + "_dg%d" % b_], writes=[wkey + "_prb"])
                P.add("act", lambda e, c4=c4: e.activation(out=dst[:, c4 * 512:(c4 + 1) * 512], in_=prb[:, :], func=AF.Copy),
                      reads=[wkey + "_prb"], writes=[wkey])

        with ExitStack() as ph:
            g1b = sb("g1b", [128, D], F32, ph)
            with ExitStack() as ph3:
                row_bcast(g1b, lambda ft: modT[:, 32 + ft, 0:1], ["modT"], "g1b", ph3)
                P.barrier()
            woc = [sb("woc%d" % i, [128, 16, 512], BF16, ph) for i in range(2)]
            wov = w_out.rearrange("(kt p) c -> p kt c", p=128)
            po = [ps("po%d" % i, [128, 512], F32, ph) for i in range(6)]
            xp = [sb("xp%d" % i, [128, 512], F32, ph) for i in range(8)]
            x1p = [sb("x1p%d" % i, [128, 512], F32, ph) for i in range(4)]
            jk = sb("jk", [128, 512], BF16, ph)
            its = [(cc, tt) for cc in range(4) for tt in range(8)]

            def ld(n):
                cc, tt = its[n]
                P.add("sp", lambda e, n=n, cc=cc, tt=tt: e.dma_start(out=xp[n % 8][:], in_=xs[tt * 128:(tt + 1) * 128, cc * 512:(cc + 1) * 512]),
                      writes=[("xp", n % 8)], group="xp%d" % (n % 8))
            for n in range(6):
                ld(n)
            for n, (cc, tt) in enumerate(its):
                wb_ = cc % 2
                if tt == 0:
                    for kt in range(16):
                        P.add("pool", lambda e, kt=kt, cc=cc, wb_=wb_: e.dma_start(out=woc[wb_][:, kt, :], in_=wov[:, kt, cc * 512:(cc + 1) * 512]),
                              writes=[("woc", wb_, kt)], group="woc%d" % wb_)
                if n + 6 < len(its):
                    ld(n + 6)
                rows = slice(tt * 128, (tt + 1) * 128)
                cols = slice(cc * 512, (cc + 1) * 512)
                pb, xb_, sb_ = n % 6, n % 8, n % 4

                def mo(e, tt=tt, pb=pb, wb_=wb_):
                    ins = None
                    for ht in range(16):
                        hsrc = ssmT if ht < 8 else convT
                        ins = e.matmul(po[pb][:, :], lhsT=hsrc[:, ht % 8, tt * 128:(tt + 1) * 128], rhs=woc[wb_][:, ht, :],
                                       start=(ht == 0), stop=(ht == 15))
                    return ins
                P.add("pe", mo, reads=["ssmT", "convT", ("woc", wb_)], writes=["po%d" % pb])
                P.add("dve", lambda e, pb=pb, sb_=sb_, cols=cols: e.tensor_tensor(out=x1p[sb_][:], in0=po[pb][:, :], in1=g1b[:, cols], op=ALU.mult),
                      reads=["po%d" % pb, "g1b"], writes=[("x1p", sb_)])
                P.add("dve", lambda e, sb_=sb_, xb_=xb_: e.tensor_tensor(out=x1p[sb_][:], in0=x1p[sb_][:], in1=xp[xb_][:], op=ALU.add),
                      reads=[("x1p", sb_), ("xp", xb_)], writes=[("x1p", sb_)])
                P.add("act", lambda e, sb_=sb_, tt=tt, cc=cc: e.activation(out=jk[:], in_=x1p[sb_][:], func=AF.Square,
                                                                        accum_out=ss2[:, tt, cc:cc + 1]),
                      reads=[("x1p", sb_)], writes=["jk", ("ss2", tt, cc)])
                P.add("act", lambda e, sb_=sb_, rows=rows, cols=cols: e.dma_start(out=scrX1[rows, cols], in_=x1p[sb_][:]),
                      reads=[("x1p", sb_)], writes=["scrX1"], group="x1p%d" % sb_)
            P.barrier()
        mix.close()
        mixer.close()

        moe = top.enter_context(ExitStack())
        acc = sb("acc", [128, 8, D], F32, moe)
        hx2T = sb("hx2T", [128, 16, NOWN], BF16, moe)
        Wt = sb("Wt", [128, 8, 64], F32, moe)
        with ExitStack() as ph:
            sc2b = sb("sc2b", [128, D], F32, ph)
            sh2b = sb("sh2b", [128, D], F32, ph)
            scale2 = sb("scale2", [128, 16], F32, ph)
            P.add("dve", lambda e: e.scalar_tensor_tensor(out=scale2[:], in0=modT[:, 64:80, 0], scalar=1.0, in1=colA[:, 48:64],
                                                          op0=ALU.add, op1=ALU.mult), reads=["modT", "colA"], writes=["scale2"])
            with ExitStack() as ph3:
                row_bcast(sc2b, lambda ft: scale2[:, ft:ft + 1], ["scale2"], "sc2b", ph3)
                P.barrier()
            with ExitStack() as ph3:
                row_bcast(sh2b, lambda ft: modT[:, 48 + ft, 0:1], ["modT"], "sh2b", ph3)
                P.barrier()
            rw = sb("rw", [128, 16, 64], F32, ph)
            P.add("sp", lambda e: e.dma_start(out=rw[:], in_=router_w.rearrange("(kt p) c -> p kt c", p=128)), writes=["rw"], group="rw")
            rb = sb("rb", [128, 64], F32, ph)
            P.add("sp", lambda e: e.dma_start(out=rb[:], in_=rbias_b), writes=["rb"], group="rb")
            rs2 = sb("rs2", [128, 8], F32, ph)
            P.add("dve", lambda e: e.tensor_reduce(out=rs2[:], in_=ss2[:], axis=AX.X, op=ALU.add), reads=["ss2"], writes=["rs2"])
            P.add("dve", lambda e: e.tensor_scalar(out=rs2[:], in0=rs2[:], scalar1=1.0 / D, scalar2=EPS, op0=ALU.mult, op1=ALU.add),
                  reads=["rs2"], writes=["rs2"])
            P.add("act", lambda e: e.activation(out=rs2[:], in_=rs2[:], func=AF.Sqrt), reads=["rs2"], writes=["rs2"])
            P.add("dve", lambda e: e.reciprocal(out=rs2[:], in_=rs2[:]), reads=["rs2"], writes=["rs2"])
            x1t = [sb("x1t%d" % i, [128, D], F32, ph) for i in range(3)]
            hf = [sb("hf%d" % i, [128, D], F32, ph) for i in range(3)]
            pth = [ps("pth%d" % i, [128, 4, 128], F32, ph) for i in range(2)]
            hfs = [sb("hfs%d" % i, [128, 4, 128], F32, ph) for i in range(2)]
            plgs = [ps("plg%d" % i, [128, 64], F32, ph) for i in range(2)]
            rt = sb("rt", [128, 12, 64], F32, ph)
            m8 = sb("m8", [128, 16], F32, ph)

            def n2A(tt):
                b_ = tt % 3
                rows = slice(tt * 128, (tt + 1) * 128)
                P.add("sp", lambda e, b_=b_, rows=rows: e.dma_start(out=x1t[b_][:], in_=scrX1[rows, :]), reads=["scrX1"],
                      writes=[("x1t", b_)], group="x1t%d" % b_)
                P.add("act", lambda e, b_=b_, tt=tt: e.activation(out=hf[b_][:], in_=x1t[b_][:], func=AF.Copy, scale=rs2[:, tt:tt + 1]),
                      reads=[("x1t", b_), "rs2"], writes=[("hf", b_)])
                P.add("dve", lambda e, b_=b_: e.tensor_tensor(out=hf[b_][:], in0=hf[b_][:], in1=sc2b[:], op=ALU.mult),
                      reads=[("hf", b_), "sc2b"], writes=[("hf", b_)])
                P.add("dve", lambda e, b_=b_: e.tensor_tensor(out=hf[b_][:, 0:1280], in0=hf[b_][:, 0:1280], in1=sh2b[:, 0:1280], op=ALU.add),
                      reads=[("hf", b_), "sh2b"], writes=[("hf", b_, 0)])
                P.add("pool", lambda e, b_=b_: e.tensor_tensor(out=hf[b_][:, 1280:2048], in0=hf[b_][:, 1280:2048], in1=sh2b[:, 1280:2048], op=ALU.add),
                      reads=[("hf", b_), "sh2b"], writes=[("hf", b_, 1)])

            def n2B(tt):
                b_ = tt % 3
                plg = plgs[tt % 2]
                pk = "plg%d" % (tt % 2)

                def tr_blk(f4):
                    pb = f4 % 2

                    def trh(e, b_=b_, f4=f4, pb=pb):
                        ins = None
                        for j in range(4):
                            ft = f4 * 4 + j
                            ins = e.transpose(out=pth[pb][:, j, :], in_=hf[b_][:, ft * 128:(ft + 1) * 128], identity=ident_f[:])
                        return ins
                    P.add("pe", trh, reads=[("hf", b_), "ident_f"], writes=["pth%d" % pb])
                    P.add("act", lambda e, pb=pb, f4=f4, tt=tt: e.activation(out=hx2T[:, f4 * 4:(f4 + 1) * 4, tt * 128:(tt + 1) * 128],
                                                                            in_=pth[pb][:], func=AF.Copy),
                          reads=["pth%d" % pb], writes=[("hx2T", f4, tt)])
                    P.add("dve", lambda e, pb=pb: e.tensor_copy(hfs[pb][:], pth[pb][:]), reads=["pth%d" % pb], writes=[("hfs", pb)])

                def mr_blk(f4):
                    pb = f4 % 2

                    def mr(e, pb=pb, f4=f4, plg=plg):
                        ins = None
                        for j in range(4):
                            ft = f4 * 4 + j
                            ins = e.matmul(plg[:, :], lhsT=hfs[pb][:, j, :], rhs=rw[:, ft, :], start=(ft == 0), stop=(ft == 15))
                        return ins
                    P.add("pe", mr, reads=[("hfs", pb), "rw"], writes=[pk])
                tr_blk(0)
                for f4 in range(4):
                    if f4 + 1 < 4:
                        tr_blk(f4 + 1)
                    mr_blk(f4)

            def n2C(tt):
                plg = plgs[tt % 2]
                pk = "plg%d" % (tt % 2)
                S_, Bi, T1, T2, MB, EM = (rt[:, i, :] for i in range(6))
                g3 = lambda ap: ap.rearrange("p (g k) -> p g k", k=8)
                rops = [
                    ("act", lambda e: e.activation(out=S_, in_=plg[:, :], func=AF.Sigmoid), [pk]),
                    ("dve", lambda e: e.tensor_tensor(out=Bi, in0=S_, in1=rb[:], op=ALU.add), ["rb"]),
                    ("dve", lambda e: e.tensor_reduce(out=m8[:, 0:8], in_=g3(Bi), axis=AX.X, op=ALU.max), []),
                    ("dve", lambda e: e.tensor_tensor(out=g3(T1), in0=g3(Bi), in1=m8[:, 0:8].unsqueeze(2).to_broadcast([128, 8, 8]), op=ALU.is_equal), []),
                    ("dve", lambda e: e.scalar_tensor_tensor(out=T1, in0=T1, scalar=-1e9, in1=Bi, op0=ALU.mult, op1=ALU.add), []),
                    ("dve", lambda e: e.tensor_reduce(out=m8[:, 8:16], in_=g3(T1), axis=AX.X, op=ALU.max), []),
                    ("dve", lambda e: e.tensor_tensor(out=m8[:, 0:8], in0=m8[:, 0:8], in1=m8[:, 8:16], op=ALU.add), []),
                    ("dve", lambda e: e.max(out=m8[:, 8:16], in_=m8[:, 0:8]), []),
                    ("dve", lambda e: e.tensor_scalar(out=m8[:, 0:8], in0=m8[:, 0:8], scalar1=m8[:, 11:12], scalar2=None, op0=ALU.is_ge), []),
                    ("dve", lambda e: e.tensor_tensor(out=g3(MB), in0=g3(Bi), in1=m8[:, 0:8].unsqueeze(2).to_broadcast([128, 8, 8]), op=ALU.mult), []),
                    ("dve", lambda e: e.tensor_scalar(out=m8[:, 0:8], in0=m8[:, 0:8], scalar1=-1.0, scalar2=1e9, op0=ALU.add, op1=ALU.mult), []),
                    ("dve", lambda e: e.tensor_tensor(out=g3(MB), in0=g3(MB), in1=m8[:, 0:8].unsqueeze(2).to_broadcast([128, 8, 8]), op=ALU.add), []),
                    ("dve", lambda e: e.max(out=m8[:, 8:16], in_=MB), []),
                    ("dve", lambda e: e.tensor_scalar(out=EM, in0=MB, scalar1=m8[:, 15:16], scalar2=None, op0=ALU.is_ge), []),
                    ("dve", lambda e: e.tensor_tensor(out=T2, in0=S_, in1=EM, op=ALU.mult), []),
                    ("dve", lambda e: e.tensor_reduce(out=m8[:, 0:1], in_=T2, axis=AX.X, op=ALU.add), []),
                    ("dve", lambda e: e.reciprocal(out=m8[:, 0:1], in_=m8[:, 0:1]), []),
                    ("dve", lambda e, tt=tt: e.tensor_scalar(out=Wt[:, tt, :], in0=T2, scalar1=m8[:, 0:1], scalar2=2.5, op0=ALU.mult, op1=ALU.mult), []),
                ]
                for (en, f_, rk) in rops:
                    P.add(en, f_, reads=["rt", "m8"] + rk, writes=["rt", "m8", ("Wt", tt)])

            for it in range(10):
                if it < 8:
                    n2A(it)
                if 1 <= it <= 8:
                    n2B(it - 1)
                if it >= 2:
                    n2C(it - 2)
            P.barrier()
        if debug:
            d_Wt = dout("d_Wt", [128, 512])
            P.add("sp", lambda e: e.dma_start(out=d_Wt, in_=Wt[:].rearrange("p a b -> p (a b)")), reads=["Wt"], writes=["d_Wt"], group="dbgW")
            d_hx2T = dout("d_hx2T", [128, 16 * NOWN], BF16)
            P.add("sp", lambda e: e.dma_start(out=d_hx2T, in_=hx2T[:].rearrange("p a b -> p (a b)")), reads=["hx2T"], writes=["d_hx2T"], group="dbgW2")

        with ExitStack() as ph:
            wg = [sb("wg%d" % i, [128, 16, 512], BF16, ph) for i in range(2)]
            wu = [sb("wu%d" % i, [128, 16, 512], BF16, ph) for i in range(2)]
            wd = [sb("wd0", [128, 4, D], BF16, ph)]
            actT = sb("actT", [128, 4, NOWN], BF16, ph)
            sgl = [sb("sgl%d" % i, [128, 512], F32, ph) for i in range(2)]
            pg = [ps("pg%d" % i, [128, 512], F32, ph) for i in range(2)]
            pu = [ps("pu%d" % i, [128, 512], F32, ph) for i in range(2)]
            pd = [ps("pd%d" % i, [128, 512], F32, ph) for i in range(3)]
            P.add("pool", lambda e: e.memset(acc[:], 0.0), writes=["acc"])
            NE = 65
            import os
            DMAONLY = os.environ.get("MOE_DMAONLY", "")
            _Padd = P.add
            if DMAONLY:
                class _PX:
                    @staticmethod
                    def add(eng, fn, reads=(), writes=(), group=None):
                        if group is None:
                            return None
                        q = {"1": "pool", "2": "sp", "3": "act"}[DMAONLY[0]]
                        return _Padd(q if DMAONLY[0] != "4" else eng, fn, reads=reads, writes=writes, group=group)
                PM = _PX
            else:
                PM = P
            for ex in range(NE):
                b_ = ex % 2
                gsrc = ew_gate[ex] if ex < 64 else sw_gate
                usrc = ew_up[ex] if ex < 64 else sw_up
                dsrc = ew_down[ex] if ex < 64 else sw_down
                PM.add("pool", lambda e, b_=b_, gsrc=gsrc: e.dma_start(out=wg[b_][:], in_=gsrc.rearrange("(kt p) c -> p kt c", p=128)),
                      writes=[("wg", b_)], group="wg%d" % b_)
                PM.add("pool", lambda e, b_=b_, usrc=usrc: e.dma_start(out=wu[b_][:], in_=usrc.rearrange("(kt p) c -> p kt c", p=128)),
                      writes=[("wu", b_)], group="wu%d" % b_)
                dv = dsrc.rearrange("(kt p) c -> p kt c", p=128)
                for hc in range(2):
                    PM.add("pool", lambda e, dv=dv, hc=hc: e.dma_start(out=wd[0][:, :, hc * 1024:(hc + 1) * 1024],
                                                                     in_=dv[:, :, hc * 1024:(hc + 1) * 1024]),
                          writes=[("wd", hc)], group="wd")
                n = 0
                for half in range(2):
                    for mt in range(4):
                        pb = n % 2
                        n += 1
                        tk = slice(half * 512, (half + 1) * 512)

                        def mgu(e, w_, pp, mt=mt, tk=tk, pb=pb, b_=b_):
                            ins = None
                            for kt in range(16):
                                ins = e.matmul(pp[pb][:, :], lhsT=w_[b_][:, kt, mt * 128:(mt + 1) * 128], rhs=hx2T[:, kt, tk],
                                               start=(kt == 0), stop=(kt == 15))
                            return ins
                        PM.add("pe", lambda e, f_=mgu: f_(e, wg, pg), reads=[("wg", b_), "hx2T"], writes=["pg%d" % pb])
                        PM.add("pe", lambda e, f_=mgu: f_(e, wu, pu), reads=[("wu", b_), "hx2T"], writes=["pu%d" % pb])
                        PM.add("act", lambda e, pb=pb: e.activation(out=sgl[pb][:], in_=pg[pb][:, :], func=AF.Silu),
                              reads=["pg%d" % pb], writes=[("sgl", pb)])
                        PM.add("dve", lambda e, pb=pb, mt=mt, tk=tk: e.tensor_tensor(out=actT[:, mt, tk], in0=sgl[pb][:], in1=pu[pb][:, :], op=ALU.mult),
                              reads=[("sgl", pb), "pu%d" % pb], writes=[("actT", mt, half)])
                n = 0
                for tt in range(8):
                    for cc in range(4):
                        pb = n % 3
                        n += 1

                        def mdn(e, tt=tt, cc=cc, pb=pb):
                            ins = None
                            for kt in range(4):
                                ins = e.matmul(pd[pb][:, :], lhsT=actT[:, kt, tt * 128:(tt + 1) * 128], rhs=wd[0][:, kt, cc * 512:(cc + 1) * 512],
                                               start=(kt == 0), stop=(kt == 3))
                            return ins
                        PM.add("pe", mdn, reads=[("actT", m_, tt // 4) for m_ in range(4)] + ["wd"], writes=["pd%d" % pb])
                        wsc = Wt[:, tt, ex:ex + 1] if ex < 64 else 1.0
                        PM.add("dve", lambda e, tt=tt, cc=cc, pb=pb, wsc=wsc: e.scalar_tensor_tensor(
                            out=acc[:, tt, cc * 512:(cc + 1) * 512], in0=pd[pb][:, :], scalar=wsc, in1=acc[:, tt, cc * 512:(cc + 1) * 512],
                            op0=ALU.mult, op1=ALU.add), reads=["pd%d" % pb, "Wt"], writes=[("acc", tt, cc)])
            P.barrier()

        with ExitStack() as ph:
            g2b = sb("g2b", [128, D], F32, ph)
            fgb = sb("fgb", [128, D], F32, ph)
            with ExitStack() as ph3:
                row_bcast(g2b, lambda ft: modT[:, 80 + ft, 0:1], ["modT"], "g2b", ph3)
                P.barrier()
            with ExitStack() as ph3:
                row_bcast(fgb, lambda ft: colA[:, 64 + ft:65 + ft], ["colA"], "fgb", ph3)
                P.barrier()
            x1f = [sb("x1f%d" % i, [128, D], F32, ph) for i in range(3)]
            fo = [sb("fo%d" % i, [128, D], F32, ph) for i in range(3)]
            fs = sb("fs", [128, 8], F32, ph)
            fj = sb("fj", [128, D], BF16, ph)
            def stageA(tt):
                b_ = tt % 3
                rows = slice(tt * 128, (tt + 1) * 128)
                for hc in range(2):
                    cs = slice(hc * 1024, (hc + 1) * 1024)
                    P.add("dve", lambda e, tt=tt, cs=cs: e.tensor_tensor(out=acc[:, tt, cs], in0=acc[:, tt, cs], in1=g2b[:, cs], op=ALU.mult),
                          reads=[("acc", tt, hc), "g2b"], writes=[("acc", tt, hc)])
                    P.add("dve", lambda e, tt=tt, b_=b_, cs=cs: e.tensor_tensor(out=acc[:, tt, cs], in0=acc[:, tt, cs], in1=x1f[b_][:, cs], op=ALU.add),
                          reads=[("acc", tt, hc), ("x1f", b_)], writes=[("acc", tt, hc)])
                    P.add("act", lambda e, tt=tt, cs=cs, hc=hc: e.activation(out=fj[:, cs], in_=acc[:, tt, cs], func=AF.Square,
                                                                          accum_out=fs2[:, tt, hc:hc + 1]),
                          reads=[("acc", tt, hc)], writes=[("fj", hc), ("fs2", tt, hc)])

            def stageB(tt):
                b_ = tt % 3
                rows = slice(tt * 128, (tt + 1) * 128)
                P.add("dve", lambda e, tt=tt: e.tensor_tensor(out=fs[:, tt:tt + 1], in0=fs2[:, tt, 0:1], in1=fs2[:, tt, 1:2], op=ALU.add),
                      reads=[("fs2", tt)], writes=[("fs", tt)])
                P.add("dve", lambda e, tt=tt: e.tensor_scalar(out=fs[:, tt:tt + 1], in0=fs[:, tt:tt + 1], scalar1=1.0 / D, scalar2=EPS,
                                                              op0=ALU.mult, op1=ALU.add), reads=[("fs", tt)], writes=[("fs", tt)])
                P.add("act", lambda e, tt=tt: e.activation(out=fs[:, tt:tt + 1], in_=fs[:, tt:tt + 1], func=AF.Sqrt), reads=[("fs", tt)], writes=[("fs", tt)])
                P.add("dve", lambda e, tt=tt: e.reciprocal(out=fs[:, tt:tt + 1], in_=fs[:, tt:tt + 1]), reads=[("fs", tt)], writes=[("fs", tt)])
                P.add("act", lambda e, tt=tt, b_=b_: e.activation(out=fo[b_][:], in_=acc[:, tt, :], func=AF.Copy, scale=fs[:, tt:tt + 1]),
                      reads=[("acc", tt), ("fs", tt)], writes=[("fo", b_)])
                P.add("dve", lambda e, b_=b_: e.tensor_tensor(out=fo[b_][:], in0=fo[b_][:], in1=fgb[:], op=ALU.mult),
                      reads=[("fo", b_), "fgb"], writes=[("fo", b_)])
                P.add("pool", lambda e, b_=b_, rows=rows: e.dma_start(out=out[rows, :], in_=fo[b_][:]), reads=[("fo", b_)], writes=["out"],
                      group="fo%d" % b_)

            fs2 = sb("fs2", [128, 8, 2], F32, ph)

            def ldf(tt):
                P.add("sp", lambda e, tt=tt: e.dma_start(out=x1f[tt % 3][:], in_=scrX1[tt * 128:(tt + 1) * 128, :]), reads=["scrX1"],
                      writes=[("x1f", tt % 3)], group="x1f%d" % (tt % 3))
            ldf(0)
            ldf(1)
            for tt in range(9):
                if tt < 8:
                    stageA(tt)
                if tt + 2 < 8:
                    ldf(tt + 2)
                if tt >= 1:
                    stageB(tt - 1)

        P.add("sp", None, reads=["out", "scrW1", "scrT", "scrW2", "scrU", "scrX1"] + list(dbg.keys()))
        P.emit()
    return nc, dbg


def prep_inputs(inp):
    f = lambda a: np.ascontiguousarray(a, dtype=np.float32)
    x, ctx, c = inp["x"], inp["ctx"], inp["c"]
    maps = []
    shared = {
        "w_ada": f(inp["w_ada"][0]), "w_in": f(inp["w_in"][0]),
        "b_ada": f(inp["b_ada"][0].reshape(96, 128)),
        "w_glu": f(inp["ssm_w_glu"][0]), "w_out": f(inp["w_out"][0]), "router_w": f(inp["router_w"][0]),
        "rbias_b": f(np.tile(inp["router_bias"][0].reshape(1, 64), (128, 1))),
        "ew_gate": f(inp["exp_w_gate"][0]), "ew_up": f(inp["exp_w_up"][0]), "ew_down": f(inp["exp_w_down"][0]),
        "sw_gate": f(inp["shared_w_gate"][0]), "sw_up": f(inp["shared_w_up"][0]), "sw_down": f(inp["shared_w_down"][0]),
    }
    for core in range(8):
        b, h = core // 2, core % 2
        xb = x[b]
        cb = ctx[b]
        conv_w = inp["conv_w"][0]
        if h == 1:
            xb = xb[::-1]
            cb = cb[::-1]
            conv_w = conv_w[::-1]
        vecsA = np.concatenate([c[b].reshape(16, 128), inp["c_ctx"].reshape(16, 128),
                                inp["norm1_g"][0].reshape(16, 128), inp["norm2_g"][0].reshape(16, 128),
                                inp["final_g"].reshape(16, 128), inp["mix_norm_g"][0].reshape(16, 128)], 0)
        vecsB = np.concatenate([inp["ssm_d"][0].reshape(8, 128), conv_w.reshape(24, 128),
                                inp["conv_b"][0].reshape(8, 128)], 0)
        sl = slice(None, None, -1) if h == 1 else slice(None)
        m = dict(shared)
        m.update(xs=f(xb), ctxs=f(cb), vecsA=f(vecsA), vecsB=f(vecsB),
                 lamre_p=f(inp["ssm_lam_re"][0][sl].reshape(64, 128)), lamim_p=f(inp["ssm_lam_im"][0][sl].reshape(64, 128)),
                 logdt_p=f(inp["ssm_log_dt"][0][sl].reshape(64, 2)),
                 ssm_b_re=f(inp["ssm_b_re"][0][sl]), ssm_b_im=f(inp["ssm_b_im"][0][sl]),
                 ssm_c_re=f(inp["ssm_c_re"][0][sl]), ssm_c_im=f(inp["ssm_c_im"][0][sl]))
        maps.append(m)
    return maps


def kernel(**inputs):
    nc, _ = build_nc(False)
    maps = prep_inputs(inputs)
    res = run_bass_kernel_spmd(nc, maps, core_ids=list(range(8)))
    outs = np.zeros((4, 2048, 2048), np.float32)
    for core in range(8):
        b, h = core // 2, core % 2
        o = res.results[core]["out"]
        if h == 0:
            outs[b, 0:1024] = o
        else:
            outs[b, 1024:2048] = o[::-1]
    return outs
```
